# Optimizing a Trainium2 kernel written in Bass

```python
import math
import jax, jax.numpy as jnp
from jax import lax
import numpy as np

D_MODEL = 1024
BATCH = 16
SEQ = 2048
DEPTH = 2

HEAD_DIM = 64
QBLOCK = 128
EPS = 1e-5
SWA_HEADS = 8
SWA_KV_HEADS = 2
SWA_WINDOW = 128
DIFF_HEADS = 4
NSA_HEADS = 8
NSA_KV_HEADS = 2
CMP_BLOCK = 32
CMP_STRIDE = 16
CMP_HIDDEN = 256
SEL_BLOCK = 64
SEL_TOPN = 16
SEL_QCHUNK = 32
NSA_WINDOW = 512
BRANCH_WIDTH = 512
N_BRANCHES = 3
N_EXPERTS = 32
TOP_K = 4
D_EXPERT = D_MODEL
SWIGLU_LIMIT = 7.0
SWIGLU_ALPHA = 1.702
MOE_BLOCK = 128
A_Q = SWA_HEADS * HEAD_DIM
A_KV = SWA_KV_HEADS * HEAD_DIM
B_QK = DIFF_HEADS * 2 * HEAD_DIM
B_V = DIFF_HEADS * 2 * HEAD_DIM
C_Q = NSA_HEADS * HEAD_DIM
C_KV = NSA_KV_HEADS * HEAD_DIM
NSA_GATES = NSA_HEADS * 3
MERGE_GATES = N_BRANCHES * D_MODEL
IN_SPLITS = (A_Q, A_KV, A_KV, B_QK, B_QK, B_V, C_Q, C_KV, C_KV, C_KV, C_KV, C_KV, C_KV, NSA_GATES, MERGE_GATES)
IN_WIDTH = sum(IN_SPLITS)

kernel_name = 'hybrid_swa_diff_nsa_moe_adaln'


def rms_norm(x, g):
    xf = x.astype(jnp.float32)
    y = xf * lax.rsqrt(jnp.mean(xf * xf, axis=-1, keepdims=True) + EPS)
    return (y * g.astype(jnp.float32)).astype(x.dtype)


def alibi_slopes(n):
    return 2.0 ** (-8.0 * jnp.arange(1, n + 1, dtype=jnp.float32) / n)


def banded_attention(q, k, v, window, slopes, sinks=None):
    B, S, Hkv, G, d = q.shape
    nb = S // QBLOCK
    n_prev = -(-(window - 1) // QBLOCK)
    ctx = (n_prev + 1) * QBLOCK
    pad = ((0, 0), (n_prev * QBLOCK, 0), (0, 0), (0, 0))
    kp = jnp.pad(k, pad)
    vp = jnp.pad(v, pad)
    qb = q.reshape(B, nb, QBLOCK, Hkv, G, d).swapaxes(0, 1)
    scale = d ** -0.5

    def block(args):
        qi, i = args
        start = i * QBLOCK
        kc = lax.dynamic_slice_in_dim(kp, start, ctx, axis=1)
        vc = lax.dynamic_slice_in_dim(vp, start, ctx, axis=1)
        s = jnp.einsum('bqhgd,bkhd->bhgqk', qi, kc).astype(jnp.float32) * scale
        t = start + jnp.arange(QBLOCK)
        pos = start - n_prev * QBLOCK + jnp.arange(ctx)
        dist = t[:, None] - pos[None, :]
        valid = (dist >= 0) & (dist < window) & (pos[None, :] >= 0)
        s = s - slopes[:, :, None, None] * dist.astype(jnp.float32)
        s = jnp.where(valid, s, -jnp.inf)
        if sinks is None:
            p = jax.nn.softmax(s, axis=-1)
        else:
            lse = jnp.logaddexp(jax.nn.logsumexp(s, axis=-1, keepdims=True), sinks[:, :, None, None])
            p = jnp.exp(s - lse)
        return jnp.einsum('bhgqk,bkhd->bqhgd', p.astype(vc.dtype), vc)

    o = lax.map(block, (qb, jnp.arange(nb)))
    return o.swapaxes(0, 1).reshape(B, S, Hkv, G, d)


def diff_attention(q, k, v, lam_params, subln_g, slopes, lam_init):
    B, S, H, _, d = q.shape
    nb = S // QBLOCK
    lp = lam_params.astype(jnp.float32)
    lam = jnp.exp(jnp.sum(lp[0] * lp[1])) - jnp.exp(jnp.sum(lp[2] * lp[3])) + lam_init
    qb = q.reshape(B, nb, QBLOCK, H, 2, d).swapaxes(0, 1)
    kpos = jnp.arange(S)
    scale = d ** -0.5

    def block(args):
        qi, i = args
        s = jnp.einsum('bqhcd,bkhcd->bhcqk', qi, k).astype(jnp.float32) * scale
        t = i * QBLOCK + jnp.arange(QBLOCK)
        dist = t[:, None] - kpos[None, :]
        s = s - slopes[:, None, None, None] * dist.astype(jnp.float32)
        s = jnp.where(dist >= 0, s, -jnp.inf)
        p = jax.nn.softmax(s, axis=-1)
        w = p[:, :, 0] - lam * p[:, :, 1]
        return jnp.einsum('bhqk,bkhe->bqhe', w.astype(v.dtype), v)

    o = lax.map(block, (qb, jnp.arange(nb)))
    o = o.swapaxes(0, 1).reshape(B, S, H, 2 * d)
    return rms_norm(o, subln_g) * (1.0 - lam_init)


def compress_blocks(x, pos, w1, b1, w2, b2):
    B, S, Hkv, d = x.shape
    nc = (S - CMP_BLOCK) // CMP_STRIDE + 1
    idx = jnp.arange(nc)[:, None] * CMP_STRIDE + jnp.arange(CMP_BLOCK)[None, :]
    blk = x[:, idx] + pos[:, None, :]
    blk = blk.transpose(0, 1, 3, 2, 4).reshape(B, nc, Hkv, CMP_BLOCK * d)
    hid = jax.nn.gelu(blk @ w1 + b1)
    return hid @ w2 + b2


def nsa_attention(q, k_cmp, v_cmp, k_slc, v_slc, k_win, v_win, gates, cmp_pos, cmp_w1, cmp_b1, cmp_w2, cmp_b2, slopes):
    B, S, Hkv, G, d = q.shape
    scale = d ** -0.5
    t = jnp.arange(S)
    kc = compress_blocks(k_cmp, cmp_pos[0], cmp_w1[0], cmp_b1[0], cmp_w2[0], cmp_b2[0])
    vc = compress_blocks(v_cmp, cmp_pos[1], cmp_w1[1], cmp_b1[1], cmp_w2[1], cmp_b2[1])
    nc = kc.shape[1]
    cstart = jnp.arange(nc) * CMP_STRIDE
    cvalid = (cstart[None, :] + CMP_BLOCK - 1) <= t[:, None]
    s = jnp.einsum('bqhgd,bchd->bhgqc', q, kc).astype(jnp.float32) * scale
    p_cmp = jax.nn.softmax(jnp.where(cvalid, s, -1e30), axis=-1) * cvalid
    o_cmp = jnp.einsum('bhgqc,bchd->bqhgd', p_cmp.astype(vc.dtype), vc)
    nsb = S // SEL_BLOCK
    sstart = jnp.arange(nsb) * SEL_BLOCK
    overlap = jnp.clip(jnp.minimum(cstart[:, None] + CMP_BLOCK, sstart[None, :] + SEL_BLOCK)
                       - jnp.maximum(cstart[:, None], sstart[None, :]), 0).astype(jnp.float32) / CMP_BLOCK
    imp = jnp.einsum('bhgqc,cn->bhqn', p_cmp, overlap)
    cur = t // SEL_BLOCK
    blk = jnp.arange(nsb)
    causal_blk = blk[None, :] <= cur[:, None]
    forced = (blk[None, :] == 0) | (blk[None, :] == cur[:, None]) | (blk[None, :] == cur[:, None] - 1)
    score = jnp.where(forced, jnp.inf, jnp.where(causal_blk, imp, -jnp.inf))
    n_sel = min(SEL_TOPN, nsb)
    top_s, top_i = lax.top_k(score, n_sel)
    sel_ok = top_s > -jnp.inf
    kb = k_slc.reshape(B, nsb, SEL_BLOCK, Hkv, d).transpose(0, 3, 1, 2, 4)
    vb = v_slc.reshape(B, nsb, SEL_BLOCK, Hkv, d).transpose(0, 3, 1, 2, 4)
    nq = S // SEL_QCHUNK
    qch = q.reshape(B, nq, SEL_QCHUNK, Hkv, G, d).swapaxes(0, 1)
    ich = top_i.reshape(B, Hkv, nq, SEL_QCHUNK, n_sel).transpose(2, 0, 1, 3, 4)
    och = sel_ok.reshape(B, Hkv, nq, SEL_QCHUNK, n_sel).transpose(2, 0, 1, 3, 4)
    bidx = jnp.arange(B)[:, None, None, None]
    hidx = jnp.arange(Hkv)[None, :, None, None]

    def chunk(args):
        qi, ii, oki, cidx = args
        kg = kb[bidx, hidx, ii]
        vg = vb[bidx, hidx, ii]
        sc = jnp.einsum('bqhgd,bhqnld->bhgqnl', qi, kg).astype(jnp.float32) * scale
        tq = cidx * SEL_QCHUNK + jnp.arange(SEL_QCHUNK)
        kpos = ii[..., None] * SEL_BLOCK + jnp.arange(SEL_BLOCK)
        dist = tq[None, None, :, None, None] - kpos
        ok = (dist >= 0) & oki[..., None]
        sc = sc - slopes[None, :, :, None, None, None] * dist[:, :, None].astype(jnp.float32)
        sc = jnp.where(ok[:, :, None], sc, -jnp.inf)
        shp = sc.shape
        pr = jax.nn.softmax(sc.reshape(shp[:4] + (shp[4] * shp[5],)), axis=-1).reshape(shp)
        return jnp.einsum('bhgqnl,bhqnld->bqhgd', pr.astype(vg.dtype), vg)

    o_slc = lax.map(chunk, (qch, ich, och, jnp.arange(nq)))
    o_slc = o_slc.swapaxes(0, 1).reshape(B, S, Hkv, G, d)
    o_win = banded_attention(q, k_win, v_win, NSA_WINDOW, slopes)
    g = jax.nn.sigmoid(gates)
    return g[..., 0:1] * o_cmp + g[..., 1:2] * o_slc + g[..., 2:3] * o_win


def hybrid_mixer(h, w_in, b_in, sinks, diff_lambda, diff_subln_g, cmp_pos, cmp_w1, cmp_b1, cmp_w2, cmp_b2, w_branch, w_out, lam_init):
    B, S, _ = h.shape
    z = h @ w_in + b_in
    offs = np.cumsum(IN_SPLITS)[:-1].tolist()
    (qa, ka, va, qb, kb, vb, qc, kcm, vcm, ksl, vsl, kwn, vwn, g_nsa, g_merge) = jnp.split(z, offs, axis=-1)
    ga = SWA_HEADS // SWA_KV_HEADS
    o_a = banded_attention(qa.reshape(B, S, SWA_KV_HEADS, ga, HEAD_DIM),
                           ka.reshape(B, S, SWA_KV_HEADS, HEAD_DIM), va.reshape(B, S, SWA_KV_HEADS, HEAD_DIM),
                           SWA_WINDOW, alibi_slopes(SWA_HEADS).reshape(SWA_KV_HEADS, ga),
                           sinks.astype(jnp.float32).reshape(SWA_KV_HEADS, ga)).reshape(B, S, BRANCH_WIDTH)
    o_b = diff_attention(qb.reshape(B, S, DIFF_HEADS, 2, HEAD_DIM), kb.reshape(B, S, DIFF_HEADS, 2, HEAD_DIM),
                         vb.reshape(B, S, DIFF_HEADS, 2 * HEAD_DIM), diff_lambda, diff_subln_g,
                         alibi_slopes(DIFF_HEADS), lam_init).reshape(B, S, BRANCH_WIDTH)
    gc = NSA_HEADS // NSA_KV_HEADS
    kv = lambda a: a.reshape(B, S, NSA_KV_HEADS, HEAD_DIM)
    o_c = nsa_attention(qc.reshape(B, S, NSA_KV_HEADS, gc, HEAD_DIM), kv(kcm), kv(vcm), kv(ksl), kv(vsl), kv(kwn), kv(vwn),
                        g_nsa.reshape(B, S, NSA_KV_HEADS, gc, 3), cmp_pos, cmp_w1, cmp_b1, cmp_w2, cmp_b2,
                        alibi_slopes(NSA_HEADS).reshape(NSA_KV_HEADS, gc)).reshape(B, S, BRANCH_WIDTH)
    gm = jax.nn.sigmoid(g_merge.reshape(B, S, N_BRANCHES, D_MODEL))
    merged = gm[:, :, 0] * (o_a @ w_branch[0]) + gm[:, :, 1] * (o_b @ w_branch[1]) + gm[:, :, 2] * (o_c @ w_branch[2])
    return merged @ w_out


def moe_ffn(h, router_w, router_b, w1, b1, w2, b2):
    B, S, D = h.shape
    N = B * S
    xt = h.reshape(N, D)
    logits = (xt @ router_w + router_b).astype(jnp.float32)
    top_v, top_e = lax.top_k(logits, TOP_K)
    gate = jax.nn.softmax(top_v, axis=-1)
    A = N * TOP_K
    e_flat = top_e.reshape(A)
    order = jnp.argsort(e_flat)
    e_sorted = e_flat[order]
    tok_sorted = order // TOP_K
    g_sorted = gate.reshape(A)[order]
    counts = jnp.bincount(e_flat, length=N_EXPERTS)
    group_start = jnp.cumsum(counts) - counts
    padded = (counts + MOE_BLOCK - 1) // MOE_BLOCK * MOE_BLOCK
    pad_end = jnp.cumsum(padded)
    pad_start = pad_end - padded
    dest = pad_start[e_sorted] + (jnp.arange(A) - group_start[e_sorted])
    n_blocks = -(-A // MOE_BLOCK) + N_EXPERTS
    P = n_blocks * MOE_BLOCK
    row_tok = jnp.zeros((P,), jnp.int32).at[dest].set(tok_sorted.astype(jnp.int32))
    row_w = jnp.zeros((P,), jnp.float32).at[dest].set(g_sorted)
    block_expert = jnp.clip(jnp.searchsorted(pad_end, jnp.arange(n_blocks) * MOE_BLOCK, side='right'), 0, N_EXPERTS - 1)

    def expert_block(args):
        rows, e = args
        xb = xt[rows]
        hb = xb @ w1[e] + b1[e]
        g, u = hb[:, :D_EXPERT], hb[:, D_EXPERT:]
        g = jnp.minimum(g, SWIGLU_LIMIT)
        u = jnp.clip(u, -SWIGLU_LIMIT, SWIGLU_LIMIT)
        glu = g * jax.nn.sigmoid(g * SWIGLU_ALPHA)
        return ((u + 1.0) * glu) @ w2[e] + b2[e]

    y = lax.map(expert_block, (row_tok.reshape(n_blocks, MOE_BLOCK), block_expert)).reshape(P, D)
    out = jnp.zeros((N, D), y.dtype).at[row_tok].add(y * row_w[:, None].astype(y.dtype))
    return out.reshape(B, S, D)


def setup_inputs(seed: int = 0) -> dict:
    key = jax.random.key(seed)
    ks = jax.random.split(key, 32)
    L, D, E, F = DEPTH, D_MODEL, N_EXPERTS, D_EXPERT
    nrm = lambda k, shape, s: jax.random.normal(k, shape, jnp.float32) * s
    return {
        'x': nrm(ks[0], (BATCH, SEQ, D), 1.0),
        'c': nrm(ks[1], (BATCH, D), 1.0),
        'mod_w': nrm(ks[2], (L, D, 6 * D), 0.5 * D ** -0.5),
        'mod_b': nrm(ks[3], (L, 6 * D), 0.02),
        'norm1_g': 1.0 + nrm(ks[4], (L, D), 0.1),
        'norm2_g': 1.0 + nrm(ks[5], (L, D), 0.1),
        'w_in': nrm(ks[6], (L, D, IN_WIDTH), D ** -0.5),
        'b_in': nrm(ks[7], (L, IN_WIDTH), 0.02),
        'sinks': nrm(ks[8], (L, SWA_HEADS), 0.5),
        'diff_lambda': nrm(ks[9], (L, 4, HEAD_DIM), 0.1),
        'diff_subln_g': 1.0 + nrm(ks[10], (L, 2 * HEAD_DIM), 0.1),
        'cmp_pos': nrm(ks[11], (L, 2, CMP_BLOCK, HEAD_DIM), 0.1),
        'cmp_w1': nrm(ks[12], (L, 2, CMP_BLOCK * HEAD_DIM, CMP_HIDDEN), (CMP_BLOCK * HEAD_DIM) ** -0.5),
        'cmp_b1': nrm(ks[13], (L, 2, CMP_HIDDEN), 0.02),
        'cmp_w2': nrm(ks[14], (L, 2, CMP_HIDDEN, HEAD_DIM), CMP_HIDDEN ** -0.5),
        'cmp_b2': nrm(ks[15], (L, 2, HEAD_DIM), 0.02),
        'w_branch': nrm(ks[16], (L, N_BRANCHES, BRANCH_WIDTH, D), BRANCH_WIDTH ** -0.5),
        'w_out': nrm(ks[17], (L, D, D), D ** -0.5),
        'router_w': nrm(ks[18], (L, D, E), D ** -0.5),
        'router_b': nrm(ks[19], (L, E), 0.01),
        'exp_w1': nrm(ks[20], (L, E, D, 2 * F), D ** -0.5),
        'exp_b1': nrm(ks[21], (L, E, 2 * F), 0.02),
        'exp_w2': nrm(ks[22], (L, E, F, D), F ** -0.5),
        'exp_b2': nrm(ks[23], (L, E, D), 0.02),
        'final_g': 1.0 + nrm(ks[24], (D,), 0.1),
    }


def reference(x, c, mod_w, mod_b, norm1_g, norm2_g, w_in, b_in, sinks, diff_lambda, diff_subln_g, cmp_pos, cmp_w1, cmp_b1, cmp_w2, cmp_b2, w_branch, w_out, router_w, router_b, exp_w1, exp_b1, exp_w2, exp_b2, final_g):
    cs = jax.nn.silu(c)
    for l in range(DEPTH):
        mod = cs @ mod_w[l] + mod_b[l]
        sh1, sc1, g1, sh2, sc2, g2 = [m[:, None, :] for m in jnp.split(mod, 6, axis=-1)]
        lam_init = 0.8 - 0.6 * math.exp(-0.3 * l)
        h = rms_norm(x, norm1_g[l]) * (1.0 + sc1) + sh1
        x = x + g1 * hybrid_mixer(h, w_in[l], b_in[l], sinks[l], diff_lambda[l], diff_subln_g[l], cmp_pos[l], cmp_w1[l],
                                  cmp_b1[l], cmp_w2[l], cmp_b2[l], w_branch[l], w_out[l], lam_init)
        h = rms_norm(x, norm2_g[l]) * (1.0 + sc2) + sh2
        x = x + g2 * moe_ffn(h, router_w[l], router_b[l], exp_w1[l], exp_b1[l], exp_w2[l], exp_b2[l])
    return rms_norm(x, final_g)
```

```python
import numpy as np
import ml_dtypes
from contextlib import ExitStack
import concourse.bass as bass
import concourse.mybir as mybir
from concourse.bass_utils import run_bass_kernel_spmd

F32 = mybir.dt.float32
BF16 = mybir.dt.bfloat16
I32 = mybir.dt.int32
AF = mybir.ActivationFunctionType
ALU = mybir.AluOpType
AX = mybir.AxisListType
NDSEM = 8


class R:
    __slots__ = ("name", "lw", "rd")

    def __init__(self, name=""):
        self.name = name
        self.lw = {}
        self.rd = {}


class T(R):
    __slots__ = ("t",)

    def __init__(self, t, name=""):
        super().__init__(name)
        self.t = t

    def __getitem__(self, k):
        return self.t[k]


class Eng:
    def __init__(self, name, h, sem, dsems):
        self.name = name
        self.h = h
        self.sem = sem
        self.count = 0
        self.known = {}
        self.dsems = dsems
        self.ndma = 0


class Sched:
    def __init__(self, nc, stack):
        self.nc = nc
        self.stack = stack
        self.E = {}
        for name, h, nd in (("pe", nc.tensor, 0), ("act", nc.scalar, NDSEM), ("dve", nc.vector, 0),
                            ("pool", nc.gpsimd, NDSEM), ("sp", nc.sync, NDSEM)):
            sem = stack.enter_context(nc.semaphore("s_" + name))
            ds = [stack.enter_context(nc.semaphore("d_%s%d" % (name, i))) for i in range(nd)]
            self.E[name] = Eng(name, h, sem, ds)
        self.dma_tokens = []
        self.nuniq = 0

    def sb(self, shape, dt, name=None, stack=None):
        self.nuniq += 1
        name = (name or "t") + "_%d" % self.nuniq
        t = (stack or self.stack).enter_context(self.nc.sbuf_tensor(name, list(shape), dt))
        return T(t, name)

    def ps(self, shape, dt=F32, name=None, stack=None):
        self.nuniq += 1
        name = (name or "p") + "_%d" % self.nuniq
        t = (stack or self.stack).enter_context(self.nc.psum_tensor(name, list(shape), dt))
        return T(t, name)

    def _wait(self, eng, tok):
        sem, val = tok
        if eng.known.get(sem, 0) >= val:
            return
        eng.h.wait_ge(sem, val)
        eng.known[sem] = val

    def _deps(self, eng, reads, writes):
        deps = []
        for r in reads:
            deps.extend(r.lw.items())
        for w in writes:
            deps.extend(w.lw.items())
            deps.extend(w.rd.items())
        for tok in deps:
            if tok[0] is eng.sem:
                if eng.name == "pe":
                    continue
                if tok[1] > eng.count:
                    continue
            self._wait(eng, tok)

    def _commit(self, tok, reads, writes):
        sem, val = tok
        for r in reads:
            if r.rd.get(sem, 0) < val:
                r.rd[sem] = val
        for w in writes:
            if w.lw.get(sem, 0) < val:
                w.lw[sem] = val

    def op(self, engname, fn, reads=(), writes=(), sig=True):
        eng = self.E[engname]
        self._deps(eng, reads, writes)
        ins = fn(eng.h)
        if sig:
            eng.count += 1
            ins.then_inc(eng.sem, 1)
            tok = (eng.sem, eng.count)
        else:
            tok = (eng.sem, eng.count + 1)
        self._commit(tok, reads, writes)
        return tok

    def dma(self, qname, out, in_, reads=(), writes=(), **kw):
        q = self.E[qname]
        i = q.ndma
        q.ndma += 1
        sem = q.dsems[i % NDSEM]
        val = 16 * (i // NDSEM + 1)
        if i >= NDSEM:
            self._wait(q, (sem, val - 16))
        self._deps(q, reads, writes)
        q.h.dma_start(out=out, in_=in_, **kw).then_inc(sem, 16)
        tok = (sem, val)
        self._commit(tok, reads, writes)
        self.dma_tokens.append(tok)
        return tok

    def idma(self, out, in_, out_off=None, in_off=None, reads=(), writes=(), bounds=None):
        q = self.E["pool"]
        i = q.ndma
        q.ndma += 1
        sem = q.dsems[i % NDSEM]
        val = 16 * (i // NDSEM + 1)
        if i >= NDSEM:
            self._wait(q, (sem, val - 16))
        self._deps(q, reads, writes)
        oo = bass.IndirectOffsetOnAxis(ap=out_off, axis=0) if out_off is not None else None
        io = bass.IndirectOffsetOnAxis(ap=in_off, axis=0) if in_off is not None else None
        if bounds is None:
            q.h.indirect_dma_start(out=out, out_offset=oo, in_=in_, in_offset=io).then_inc(sem, 16)
        else:
            if getattr(self, "bound_reg", None) is None:
                self.bound_reg = q.h.alloc_register("bnd")
                q.h.reg_mov(self.bound_reg, 65535)
            q.h.indirect_dma_start(out=out, out_offset=oo, in_=in_, in_offset=io, bounds_check=self.bound_reg, oob_is_err=False).then_inc(sem, 16)
        tok = (sem, val)
        self._commit(tok, reads, writes)
        self.dma_tokens.append(tok)
        return tok

    def barrier(self):
        toks = []
        for e in self.E.values():
            if e.count > 0:
                toks.append((e.sem, e.count))
            for j, s in enumerate(e.dsems):
                n = (e.ndma - j + NDSEM - 1) // NDSEM
                if n > 0:
                    toks.append((s, 16 * n))
        for e in self.E.values():
            for tok in toks:
                if tok[0] is e.sem:
                    continue
                self._wait(e, tok)

    def finish(self):
        sp = self.E["sp"]
        for e in self.E.values():
            for j, s in enumerate(e.dsems):
                n = (e.ndma - j + NDSEM - 1) // NDSEM
                if n > 0:
                    self._wait(sp, (s, 16 * n))


NCORES = 8
L_DEPTH = 2
D = 1024
S = 2048
NSEQ = 2
NT = S // 128
EPS = 1e-5
INW = 6680
SCALE = 0.125
NEG = -30000.0
NE = 32
SLOT_COL = ([0 + 64 * i for i in range(8)] + [512 + 64 * i for i in range(2)] + [768 + 64 * i for i in range(8)]
            + [1280 + 64 * i for i in range(8)] + [2304 + 64 * i for i in range(8)] + [2816 + 64 * i for i in range(2)]
            + [2944 + 64 * i for i in range(2)] + [3072 + 64 * i for i in range(2)] + [3328 + 64 * i for i in range(2)])
NSLOT = len(SLOT_COL)
SL_QA, SL_KA, SL_QB, SL_KB, SL_QC, SL_KCM, SL_VCM, SL_KSL, SL_KWN = 0, 8, 10, 18, 26, 34, 36, 38, 40
C_VA, C_VB, C_VSL, C_VWN, C_GN, C_GM = 640, 1792, 3200, 3456, 3584, 3608


def host_consts():
    bf = ml_dtypes.bfloat16
    c = {}
    t = np.arange(S)
    a_t, b_t = (t // 128).astype(np.float32), (t % 128).astype(np.float32)
    aug = np.zeros((NSLOT, 4, S), np.float32)
    kaug = np.stack([a_t, b_t, np.ones(S, np.float32), np.ones(S, np.float32)])

    def qaug(slope):
        return np.stack([np.full(S, 1024.0 * slope, np.float32), np.full(S, 8.0 * slope, np.float32),
                         -1024.0 * slope * a_t, -8.0 * slope * b_t])
    for i in range(8):
        aug[SL_QA + i] = qaug(2.0 ** -(i + 1))
        aug[SL_QC + i] = qaug(2.0 ** -(i + 1))
        aug[SL_QB + i] = qaug(2.0 ** (-2.0 * (i // 2 + 1)))
        aug[SL_KB + i] = kaug
    for i in range(2):
        aug[SL_KA + i] = kaug
        aug[SL_KSL + i] = kaug
        aug[SL_KWN + i] = kaug
    c["aug"] = aug.astype(bf)
    c["ident"] = np.eye(128, dtype=np.float32).astype(bf)
    c["ident32"] = np.eye(128, dtype=np.float32)
    sk = np.arange(128)[:, None]
    tq = np.arange(128)[None, :]
    c["mdiag"] = np.where(tq >= sk, 0.0, NEG).astype(bf)
    c["medge"] = np.where(tq < sk, 0.0, NEG).astype(bf)
    cc = np.arange(128)[:, None]
    c["cmaskT"] = np.where((16 * cc + 31 <= t[None, :]) & (cc < 127), 0.0, NEG).astype(bf)
    nb = np.arange(32)
    c["eblk"] = (t[None, :] // 64 == nb[:, None]).astype(np.float32).astype(bf)
    cur = t // 64
    forced = (nb[None, :] == 0) | (nb[None, :] == cur[:, None]) | (nb[None, :] == cur[:, None] - 1)
    causal = nb[None, :] <= cur[:, None]
    A = np.where(forced, 1e9 + 1e6 * nb[None, :], np.where(causal, 0.0, -1e9 - 1e6 * nb[None, :]))
    c["atab"] = A.astype(np.float32)
    cstart = np.arange(127) * 16
    sstart = nb * 64
    ov = np.clip(np.minimum(cstart[:, None] + 32, sstart[None, :] + 64) - np.maximum(cstart[:, None], sstart[None, :]), 0, None) / 32.0
    ovp = np.zeros((128, 32), np.float32)
    ovp[:127] = ov
    c["overlap"] = ovp.astype(bf)
    c["ltri"] = (np.arange(128)[:, None] < np.arange(128)[None, :]).astype(np.float32).astype(bf)
    c["ones128"] = np.ones((128, 128), np.float32).astype(bf)
    c["b512"] = np.broadcast_to((512.0 * np.arange(64, dtype=np.float32))[None, :], (128, 64)).copy()
    c["rowiota"] = (np.arange(8, dtype=np.float32)[None, :] * 128 + np.arange(128, dtype=np.float32)[:, None]).copy()
    c["piota"] = np.arange(128, dtype=np.float32).reshape(128, 1).copy()
    return c


def host_inputs(inp, core):
    b0 = core * NSEQ
    m = {}
    m["x"] = np.ascontiguousarray(inp["x"][b0:b0 + NSEQ].reshape(NSEQ * S, D))
    m["cT"] = np.ascontiguousarray(inp["c"][b0:b0 + NSEQ].T)
    for k in ("mod_w", "mod_b", "norm1_g", "norm2_g", "w_in", "b_in", "sinks", "diff_subln_g", "cmp_w1", "cmp_w2",
              "cmp_b2", "w_branch", "w_out", "router_w", "router_b", "exp_b2"):
        m[k] = inp[k]
    m["final_g"] = inp["final_g"].reshape(1, D)
    m["b_inT"] = np.ascontiguousarray(np.stack([inp["b_in"][:, c0:c0 + 64] for c0 in SLOT_COL], axis=2))
    m["diff_lambda"] = inp["diff_lambda"].reshape(L_DEPTH, 256)
    m["cmp_posT"] = np.ascontiguousarray(inp["cmp_pos"].transpose(0, 1, 3, 2))
    m["cmp_b1T"] = np.ascontiguousarray(inp["cmp_b1"].reshape(L_DEPTH, 2, 2, 128).transpose(0, 1, 3, 2))
    m["cmp_b2T"] = np.ascontiguousarray(inp["cmp_b2"].reshape(L_DEPTH, 2, 64, 1))
    m["exp_b1E"] = np.ascontiguousarray(inp["exp_b1"].reshape(L_DEPTH, NE, 2, 128, 8).transpose(0, 1, 3, 2, 4)).reshape(L_DEPTH * NE * 128, 16)
    m["exp_w1"] = inp["exp_w1"].reshape(L_DEPTH * NE * 256, 4 * 2 * D)
    m["exp_w2"] = inp["exp_w2"].reshape(L_DEPTH * NE * 128, 8 * D)
    return m


IN_SHAPES = {
    "x": ([NSEQ * S, D], F32), "cT": ([D, NSEQ], F32), "mod_w": ([L_DEPTH, D, 6 * D], F32), "mod_b": ([L_DEPTH, 6 * D], F32),
    "norm1_g": ([L_DEPTH, D], F32), "norm2_g": ([L_DEPTH, D], F32), "w_in": ([L_DEPTH, D, INW], F32), "b_in": ([L_DEPTH, INW], F32),
    "sinks": ([L_DEPTH, 8], F32), "diff_subln_g": ([L_DEPTH, 128], F32), "cmp_w1": ([L_DEPTH, 2, 2048, 256], F32),
    "cmp_w2": ([L_DEPTH, 2, 256, 64], F32), "cmp_b2": ([L_DEPTH, 2, 64], F32), "w_branch": ([L_DEPTH, 3, 512, D], F32),
    "w_out": ([L_DEPTH, D, D], F32), "router_w": ([L_DEPTH, D, NE], F32), "router_b": ([L_DEPTH, NE], F32),
    "exp_w1": ([L_DEPTH * NE * 256, 8 * D], F32), "exp_w2": ([L_DEPTH * NE * 128, 8 * D], F32), "exp_b2": ([L_DEPTH, NE, D], F32),
    "final_g": ([1, D], F32), "b_inT": ([L_DEPTH, 64, NSLOT], F32), "diff_lambda": ([L_DEPTH, 256], F32),
    "cmp_posT": ([L_DEPTH, 2, 64, 32], F32), "cmp_b1T": ([L_DEPTH, 2, 128, 2], F32), "cmp_b2T": ([L_DEPTH, 2, 64, 1], F32),
    "exp_b1E": ([L_DEPTH * NE * 128, 16], F32),
    "ltri": ([128, 128], BF16), "ones128": ([128, 128], BF16), "b512": ([128, 64], F32), "rowiota": ([128, 8], F32), "piota": ([128, 1], F32),
    "aug": ([NSLOT, 4, S], BF16), "ident": ([128, 128], BF16), "ident32": ([128, 128], F32), "mdiag": ([128, 128], BF16),
    "medge": ([128, 128], BF16), "cmaskT": ([128, S], BF16), "eblk": ([32, S], BF16), "atab": ([S, 32], F32),
    "overlap": ([128, 32], BF16),
}


def bcast_rows(ap1d_row, nparts):
    return ap1d_row.broadcast_to([nparts, ap1d_row.shape[-1]])


class K:
    def __init__(self, dbg=(), layers=L_DEPTH, nseq=NSEQ, stop_after=None, only=None):
        self.dbg = set(dbg)
        self.only = only
        self.layers = layers
        self.nseq = nseq
        self.stop_after = stop_after
        nc = self.nc = bass.Bass("TRN2", target_bir_lowering=False)
        self.I = {k: nc.dram_tensor(k, list(sh), dt, kind="ExternalInput").ap() for k, (sh, dt) in IN_SHAPES.items()}
        self.out = nc.dram_tensor("out", [NSEQ * S, D], F32, kind="ExternalOutput").ap()
        self.Dr = {}
        self.DR = {}

    def dram(self, name, shape, dt):
        kind = "ExternalOutput" if name in self.dbg else "Internal"
        self.Dr[name] = self.nc.dram_tensor(name, list(shape), dt, kind=kind).ap()
        self.DR[name] = R(name)
        return self.Dr[name]

    def mm(self, out, lhsT, rhs, start, stop, reads, writes, sig=True):
        return self.s.op("pe", lambda h: h.matmul(out, lhsT=lhsT, rhs=rhs, start=start, stop=stop), reads=reads, writes=writes, sig=sig)

    def tr(self, out, in_, ident, reads, writes, sig=True):
        return self.s.op("pe", lambda h: h.transpose(out=out, in_=in_, identity=ident), reads=reads, writes=writes, sig=sig)

    def rstd_of(self, xt, junk, ss, n=D):
        s = self.s
        s.op("act", lambda h: h.activation(out=junk[1], in_=xt[1], func=AF.Square, accum_out=ss[:]), reads=[xt[0]], writes=[junk[0], ss])
        s.op("dve", lambda h: h.tensor_scalar(out=ss[:], in0=ss[:], scalar1=1.0 / n, scalar2=EPS, op0=ALU.mult, op1=ALU.add), reads=[ss], writes=[ss])
        s.op("act", lambda h: h.sqrt(out=ss[:], in_=ss[:]), reads=[ss], writes=[ss])
        s.op("dve", lambda h: h.reciprocal(out=ss[:], in_=ss[:]), reads=[ss], writes=[ss])

    def build(self):
        nc = self.nc
        with ExitStack() as st:
            s = self.s = Sched(nc, st)
            self.P = [s.ps([128, 512], F32, "bank%d" % i) for i in range(8)]
            self.ident = s.sb([128, 128], BF16, "ident")
            self.ident32 = s.sb([128, 128], F32, "ident32")
            s.dma("sp", self.ident[:], self.I["ident"], writes=[self.ident])
            s.dma("sp", self.ident32[:], self.I["ident32"], writes=[self.ident32])
            self.dram("mod_d", [L_DEPTH, NSEQ, 6 * D], F32)
            self.dram("xres", [NSEQ * S, D], F32)
            self.dram("QT_d", [NSLOT, 64, S], BF16)
            self.dram("VA_d", [S, 2, 65], BF16)
            self.dram("VB_d", [S, 4, 129], BF16)
            self.dram("VSL_d", [S, 2, 65], BF16)
            self.dram("VWN_d", [S, 2, 65], BF16)
            self.dram("GN_d", [S, 24], F32)
            self.dram("GM_d", [S, 3072], F32)
            self.dram("KC_d", [2, 64, 128], BF16)
            self.dram("VC_d", [2, 128, 97], BF16)
            self.dram("O_d", [S, 1536], BF16)
            self.ntt = self.nseq * NT
            self.nblk = self.ntt + NE
            self.dram("xs_d", [self.nblk * 512, D], BF16)
            self.dram("ys_d", [self.nblk * 512, D], F32)
            self.dram("dbg_d", [128, 4096], F32)
            with ExitStack() as zs:
                zt = s.sb([128, 8192], BF16, "zt", zs)
                s.op("pool", lambda h: h.memset(zt[:], 0.0), writes=[zt])
                xv = self.Dr["xs_d"].rearrange("(a p r) d -> a p (r d)", p=128, r=8)
                for a in range(xv.shape[0]):
                    s.dma("sp", xv[a], zt[:], reads=[zt], writes=[self.DR["xs_d"]])
                s.barrier()
            try:
                self.body()
            except StopIteration:
                pass
            s.barrier()
            s.finish()
        return nc

    def phase_end(self, name):
        self.s.barrier()
        if self.stop_after == name:
            raise StopIteration

    def body(self):
        for l in range(self.layers):
            self.runp("mod%d" % l, self.phase_mod, l)
            for q in range(self.nseq):
                for nm, fn in (("inproj", self.phase_inproj), ("cmp", self.phase_cmp), ("swa", self.phase_swa), ("diff", self.phase_diff),
                               ("nsa", self.phase_nsa), ("merge", self.phase_merge)):
                    self.runp("%s%d_%d" % (nm, l, q), fn, l, q)
            self.runp("moe%d" % l, self.phase_moe2, l)

    def runp(self, name, fn, *args):
        if self.only is None or name in self.only:
            fn(*args)
        self.phase_end(name)

    def phase_mod(self, l):
        s, I = self.s, self.I
        with ExitStack() as ps:
            cT = s.sb([128, 8, NSEQ], F32, "cT", ps)
            cs = s.sb([128, 8, NSEQ], F32, "cs", ps)
            modb = s.sb([NSEQ, 6 * D], F32, "modb", ps)
            mods = s.sb([NSEQ, 6 * D], F32, "mods", ps)
            wb = [s.sb([128, 8, 512], F32, "modw", ps) for _ in range(2)]
            s.dma("sp", cT[:], I["cT"].rearrange("(kc p) b -> p kc b", p=128), writes=[cT])
            s.dma("sp", modb[:], I["mod_b"][l:l + 1, :].broadcast_to([NSEQ, 6 * D]), writes=[modb])
            s.op("act", lambda h: h.activation(out=cs[:], in_=cT[:], func=AF.Silu), reads=[cT], writes=[cs])
            wsrc = I["mod_w"][l].rearrange("(kc p) n -> p kc n", p=128)
            for cg in range(12):
                w = wb[cg % 2]
                s.dma("sp", w[:], wsrc[:, :, cg * 512:(cg + 1) * 512], writes=[w])
                pm = self.P[cg % 2]
                for kc in range(8):
                    self.mm(pm[0:NSEQ, :], cs[:, kc, :], w[:, kc, :], kc == 0, kc == 7, [cs, w], [pm], sig=(kc == 7))
                s.op("dve", lambda h: h.tensor_tensor(out=mods[:, cg * 512:(cg + 1) * 512], in0=pm[0:NSEQ, :], in1=modb[:, cg * 512:(cg + 1) * 512], op=ALU.add),
                     reads=[pm, modb], writes=[mods])
            for seg in (1, 4):
                s.op("dve", lambda h: h.tensor_scalar_add(out=mods[:, seg * D:(seg + 1) * D], in0=mods[:, seg * D:(seg + 1) * D], scalar1=1.0), reads=[mods], writes=[mods])
            s.dma("sp", self.Dr["mod_d"][l], mods[:], reads=[mods], writes=[self.DR["mod_d"]])

    def mod_bc(self, l, q, seg, tile):
        src = self.Dr["mod_d"][l, q:q + 1, seg * D:(seg + 1) * D].broadcast_to([128, D])
        self.s.dma("sp", tile[:], src, reads=[self.DR["mod_d"]], writes=[tile])

    def xsrc(self, l, q):
        return (self.I["x"] if l == 0 else self.Dr["xres"]), ([] if l == 0 else [self.DR["xres"]])

    def norm_tiles(self, ps, l, q, gname, seg_sc, seg_sh):
        s, I = self.s, self.I
        gsc = s.sb([128, D], F32, "gsc", ps)
        sh = s.sb([128, D], F32, "sh", ps)
        gt = s.sb([128, D], F32, "gt", ps)
        s.dma("sp", gt[:], I[gname][l:l + 1, :].broadcast_to([128, D]), writes=[gt])
        self.mod_bc(l, q, seg_sc, gsc)
        self.mod_bc(l, q, seg_sh, sh)
        s.op("dve", lambda h: h.tensor_tensor(out=gsc[:], in0=gsc[:], in1=gt[:], op=ALU.mult), reads=[gsc, gt], writes=[gsc])
        return gsc, sh

    def phase_inproj(self, l, q):
        s, I, Dr, DR = self.s, self.I, self.Dr, self.DR
        xsrc, xres_r = self.xsrc(l, q)
        with ExitStack() as ps:
            gsc, sh = self.norm_tiles(ps, l, q, "norm1_g", 1, 0)
            hT = s.sb([128, 8, S], BF16, "hT", ps)
            xt = [s.sb([128, D], F32, "xt", ps) for _ in range(2)]
            junk = s.sb([128, D], F32, "junk", ps)
            hb = [s.sb([128, D], BF16, "hb", ps) for _ in range(2)]
            ss = [s.sb([128, 1], F32, "ss", ps) for _ in range(2)]
            for tt in range(NT):
                x, h, sq = xt[tt % 2], hb[tt % 2], ss[tt % 2]
                r0 = q * S + tt * 128
                s.dma("sp", x[:], xsrc[r0:r0 + 128, :], reads=xres_r, writes=[x])
                self.rstd_of((x, x[:]), (junk, junk[:]), sq)
                s.op("dve", lambda hh: hh.scalar_tensor_tensor(out=x[:], in0=x[:], scalar=sq[:, 0:1], in1=gsc[:], op0=ALU.mult, op1=ALU.mult), reads=[x, sq, gsc], writes=[x])
                s.op("pool", lambda hh: hh.tensor_tensor(out=h[:], in0=x[:], in1=sh[:], op=ALU.add), reads=[x, sh], writes=[h])
                pt = self.P[tt % 2]
                ptb = pt[:].bitcast(BF16)
                for kc in range(8):
                    self.tr(ptb[:, kc * 128:(kc + 1) * 128], h[:, kc * 128:(kc + 1) * 128], self.ident[:], [h, self.ident], [pt], sig=(kc == 7))
                s.op("act", lambda hh: hh.copy(out=hT[:, :, tt * 128:(tt + 1) * 128], in_=ptb.rearrange("p (k t) -> p k t", k=8)), reads=[pt], writes=[hT])
            binT = s.sb([64, NSLOT], F32, "binT", ps)
            s.dma("sp", binT[:], I["b_inT"][l], writes=[binT])
            wsrc = I["w_in"][l].rearrange("(kc p) n -> p kc n", p=128)
            wbuf = [s.sb([128, 8, 512], BF16, "wblk", ps) for _ in range(2)]
            stg = [s.sb([64, S], BF16, "stg", ps) for _ in range(2)]
            groups = [(SL_QA, 8), (SL_KA, 2), (SL_QB, 8), (SL_KB, 8), (SL_QC, 8), (SL_KCM, 4), (SL_KSL, 2), (SL_KWN, 2)]
            nw = 0
            nmm = 0
            for (s0, ns) in groups:
                w = wbuf[nw % 2]
                nw += 1
                c0 = SLOT_COL[s0]
                s.dma("pool", w[:, :, 0:ns * 64], wsrc[:, :, c0:c0 + ns * 64], writes=[w])
                for si in range(ns):
                    slot = s0 + si
                    sg = stg[slot % 2]
                    for tg in range(4):
                        pm = self.P[2 + nmm % 4]
                        nmm += 1
                        for kc in range(8):
                            self.mm(pm[0:64, :], w[:, kc, si * 64:(si + 1) * 64], hT[:, kc, tg * 512:(tg + 1) * 512], kc == 0, kc == 7, [w, hT], [pm], sig=(kc == 7))
                        s.op("act", lambda hh: hh.activation(out=sg[:, tg * 512:(tg + 1) * 512], in_=pm[0:64, :], func=AF.Identity, bias=binT[:, slot:slot + 1]),
                             reads=[pm, binT], writes=[sg])
                    s.dma("sp", Dr["QT_d"][slot], sg[:], reads=[sg], writes=[DR["QT_d"]])
            binb = s.sb([128, INW], F32, "binb", ps)
            s.dma("sp", binb[:], I["b_in"][l:l + 1, :].broadcast_to([128, INW]), writes=[binb])
            vt = {}
            for nm, nh, dv in (("VA_d", 2, 64), ("VB_d", 4, 128), ("VSL_d", 2, 64), ("VWN_d", 2, 64)):
                vt[nm] = [s.sb([128, nh, dv + 1], BF16, "vt", ps) for _ in range(2)]
                for v in vt[nm]:
                    s.op("pool", lambda hh: hh.memset(v[:, :, dv:dv + 1], 1.0), writes=[v])
            gnt = [s.sb([128, 24], F32, "gnt", ps) for _ in range(2)]
            gmt = [s.sb([128, 512], F32, "gmt", ps) for _ in range(2)]
            blocks = [("VA_d", C_VA, 128, 2, 64), ("VB_d", C_VB, 512, 4, 128), ("VSL_d", C_VSL, 128, 2, 64), ("VWN_d", C_VWN, 128, 2, 64),
                      ("GN_d", C_GN, 24, 0, 0)] + [("GM_d", C_GM + 512 * i, 512, i, 0) for i in range(6)]
            for (nm, c0, ncol, nh, dv) in blocks:
                w = wbuf[nw % 2]
                nw += 1
                s.dma("pool", w[:, :, 0:ncol], wsrc[:, :, c0:c0 + ncol], writes=[w])
                for tt in range(NT):
                    pm = self.P[2 + nmm % 4]
                    nmm += 1
                    for kc in range(8):
                        self.mm(pm[:, 0:ncol], hT[:, kc, tt * 128:(tt + 1) * 128], w[:, kc, 0:ncol], kc == 0, kc == 7, [w, hT], [pm], sig=(kc == 7))
                    rows = slice(tt * 128, (tt + 1) * 128)
                    if nm == "GN_d":
                        g = gnt[tt % 2]
                        s.op("dve", lambda hh: hh.tensor_tensor(out=g[:], in0=pm[:, 0:24], in1=binb[:, c0:c0 + 24], op=ALU.add), reads=[pm, binb], writes=[g])
                        s.op("act", lambda hh: hh.activation(out=g[:], in_=g[:], func=AF.Sigmoid), reads=[g], writes=[g])
                        s.dma("sp", Dr["GN_d"][rows, :], g[:], reads=[g], writes=[DR["GN_d"]])
                    elif nm == "GM_d":
                        g = gmt[tt % 2]
                        s.op("dve", lambda hh: hh.tensor_tensor(out=g[:], in0=pm[:, :], in1=binb[:, c0:c0 + 512], op=ALU.add), reads=[pm, binb], writes=[g])
                        s.op("act", lambda hh: hh.activation(out=g[:], in_=g[:], func=AF.Sigmoid), reads=[g], writes=[g])
                        s.dma("sp", Dr["GM_d"][rows, nh * 512:(nh + 1) * 512], g[:], reads=[g], writes=[DR["GM_d"]])
                    else:
                        v = vt[nm][tt % 2]
                        s.op("dve", lambda hh: hh.tensor_tensor(out=v[:, :, 0:dv], in0=pm[:, 0:ncol].rearrange("p (h d) -> p h d", d=dv),
                                                               in1=binb[:, c0:c0 + ncol].rearrange("p (h d) -> p h d", d=dv), op=ALU.add), reads=[pm, binb], writes=[v])
                        s.dma("sp", Dr[nm][rows], v[:], reads=[v], writes=[DR[nm]])

    def phase_cmp(self, l, q):
        s, I, Dr, DR = self.s, self.I, self.Dr, self.DR
        with ExitStack() as ps:
            ovl = self.load_const(ps, "overlap", [128, 32], BF16)
            w1 = s.sb([64, 32, 256], BF16, "cw1", ps)
            w2 = s.sb([128, 2, 64], BF16, "cw2", ps)
            posT = s.sb([64, 32], F32, "posT", ps)
            posb = s.sb([64, 32], BF16, "posb", ps)
            b1T = s.sb([128, 2], F32, "b1T", ps)
            bias = s.sb([128, 2], F32, "cbias", ps)
            b2T = s.sb([64, 1], F32, "b2T", ps)
            b2b = s.sb([128, 64], F32, "b2b", ps)
            xT = s.sb([64, S], BF16, "cxT", ps)
            hid = s.sb([128, 2, 128], BF16, "hid", ps)
            y = s.sb([128, 128], F32, "cy", ps)
            u = s.sb([128, 128], F32, "cu", ps)
            kct = s.sb([64, 128], BF16, "kct", ps)
            vct = s.sb([128, 97], BF16, "vct", ps)
            s.op("pool", lambda h: h.memset(kct[:], 0.0), writes=[kct])
            s.op("pool", lambda h: h.memset(vct[:], 0.0), writes=[vct])
            for which in range(2):
                s.dma("pool", w1[:], I["cmp_w1"][l, which].rearrange("(l d) f -> d l f", d=64), writes=[w1])
                s.dma("pool", w2[:], I["cmp_w2"][l, which].rearrange("(c p) d -> p c d", p=128), writes=[w2])
                s.dma("sp", posT[:], I["cmp_posT"][l, which], writes=[posT])
                s.dma("sp", b1T[:], I["cmp_b1T"][l, which], writes=[b1T])
                s.dma("sp", b2T[:], I["cmp_b2T"][l, which], writes=[b2T])
                s.dma("sp", b2b[:], I["cmp_b2"][l, which:which + 1, :].broadcast_to([128, 64]), writes=[b2b])
                s.op("act", lambda h: h.copy(out=posb[:], in_=posT[:]), reads=[posT], writes=[posb])
                for ch in range(2):
                    pm = self.P[ch]
                    for li in range(32):
                        self.mm(pm[:, 0:1], w1[:, li, ch * 128:(ch + 1) * 128], posb[:, li:li + 1], li == 0, li == 31, [w1, posb], [pm], sig=(li == 31))
                    s.op("dve", lambda h: h.tensor_tensor(out=bias[:, ch:ch + 1], in0=pm[:, 0:1], in1=b1T[:, ch:ch + 1], op=ALU.add), reads=[pm, b1T], writes=[bias])
                for hk in range(2):
                    s.dma("sp", xT[:], Dr["QT_d"][SL_KCM + which * 2 + hk], reads=[DR["QT_d"]], writes=[xT])
                    for ch in range(2):
                        pm = self.P[2 + ch]
                        for li in range(32):
                            self.mm(pm[:, 0:127], w1[:, li, ch * 128:(ch + 1) * 128], xT[:, li:li + 16 * 126 + 1:16], li == 0, li == 31, [w1, xT], [pm], sig=(li == 31))
                        s.op("act", lambda h: h.activation(out=y[:, 0:127], in_=pm[:, 0:127], func=AF.Identity, bias=bias[:, ch:ch + 1]), reads=[pm, bias], writes=[y])
                        s.op("dve", lambda h: h.tensor_tensor(out=u[:, 0:127], in0=y[:, 0:127], in1=y[:, 0:127], op=ALU.mult), reads=[y], writes=[u])
                        s.op("dve", lambda h: h.tensor_scalar(out=u[:, 0:127], in0=u[:, 0:127], scalar1=0.044715, scalar2=1.0, op0=ALU.mult, op1=ALU.add), reads=[u], writes=[u])
                        s.op("dve", lambda h: h.tensor_tensor(out=u[:, 0:127], in0=u[:, 0:127], in1=y[:, 0:127], op=ALU.mult), reads=[u, y], writes=[u])
                        s.op("act", lambda h: h.activation(out=u[:, 0:127], in_=u[:, 0:127], func=AF.Sigmoid, scale=1.5957691216057308), reads=[u], writes=[u])
                        s.op("dve", lambda h: h.tensor_tensor(out=hid[:, ch, 0:127], in0=u[:, 0:127], in1=y[:, 0:127], op=ALU.mult), reads=[u, y], writes=[hid])
                    pm2 = self.P[4 + hk]
                    if which == 0:
                        for ch in range(2):
                            self.mm(pm2[0:64, 0:127], w2[:, ch, :], hid[:, ch, 0:127], ch == 0, ch == 1, [w2, hid], [pm2], sig=(ch == 1))
                        s.op("act", lambda h: h.activation(out=kct[:, 0:127], in_=pm2[0:64, 0:127], func=AF.Identity, bias=b2T[:, 0:1]), reads=[pm2, b2T], writes=[kct])
                        s.dma("sp", Dr["KC_d"][hk], kct[:], reads=[kct], writes=[DR["KC_d"]])
                    else:
                        for ch in range(2):
                            self.mm(pm2[0:127, 0:64], hid[:, ch, 0:127], w2[:, ch, :], ch == 0, ch == 1, [w2, hid], [pm2], sig=(ch == 1))
                        s.op("dve", lambda h: h.tensor_tensor(out=vct[0:127, 0:64], in0=pm2[0:127, 0:64], in1=b2b[0:127, :], op=ALU.add), reads=[pm2, b2b], writes=[vct])
                        s.op("pool", lambda h: h.memset(vct[:, 64:65], 1.0), writes=[vct])
                        s.op("pool", lambda h: h.tensor_copy(out=vct[:, 65:97], in_=ovl[:]), reads=[ovl], writes=[vct])
                        s.dma("sp", Dr["VC_d"][hk], vct[:], reads=[vct], writes=[DR["VC_d"]])

    def load_heads(self, tile, slots, grouped):
        s = self.s
        for g, slot in enumerate(slots):
            dst0 = tile[0:64, g, :] if grouped else tile[0:64, :]
            dst1 = tile[64:68, g, :] if grouped else tile[64:68, :]
            s.dma("sp", dst0, self.Dr["QT_d"][slot], reads=[self.DR["QT_d"]], writes=[tile])
            s.dma("sp", dst1, self.I["aug"][slot], writes=[tile])

    def attn_stream(self, items, banks, pts):
        s = self.s
        n = len(items)

        def emit_qk(k):
            it = items[k]
            sc = banks[k % len(banks)]
            nq = len(it["qk"])
            for m, (outfn, lhsT, rhs, start, stop, reads) in enumerate(it["qk"]):
                self.mm(outfn(sc), lhsT, rhs, start, stop, reads, [sc], sig=(m == nq - 1))

        emit_qk(0)
        if n > 1:
            emit_qk(1)
        for k in range(n):
            if k + 2 < n:
                emit_qk(k + 2)
            it = items[k]
            sc = banks[k % len(banks)]
            pT = pts[k % len(pts)]
            kp, N = it["kp"], it["N"]
            s.op("act", lambda h: h.activation(out=pT[0:kp, 0:N], in_=sc[0:kp, 0:N], func=AF.Exp, scale=SCALE), reads=[sc], writes=[pT])
            it["pv"](pT)

    def load_const(self, ps, name, shape, dt):
        t = self.s.sb(shape, dt, name, ps)
        self.s.dma("sp", t[:], self.I[name], writes=[t])
        return t

    def phase_swa(self, l, q):
        s, I, Dr, DR = self.s, self.I, self.Dr, self.DR
        ident = self.ident
        with ExitStack() as ps:
            mdiag = self.load_const(ps, "mdiag", [128, 128], BF16)
            medge = self.load_const(ps, "medge", [128, 128], BF16)
            esink = s.sb([128, 8], F32, "esink", ps)
            s.dma("sp", esink[:], I["sinks"][l:l + 1, :].broadcast_to([128, 8]), writes=[esink])
            s.op("act", lambda h: h.activation(out=esink[:], in_=esink[:], func=AF.Exp), reads=[esink], writes=[esink])
            QG = s.sb([68, 4, S], BF16, "QG", ps)
            KT = s.sb([68, S], BF16, "KT", ps)
            V = s.sb([128, NT, 65], BF16, "V", ps)
            pts = [s.sb([128, 512], BF16, "pT", ps) for _ in range(3)]
            ot = [s.sb([128, 4, 64], BF16, "ot", ps) for _ in range(2)]
            den = [s.sb([128, 4], F32, "den", ps) for _ in range(2)]
            for hk in range(2):
                self.load_heads(QG, [SL_QA + hk * 4 + g for g in range(4)], True)
                self.load_heads(KT, [SL_KA + hk], False)
                s.dma("sp", V[:], Dr["VA_d"][:, hk, :].rearrange("(n p) e -> p n e", p=128), reads=[DR["VA_d"]], writes=[V])
                for j in range(NT):
                    acc = self.P[4 + j % 2]
                    tiles = [i for i in (j - 1, j) if i >= 0]
                    items = []
                    for idx, i in enumerate(tiles):
                        mask = mdiag if i == j else medge
                        o3 = lambda sc: sc[:, :].rearrange("p (g t) -> p g t", g=4)
                        qk = [(o3, KT[:, i * 128:(i + 1) * 128], QG[:, :, j * 128:(j + 1) * 128], True, False, [KT, QG]),
                              (o3, ident[:], mask[:].unsqueeze(1).broadcast_to([128, 4, 128]), False, True, [ident, mask])]

                        def pv(pT, i=i, idx=idx, acc=acc, last=len(tiles) - 1):
                            for g in range(4):
                                self.mm(acc[:, g * 65:(g + 1) * 65], pT[:, g * 128:(g + 1) * 128], V[:, i, :], idx == 0 and g == 0, idx == last, [pT, V], [acc], sig=(g == 3))
                        items.append(dict(kp=128, N=512, qk=qk, pv=pv))
                    self.attn_stream(items, self.P[0:3], pts)
                    a3 = acc[:, 0:260].rearrange("p (g e) -> p g e", e=65)
                    dn, o = den[j % 2], ot[j % 2]
                    s.op("dve", lambda h: h.tensor_tensor(out=dn[:].unsqueeze(2), in0=a3[:, :, 64:65], in1=esink[:, hk * 4:(hk + 1) * 4].unsqueeze(2), op=ALU.add), reads=[acc, esink], writes=[dn])
                    s.op("dve", lambda h: h.reciprocal(out=dn[:], in_=dn[:]), reads=[dn], writes=[dn])
                    s.op("dve", lambda h: h.tensor_tensor(out=o[:], in0=a3[:, :, 0:64], in1=dn[:].unsqueeze(2).broadcast_to([128, 4, 64]), op=ALU.mult), reads=[acc, dn], writes=[o])
                    s.dma("sp", Dr["O_d"][j * 128:(j + 1) * 128, hk * 256:(hk + 1) * 256], o[:].rearrange("p g d -> p (g d)"), reads=[o], writes=[DR["O_d"]])

    def phase_diff(self, l, q):
        s, I, Dr, DR = self.s, self.I, self.Dr, self.DR
        ident = self.ident
        lam_init = 0.8 - 0.6 * float(np.exp(-0.3 * l))
        with ExitStack() as ps:
            mdiag = self.load_const(ps, "mdiag", [128, 128], BF16)
            dl = s.sb([128, 256], F32, "dl", ps)
            s.dma("sp", dl[:], I["diff_lambda"][l:l + 1, :].broadcast_to([128, 256]), writes=[dl])
            pr = s.sb([128, 2, 64], F32, "pr", ps)
            d4 = dl[:].rearrange("p (a b d) -> p a b d", a=2, b=2)
            s.op("dve", lambda h: h.tensor_tensor(out=pr[:], in0=d4[:, :, 0, :], in1=d4[:, :, 1, :], op=ALU.mult), reads=[dl], writes=[pr])
            e2 = s.sb([128, 2], F32, "e2", ps)
            s.op("dve", lambda h: h.reduce_sum(out=e2[:], in_=pr[:], axis=AX.X), reads=[pr], writes=[e2])
            s.op("act", lambda h: h.activation(out=e2[:], in_=e2[:], func=AF.Exp), reads=[e2], writes=[e2])
            nlam = s.sb([128, 1], F32, "nlam", ps)
            s.op("dve", lambda h: h.scalar_tensor_tensor(out=nlam[:], in0=e2[:, 0:1], scalar=lam_init, in1=e2[:, 1:2], op0=ALU.add, op1=ALU.subtract), reads=[e2], writes=[nlam])
            s.op("dve", lambda h: h.tensor_scalar_mul(out=nlam[:], in0=nlam[:], scalar1=-1.0), reads=[nlam], writes=[nlam])
            gsub = s.sb([128, 128], F32, "gsub", ps)
            s.dma("sp", gsub[:], I["diff_subln_g"][l:l + 1, :].broadcast_to([128, 128]), writes=[gsub])
            s.op("dve", lambda h: h.tensor_scalar_mul(out=gsub[:], in0=gsub[:], scalar1=1.0 - lam_init), reads=[gsub], writes=[gsub])
            KT = [s.sb([68, S], BF16, "KTb", ps) for _ in range(2)]
            QT = [s.sb([68, S], BF16, "QTb", ps) for _ in range(2)]
            V = s.sb([128, NT, 129], BF16, "Vb", ps)
            pts = [s.sb([128, 512], BF16, "pT", ps) for _ in range(3)]
            t0 = [s.sb([128, 128], F32, "t0", ps) for _ in range(2)]
            junk = s.sb([128, 128], F32, "junkb", ps)
            rr = [s.sb([128, 4], F32, "rr", ps) for _ in range(2)]
            ob = [s.sb([128, 128], BF16, "ob", ps) for _ in range(2)]
            nfin = 0
            for hd in range(4):
                for c in range(2):
                    self.load_heads(KT[c], [SL_KB + 2 * hd + c], False)
                    self.load_heads(QT[c], [SL_QB + 2 * hd + c], False)
                s.dma("sp", V[:], Dr["VB_d"][:, hd, :].rearrange("(n p) e -> p n e", p=128), reads=[DR["VB_d"]], writes=[V])
                for G in range(4):
                    def accap(c, jj):
                        return self.P[4 + c * 2 + jj // 2], (jj % 2) * 129
                    items = []
                    for c in range(2):
                        for i in range(0, 4 * G + 4):
                            j0 = max(i, 4 * G)
                            N = (4 * G + 4 - j0) * 128
                            kt = KT[c][:, i * 128:(i + 1) * 128]
                            qk = []
                            if i >= 4 * G:
                                qk.append((lambda sc: sc[:, 0:128], kt, QT[c][:, j0 * 128:(j0 + 1) * 128], True, False, [KT[c], QT[c]]))
                                qk.append((lambda sc: sc[:, 0:128], ident[:], mdiag[:], False, True, [ident, mdiag]))
                                if N > 128:
                                    qk.append((lambda sc, N=N: sc[:, 128:N], kt, QT[c][:, (j0 + 1) * 128:(4 * G + 4) * 128], True, True, [KT[c], QT[c]]))
                            else:
                                qk.append((lambda sc, N=N: sc[:, 0:N], kt, QT[c][:, j0 * 128:(4 * G + 4) * 128], True, True, [KT[c], QT[c]]))

                            def pv(pT, c=c, i=i, j0=j0, G=G):
                                for jj in range(j0, 4 * G + 4):
                                    bank, off = accap(c, jj - 4 * G)
                                    self.mm(bank[:, off:off + 129], pT[:, (jj - j0) * 128:(jj - j0 + 1) * 128], V[:, i, :], i == 0 and (jj - 4 * G) % 2 == 0, i == jj, [pT, V], [bank], sig=(jj == 4 * G + 3))
                            items.append(dict(kp=128, N=N, qk=qk, pv=pv))
                    self.attn_stream(items, self.P[0:3], pts)
                    for jj in range(4):
                        b0, o0 = accap(0, jj)
                        b1, o1 = accap(1, jj)
                        r, t, o = rr[nfin % 2], t0[nfin % 2], ob[nfin % 2]
                        nfin += 1
                        s.op("dve", lambda h: h.reciprocal(out=r[:, 0:1], in_=b0[:, o0 + 128:o0 + 129]), reads=[b0], writes=[r])
                        s.op("dve", lambda h: h.reciprocal(out=r[:, 1:2], in_=b1[:, o1 + 128:o1 + 129]), reads=[b1], writes=[r])
                        s.op("dve", lambda h: h.tensor_tensor(out=r[:, 1:2], in0=r[:, 1:2], in1=nlam[:], op=ALU.mult), reads=[r, nlam], writes=[r])
                        s.op("dve", lambda h: h.tensor_scalar_mul(out=t[:], in0=b0[:, o0:o0 + 128], scalar1=r[:, 0:1]), reads=[b0, r], writes=[t])
                        s.op("dve", lambda h: h.scalar_tensor_tensor(out=t[:], in0=b1[:, o1:o1 + 128], scalar=r[:, 1:2], in1=t[:], op0=ALU.mult, op1=ALU.add), reads=[b1, r, t], writes=[t])
                        s.op("act", lambda h: h.activation(out=junk[:], in_=t[:], func=AF.Square, accum_out=r[:, 2:3]), reads=[t], writes=[junk, r])
                        s.op("dve", lambda h: h.tensor_scalar(out=r[:, 2:3], in0=r[:, 2:3], scalar1=1.0 / 128, scalar2=EPS, op0=ALU.mult, op1=ALU.add), reads=[r], writes=[r])
                        s.op("act", lambda h: h.sqrt(out=r[:, 2:3], in_=r[:, 2:3]), reads=[r], writes=[r])
                        s.op("dve", lambda h: h.reciprocal(out=r[:, 2:3], in_=r[:, 2:3]), reads=[r], writes=[r])
                        s.op("dve", lambda h: h.scalar_tensor_tensor(out=o[:], in0=t[:], scalar=r[:, 2:3], in1=gsub[:], op0=ALU.mult, op1=ALU.mult), reads=[t, r, gsub], writes=[o])
                        row0 = (4 * G + jj) * 128
                        s.dma("sp", Dr["O_d"][row0:row0 + 128, 512 + hd * 128:512 + (hd + 1) * 128], o[:], reads=[o], writes=[DR["O_d"]])

    def phase_nsa(self, l, q):
        s, I, Dr, DR = self.s, self.I, self.Dr, self.DR
        ident = self.ident
        with ExitStack() as ps:
            mdiag = self.load_const(ps, "mdiag", [128, 128], BF16)
            medge = self.load_const(ps, "medge", [128, 128], BF16)
            cmaskT = self.load_const(ps, "cmaskT", [128, S], BF16)
            eblk = self.load_const(ps, "eblk", [32, S], BF16)
            atab = s.sb([128, NT, 32], F32, "atab", ps)
            s.dma("sp", atab[:], I["atab"].rearrange("(n p) b -> p n b", p=128), writes=[atab])
            GN = s.sb([128, NT, 24], F32, "GN", ps)
            s.dma("sp", GN[:], Dr["GN_d"].rearrange("(n p) c -> p n c", p=128), reads=[DR["GN_d"]], writes=[GN])
            QG = s.sb([68, 4, S], BF16, "QGc", ps)
            KSL = s.sb([68, S], BF16, "KSL", ps)
            KWN = s.sb([68, S], BF16, "KWN", ps)
            KCT = s.sb([64, 128], BF16, "KCT", ps)
            VC = s.sb([128, 97], BF16, "VC", ps)
            VSL = s.sb([128, NT, 65], BF16, "VSL", ps)
            VWN = s.sb([128, NT, 65], BF16, "VWN", ps)
            OC = s.sb([128, NT, 4, 64], F32, "OC", ps)
            SELT = s.sb([32, S], BF16, "SELT", ps)
            pts = [s.sb([128, 512], BF16, "pT", ps) for _ in range(3)]
            dn = [s.sb([128, 12], F32, "dn", ps) for _ in range(2)]
            imp = [s.sb([128, 32], F32, "imp", ps) for _ in range(2)]
            tmp32 = [s.sb([128, 32], F32, "tmp32", ps) for _ in range(2)]
            m8 = [s.sb([128, 16], F32, "m8", ps) for _ in range(2)]
            oo = [s.sb([128, 4, 64], F32, "oo", ps) for _ in range(2)]
            ot = [s.sb([128, 4, 64], F32, "otmp", ps) for _ in range(2)]
            ob = [s.sb([128, 4, 64], BF16, "obf", ps) for _ in range(2)]
            o3 = lambda sc: sc[:, :].rearrange("p (g t) -> p g t", g=4)
            o3c = lambda sc: sc[0:127, :].rearrange("p (g t) -> p g t", g=4)
            b4 = lambda ap: ap.unsqueeze(1).broadcast_to([ap.shape[0], 4, 128])
            for hk in range(2):
                self.load_heads(QG, [SL_QC + hk * 4 + g for g in range(4)], True)
                self.load_heads(KSL, [SL_KSL + hk], False)
                self.load_heads(KWN, [SL_KWN + hk], False)
                s.dma("sp", KCT[:], Dr["KC_d"][hk], reads=[DR["KC_d"]], writes=[KCT])
                s.dma("sp", VC[:], Dr["VC_d"][hk], reads=[DR["VC_d"]], writes=[VC])
                s.dma("sp", VSL[:], Dr["VSL_d"][:, hk, :].rearrange("(n p) e -> p n e", p=128), reads=[DR["VSL_d"]], writes=[VSL])
                s.dma("sp", VWN[:], Dr["VWN_d"][:, hk, :].rearrange("(n p) e -> p n e", p=128), reads=[DR["VWN_d"]], writes=[VWN])
                for j in range(NT):
                    acc = self.P[4 + j % 2]
                    jc = slice(j * 128, (j + 1) * 128)
                    qk = [(o3c, KCT[:, 0:127], QG[0:64, :, jc], True, False, [KCT, QG]),
                          (o3c, ident[0:127, 0:127], b4(cmaskT[0:127, jc]), False, True, [ident, cmaskT])]

                    def pv(pT, acc=acc):
                        for g in range(4):
                            self.mm(acc[:, g * 97:(g + 1) * 97], pT[0:127, g * 128:(g + 1) * 128], VC[0:127, :], g == 0, True, [pT, VC], [acc], sig=(g == 3))
                    self.attn_stream([dict(kp=127, N=512, qk=qk, pv=pv)], self.P[0:3], pts)
                    a3 = acc[:, 0:388].rearrange("p (g e) -> p g e", e=97)
                    d, im, tm, mm8 = dn[j % 2], imp[j % 2], tmp32[j % 2], m8[j % 2]
                    s.op("dve", lambda h: h.tensor_scalar_max(out=d[:, 0:4].unsqueeze(2), in0=a3[:, :, 64:65], scalar1=1e-30), reads=[acc], writes=[d])
                    s.op("dve", lambda h: h.reciprocal(out=d[:, 0:4], in_=d[:, 0:4]), reads=[d], writes=[d])
                    s.op("dve", lambda h: h.tensor_tensor(out=OC[:, j], in0=a3[:, :, 0:64], in1=d[:, 0:4].unsqueeze(2).broadcast_to([128, 4, 64]), op=ALU.mult), reads=[acc, d], writes=[OC])
                    s.op("dve", lambda h: h.tensor_scalar_mul(out=im[:], in0=a3[:, 0, 65:97], scalar1=d[:, 0:1]), reads=[acc, d], writes=[im])
                    for g in range(1, 4):
                        s.op("dve", lambda h, g=g: h.scalar_tensor_tensor(out=im[:], in0=a3[:, g, 65:97], scalar=d[:, g:g + 1], in1=im[:], op0=ALU.mult, op1=ALU.add), reads=[acc, d, im], writes=[im])
                    s.op("dve", lambda h: h.tensor_tensor(out=im[:], in0=im[:], in1=atab[:, j, :], op=ALU.add), reads=[im, atab], writes=[im])
                    s.op("dve", lambda h: h.max(out=mm8[:, 0:8], in_=im[:]), reads=[im], writes=[mm8])
                    s.op("dve", lambda h: h.match_replace(out=tm[:], in_to_replace=mm8[:, 0:8], in_values=im[:], imm_value=-3e9), reads=[im, mm8], writes=[tm])
                    s.op("dve", lambda h: h.max(out=mm8[:, 8:16], in_=tm[:]), reads=[tm], writes=[mm8])
                    s.op("dve", lambda h: h.tensor_scalar(out=tm[:], in0=im[:], scalar1=mm8[:, 15:16], scalar2=NEG, op0=ALU.is_lt, op1=ALU.mult), reads=[im, mm8], writes=[tm])
                    pt = self.P[6 + j % 2]
                    self.tr(pt[0:32, 0:128], tm[:], self.ident32[:], [tm, self.ident32], [pt])
                    s.op("act", lambda h: h.copy(out=SELT[:, jc], in_=pt[0:32, 0:128]), reads=[pt], writes=[SELT])
                for j in range(NT):
                    jc = slice(j * 128, (j + 1) * 128)
                    accS, accW = self.P[4 + 2 * (j % 2)], self.P[5 + 2 * (j % 2)]
                    items = []
                    for i in range(0, j + 1):
                        ic = slice(i * 128, (i + 1) * 128)
                        qk = [(o3, KSL[:, ic], QG[:, :, jc], True, False, [KSL, QG]),
                              (o3, eblk[:, ic], b4(SELT[:, jc]), False, i != j, [eblk, SELT])]
                        if i == j:
                            qk.append((o3, ident[:], b4(mdiag[:]), False, True, [ident, mdiag]))

                        def pv(pT, i=i, j=j, acc=accS):
                            for g in range(4):
                                self.mm(acc[:, g * 65:(g + 1) * 65], pT[:, g * 128:(g + 1) * 128], VSL[:, i, :], i == 0 and g == 0, i == j, [pT, VSL], [acc], sig=(g == 3))
                        items.append(dict(kp=128, N=512, qk=qk, pv=pv))
                    i0 = max(0, j - 4)
                    for i in range(i0, j + 1):
                        ic = slice(i * 128, (i + 1) * 128)
                        masked = (i == j) or (i == j - 4)
                        qk = [(o3, KWN[:, ic], QG[:, :, jc], True, not masked, [KWN, QG])]
                        if masked:
                            mk = mdiag if i == j else medge
                            qk.append((o3, ident[:], b4(mk[:]), False, True, [ident, mk]))

                        def pv(pT, i=i, j=j, i0=i0, acc=accW):
                            for g in range(4):
                                self.mm(acc[:, g * 65:(g + 1) * 65], pT[:, g * 128:(g + 1) * 128], VWN[:, i, :], i == i0 and g == 0, i == j, [pT, VWN], [acc], sig=(g == 3))
                        items.append(dict(kp=128, N=512, qk=qk, pv=pv))
                    self.attn_stream(items, self.P[0:3], pts)
                    d, o, t, obf = dn[j % 2], oo[j % 2], ot[j % 2], ob[j % 2]
                    aS = accS[:, 0:260].rearrange("p (g e) -> p g e", e=65)
                    aW = accW[:, 0:260].rearrange("p (g e) -> p g e", e=65)
                    gv = GN[:, j, hk * 12:(hk + 1) * 12].rearrange("p (g r) -> p g r", r=3)
                    s.op("dve", lambda h: h.reciprocal(out=d[:, 4:8].unsqueeze(2), in_=aS[:, :, 64:65]), reads=[accS], writes=[d])
                    s.op("dve", lambda h: h.reciprocal(out=d[:, 8:12].unsqueeze(2), in_=aW[:, :, 64:65]), reads=[accW], writes=[d])
                    s.op("dve", lambda h: h.tensor_tensor(out=d[:, 4:8].unsqueeze(2), in0=d[:, 4:8].unsqueeze(2), in1=gv[:, :, 1:2], op=ALU.mult), reads=[d, GN], writes=[d])
                    s.op("dve", lambda h: h.tensor_tensor(out=d[:, 8:12].unsqueeze(2), in0=d[:, 8:12].unsqueeze(2), in1=gv[:, :, 2:3], op=ALU.mult), reads=[d, GN], writes=[d])
                    s.op("pool", lambda h: h.tensor_tensor(out=o[:], in0=OC[:, j], in1=gv[:, :, 0:1].broadcast_to([128, 4, 64]), op=ALU.mult), reads=[OC, GN], writes=[o])
                    s.op("dve", lambda h: h.tensor_tensor(out=t[:], in0=aS[:, :, 0:64], in1=d[:, 4:8].unsqueeze(2).broadcast_to([128, 4, 64]), op=ALU.mult), reads=[accS, d], writes=[t])
                    s.op("pool", lambda h: h.tensor_tensor(out=o[:], in0=o[:], in1=t[:], op=ALU.add), reads=[o, t], writes=[o])
                    s.op("dve", lambda h: h.tensor_tensor(out=t[:], in0=aW[:, :, 0:64], in1=d[:, 8:12].unsqueeze(2).broadcast_to([128, 4, 64]), op=ALU.mult), reads=[accW, d], writes=[t])
                    s.op("pool", lambda h: h.tensor_tensor(out=obf[:], in0=o[:], in1=t[:], op=ALU.add), reads=[o, t], writes=[obf])
                    s.dma("sp", Dr["O_d"][jc, 1024 + hk * 256:1024 + (hk + 1) * 256], obf[:].rearrange("p g d -> p (g d)"), reads=[obf], writes=[DR["O_d"]])

    def phase_merge(self, l, q):
        s, I, Dr, DR = self.s, self.I, self.Dr, self.DR
        xsrc, xres_r = self.xsrc(l, q)
        with ExitStack() as ps:
            wbr = s.sb([128, 12, D], BF16, "wbr", ps)
            wout = s.sb([128, 8, D], BF16, "wout", ps)
            for r in range(3):
                for hh in range(2):
                    s.dma("pool", wbr[:, r * 4:(r + 1) * 4, hh * 512:(hh + 1) * 512],
                          I["w_branch"][l, r].rearrange("(kc p) n -> p kc n", p=128)[:, :, hh * 512:(hh + 1) * 512], writes=[wbr])
            for k4 in range(2):
                for hh in range(2):
                    s.dma("pool", wout[:, k4 * 4:(k4 + 1) * 4, hh * 512:(hh + 1) * 512],
                          I["w_out"][l].rearrange("(kc p) n -> p kc n", p=128)[:, k4 * 4:(k4 + 1) * 4, hh * 512:(hh + 1) * 512], writes=[wout])
            g1 = s.sb([128, D], F32, "g1", ps)
            self.mod_bc(l, q, 2, g1)
            Ot = [s.sb([128, 1536], BF16, "Ot", ps) for _ in range(2)]
            GM = [s.sb([128, 3072], F32, "GMt", ps) for _ in range(2)]
            xt = [s.sb([128, D], F32, "xt", ps) for _ in range(2)]
            OT = [s.sb([128, 12, 128], BF16, "OT", ps) for _ in range(2)]
            MT = [s.sb([128, 8, 128], BF16, "MT", ps) for _ in range(2)]
            mg = s.sb([128, D], F32, "mg", ps)
            mgb = s.sb([128, D], BF16, "mgb", ps)
            tmp = [s.sb([128, 512], F32, "mtmp", ps) for _ in range(2)]
            nb = 0
            for tt in range(NT):
                rows = slice(tt * 128, (tt + 1) * 128)
                grow = slice(q * S + tt * 128, q * S + (tt + 1) * 128)
                O, G, x, ot, mt = Ot[tt % 2], GM[tt % 2], xt[tt % 2], OT[tt % 2], MT[tt % 2]
                s.dma("sp", O[:], Dr["O_d"][rows, :], reads=[DR["O_d"]], writes=[O])
                s.dma("sp", G[:], Dr["GM_d"][rows, :], reads=[DR["GM_d"]], writes=[G])
                s.dma("sp", x[:], xsrc[grow, :], reads=xres_r, writes=[x])
                pa, pb = self.P[0], self.P[1]
                pab, pbb = pa[:].bitcast(BF16), pb[:].bitcast(BF16)
                for kc in range(8):
                    self.tr(pab[:, kc * 128:(kc + 1) * 128], O[:, kc * 128:(kc + 1) * 128], self.ident[:], [O, self.ident], [pa], sig=(kc == 7))
                for kc in range(4):
                    self.tr(pbb[:, kc * 128:(kc + 1) * 128], O[:, (8 + kc) * 128:(9 + kc) * 128], self.ident[:], [O, self.ident], [pb], sig=(kc == 3))
                s.op("act", lambda h: h.copy(out=ot[:, 0:8, :], in_=pab.rearrange("p (k t) -> p k t", k=8)), reads=[pa], writes=[ot])
                s.op("act", lambda h: h.copy(out=ot[:, 8:12, :], in_=pbb[:, 0:512].rearrange("p (k t) -> p k t", k=4)), reads=[pb], writes=[ot])
                for r in range(3):
                    for half in range(2):
                        pm = self.P[2 + nb % 4]
                        tp = tmp[nb % 2]
                        nb += 1
                        hc = slice(half * 512, (half + 1) * 512)
                        for kc in range(4):
                            self.mm(pm[:, :], ot[:, r * 4 + kc, :], wbr[:, r * 4 + kc, hc], kc == 0, kc == 3, [ot, wbr], [pm], sig=(kc == 3))
                        gsl = G[:, r * 1024 + half * 512:r * 1024 + (half + 1) * 512]
                        if r == 0:
                            s.op("dve", lambda h: h.tensor_tensor(out=mg[:, hc], in0=pm[:, :], in1=gsl, op=ALU.mult), reads=[pm, G], writes=[mg])
                        else:
                            s.op("dve", lambda h: h.tensor_tensor(out=tp[:], in0=pm[:, :], in1=gsl, op=ALU.mult), reads=[pm, G], writes=[tp])
                            s.op("pool", lambda h: h.tensor_tensor(out=mg[:, hc], in0=mg[:, hc], in1=tp[:], op=ALU.add), reads=[mg, tp], writes=[mg])
                s.op("act", lambda h: h.copy(out=mgb[:], in_=mg[:]), reads=[mg], writes=[mgb])
                for kc in range(8):
                    self.tr(pab[:, kc * 128:(kc + 1) * 128], mgb[:, kc * 128:(kc + 1) * 128], self.ident[:], [mgb, self.ident], [pa], sig=(kc == 7))
                s.op("act", lambda h: h.copy(out=mt[:], in_=pab.rearrange("p (k t) -> p k t", k=8)), reads=[pa], writes=[mt])
                for half in range(2):
                    pm = self.P[2 + nb % 4]
                    tp = tmp[nb % 2]
                    nb += 1
                    hc = slice(half * 512, (half + 1) * 512)
                    for kc in range(8):
                        self.mm(pm[:, :], mt[:, kc, :], wout[:, kc, hc], kc == 0, kc == 7, [mt, wout], [pm], sig=(kc == 7))
                    s.op("dve", lambda h: h.tensor_tensor(out=tp[:], in0=pm[:, :], in1=g1[:, hc], op=ALU.mult), reads=[pm, g1], writes=[tp])
                    s.op("pool", lambda h: h.tensor_tensor(out=x[:, hc], in0=x[:, hc], in1=tp[:], op=ALU.add), reads=[x, tp], writes=[x])
                s.dma("sp", Dr["xres"][grow, :], x[:], reads=[x], writes=[DR["xres"]])

    def phase_moe(self, l, q):
        s, I, Dr, DR = self.s, self.I, self.Dr, self.DR
        last = (l == L_DEPTH - 1)
        with ExitStack() as ps:
            g2 = s.sb([128, D], F32, "g2", ps)
            self.mod_bc(l, q, 5, g2)
            H2T = s.sb([128, 8, S], BF16, "H2T", ps)
            ACC = s.sb([128, NT, D], F32, "ACC", ps)
            GATE = s.sb([128, NT, NE], F32, "GATE", ps)
            rw = s.sb([128, 8, NE], F32, "rw", ps)
            rb = s.sb([128, NE], F32, "rb", ps)
            b2 = s.sb([NE, D], F32, "b2", ps)
            b1 = s.sb([128, NE, 16], F32, "b1", ps)
            s.dma("sp", rw[:], I["router_w"][l].rearrange("(kc p) e -> p kc e", p=128), writes=[rw])
            s.dma("sp", rb[:], I["router_b"][l:l + 1, :].broadcast_to([128, NE]), writes=[rb])
            s.dma("sp", b2[:], I["exp_b2"][l], writes=[b2])
            s.dma("sp", b1[:], I["exp_b1T"][l], writes=[b1])
            with ExitStack() as p1:
                gsc, sh = self.norm_tiles(p1, l, q, "norm2_g", 4, 3)
                xt = [s.sb([128, D], F32, "xt", p1) for _ in range(2)]
                hf = [s.sb([128, D], F32, "hf", p1) for _ in range(2)]
                hb = [s.sb([128, D], BF16, "hb", p1) for _ in range(2)]
                hT32 = s.sb([128, 8, 128], F32, "hT32", p1)
                junk = s.sb([128, D], F32, "junk", p1)
                ss = [s.sb([128, 1], F32, "ss", p1) for _ in range(2)]
                lg = [s.sb([128, NE], F32, "lg", p1) for _ in range(2)]
                ex = [s.sb([128, NE], F32, "ex", p1) for _ in range(2)]
                m8 = [s.sb([128, 8], F32, "m8", p1) for _ in range(2)]
                sm = [s.sb([128, 2], F32, "sm", p1) for _ in range(2)]
                gT = [s.sb([NE, 128], F32, "gT", p1) for _ in range(2)]
                for tt in range(NT):
                    grow = slice(q * S + tt * 128, q * S + (tt + 1) * 128)
                    tc = slice(tt * 128, (tt + 1) * 128)
                    x, h32, h16, sq = xt[tt % 2], hf[tt % 2], hb[tt % 2], ss[tt % 2]
                    s.dma("sp", x[:], Dr["xres"][grow, :], reads=[DR["xres"]], writes=[x])
                    self.rstd_of((x, x[:]), (junk, junk[:]), sq)
                    s.op("dve", lambda hh: hh.scalar_tensor_tensor(out=x[:], in0=x[:], scalar=sq[:, 0:1], in1=gsc[:], op0=ALU.mult, op1=ALU.mult), reads=[x, sq, gsc], writes=[x])
                    s.op("pool", lambda hh: hh.tensor_tensor(out=h32[:], in0=x[:], in1=sh[:], op=ALU.add), reads=[x, sh], writes=[h32])
                    s.op("act", lambda hh: hh.copy(out=h16[:], in_=h32[:]), reads=[h32], writes=[h16])
                    pt = self.P[0]
                    ptb = pt[:].bitcast(BF16)
                    for kc in range(8):
                        self.tr(ptb[:, kc * 128:(kc + 1) * 128], h16[:, kc * 128:(kc + 1) * 128], self.ident[:], [h16, self.ident], [pt], sig=(kc == 7))
                    s.op("act", lambda hh: hh.copy(out=H2T[:, :, tc], in_=ptb.rearrange("p (k t) -> p k t", k=8)), reads=[pt], writes=[H2T])
                    for hh2 in range(2):
                        pf = self.P[1 + hh2]
                        for k4 in range(4):
                            kc = hh2 * 4 + k4
                            self.tr(pf[:, k4 * 128:(k4 + 1) * 128], h32[:, kc * 128:(kc + 1) * 128], self.ident32[:], [h32, self.ident32], [pf], sig=(k4 == 3))
                        s.op("dve", lambda hh: hh.tensor_copy(out=hT32[:, hh2 * 4:(hh2 + 1) * 4, :], in_=pf[:, :].rearrange("p (k t) -> p k t", k=4)), reads=[pf], writes=[hT32])
                    pl = self.P[3]
                    for kc in range(8):
                        self.mm(pl[:, 0:NE], hT32[:, kc, :], rw[:, kc, :], kc == 0, kc == 7, [hT32, rw], [pl], sig=(kc == 7))
                    lgt, et, mt, st, gt_ = lg[tt % 2], ex[tt % 2], m8[tt % 2], sm[tt % 2], gT[tt % 2]
                    s.op("dve", lambda hh: hh.tensor_tensor(out=lgt[:], in0=pl[:, 0:NE], in1=rb[:], op=ALU.add), reads=[pl, rb], writes=[lgt])
                    s.op("dve", lambda hh: hh.max(out=mt[:], in_=lgt[:]), reads=[lgt], writes=[mt])
                    s.op("dve", lambda hh: hh.tensor_scalar_mul(out=st[:, 0:1], in0=mt[:, 0:1], scalar1=-1.0), reads=[mt], writes=[st])
                    s.op("act", lambda hh: hh.activation(out=et[:], in_=lgt[:], func=AF.Exp, bias=st[:, 0:1]), reads=[lgt, st], writes=[et])
                    s.op("dve", lambda hh: hh.tensor_scalar(out=lgt[:], in0=lgt[:], scalar1=mt[:, 3:4], scalar2=None, op0=ALU.is_ge), reads=[lgt, mt], writes=[lgt])
                    s.op("dve", lambda hh: hh.tensor_tensor(out=et[:], in0=et[:], in1=lgt[:], op=ALU.mult), reads=[et, lgt], writes=[et])
                    s.op("dve", lambda hh: hh.reduce_sum(out=st[:, 1:2], in_=et[:], axis=AX.X), reads=[et], writes=[st])
                    s.op("dve", lambda hh: hh.reciprocal(out=st[:, 1:2], in_=st[:, 1:2]), reads=[st], writes=[st])
                    s.op("dve", lambda hh: hh.tensor_scalar_mul(out=GATE[:, tt, :], in0=et[:], scalar1=st[:, 1:2]), reads=[et, st], writes=[GATE])
                    pg = self.P[4]
                    self.tr(pg[0:NE, 0:128], GATE[:, tt, :], self.ident32[:], [GATE, self.ident32], [pg])
                    s.op("act", lambda hh: hh.copy(out=gt_[:], in_=pg[0:NE, 0:128]), reads=[pg], writes=[gt_])
                    for half in range(2):
                        pa = self.P[5 + half]
                        self.mm(pa[:, :], gt_[:], b2[:, half * 512:(half + 1) * 512], True, True, [gt_, b2], [pa])
                        s.op("act", lambda hh: hh.copy(out=ACC[:, tt, half * 512:(half + 1) * 512], in_=pa[:, :]), reads=[pa], writes=[ACC])
                s.barrier()
            with ExitStack() as p2:
                W1 = [s.sb([128, 8, 1024], BF16, "W1", p2) for _ in range(2)]
                W2 = [s.sb([128, 4, 1024], BF16, "W2", p2) for _ in range(2)]
                Ab = [s.sb([128, 4, 512], BF16, "Ab", p2) for _ in range(2)]
                Gt = [s.sb([128, 512], F32, "Gt", p2) for _ in range(2)]
                St = [s.sb([128, 512], F32, "St", p2) for _ in range(2)]
                Ut = [s.sb([128, 512], F32, "Ut", p2) for _ in range(2)]
                k = 0
                na = 0
                nf = 0
                ny = 0
                for e in range(NE):
                    w1src = I["exp_w1"][l, e].rearrange("(kc p) n -> p kc n", p=128)
                    w2src = I["exp_w2"][l, e].rearrange("(fc p) n -> p fc n", p=128)
                    for hf_ in range(2):
                        w1, w2 = W1[k % 2], W2[k % 2]
                        k += 1
                        s.dma("pool", w1[:, :, 0:512], w1src[:, :, hf_ * 512:(hf_ + 1) * 512], writes=[w1])
                        s.dma("pool", w1[:, :, 512:1024], w1src[:, :, 1024 + hf_ * 512:1024 + (hf_ + 1) * 512], writes=[w1])
                        for hh in range(2):
                            s.dma("pool", w2[:, :, hh * 512:(hh + 1) * 512], w2src[:, hf_ * 4:(hf_ + 1) * 4, hh * 512:(hh + 1) * 512], writes=[w2])
                        for tg in range(4):
                            A = Ab[na % 2]
                            na += 1
                            tgc = slice(tg * 512, (tg + 1) * 512)
                            for fc in range(4):
                                psG, psU = self.P[(nf % 2) * 2], self.P[(nf % 2) * 2 + 1]
                                G, Sg, U = Gt[nf % 2], St[nf % 2], Ut[nf % 2]
                                nf += 1
                                for kc in range(8):
                                    self.mm(psG[:, :], w1[:, kc, fc * 128:(fc + 1) * 128], H2T[:, kc, tgc], kc == 0, kc == 7, [w1, H2T], [psG], sig=(kc == 7))
                                for kc in range(8):
                                    self.mm(psU[:, :], w1[:, kc, 512 + fc * 128:512 + (fc + 1) * 128], H2T[:, kc, tgc], kc == 0, kc == 7, [w1, H2T], [psU], sig=(kc == 7))
                                ig = hf_ * 4 + fc
                                s.op("dve", lambda hh: hh.tensor_scalar(out=G[:], in0=psG[:, :], scalar1=b1[:, e, ig:ig + 1], scalar2=7.0, op0=ALU.add, op1=ALU.min), reads=[psG, b1], writes=[G])
                                s.op("act", lambda hh: hh.activation(out=Sg[:], in_=G[:], func=AF.Sigmoid, scale=1.702), reads=[G], writes=[Sg])
                                s.op("dve", lambda hh: hh.tensor_scalar(out=U[:], in0=psU[:, :], scalar1=b1[:, e, 8 + ig:8 + ig + 1], scalar2=7.0, op0=ALU.add, op1=ALU.min), reads=[psU, b1], writes=[U])
                                s.op("pool", lambda hh: hh.tensor_scalar(out=U[:], in0=U[:], scalar1=-7.0, scalar2=1.0, op0=ALU.max, op1=ALU.add), reads=[U], writes=[U])
                                s.op("pool", lambda hh: hh.tensor_tensor(out=G[:], in0=G[:], in1=Sg[:], op=ALU.mult), reads=[G, Sg], writes=[G])
                                s.op("pool", lambda hh: hh.tensor_tensor(out=A[:, fc, :], in0=G[:], in1=U[:], op=ALU.mult), reads=[G, U], writes=[A])
                            for t4 in range(4):
                                tt = tg * 4 + t4
                                for dh in range(2):
                                    psY = self.P[4 + ny % 4]
                                    ny += 1
                                    dc = slice(dh * 512, (dh + 1) * 512)
                                    for fc in range(4):
                                        self.mm(psY[:, :], A[:, fc, t4 * 128:(t4 + 1) * 128], w2[:, fc, dc], fc == 0, fc == 3, [A, w2], [psY], sig=(fc == 3))
                                    s.op("dve", lambda hh: hh.scalar_tensor_tensor(out=ACC[:, tt, dc], in0=psY[:, :], scalar=GATE[:, tt, e:e + 1], in1=ACC[:, tt, dc], op0=ALU.mult, op1=ALU.add),
                                         reads=[psY, GATE, ACC], writes=[ACC])
                s.barrier()
            with ExitStack() as p3:
                xt = [s.sb([128, D], F32, "xt", p3) for _ in range(2)]
                tmp = [s.sb([128, D], F32, "tmp3", p3) for _ in range(2)]
                junk = s.sb([128, D], F32, "junk", p3)
                ss = [s.sb([128, 1], F32, "ss", p3) for _ in range(2)]
                if last:
                    fg = s.sb([128, D], F32, "fg", p3)
                    s.dma("sp", fg[:], I["final_g"].broadcast_to([128, D]), writes=[fg])
                for tt in range(NT):
                    grow = slice(q * S + tt * 128, q * S + (tt + 1) * 128)
                    x, tp, sq = xt[tt % 2], tmp[tt % 2], ss[tt % 2]
                    s.dma("sp", x[:], Dr["xres"][grow, :], reads=[DR["xres"]], writes=[x])
                    s.op("dve", lambda hh: hh.tensor_tensor(out=tp[:], in0=ACC[:, tt, :], in1=g2[:], op=ALU.mult), reads=[ACC, g2], writes=[tp])
                    s.op("pool", lambda hh: hh.tensor_tensor(out=x[:], in0=x[:], in1=tp[:], op=ALU.add), reads=[x, tp], writes=[x])
                    if last:
                        self.rstd_of((x, x[:]), (junk, junk[:]), sq)
                        s.op("dve", lambda hh: hh.scalar_tensor_tensor(out=x[:], in0=x[:], scalar=sq[:, 0:1], in1=fg[:], op0=ALU.mult, op1=ALU.mult), reads=[x, sq, fg], writes=[x])
                        s.dma("sp", self.out[grow, :], x[:], reads=[x], writes=[R("out")])
                    else:
                        s.dma("sp", Dr["xres"][grow, :], x[:], reads=[x], writes=[DR["xres"]])
                s.barrier()


    def phase_moe2(self, l):
        s, I, Dr, DR = self.s, self.I, self.Dr, self.DR
        last = (l == L_DEPTH - 1)
        NTT, NB = self.ntt, self.nblk
        with ExitStack() as ps:
            g2 = []
            for q in range(self.nseq):
                t = s.sb([128, D], F32, "g2", ps)
                self.mod_bc(l, q, 5, t)
                g2.append(t)
            GATE = s.sb([128, NTT, NE], F32, "GATE", ps)
            GT = s.sb([NE, NTT, 128], F32, "GT", ps)
            IDX4 = s.sb([128, NTT, 4], I32, "IDX4", ps)
            G4 = s.sb([128, NTT, 4], F32, "G4", ps)
            IDXW = s.sb([128, NB, 2], I32, "IDXW", ps)
            IDXB = s.sb([128, NB], I32, "IDXB", ps)
            b2 = s.sb([NE, D], F32, "b2", ps)
            s.dma("sp", b2[:], I["exp_b2"][l], writes=[b2])
            with ExitStack() as p1:
                HB = s.sb([128, NTT, D], BF16, "HB", p1)
                MASK = s.sb([128, NTT, NE], BF16, "MASK", p1)
                DEST = s.sb([128, NTT, NE], F32, "DEST", p1)
                rw = s.sb([128, 8, NE], F32, "rw", p1)
                rb = s.sb([128, NE], F32, "rb", p1)
                s.dma("sp", rw[:], I["router_w"][l].rearrange("(kc p) e -> p kc e", p=128), writes=[rw])
                s.dma("sp", rb[:], I["router_b"][l:l + 1, :].broadcast_to([128, NE]), writes=[rb])
                ltri = self.load_const(p1, "ltri", [128, 128], BF16)
                ones = self.load_const(p1, "ones128", [128, 128], BF16)
                b512 = self.load_const(p1, "b512", [128, 64], F32)
                rowiota = self.load_const(p1, "rowiota", [128, 8], F32)
                piota = self.load_const(p1, "piota", [128, 1], F32)
                xt = [s.sb([128, D], F32, "xt", p1) for _ in range(2)]
                hf = [s.sb([128, D], F32, "hf", p1) for _ in range(2)]
                hT32 = s.sb([128, 8, 128], F32, "hT32", p1)
                junk = s.sb([128, D], F32, "junk", p1)
                ss = [s.sb([128, 1], F32, "ss", p1) for _ in range(2)]
                lg = [s.sb([128, NE], F32, "lg", p1) for _ in range(2)]
                ex = [s.sb([128, NE], F32, "ex", p1) for _ in range(2)]
                m8 = [s.sb([128, 8], F32, "m8", p1) for _ in range(2)]
                sm = [s.sb([128, 2], F32, "sm", p1) for _ in range(2)]
                gsc = sh = None
                for tt in range(NTT):
                    q = tt // NT
                    if tt % NT == 0:
                        gsc, sh = self.norm_tiles(p1, l, q, "norm2_g", 4, 3)
                    grow = slice(tt * 128, (tt + 1) * 128)
                    x, h32, sq = xt[tt % 2], hf[tt % 2], ss[tt % 2]
                    s.dma("sp", x[:], Dr["xres"][grow, :], reads=[DR["xres"]], writes=[x])
                    self.rstd_of((x, x[:]), (junk, junk[:]), sq)
                    s.op("dve", lambda hh: hh.scalar_tensor_tensor(out=x[:], in0=x[:], scalar=sq[:, 0:1], in1=gsc[:], op0=ALU.mult, op1=ALU.mult), reads=[x, sq, gsc], writes=[x])
                    s.op("pool", lambda hh: hh.tensor_tensor(out=h32[:], in0=x[:], in1=sh[:], op=ALU.add), reads=[x, sh], writes=[h32])
                    s.op("act", lambda hh: hh.copy(out=HB[:, tt, :], in_=h32[:]), reads=[h32], writes=[HB])
                    for hh2 in range(2):
                        pf = self.P[1 + hh2]
                        for k4 in range(4):
                            kc = hh2 * 4 + k4
                            self.tr(pf[:, k4 * 128:(k4 + 1) * 128], h32[:, kc * 128:(kc + 1) * 128], self.ident32[:], [h32, self.ident32], [pf], sig=(k4 == 3))
                        s.op("dve", lambda hh: hh.tensor_copy(out=hT32[:, hh2 * 4:(hh2 + 1) * 4, :], in_=pf[:, :].rearrange("p (k t) -> p k t", k=4)), reads=[pf], writes=[hT32])
                    pl = self.P[3]
                    for kc in range(8):
                        self.mm(pl[:, 0:NE], hT32[:, kc, :], rw[:, kc, :], kc == 0, kc == 7, [hT32, rw], [pl], sig=(kc == 7))
                    lgt, et, mt, st = lg[tt % 2], ex[tt % 2], m8[tt % 2], sm[tt % 2]
                    s.op("dve", lambda hh: hh.tensor_tensor(out=lgt[:], in0=pl[:, 0:NE], in1=rb[:], op=ALU.add), reads=[pl, rb], writes=[lgt])
                    s.op("dve", lambda hh: hh.max(out=mt[:], in_=lgt[:]), reads=[lgt], writes=[mt])
                    s.op("dve", lambda hh: hh.tensor_scalar_mul(out=st[:, 0:1], in0=mt[:, 0:1], scalar1=-1.0), reads=[mt], writes=[st])
                    s.op("act", lambda hh: hh.activation(out=et[:], in_=lgt[:], func=AF.Exp, bias=st[:, 0:1]), reads=[lgt, st], writes=[et])
                    s.op("dve", lambda hh: hh.tensor_scalar(out=lgt[:], in0=lgt[:], scalar1=mt[:, 3:4], scalar2=None, op0=ALU.is_ge), reads=[lgt, mt], writes=[lgt])
                    s.op("dve", lambda hh: hh.tensor_copy(out=MASK[:, tt, :], in_=lgt[:]), reads=[lgt], writes=[MASK])
                    s.op("dve", lambda hh: hh.tensor_tensor(out=et[:], in0=et[:], in1=lgt[:], op=ALU.mult), reads=[et, lgt], writes=[et])
                    s.op("dve", lambda hh: hh.reduce_sum(out=st[:, 1:2], in_=et[:], axis=AX.X), reads=[et], writes=[st])
                    s.op("dve", lambda hh: hh.reciprocal(out=st[:, 1:2], in_=st[:, 1:2]), reads=[st], writes=[st])
                    s.op("dve", lambda hh: hh.tensor_scalar_mul(out=GATE[:, tt, :], in0=et[:], scalar1=st[:, 1:2]), reads=[et, st], writes=[GATE])
                    pg = self.P[4 + tt % 2]
                    self.tr(pg[0:NE, 0:128], GATE[:, tt, :], self.ident32[:], [GATE, self.ident32], [pg])
                    s.op("act", lambda hh: hh.copy(out=GT[:, tt, :], in_=pg[0:NE, 0:128]), reads=[pg], writes=[GT])
                run = s.sb([128, NE], F32, "run", p1)
                s.op("dve", lambda hh: hh.memset(run[:], 0.0), writes=[run])
                for tt in range(NTT):
                    pr = self.P[6 + tt % 2]
                    self.mm(pr[:, 0:NE], ltri[:], MASK[:, tt, :], True, True, [ltri, MASK], [pr], sig=False)
                    self.mm(pr[:, NE:2 * NE], ones[:], MASK[:, tt, :], False, True, [ones, MASK], [pr])
                    s.op("dve", lambda hh: hh.tensor_tensor(out=DEST[:, tt, :], in0=pr[:, 0:NE], in1=run[:], op=ALU.add), reads=[pr, run], writes=[DEST])
                    s.op("dve", lambda hh: hh.tensor_tensor(out=run[:], in0=run[:], in1=pr[:, NE:2 * NE], op=ALU.add), reads=[pr, run], writes=[run])
                padded = s.sb([128, NE], F32, "padded", p1)
                tmpe = s.sb([128, NE], F32, "tmpe", p1)
                cum = [s.sb([128, NE], F32, "cum", p1) for _ in range(2)]
                s.op("dve", lambda hh: hh.memset(padded[:], 0.0), writes=[padded])
                for jb in range(NTT // 4):
                    s.op("dve", lambda hh, jb=jb: hh.scalar_tensor_tensor(out=padded[:], in0=run[:], scalar=512.0 * jb, in1=padded[:], op0=ALU.is_gt, op1=ALU.add), reads=[run, padded], writes=[padded])
                s.op("dve", lambda hh: hh.tensor_scalar_mul(out=padded[:], in0=padded[:], scalar1=512.0), reads=[padded], writes=[padded])
                s.op("dve", lambda hh: hh.tensor_copy(out=cum[0][:], in_=padded[:]), reads=[padded], writes=[cum[0]])
                ci = 0
                for shf in (1, 2, 4, 8, 16):
                    a, b = cum[ci], cum[1 - ci]
                    s.op("dve", lambda hh: hh.tensor_copy(out=b[:, 0:shf], in_=a[:, 0:shf]), reads=[a], writes=[b])
                    s.op("dve", lambda hh: hh.tensor_tensor(out=b[:, shf:NE], in0=a[:, shf:NE], in1=a[:, 0:NE - shf], op=ALU.add), reads=[a], writes=[b])
                    ci = 1 - ci
                pend = cum[ci]
                pstart = s.sb([128, NE], F32, "pstart", p1)
                s.op("dve", lambda hh: hh.tensor_tensor(out=pstart[:], in0=pend[:], in1=padded[:], op=ALU.subtract), reads=[pend, padded], writes=[pstart])
                s.op("dve", lambda hh: hh.tensor_tensor(out=DEST[:], in0=DEST[:], in1=pstart[:].unsqueeze(1).broadcast_to([128, NTT, NE]), op=ALU.add), reads=[DEST, pstart], writes=[DEST])
                s.op("dve", lambda hh: hh.scalar_tensor_tensor(out=DEST[:], in0=DEST[:], scalar=1.0, in1=MASK[:], op0=ALU.add, op1=ALU.mult), reads=[DEST, MASK], writes=[DEST])
                d4 = s.sb([128, NTT, 4], F32, "d4", p1)
                for tt in range(NTT):
                    mt, et = m8[tt % 2], ex[tt % 2]
                    s.op("dve", lambda hh: hh.max(out=mt[:], in_=DEST[:, tt, :]), reads=[DEST], writes=[mt])
                    s.op("dve", lambda hh: hh.tensor_scalar_add(out=d4[:, tt, :], in0=mt[:, 0:4], scalar1=-1.0), reads=[mt], writes=[d4])
                    for k in range(4):
                        s.op("dve", lambda hh: hh.scalar_tensor_tensor(out=et[:], in0=DEST[:, tt, :], scalar=mt[:, k:k + 1], in1=GATE[:, tt, :], op0=ALU.is_equal, op1=ALU.mult), reads=[DEST, mt, GATE], writes=[et])
                        s.op("dve", lambda hh: hh.reduce_sum(out=G4[:, tt, k:k + 1], in_=et[:], axis=AX.X), reads=[et], writes=[G4])
                s.op("dve", lambda hh: hh.tensor_copy(out=IDX4[:], in_=d4[:]), reads=[d4], writes=[IDX4])
                bexp = s.sb([128, 64], F32, "bexp", p1)
                s.op("dve", lambda hh: hh.memset(bexp[:], 0.0), writes=[bexp])
                for e in range(NE):
                    s.op("dve", lambda hh: hh.scalar_tensor_tensor(out=bexp[:], in0=b512[:], scalar=pend[:, e:e + 1], in1=bexp[:], op0=ALU.is_ge, op1=ALU.add), reads=[b512, pend, bexp], writes=[bexp])
                boob = s.sb([128, 64], F32, "boob", p1)
                s.op("dve", lambda hh: hh.tensor_scalar(out=boob[:], in0=bexp[:], scalar1=float(NE) - 0.5, scalar2=1.0e6, op0=ALU.is_ge, op1=ALU.mult), reads=[bexp], writes=[boob])
                s.op("dve", lambda hh: hh.tensor_scalar_min(out=bexp[:], in0=bexp[:], scalar1=float(NE - 1)), reads=[bexp], writes=[bexp])
                iwf = s.sb([128, NB, 2], F32, "iwf", p1)
                ibf = s.sb([128, NB], F32, "ibf", p1)
                e1k = s.sb([128, 64], F32, "e1k", p1)
                s.op("dve", lambda hh: hh.tensor_scalar(out=e1k[:], in0=bexp[:], scalar1=256.0, scalar2=float(l * NE * 256), op0=ALU.mult, op1=ALU.add), reads=[bexp], writes=[e1k])
                s.op("dve", lambda hh: hh.tensor_tensor(out=e1k[:], in0=e1k[:], in1=boob[:], op=ALU.add), reads=[e1k, boob], writes=[e1k])
                s.op("dve", lambda hh: hh.tensor_tensor(out=iwf[:], in0=rowiota[:, 0:2].unsqueeze(1).broadcast_to([128, NB, 2]), in1=e1k[:, 0:NB].unsqueeze(2).broadcast_to([128, NB, 2]), op=ALU.add), reads=[rowiota, e1k], writes=[iwf])
                s.op("dve", lambda hh: hh.tensor_copy(out=IDXW[:], in_=iwf[:]), reads=[iwf], writes=[IDXW])
                s.op("dve", lambda hh: hh.tensor_scalar(out=ibf[:], in0=bexp[:, 0:NB], scalar1=128.0, scalar2=piota[:, 0:1], op0=ALU.mult, op1=ALU.add), reads=[bexp, piota], writes=[ibf])
                s.op("dve", lambda hh: hh.tensor_scalar_add(out=ibf[:], in0=ibf[:], scalar1=float(l * NE * 128)), reads=[ibf], writes=[ibf])
                s.op("dve", lambda hh: hh.tensor_tensor(out=ibf[:], in0=ibf[:], in1=boob[:, 0:NB], op=ALU.add), reads=[ibf, boob], writes=[ibf])
                s.op("dve", lambda hh: hh.tensor_copy(out=IDXB[:], in_=ibf[:]), reads=[ibf], writes=[IDXB])
                if "dbg_d" in self.dbg:
                    dbt = s.sb([128, 4096], F32, "dbt", p1)
                    s.op("dve", lambda hh: hh.memset(dbt[:], 0.0), writes=[dbt])
                    s.op("dve", lambda hh: hh.tensor_copy(out=dbt[:, 0:32], in_=run[:]), reads=[run], writes=[dbt])
                    s.op("dve", lambda hh: hh.tensor_copy(out=dbt[:, 32:64], in_=pend[:]), reads=[pend], writes=[dbt])
                    s.op("dve", lambda hh: hh.tensor_copy(out=dbt[:, 64:128], in_=bexp[:]), reads=[bexp], writes=[dbt])
                    s.op("dve", lambda hh: hh.tensor_copy(out=dbt[:, 128:128 + NTT * 4], in_=d4[:].rearrange("p t k -> p (t k)")), reads=[d4], writes=[dbt])
                    s.op("dve", lambda hh: hh.tensor_copy(out=dbt[:, 512:512 + NTT * 4], in_=G4[:].rearrange("p t k -> p (t k)")), reads=[G4], writes=[dbt])
                    s.dma("sp", Dr["dbg_d"], dbt[:], reads=[dbt], writes=[DR["dbg_d"]])
                for tt in range(NTT):
                    for k in range(4):
                        s.idma(Dr["xs_d"][:, :], HB[:, tt, :], out_off=IDX4[:, tt, k:k + 1], reads=[HB, IDX4], writes=[])
                s.barrier()
            with ExitStack() as p2:
                W1 = [s.sb([128, 8, 2 * D], BF16, "W1", p2) for _ in range(2)]
                W2 = [s.sb([128, 8, D], BF16, "W2", p2) for _ in range(2)]
                B1 = [s.sb([128, 16], F32, "B1", p2) for _ in range(2)]
                XS = [s.sb([128, 4, D], BF16, "XS", p2) for _ in range(2)]
                XT = [s.sb([128, 8, 512], BF16, "XT", p2) for _ in range(2)]
                Ab = [s.sb([128, 8, 512], BF16, "Ab", p2) for _ in range(2)]
                Gt = [s.sb([128, 512], F32, "Gt", p2) for _ in range(2)]
                St = [s.sb([128, 512], F32, "St", p2) for _ in range(2)]
                Ut = [s.sb([128, 512], F32, "Ut", p2) for _ in range(2)]
                Yt = [s.sb([128, D], F32, "Yt", p2) for _ in range(2)]
                for wt_ in W1 + W2 + B1:
                    s.op("pool", lambda hh, wt_=wt_: hh.memset(wt_[:], 0.0), writes=[wt_])
                nf = 0
                ny = 0
                for b in range(NB):
                    w1, w2, b1, xs, xT, A = W1[b % 2], W2[b % 2], B1[b % 2], XS[b % 2], XT[b % 2], Ab[b % 2]
                    for j2 in range(2):
                        s.idma(w1[:, j2 * 4:(j2 + 1) * 4, :].rearrange("p a b -> p (a b)"), I["exp_w1"][:, :], in_off=IDXW[:, b, j2:j2 + 1], reads=[IDXW], writes=[w1], bounds=65535)
                    s.idma(w2[:].rearrange("p a b -> p (a b)"), I["exp_w2"][:, :], in_off=IDXB[:, b:b + 1], reads=[IDXB], writes=[w2], bounds=65535)
                    s.idma(b1[:, :], I["exp_b1E"][:, :], in_off=IDXB[:, b:b + 1], reads=[IDXB], writes=[b1], bounds=L_DEPTH * NE * 128 - 1)
                    s.dma("sp", xs[:], Dr["xs_d"][b * 512:(b + 1) * 512, :].rearrange("(t p) d -> p t d", p=128), reads=[DR["xs_d"]], writes=[xs])
                    for t4 in range(4):
                        pt = self.P[t4 % 2]
                        ptb = pt[:].bitcast(BF16)
                        for kc in range(8):
                            kb = (kc // 4) * 512 + (kc % 4)
                            self.tr(ptb[:, kc * 128:(kc + 1) * 128], xs[:, t4, kb:kb + 509:4], self.ident[:], [xs, self.ident], [pt], sig=(kc == 7))
                        eng = "act" if t4 % 2 == 0 else "dve"
                        if eng == "act":
                            s.op("act", lambda hh: hh.copy(out=xT[:, :, t4 * 128:(t4 + 1) * 128], in_=ptb.rearrange("p (k t) -> p k t", k=8)), reads=[pt], writes=[xT])
                        else:
                            s.op("dve", lambda hh: hh.tensor_copy(out=xT[:, :, t4 * 128:(t4 + 1) * 128], in_=ptb.rearrange("p (k t) -> p k t", k=8)), reads=[pt], writes=[xT])
                    for fc in range(8):
                        psG, psU = self.P[2 + (nf % 2) * 2], self.P[3 + (nf % 2) * 2]
                        G, Sg, U = Gt[nf % 2], St[nf % 2], Ut[nf % 2]
                        nf += 1
                        for kc in range(8):
                            self.mm(psG[:, :], w1[:, kc, fc:D:8], xT[:, kc, :], kc == 0, kc == 7, [w1, xT], [psG], sig=(kc == 7))
                        for kc in range(8):
                            self.mm(psU[:, :], w1[:, kc, D + fc:2 * D:8], xT[:, kc, :], kc == 0, kc == 7, [w1, xT], [psU], sig=(kc == 7))
                        s.op("dve", lambda hh: hh.tensor_scalar(out=G[:], in0=psG[:, :], scalar1=b1[:, fc:fc + 1], scalar2=7.0, op0=ALU.add, op1=ALU.min), reads=[psG, b1], writes=[G])
                        s.op("act", lambda hh: hh.activation(out=Sg[:], in_=G[:], func=AF.Sigmoid, scale=1.702), reads=[G], writes=[Sg])
                        s.op("act", lambda hh: hh.activation(out=U[:], in_=psU[:, :], func=AF.Identity, bias=b1[:, 8 + fc:8 + fc + 1]), reads=[psU, b1], writes=[U])
                        s.op("dve", lambda hh: hh.tensor_scalar(out=U[:], in0=U[:], scalar1=7.0, scalar2=-7.0, op0=ALU.min, op1=ALU.max), reads=[U], writes=[U])
                        s.op("dve", lambda hh: hh.tensor_tensor(out=G[:], in0=G[:], in1=Sg[:], op=ALU.mult), reads=[G, Sg], writes=[G])
                        s.op("dve", lambda hh: hh.scalar_tensor_tensor(out=A[:, fc, :], in0=U[:], scalar=1.0, in1=G[:], op0=ALU.add, op1=ALU.mult), reads=[U, G], writes=[A])
                    for t4 in range(4):
                        y = Yt[ny % 2]
                        for dh in range(2):
                            psY = self.P[6 + ny % 2] if dh == 0 else self.P[(ny % 2)]
                            dc = slice(dh * 512, (dh + 1) * 512)
                            for fc in range(8):
                                self.mm(psY[:, :], A[:, fc, t4 * 128:(t4 + 1) * 128], w2[:, fc, dc], fc == 0, fc == 7, [A, w2], [psY], sig=(fc == 7))
                            s.op("act", lambda hh: hh.copy(out=y[:, dc], in_=psY[:, :]), reads=[psY], writes=[y])
                        ny += 1
                        r0 = b * 512 + t4 * 128
                        s.dma("sp", Dr["ys_d"][r0:r0 + 128, :], y[:], reads=[y], writes=[DR["ys_d"]])
                s.barrier()
            with ExitStack() as p3:
                xt = [s.sb([128, D], F32, "xt", p3) for _ in range(2)]
                acc = [s.sb([128, D], F32, "acc3", p3) for _ in range(2)]
                Yk = [s.sb([128, D], F32, "Yk", p3) for _ in range(4)]
                junk = s.sb([128, D], F32, "junk", p3)
                ss = [s.sb([128, 1], F32, "ss", p3) for _ in range(2)]
                if last:
                    fg = s.sb([128, D], F32, "fg", p3)
                    s.dma("sp", fg[:], I["final_g"].broadcast_to([128, D]), writes=[fg])
                nk = 0
                for tt in range(NTT):
                    q = tt // NT
                    grow = slice(tt * 128, (tt + 1) * 128)
                    x, ac, sq = xt[tt % 2], acc[tt % 2], ss[tt % 2]
                    s.dma("sp", x[:], Dr["xres"][grow, :], reads=[DR["xres"]], writes=[x])
                    pa = [self.P[(tt % 2) * 2], self.P[(tt % 2) * 2 + 1]]
                    for half in range(2):
                        self.mm(pa[half][:, :], GT[:, tt, :], b2[:, half * 512:(half + 1) * 512], True, True, [GT, b2], [pa[half]])
                    for k in range(4):
                        yk = Yk[nk % 4]
                        nk += 1
                        s.idma(yk[:, :], Dr["ys_d"][:, :], in_off=IDX4[:, tt, k:k + 1], reads=[IDX4, DR["ys_d"]], writes=[yk])
                        for half in range(2):
                            hc = slice(half * 512, (half + 1) * 512)
                            in1 = pa[half][:, :] if k == 0 else ac[:, hc]
                            rd = [yk, G4] + ([pa[half]] if k == 0 else [ac])
                            s.op("dve", lambda hh, in1=in1, hc=hc, yk=yk, k=k: hh.scalar_tensor_tensor(out=ac[:, hc], in0=yk[:, hc], scalar=G4[:, tt, k:k + 1], in1=in1, op0=ALU.mult, op1=ALU.add), reads=rd, writes=[ac])
                    s.op("pool", lambda hh: hh.tensor_tensor(out=ac[:], in0=ac[:], in1=g2[q][:], op=ALU.mult), reads=[ac, g2[q]], writes=[ac])
                    s.op("dve", lambda hh: hh.tensor_tensor(out=x[:], in0=x[:], in1=ac[:], op=ALU.add), reads=[x, ac], writes=[x])
                    if last:
                        self.rstd_of((x, x[:]), (junk, junk[:]), sq)
                        s.op("dve", lambda hh: hh.scalar_tensor_tensor(out=x[:], in0=x[:], scalar=sq[:, 0:1], in1=fg[:], op0=ALU.mult, op1=ALU.mult), reads=[x, sq, fg], writes=[x])
                        s.dma("sp", self.out[grow, :], x[:], reads=[x], writes=[R("out")])
                    else:
                        s.dma("sp", Dr["xres"][grow, :], x[:], reads=[x], writes=[DR["xres"]])
                s.barrier()


_CONSTS = None


def run(inputs, dbg=(), layers=L_DEPTH, nseq=NSEQ, stop_after=None, cores=NCORES, trace=False, only=None):
    global _CONSTS
    if _CONSTS is None:
        _CONSTS = host_consts()
    inp = {k: np.asarray(v) for k, v in inputs.items()}
    k = K(dbg=dbg, layers=layers, nseq=nseq, stop_after=stop_after, only=only)
    nc = k.build()
    in_maps = []
    for c in range(cores):
        m = host_inputs(inp, c)
        m.update(_CONSTS)
        in_maps.append(m)
    res = run_bass_kernel_spmd(nc, in_maps, core_ids=list(range(cores)), trace=trace)
    return res


def kernel(**inputs):
    res = run(inputs)
    out = np.concatenate([np.asarray(r["out"]).reshape(NSEQ, S, D) for r in res.results], axis=0)
    return out.astype(np.float32)
```

```python
import numpy as np
import ml_dtypes
from contextlib import ExitStack
import concourse.bass as bass
import concourse.mybir as mybir
from concourse.bass_utils import run_bass_kernel_spmd

F32 = mybir.dt.float32
BF16 = mybir.dt.bfloat16
I32 = mybir.dt.int32
AF = mybir.ActivationFunctionType
ALU = mybir.AluOpType
AX = mybir.AxisListType
NDSEM = 8


class R:
    __slots__ = ("name", "lw", "rd")

    def __init__(self, name=""):
        self.name = name
        self.lw = {}
        self.rd = {}


class T(R):
    __slots__ = ("t",)

    def __init__(self, t, name=""):
        super().__init__(name)
        self.t = t

    def __getitem__(self, k):
        return self.t[k]


class Eng:
    def __init__(self, name, h, sem, dsems):
        self.name = name
        self.h = h
        self.sem = sem
        self.count = 0
        self.known = {}
        self.dsems = dsems
        self.ndma = 0


class Sched:
    def __init__(self, nc, stack):
        self.nc = nc
        self.stack = stack
        self.E = {}
        for name, h, nd in (("pe", nc.tensor, 0), ("act", nc.scalar, NDSEM), ("dve", nc.vector, 0),
                            ("pool", nc.gpsimd, NDSEM), ("sp", nc.sync, NDSEM)):
            sem = stack.enter_context(nc.semaphore("s_" + name))
            ds = [stack.enter_context(nc.semaphore("d_%s%d" % (name, i))) for i in range(nd)]
            self.E[name] = Eng(name, h, sem, ds)
        self.dma_tokens = []
        self.nuniq = 0

    def sb(self, shape, dt, name=None, stack=None):
        self.nuniq += 1
        name = (name or "t") + "_%d" % self.nuniq
        t = (stack or self.stack).enter_context(self.nc.sbuf_tensor(name, list(shape), dt))
        return T(t, name)

    def ps(self, shape, dt=F32, name=None, stack=None):
        self.nuniq += 1
        name = (name or "p") + "_%d" % self.nuniq
        t = (stack or self.stack).enter_context(self.nc.psum_tensor(name, list(shape), dt))
        return T(t, name)

    def _wait(self, eng, tok):
        sem, val = tok
        if eng.known.get(sem, 0) >= val:
            return
        eng.h.wait_ge(sem, val)
        eng.known[sem] = val

    def _deps(self, eng, reads, writes):
        deps = []
        for r in reads:
            deps.extend(r.lw.items())
        for w in writes:
            deps.extend(w.lw.items())
            deps.extend(w.rd.items())
        for tok in deps:
            if tok[0] is eng.sem:
                if eng.name == "pe":
                    continue
                if tok[1] > eng.count:
                    continue
            self._wait(eng, tok)

    def _commit(self, tok, reads, writes):
        sem, val = tok
        for r in reads:
            if r.rd.get(sem, 0) < val:
                r.rd[sem] = val
        for w in writes:
            if w.lw.get(sem, 0) < val:
                w.lw[sem] = val

    def op(self, engname, fn, reads=(), writes=(), sig=True):
        eng = self.E[engname]
        self._deps(eng, reads, writes)
        ins = fn(eng.h)
        if sig:
            eng.count += 1
            ins.then_inc(eng.sem, 1)
            tok = (eng.sem, eng.count)
        else:
            tok = (eng.sem, eng.count + 1)
        self._commit(tok, reads, writes)
        return tok

    def dma(self, qname, out, in_, reads=(), writes=(), **kw):
        q = self.E[qname]
        i = q.ndma
        q.ndma += 1
        sem = q.dsems[i % NDSEM]
        val = 16 * (i // NDSEM + 1)
        if i >= NDSEM:
            self._wait(q, (sem, val - 16))
        self._deps(q, reads, writes)
        q.h.dma_start(out=out, in_=in_, **kw).then_inc(sem, 16)
        tok = (sem, val)
        self._commit(tok, reads, writes)
        self.dma_tokens.append(tok)
        return tok

    def idma(self, out, in_, out_off=None, in_off=None, reads=(), writes=(), bounds=None):
        q = self.E["pool"]
        i = q.ndma
        q.ndma += 1
        sem = q.dsems[i % NDSEM]
        val = 16 * (i // NDSEM + 1)
        if i >= NDSEM:
            self._wait(q, (sem, val - 16))
        self._deps(q, reads, writes)
        oo = bass.IndirectOffsetOnAxis(ap=out_off, axis=0) if out_off is not None else None
        io = bass.IndirectOffsetOnAxis(ap=in_off, axis=0) if in_off is not None else None
        if bounds is None:
            q.h.indirect_dma_start(out=out, out_offset=oo, in_=in_, in_offset=io).then_inc(sem, 16)
        else:
            if getattr(self, "bound_reg", None) is None:
                self.bound_reg = q.h.alloc_register("bnd")
                q.h.reg_mov(self.bound_reg, 65535)
            q.h.indirect_dma_start(out=out, out_offset=oo, in_=in_, in_offset=io, bounds_check=self.bound_reg, oob_is_err=False).then_inc(sem, 16)
        tok = (sem, val)
        self._commit(tok, reads, writes)
        self.dma_tokens.append(tok)
        return tok

    def barrier(self):
        toks = []
        for e in self.E.values():
            if e.count > 0:
                toks.append((e.sem, e.count))
            for j, s in enumerate(e.dsems):
                n = (e.ndma - j + NDSEM - 1) // NDSEM
                if n > 0:
                    toks.append((s, 16 * n))
        for e in self.E.values():
            for tok in toks:
                if tok[0] is e.sem:
                    continue
                self._wait(e, tok)

    def finish(self):
        sp = self.E["sp"]
        for e in self.E.values():
            for j, s in enumerate(e.dsems):
                n = (e.ndma - j + NDSEM - 1) // NDSEM
                if n > 0:
                    self._wait(sp, (s, 16 * n))


NCORES = 8
L_DEPTH = 2
D = 1024
S = 2048
NSEQ = 2
NT = S // 128
EPS = 1e-5
INW = 6680
SCALE = 0.125
NEG = -30000.0
NE = 32
SLOT_COL = ([0 + 64 * i for i in range(8)] + [512 + 64 * i for i in range(2)] + [768 + 64 * i for i in range(8)]
            + [1280 + 64 * i for i in range(8)] + [2304 + 64 * i for i in range(8)] + [2816 + 64 * i for i in range(2)]
            + [2944 + 64 * i for i in range(2)] + [3072 + 64 * i for i in range(2)] + [3328 + 64 * i for i in range(2)])
NSLOT = len(SLOT_COL)
SL_QA, SL_KA, SL_QB, SL_KB, SL_QC, SL_KCM, SL_VCM, SL_KSL, SL_KWN = 0, 8, 10, 18, 26, 34, 36, 38, 40
C_VA, C_VB, C_VSL, C_VWN, C_GN, C_GM = 640, 1792, 3200, 3456, 3584, 3608


def host_consts():
    bf = ml_dtypes.bfloat16
    c = {}
    t = np.arange(S)
    a_t, b_t = (t // 128).astype(np.float32), (t % 128).astype(np.float32)
    aug = np.zeros((NSLOT, 4, S), np.float32)
    kaug = np.stack([a_t, b_t, np.ones(S, np.float32), np.ones(S, np.float32)])

    def qaug(slope):
        return np.stack([np.full(S, 1024.0 * slope, np.float32), np.full(S, 8.0 * slope, np.float32),
                         -1024.0 * slope * a_t, -8.0 * slope * b_t])
    for i in range(8):
        aug[SL_QA + i] = qaug(2.0 ** -(i + 1))
        aug[SL_QC + i] = qaug(2.0 ** -(i + 1))
        aug[SL_QB + i] = qaug(2.0 ** (-2.0 * (i // 2 + 1)))
        aug[SL_KB + i] = kaug
    for i in range(2):
        aug[SL_KA + i] = kaug
        aug[SL_KSL + i] = kaug
        aug[SL_KWN + i] = kaug
    c["aug"] = aug.astype(bf)
    c["ident"] = np.eye(128, dtype=np.float32).astype(bf)
    c["ident32"] = np.eye(128, dtype=np.float32)
    sk = np.arange(128)[:, None]
    tq = np.arange(128)[None, :]
    c["mdiag"] = np.where(tq >= sk, 0.0, NEG).astype(bf)
    c["medge"] = np.where(tq < sk, 0.0, NEG).astype(bf)
    cc = np.arange(128)[:, None]
    c["cmaskT"] = np.where((16 * cc + 31 <= t[None, :]) & (cc < 127), 0.0, NEG).astype(bf)
    nb = np.arange(32)
    c["eblk"] = (t[None, :] // 64 == nb[:, None]).astype(np.float32).astype(bf)
    cur = t // 64
    forced = (nb[None, :] == 0) | (nb[None, :] == cur[:, None]) | (nb[None, :] == cur[:, None] - 1)
    causal = nb[None, :] <= cur[:, None]
    A = np.where(forced, 1e9 + 1e6 * nb[None, :], np.where(causal, 0.0, -1e9 - 1e6 * nb[None, :]))
    c["atab"] = A.astype(np.float32)
    cstart = np.arange(127) * 16
    sstart = nb * 64
    ov = np.clip(np.minimum(cstart[:, None] + 32, sstart[None, :] + 64) - np.maximum(cstart[:, None], sstart[None, :]), 0, None) / 32.0
    ovp = np.zeros((128, 32), np.float32)
    ovp[:127] = ov
    c["overlap"] = ovp.astype(bf)
    c["ltri"] = (np.arange(128)[:, None] < np.arange(128)[None, :]).astype(np.float32).astype(bf)
    c["ones128"] = np.ones((128, 128), np.float32).astype(bf)
    c["b512"] = np.broadcast_to((512.0 * np.arange(64, dtype=np.float32))[None, :], (128, 64)).copy()
    c["rowiota"] = (np.arange(8, dtype=np.float32)[None, :] * 128 + np.arange(128, dtype=np.float32)[:, None]).copy()
    c["piota"] = np.arange(128, dtype=np.float32).reshape(128, 1).copy()
    return c


def host_inputs(inp, core):
    b0 = core * NSEQ
    m = {}
    m["x"] = np.ascontiguousarray(inp["x"][b0:b0 + NSEQ].reshape(NSEQ * S, D))
    m["cT"] = np.ascontiguousarray(inp["c"][b0:b0 + NSEQ].T)
    for k in ("mod_w", "mod_b", "norm1_g", "norm2_g", "w_in", "b_in", "sinks", "diff_subln_g", "cmp_w1", "cmp_w2",
              "cmp_b2", "w_branch", "w_out", "router_w", "router_b", "exp_b2"):
        m[k] = inp[k]
    m["final_g"] = inp["final_g"].reshape(1, D)
    m["b_inT"] = np.ascontiguousarray(np.stack([inp["b_in"][:, c0:c0 + 64] for c0 in SLOT_COL], axis=2))
    m["diff_lambda"] = inp["diff_lambda"].reshape(L_DEPTH, 256)
    m["cmp_posT"] = np.ascontiguousarray(inp["cmp_pos"].transpose(0, 1, 3, 2))
    m["cmp_b1T"] = np.ascontiguousarray(inp["cmp_b1"].reshape(L_DEPTH, 2, 2, 128).transpose(0, 1, 3, 2))
    m["cmp_b2T"] = np.ascontiguousarray(inp["cmp_b2"].reshape(L_DEPTH, 2, 64, 1))
    m["exp_b1E"] = np.ascontiguousarray(inp["exp_b1"].reshape(L_DEPTH, NE, 2, 128, 8).transpose(0, 1, 3, 2, 4)).reshape(L_DEPTH * NE * 128, 16)
    m["exp_w1"] = inp["exp_w1"].reshape(L_DEPTH * NE * 256, 4 * 2 * D)
    m["exp_w2"] = inp["exp_w2"].reshape(L_DEPTH * NE * 128, 8 * D)
    return m


IN_SHAPES = {
    "x": ([NSEQ * S, D], F32), "cT": ([D, NSEQ], F32), "mod_w": ([L_DEPTH, D, 6 * D], F32), "mod_b": ([L_DEPTH, 6 * D], F32),
    "norm1_g": ([L_DEPTH, D], F32), "norm2_g": ([L_DEPTH, D], F32), "w_in": ([L_DEPTH, D, INW], F32), "b_in": ([L_DEPTH, INW], F32),
    "sinks": ([L_DEPTH, 8], F32), "diff_subln_g": ([L_DEPTH, 128], F32), "cmp_w1": ([L_DEPTH, 2, 2048, 256], F32),
    "cmp_w2": ([L_DEPTH, 2, 256, 64], F32), "cmp_b2": ([L_DEPTH, 2, 64], F32), "w_branch": ([L_DEPTH, 3, 512, D], F32),
    "w_out": ([L_DEPTH, D, D], F32), "router_w": ([L_DEPTH, D, NE], F32), "router_b": ([L_DEPTH, NE], F32),
    "exp_w1": ([L_DEPTH * NE * 256, 8 * D], F32), "exp_w2": ([L_DEPTH * NE * 128, 8 * D], F32), "exp_b2": ([L_DEPTH, NE, D], F32),
    "final_g": ([1, D], F32), "b_inT": ([L_DEPTH, 64, NSLOT], F32), "diff_lambda": ([L_DEPTH, 256], F32),
    "cmp_posT": ([L_DEPTH, 2, 64, 32], F32), "cmp_b1T": ([L_DEPTH, 2, 128, 2], F32), "cmp_b2T": ([L_DEPTH, 2, 64, 1], F32),
    "exp_b1E": ([L_DEPTH * NE * 128, 16], F32),
    "ltri": ([128, 128], BF16), "ones128": ([128, 128], BF16), "b512": ([128, 64], F32), "rowiota": ([128, 8], F32), "piota": ([128, 1], F32),
    "aug": ([NSLOT, 4, S], BF16), "ident": ([128, 128], BF16), "ident32": ([128, 128], F32), "mdiag": ([128, 128], BF16),
    "medge": ([128, 128], BF16), "cmaskT": ([128, S], BF16), "eblk": ([32, S], BF16), "atab": ([S, 32], F32),
    "overlap": ([128, 32], BF16),
}


def bcast_rows(ap1d_row, nparts):
    return ap1d_row.broadcast_to([nparts, ap1d_row.shape[-1]])


class K:
    def __init__(self, dbg=(), layers=L_DEPTH, nseq=NSEQ, stop_after=None, only=None):
        self.dbg = set(dbg)
        self.only = only
        self.layers = layers
        self.nseq = nseq
        self.stop_after = stop_after
        nc = self.nc = bass.Bass("TRN2", target_bir_lowering=False)
        self.I = {k: nc.dram_tensor(k, list(sh), dt, kind="ExternalInput").ap() for k, (sh, dt) in IN_SHAPES.items()}
        self.out = nc.dram_tensor("out", [NSEQ * S, D], F32, kind="ExternalOutput").ap()
        self.Dr = {}
        self.DR = {}

    def dram(self, name, shape, dt):
        kind = "ExternalOutput" if name in self.dbg else "Internal"
        self.Dr[name] = self.nc.dram_tensor(name, list(shape), dt, kind=kind).ap()
        self.DR[name] = R(name)
        return self.Dr[name]

    def mm(self, out, lhsT, rhs, start, stop, reads, writes, sig=True):
        return self.s.op("pe", lambda h: h.matmul(out, lhsT=lhsT, rhs=rhs, start=start, stop=stop), reads=reads, writes=writes, sig=sig)

    def tr(self, out, in_, ident, reads, writes, sig=True):
        return self.s.op("pe", lambda h: h.transpose(out=out, in_=in_, identity=ident), reads=reads, writes=writes, sig=sig)

    def rstd_of(self, xt, junk, ss, n=D):
        s = self.s
        s.op("act", lambda h: h.activation(out=junk[1], in_=xt[1], func=AF.Square, accum_out=ss[:]), reads=[xt[0]], writes=[junk[0], ss])
        s.op("dve", lambda h: h.tensor_scalar(out=ss[:], in0=ss[:], scalar1=1.0 / n, scalar2=EPS, op0=ALU.mult, op1=ALU.add), reads=[ss], writes=[ss])
        s.op("act", lambda h: h.sqrt(out=ss[:], in_=ss[:]), reads=[ss], writes=[ss])
        s.op("dve", lambda h: h.reciprocal(out=ss[:], in_=ss[:]), reads=[ss], writes=[ss])

    def build(self):
        nc = self.nc
        with ExitStack() as st:
            s = self.s = Sched(nc, st)
            self.P = [s.ps([128, 512], F32, "bank%d" % i) for i in range(8)]
            self.ident = s.sb([128, 128], BF16, "ident")
            self.ident32 = s.sb([128, 128], F32, "ident32")
            s.dma("sp", self.ident[:], self.I["ident"], writes=[self.ident])
            s.dma("sp", self.ident32[:], self.I["ident32"], writes=[self.ident32])
            self.dram("mod_d", [L_DEPTH, NSEQ, 6 * D], F32)
            self.dram("xres", [NSEQ * S, D], F32)
            self.dram("QT_d", [NSLOT, 64, S], BF16)
            self.dram("VA_d", [S, 2, 65], BF16)
            self.dram("VB_d", [S, 4, 129], BF16)
            self.dram("VSL_d", [S, 2, 65], BF16)
            self.dram("VWN_d", [S, 2, 65], BF16)
            self.dram("GN_d", [S, 24], F32)
            self.dram("GM_d", [S, 3072], F32)
            self.dram("KC_d", [2, 64, 128], BF16)
            self.dram("VC_d", [2, 128, 97], BF16)
            self.dram("O_d", [S, 1536], BF16)
            self.ntt = self.nseq * NT
            self.nblk = self.ntt + NE
            self.dram("xs_d", [self.nblk * 512, D], BF16)
            self.dram("ys_d", [self.nblk * 512, D], F32)
            self.dram("dbg_d", [128, 4096], F32)
            with ExitStack() as zs:
                zt = s.sb([128, 8192], BF16, "zt", zs)
                s.op("pool", lambda h: h.memset(zt[:], 0.0), writes=[zt])
                xv = self.Dr["xs_d"].rearrange("(a p r) d -> a p (r d)", p=128, r=8)
                for a in range(xv.shape[0]):
                    s.dma("sp", xv[a], zt[:], reads=[zt], writes=[self.DR["xs_d"]])
                s.barrier()
            try:
                self.body()
            except StopIteration:
                pass
            s.barrier()
            s.finish()
        return nc

    def phase_end(self, name):
        self.s.barrier()
        if self.stop_after == name:
            raise StopIteration

    def body(self):
        for l in range(self.layers):
            self.runp("mod%d" % l, self.phase_mod, l)
            for q in range(self.nseq):
                for nm, fn in (("inproj", self.phase_inproj), ("cmp", self.phase_cmp), ("swa", self.phase_swa), ("diff", self.phase_diff),
                               ("nsa", self.phase_nsa), ("merge", self.phase_merge)):
                    self.runp("%s%d_%d" % (nm, l, q), fn, l, q)
            self.runp("moe%d" % l, self.phase_moe2, l)

    def runp(self, name, fn, *args):
        if self.only is None or name in self.only:
            fn(*args)
        self.phase_end(name)

    def phase_mod(self, l):
        s, I = self.s, self.I
        with ExitStack() as ps:
            cT = s.sb([128, 8, NSEQ], F32, "cT", ps)
            cs = s.sb([128, 8, NSEQ], F32, "cs", ps)
            modb = s.sb([NSEQ, 6 * D], F32, "modb", ps)
            mods = s.sb([NSEQ, 6 * D], F32, "mods", ps)
            wb = [s.sb([128, 8, 512], F32, "modw", ps) for _ in range(2)]
            s.dma("sp", cT[:], I["cT"].rearrange("(kc p) b -> p kc b", p=128), writes=[cT])
            s.dma("sp", modb[:], I["mod_b"][l:l + 1, :].broadcast_to([NSEQ, 6 * D]), writes=[modb])
            s.op("act", lambda h: h.activation(out=cs[:], in_=cT[:], func=AF.Silu), reads=[cT], writes=[cs])
            wsrc = I["mod_w"][l].rearrange("(kc p) n -> p kc n", p=128)
            for cg in range(12):
                w = wb[cg % 2]
                s.dma("sp", w[:], wsrc[:, :, cg * 512:(cg + 1) * 512], writes=[w])
                pm = self.P[cg % 2]
                for kc in range(8):
                    self.mm(pm[0:NSEQ, :], cs[:, kc, :], w[:, kc, :], kc == 0, kc == 7, [cs, w], [pm], sig=(kc == 7))
                s.op("dve", lambda h: h.tensor_tensor(out=mods[:, cg * 512:(cg + 1) * 512], in0=pm[0:NSEQ, :], in1=modb[:, cg * 512:(cg + 1) * 512], op=ALU.add),
                     reads=[pm, modb], writes=[mods])
            for seg in (1, 4):
                s.op("dve", lambda h: h.tensor_scalar_add(out=mods[:, seg * D:(seg + 1) * D], in0=mods[:, seg * D:(seg + 1) * D], scalar1=1.0), reads=[mods], writes=[mods])
            s.dma("sp", self.Dr["mod_d"][l], mods[:], reads=[mods], writes=[self.DR["mod_d"]])

    def mod_bc(self, l, q, seg, tile):
        src = self.Dr["mod_d"][l, q:q + 1, seg * D:(seg + 1) * D].broadcast_to([128, D])
        self.s.dma("sp", tile[:], src, reads=[self.DR["mod_d"]], writes=[tile])

    def xsrc(self, l, q):
        return (self.I["x"] if l == 0 else self.Dr["xres"]), ([] if l == 0 else [self.DR["xres"]])

    def norm_tiles(self, ps, l, q, gname, seg_sc, seg_sh):
        s, I = self.s, self.I
        gsc = s.sb([128, D], F32, "gsc", ps)
        sh = s.sb([128, D], F32, "sh", ps)
        gt = s.sb([128, D], F32, "gt", ps)
        s.dma("sp", gt[:], I[gname][l:l + 1, :].broadcast_to([128, D]), writes=[gt])
        self.mod_bc(l, q, seg_sc, gsc)
        self.mod_bc(l, q, seg_sh, sh)
        s.op("dve", lambda h: h.tensor_tensor(out=gsc[:], in0=gsc[:], in1=gt[:], op=ALU.mult), reads=[gsc, gt], writes=[gsc])
        return gsc, sh

    def phase_inproj(self, l, q):
        s, I, Dr, DR = self.s, self.I, self.Dr, self.DR
        xsrc, xres_r = self.xsrc(l, q)
        with ExitStack() as ps:
            gsc, sh = self.norm_tiles(ps, l, q, "norm1_g", 1, 0)
            hT = s.sb([128, 8, S], BF16, "hT", ps)
            xt = [s.sb([128, D], F32, "xt", ps) for _ in range(2)]
            junk = s.sb([128, D], F32, "junk", ps)
            hb = [s.sb([128, D], BF16, "hb", ps) for _ in range(2)]
            ss = [s.sb([128, 1], F32, "ss", ps) for _ in range(2)]
            for tt in range(NT):
                x, h, sq = xt[tt % 2], hb[tt % 2], ss[tt % 2]
                r0 = q * S + tt * 128
                s.dma("sp", x[:], xsrc[r0:r0 + 128, :], reads=xres_r, writes=[x])
                self.rstd_of((x, x[:]), (junk, junk[:]), sq)
                s.op("dve", lambda hh: hh.scalar_tensor_tensor(out=x[:], in0=x[:], scalar=sq[:, 0:1], in1=gsc[:], op0=ALU.mult, op1=ALU.mult), reads=[x, sq, gsc], writes=[x])
                s.op("pool", lambda hh: hh.tensor_tensor(out=h[:], in0=x[:], in1=sh[:], op=ALU.add), reads=[x, sh], writes=[h])
                pt = self.P[tt % 2]
                ptb = pt[:].bitcast(BF16)
                for kc in range(8):
                    self.tr(ptb[:, kc * 128:(kc + 1) * 128], h[:, kc * 128:(kc + 1) * 128], self.ident[:], [h, self.ident], [pt], sig=(kc == 7))
                s.op("act", lambda hh: hh.copy(out=hT[:, :, tt * 128:(tt + 1) * 128], in_=ptb.rearrange("p (k t) -> p k t", k=8)), reads=[pt], writes=[hT])
            binT = s.sb([64, NSLOT], F32, "binT", ps)
            s.dma("sp", binT[:], I["b_inT"][l], writes=[binT])
            wsrc = I["w_in"][l].rearrange("(kc p) n -> p kc n", p=128)
            wbuf = [s.sb([128, 8, 512], BF16, "wblk", ps) for _ in range(2)]
            stg = [s.sb([64, S], BF16, "stg", ps) for _ in range(2)]
            groups = [(SL_QA, 8), (SL_KA, 2), (SL_QB, 8), (SL_KB, 8), (SL_QC, 8), (SL_KCM, 4), (SL_KSL, 2), (SL_KWN, 2)]
            nw = 0
            nmm = 0
            for (s0, ns) in groups:
                w = wbuf[nw % 2]
                nw += 1
                c0 = SLOT_COL[s0]
                s.dma("pool", w[:, :, 0:ns * 64], wsrc[:, :, c0:c0 + ns * 64], writes=[w])
                for si in range(ns):
                    slot = s0 + si
                    sg = stg[slot % 2]
                    for tg in range(4):
                        pm = self.P[2 + nmm % 4]
                        nmm += 1
                        for kc in range(8):
                            self.mm(pm[0:64, :], w[:, kc, si * 64:(si + 1) * 64], hT[:, kc, tg * 512:(tg + 1) * 512], kc == 0, kc == 7, [w, hT], [pm], sig=(kc == 7))
                        s.op("act", lambda hh: hh.activation(out=sg[:, tg * 512:(tg + 1) * 512], in_=pm[0:64, :], func=AF.Identity, bias=binT[:, slot:slot + 1]),
                             reads=[pm, binT], writes=[sg])
                    s.dma("sp", Dr["QT_d"][slot], sg[:], reads=[sg], writes=[DR["QT_d"]])
            binb = s.sb([128, INW], F32, "binb", ps)
            s.dma("sp", binb[:], I["b_in"][l:l + 1, :].broadcast_to([128, INW]), writes=[binb])
            vt = {}
            for nm, nh, dv in (("VA_d", 2, 64), ("VB_d", 4, 128), ("VSL_d", 2, 64), ("VWN_d", 2, 64)):
                vt[nm] = [s.sb([128, nh, dv + 1], BF16, "vt", ps) for _ in range(2)]
                for v in vt[nm]:
                    s.op("pool", lambda hh: hh.memset(v[:, :, dv:dv + 1], 1.0), writes=[v])
            gnt = [s.sb([128, 24], F32, "gnt", ps) for _ in range(2)]
            gmt = [s.sb([128, 512], F32, "gmt", ps) for _ in range(2)]
            blocks = [("VA_d", C_VA, 128, 2, 64), ("VB_d", C_VB, 512, 4, 128), ("VSL_d", C_VSL, 128, 2, 64), ("VWN_d", C_VWN, 128, 2, 64),
                      ("GN_d", C_GN, 24, 0, 0)] + [("GM_d", C_GM + 512 * i, 512, i, 0) for i in range(6)]
            for (nm, c0, ncol, nh, dv) in blocks:
                w = wbuf[nw % 2]
                nw += 1
                s.dma("pool", w[:, :, 0:ncol], wsrc[:, :, c0:c0 + ncol], writes=[w])
                for tt in range(NT):
                    pm = self.P[2 + nmm % 4]
                    nmm += 1
                    for kc in range(8):
                        self.mm(pm[:, 0:ncol], hT[:, kc, tt * 128:(tt + 1) * 128], w[:, kc, 0:ncol], kc == 0, kc == 7, [w, hT], [pm], sig=(kc == 7))
                    rows = slice(tt * 128, (tt + 1) * 128)
                    if nm == "GN_d":
                        g = gnt[tt % 2]
                        s.op("dve", lambda hh: hh.tensor_tensor(out=g[:], in0=pm[:, 0:24], in1=binb[:, c0:c0 + 24], op=ALU.add), reads=[pm, binb], writes=[g])
                        s.op("act", lambda hh: hh.activation(out=g[:], in_=g[:], func=AF.Sigmoid), reads=[g], writes=[g])
                        s.dma("sp", Dr["GN_d"][rows, :], g[:], reads=[g], writes=[DR["GN_d"]])
                    elif nm == "GM_d":
                        g = gmt[tt % 2]
                        s.op("dve", lambda hh: hh.tensor_tensor(out=g[:], in0=pm[:, :], in1=binb[:, c0:c0 + 512], op=ALU.add), reads=[pm, binb], writes=[g])
                        s.op("act", lambda hh: hh.activation(out=g[:], in_=g[:], func=AF.Sigmoid), reads=[g], writes=[g])
                        s.dma("sp", Dr["GM_d"][rows, nh * 512:(nh + 1) * 512], g[:], reads=[g], writes=[DR["GM_d"]])
                    else:
                        v = vt[nm][tt % 2]
                        s.op("dve", lambda hh: hh.tensor_tensor(out=v[:, :, 0:dv], in0=pm[:, 0:ncol].rearrange("p (h d) -> p h d", d=dv),
                                                               in1=binb[:, c0:c0 + ncol].rearrange("p (h d) -> p h d", d=dv), op=ALU.add), reads=[pm, binb], writes=[v])
                        s.dma("sp", Dr[nm][rows], v[:], reads=[v], writes=[DR[nm]])

    def phase_cmp(self, l, q):
        s, I, Dr, DR = self.s, self.I, self.Dr, self.DR
        with ExitStack() as ps:
            ovl = self.load_const(ps, "overlap", [128, 32], BF16)
            w1 = s.sb([64, 32, 256], BF16, "cw1", ps)
            w2 = s.sb([128, 2, 64], BF16, "cw2", ps)
            posT = s.sb([64, 32], F32, "posT", ps)
            posb = s.sb([64, 32], BF16, "posb", ps)
            b1T = s.sb([128, 2], F32, "b1T", ps)
            bias = s.sb([128, 2], F32, "cbias", ps)
            b2T = s.sb([64, 1], F32, "b2T", ps)
            b2b = s.sb([128, 64], F32, "b2b", ps)
            xT = s.sb([64, S], BF16, "cxT", ps)
            hid = s.sb([128, 2, 128], BF16, "hid", ps)
            y = s.sb([128, 128], F32, "cy", ps)
            u = s.sb([128, 128], F32, "cu", ps)
            kct = s.sb([64, 128], BF16, "kct", ps)
            vct = s.sb([128, 97], BF16, "vct", ps)
            s.op("pool", lambda h: h.memset(kct[:], 0.0), writes=[kct])
            s.op("pool", lambda h: h.memset(vct[:], 0.0), writes=[vct])
            for which in range(2):
                s.dma("pool", w1[:], I["cmp_w1"][l, which].rearrange("(l d) f -> d l f", d=64), writes=[w1])
                s.dma("pool", w2[:], I["cmp_w2"][l, which].rearrange("(c p) d -> p c d", p=128), writes=[w2])
                s.dma("sp", posT[:], I["cmp_posT"][l, which], writes=[posT])
                s.dma("sp", b1T[:], I["cmp_b1T"][l, which], writes=[b1T])
                s.dma("sp", b2T[:], I["cmp_b2T"][l, which], writes=[b2T])
                s.dma("sp", b2b[:], I["cmp_b2"][l, which:which + 1, :].broadcast_to([128, 64]), writes=[b2b])
                s.op("act", lambda h: h.copy(out=posb[:], in_=posT[:]), reads=[posT], writes=[posb])
                for ch in range(2):
                    pm = self.P[ch]
                    for li in range(32):
                        self.mm(pm[:, 0:1], w1[:, li, ch * 128:(ch + 1) * 128], posb[:, li:li + 1], li == 0, li == 31, [w1, posb], [pm], sig=(li == 31))
                    s.op("dve", lambda h: h.tensor_tensor(out=bias[:, ch:ch + 1], in0=pm[:, 0:1], in1=b1T[:, ch:ch + 1], op=ALU.add), reads=[pm, b1T], writes=[bias])
                for hk in range(2):
                    s.dma("sp", xT[:], Dr["QT_d"][SL_KCM + which * 2 + hk], reads=[DR["QT_d"]], writes=[xT])
                    for ch in range(2):
                        pm = self.P[2 + ch]
                        for li in range(32):
                            self.mm(pm[:, 0:127], w1[:, li, ch * 128:(ch + 1) * 128], xT[:, li:li + 16 * 126 + 1:16], li == 0, li == 31, [w1, xT], [pm], sig=(li == 31))
                        s.op("act", lambda h: h.activation(out=y[:, 0:127], in_=pm[:, 0:127], func=AF.Identity, bias=bias[:, ch:ch + 1]), reads=[pm, bias], writes=[y])
                        s.op("dve", lambda h: h.tensor_tensor(out=u[:, 0:127], in0=y[:, 0:127], in1=y[:, 0:127], op=ALU.mult), reads=[y], writes=[u])
                        s.op("dve", lambda h: h.tensor_scalar(out=u[:, 0:127], in0=u[:, 0:127], scalar1=0.044715, scalar2=1.0, op0=ALU.mult, op1=ALU.add), reads=[u], writes=[u])
                        s.op("dve", lambda h: h.tensor_tensor(out=u[:, 0:127], in0=u[:, 0:127], in1=y[:, 0:127], op=ALU.mult), reads=[u, y], writes=[u])
                        s.op("act", lambda h: h.activation(out=u[:, 0:127], in_=u[:, 0:127], func=AF.Sigmoid, scale=1.5957691216057308), reads=[u], writes=[u])
                        s.op("dve", lambda h: h.tensor_tensor(out=hid[:, ch, 0:127], in0=u[:, 0:127], in1=y[:, 0:127], op=ALU.mult), reads=[u, y], writes=[hid])
                    pm2 = self.P[4 + hk]
                    if which == 0:
                        for ch in range(2):
                            self.mm(pm2[0:64, 0:127], w2[:, ch, :], hid[:, ch, 0:127], ch == 0, ch == 1, [w2, hid], [pm2], sig=(ch == 1))
                        s.op("act", lambda h: h.activation(out=kct[:, 0:127], in_=pm2[0:64, 0:127], func=AF.Identity, bias=b2T[:, 0:1]), reads=[pm2, b2T], writes=[kct])
                        s.dma("sp", Dr["KC_d"][hk], kct[:], reads=[kct], writes=[DR["KC_d"]])
                    else:
                        for ch in range(2):
                            self.mm(pm2[0:127, 0:64], hid[:, ch, 0:127], w2[:, ch, :], ch == 0, ch == 1, [w2, hid], [pm2], sig=(ch == 1))
                        s.op("dve", lambda h: h.tensor_tensor(out=vct[0:127, 0:64], in0=pm2[0:127, 0:64], in1=b2b[0:127, :], op=ALU.add), reads=[pm2, b2b], writes=[vct])
                        s.op("pool", lambda h: h.memset(vct[:, 64:65], 1.0), writes=[vct])
                        s.op("pool", lambda h: h.tensor_copy(out=vct[:, 65:97], in_=ovl[:]), reads=[ovl], writes=[vct])
                        s.dma("sp", Dr["VC_d"][hk], vct[:], reads=[vct], writes=[DR["VC_d"]])

    def load_heads(self, tile, slots, grouped):
        s = self.s
        for g, slot in enumerate(slots):
            dst0 = tile[0:64, g, :] if grouped else tile[0:64, :]
            dst1 = tile[64:68, g, :] if grouped else tile[64:68, :]
            s.dma("sp", dst0, self.Dr["QT_d"][slot], reads=[self.DR["QT_d"]], writes=[tile])
            s.dma("sp", dst1, self.I["aug"][slot], writes=[tile])

    def attn_stream(self, items, banks, pts):
        s = self.s
        n = len(items)

        def emit_qk(k):
            it = items[k]
            sc = banks[k % len(banks)]
            nq = len(it["qk"])
            for m, (outfn, lhsT, rhs, start, stop, reads) in enumerate(it["qk"]):
                self.mm(outfn(sc), lhsT, rhs, start, stop, reads, [sc], sig=(m == nq - 1))

        emit_qk(0)
        if n > 1:
            emit_qk(1)
        for k in range(n):
            if k + 2 < n:
                emit_qk(k + 2)
            it = items[k]
            sc = banks[k % len(banks)]
            pT = pts[k % len(pts)]
            kp, N = it["kp"], it["N"]
            s.op("act", lambda h: h.activation(out=pT[0:kp, 0:N], in_=sc[0:kp, 0:N], func=AF.Exp, scale=SCALE), reads=[sc], writes=[pT])
            it["pv"](pT)

    def load_const(self, ps, name, shape, dt):
        t = self.s.sb(shape, dt, name, ps)
        self.s.dma("sp", t[:], self.I[name], writes=[t])
        return t

    def phase_swa(self, l, q):
        s, I, Dr, DR = self.s, self.I, self.Dr, self.DR
        ident = self.ident
        with ExitStack() as ps:
            mdiag = self.load_const(ps, "mdiag", [128, 128], BF16)
            medge = self.load_const(ps, "medge", [128, 128], BF16)
            esink = s.sb([128, 8], F32, "esink", ps)
            s.dma("sp", esink[:], I["sinks"][l:l + 1, :].broadcast_to([128, 8]), writes=[esink])
            s.op("act", lambda h: h.activation(out=esink[:], in_=esink[:], func=AF.Exp), reads=[esink], writes=[esink])
            QG = s.sb([68, 4, S], BF16, "QG", ps)
            KT = s.sb([68, S], BF16, "KT", ps)
            V = s.sb([128, NT, 65], BF16, "V", ps)
            pts = [s.sb([128, 512], BF16, "pT", ps) for _ in range(3)]
            ot = [s.sb([128, 4, 64], BF16, "ot", ps) for _ in range(2)]
            den = [s.sb([128, 4], F32, "den", ps) for _ in range(2)]
            for hk in range(2):
                self.load_heads(QG, [SL_QA + hk * 4 + g for g in range(4)], True)
                self.load_heads(KT, [SL_KA + hk], False)
                s.dma("sp", V[:], Dr["VA_d"][:, hk, :].rearrange("(n p) e -> p n e", p=128), reads=[DR["VA_d"]], writes=[V])
                for j in range(NT):
                    acc = self.P[4 + j % 2]
                    tiles = [i for i in (j - 1, j) if i >= 0]
                    items = []
                    for idx, i in enumerate(tiles):
                        mask = mdiag if i == j else medge
                        o3 = lambda sc: sc[:, :].rearrange("p (g t) -> p g t", g=4)
                        qk = [(o3, KT[:, i * 128:(i + 1) * 128], QG[:, :, j * 128:(j + 1) * 128], True, False, [KT, QG]),
                              (o3, ident[:], mask[:].unsqueeze(1).broadcast_to([128, 4, 128]), False, True, [ident, mask])]

                        def pv(pT, i=i, idx=idx, acc=acc, last=len(tiles) - 1):
                            for g in range(4):
                                self.mm(acc[:, g * 65:(g + 1) * 65], pT[:, g * 128:(g + 1) * 128], V[:, i, :], idx == 0 and g == 0, idx == last, [pT, V], [acc], sig=(g == 3))
                        items.append(dict(kp=128, N=512, qk=qk, pv=pv))
                    self.attn_stream(items, self.P[0:3], pts)
                    a3 = acc[:, 0:260].rearrange("p (g e) -> p g e", e=65)
                    dn, o = den[j % 2], ot[j % 2]
                    s.op("dve", lambda h: h.tensor_tensor(out=dn[:].unsqueeze(2), in0=a3[:, :, 64:65], in1=esink[:, hk * 4:(hk + 1) * 4].unsqueeze(2), op=ALU.add), reads=[acc, esink], writes=[dn])
                    s.op("dve", lambda h: h.reciprocal(out=dn[:], in_=dn[:]), reads=[dn], writes=[dn])
                    s.op("dve", lambda h: h.tensor_tensor(out=o[:], in0=a3[:, :, 0:64], in1=dn[:].unsqueeze(2).broadcast_to([128, 4, 64]), op=ALU.mult), reads=[acc, dn], writes=[o])
                    s.dma("sp", Dr["O_d"][j * 128:(j + 1) * 128, hk * 256:(hk + 1) * 256], o[:].rearrange("p g d -> p (g d)"), reads=[o], writes=[DR["O_d"]])

    def phase_diff(self, l, q):
        s, I, Dr, DR = self.s, self.I, self.Dr, self.DR
        ident = self.ident
        lam_init = 0.8 - 0.6 * float(np.exp(-0.3 * l))
        with ExitStack() as ps:
            mdiag = self.load_const(ps, "mdiag", [128, 128], BF16)
            dl = s.sb([128, 256], F32, "dl", ps)
            s.dma("sp", dl[:], I["diff_lambda"][l:l + 1, :].broadcast_to([128, 256]), writes=[dl])
            pr = s.sb([128, 2, 64], F32, "pr", ps)
            d4 = dl[:].rearrange("p (a b d) -> p a b d", a=2, b=2)
            s.op("dve", lambda h: h.tensor_tensor(out=pr[:], in0=d4[:, :, 0, :], in1=d4[:, :, 1, :], op=ALU.mult), reads=[dl], writes=[pr])
            e2 = s.sb([128, 2], F32, "e2", ps)
            s.op("dve", lambda h: h.reduce_sum(out=e2[:], in_=pr[:], axis=AX.X), reads=[pr], writes=[e2])
            s.op("act", lambda h: h.activation(out=e2[:], in_=e2[:], func=AF.Exp), reads=[e2], writes=[e2])
            nlam = s.sb([128, 1], F32, "nlam", ps)
            s.op("dve", lambda h: h.scalar_tensor_tensor(out=nlam[:], in0=e2[:, 0:1], scalar=lam_init, in1=e2[:, 1:2], op0=ALU.add, op1=ALU.subtract), reads=[e2], writes=[nlam])
            s.op("dve", lambda h: h.tensor_scalar_mul(out=nlam[:], in0=nlam[:], scalar1=-1.0), reads=[nlam], writes=[nlam])
            gsub = s.sb([128, 128], F32, "gsub", ps)
            s.dma("sp", gsub[:], I["diff_subln_g"][l:l + 1, :].broadcast_to([128, 128]), writes=[gsub])
            s.op("dve", lambda h: h.tensor_scalar_mul(out=gsub[:], in0=gsub[:], scalar1=1.0 - lam_init), reads=[gsub], writes=[gsub])
            KT = [s.sb([68, S], BF16, "KTb", ps) for _ in range(2)]
            QT = [s.sb([68, S], BF16, "QTb", ps) for _ in range(2)]
            V = s.sb([128, NT, 129], BF16, "Vb", ps)
            pts = [s.sb([128, 512], BF16, "pT", ps) for _ in range(3)]
            t0 = [s.sb([128, 128], F32, "t0", ps) for _ in range(2)]
            junk = s.sb([128, 128], F32, "junkb", ps)
            rr = [s.sb([128, 4], F32, "rr", ps) for _ in range(2)]
            ob = [s.sb([128, 128], BF16, "ob", ps) for _ in range(2)]
            nfin = 0
            for hd in range(4):
                for c in range(2):
                    self.load_heads(KT[c], [SL_KB + 2 * hd + c], False)
                    self.load_heads(QT[c], [SL_QB + 2 * hd + c], False)
                s.dma("sp", V[:], Dr["VB_d"][:, hd, :].rearrange("(n p) e -> p n e", p=128), reads=[DR["VB_d"]], writes=[V])
                for G in range(4):
                    def accap(c, jj):
                        return self.P[4 + c * 2 + jj // 2], (jj % 2) * 129
                    items = []
                    for c in range(2):
                        for i in range(0, 4 * G + 4):
                            j0 = max(i, 4 * G)
                            N = (4 * G + 4 - j0) * 128
                            kt = KT[c][:, i * 128:(i + 1) * 128]
                            qk = []
                            if i >= 4 * G:
                                qk.append((lambda sc: sc[:, 0:128], kt, QT[c][:, j0 * 128:(j0 + 1) * 128], True, False, [KT[c], QT[c]]))
                                qk.append((lambda sc: sc[:, 0:128], ident[:], mdiag[:], False, True, [ident, mdiag]))
                                if N > 128:
                                    qk.append((lambda sc, N=N: sc[:, 128:N], kt, QT[c][:, (j0 + 1) * 128:(4 * G + 4) * 128], True, True, [KT[c], QT[c]]))
                            else:
                                qk.append((lambda sc, N=N: sc[:, 0:N], kt, QT[c][:, j0 * 128:(4 * G + 4) * 128], True, True, [KT[c], QT[c]]))

                            def pv(pT, c=c, i=i, j0=j0, G=G):
                                for jj in range(j0, 4 * G + 4):
                                    bank, off = accap(c, jj - 4 * G)
                                    self.mm(bank[:, off:off + 129], pT[:, (jj - j0) * 128:(jj - j0 + 1) * 128], V[:, i, :], i == 0 and (jj - 4 * G) % 2 == 0, i == jj, [pT, V], [bank], sig=(jj == 4 * G + 3))
                            items.append(dict(kp=128, N=N, qk=qk, pv=pv))
                    self.attn_stream(items, self.P[0:3], pts)
                    for jj in range(4):
                        b0, o0 = accap(0, jj)
                        b1, o1 = accap(1, jj)
                        r, t, o = rr[nfin % 2], t0[nfin % 2], ob[nfin % 2]
                        nfin += 1
                        s.op("dve", lambda h: h.reciprocal(out=r[:, 0:1], in_=b0[:, o0 + 128:o0 + 129]), reads=[b0], writes=[r])
                        s.op("dve", lambda h: h.reciprocal(out=r[:, 1:2], in_=b1[:, o1 + 128:o1 + 129]), reads=[b1], writes=[r])
                        s.op("dve", lambda h: h.tensor_tensor(out=r[:, 1:2], in0=r[:, 1:2], in1=nlam[:], op=ALU.mult), reads=[r, nlam], writes=[r])
                        s.op("dve", lambda h: h.tensor_scalar_mul(out=t[:], in0=b0[:, o0:o0 + 128], scalar1=r[:, 0:1]), reads=[b0, r], writes=[t])
                        s.op("dve", lambda h: h.scalar_tensor_tensor(out=t[:], in0=b1[:, o1:o1 + 128], scalar=r[:, 1:2], in1=t[:], op0=ALU.mult, op1=ALU.add), reads=[b1, r, t], writes=[t])
                        s.op("act", lambda h: h.activation(out=junk[:], in_=t[:], func=AF.Square, accum_out=r[:, 2:3]), reads=[t], writes=[junk, r])
                        s.op("dve", lambda h: h.tensor_scalar(out=r[:, 2:3], in0=r[:, 2:3], scalar1=1.0 / 128, scalar2=EPS, op0=ALU.mult, op1=ALU.add), reads=[r], writes=[r])
                        s.op("act", lambda h: h.sqrt(out=r[:, 2:3], in_=r[:, 2:3]), reads=[r], writes=[r])
                        s.op("dve", lambda h: h.reciprocal(out=r[:, 2:3], in_=r[:, 2:3]), reads=[r], writes=[r])
                        s.op("dve", lambda h: h.scalar_tensor_tensor(out=o[:], in0=t[:], scalar=r[:, 2:3], in1=gsub[:], op0=ALU.mult, op1=ALU.mult), reads=[t, r, gsub], writes=[o])
                        row0 = (4 * G + jj) * 128
                        s.dma("sp", Dr["O_d"][row0:row0 + 128, 512 + hd * 128:512 + (hd + 1) * 128], o[:], reads=[o], writes=[DR["O_d"]])

    def phase_nsa(self, l, q):
        s, I, Dr, DR = self.s, self.I, self.Dr, self.DR
        ident = self.ident
        with ExitStack() as ps:
            mdiag = self.load_const(ps, "mdiag", [128, 128], BF16)
            medge = self.load_const(ps, "medge", [128, 128], BF16)
            cmaskT = self.load_const(ps, "cmaskT", [128, S], BF16)
            eblk = self.load_const(ps, "eblk", [32, S], BF16)
            atab = s.sb([128, NT, 32], F32, "atab", ps)
            s.dma("sp", atab[:], I["atab"].rearrange("(n p) b -> p n b", p=128), writes=[atab])
            GN = s.sb([128, NT, 24], F32, "GN", ps)
            s.dma("sp", GN[:], Dr["GN_d"].rearrange("(n p) c -> p n c", p=128), reads=[DR["GN_d"]], writes=[GN])
            QG = s.sb([68, 4, S], BF16, "QGc", ps)
            KSL = s.sb([68, S], BF16, "KSL", ps)
            KWN = s.sb([68, S], BF16, "KWN", ps)
            KCT = s.sb([64, 128], BF16, "KCT", ps)
            VC = s.sb([128, 97], BF16, "VC", ps)
            VSL = s.sb([128, NT, 65], BF16, "VSL", ps)
            VWN = s.sb([128, NT, 65], BF16, "VWN", ps)
            OC = s.sb([128, NT, 4, 64], F32, "OC", ps)
            SELT = s.sb([32, S], BF16, "SELT", ps)
            pts = [s.sb([128, 512], BF16, "pT", ps) for _ in range(3)]
            dn = [s.sb([128, 12], F32, "dn", ps) for _ in range(2)]
            imp = [s.sb([128, 32], F32, "imp", ps) for _ in range(2)]
            tmp32 = [s.sb([128, 32], F32, "tmp32", ps) for _ in range(2)]
            m8 = [s.sb([128, 16], F32, "m8", ps) for _ in range(2)]
            oo = [s.sb([128, 4, 64], F32, "oo", ps) for _ in range(2)]
            ot = [s.sb([128, 4, 64], F32, "otmp", ps) for _ in range(2)]
            ob = [s.sb([128, 4, 64], BF16, "obf", ps) for _ in range(2)]
            o3 = lambda sc: sc[:, :].rearrange("p (g t) -> p g t", g=4)
            o3c = lambda sc: sc[0:127, :].rearrange("p (g t) -> p g t", g=4)
            b4 = lambda ap: ap.unsqueeze(1).broadcast_to([ap.shape[0], 4, 128])
            for hk in range(2):
                self.load_heads(QG, [SL_QC + hk * 4 + g for g in range(4)], True)
                self.load_heads(KSL, [SL_KSL + hk], False)
                self.load_heads(KWN, [SL_KWN + hk], False)
                s.dma("sp", KCT[:], Dr["KC_d"][hk], reads=[DR["KC_d"]], writes=[KCT])
                s.dma("sp", VC[:], Dr["VC_d"][hk], reads=[DR["VC_d"]], writes=[VC])
                s.dma("sp", VSL[:], Dr["VSL_d"][:, hk, :].rearrange("(n p) e -> p n e", p=128), reads=[DR["VSL_d"]], writes=[VSL])
                s.dma("sp", VWN[:], Dr["VWN_d"][:, hk, :].rearrange("(n p) e -> p n e", p=128), reads=[DR["VWN_d"]], writes=[VWN])
                for j in range(NT):
                    acc = self.P[4 + j % 2]
                    jc = slice(j * 128, (j + 1) * 128)
                    qk = [(o3c, KCT[:, 0:127], QG[0:64, :, jc], True, False, [KCT, QG]),
                          (o3c, ident[0:127, 0:127], b4(cmaskT[0:127, jc]), False, True, [ident, cmaskT])]

                    def pv(pT, acc=acc):
                        for g in range(4):
                            self.mm(acc[:, g * 97:(g + 1) * 97], pT[0:127, g * 128:(g + 1) * 128], VC[0:127, :], g == 0, True, [pT, VC], [acc], sig=(g == 3))
                    self.attn_stream([dict(kp=127, N=512, qk=qk, pv=pv)], self.P[0:3], pts)
                    a3 = acc[:, 0:388].rearrange("p (g e) -> p g e", e=97)
                    d, im, tm, mm8 = dn[j % 2], imp[j % 2], tmp32[j % 2], m8[j % 2]
                    s.op("dve", lambda h: h.tensor_scalar_max(out=d[:, 0:4].unsqueeze(2), in0=a3[:, :, 64:65], scalar1=1e-30), reads=[acc], writes=[d])
                    s.op("dve", lambda h: h.reciprocal(out=d[:, 0:4], in_=d[:, 0:4]), reads=[d], writes=[d])
                    s.op("dve", lambda h: h.tensor_tensor(out=OC[:, j], in0=a3[:, :, 0:64], in1=d[:, 0:4].unsqueeze(2).broadcast_to([128, 4, 64]), op=ALU.mult), reads=[acc, d], writes=[OC])
                    s.op("dve", lambda h: h.tensor_scalar_mul(out=im[:], in0=a3[:, 0, 65:97], scalar1=d[:, 0:1]), reads=[acc, d], writes=[im])
                    for g in range(1, 4):
                        s.op("dve", lambda h, g=g: h.scalar_tensor_tensor(out=im[:], in0=a3[:, g, 65:97], scalar=d[:, g:g + 1], in1=im[:], op0=ALU.mult, op1=ALU.add), reads=[acc, d, im], writes=[im])
                    s.op("dve", lambda h: h.tensor_tensor(out=im[:], in0=im[:], in1=atab[:, j, :], op=ALU.add), reads=[im, atab], writes=[im])
                    s.op("dve", lambda h: h.max(out=mm8[:, 0:8], in_=im[:]), reads=[im], writes=[mm8])
                    s.op("dve", lambda h: h.match_replace(out=tm[:], in_to_replace=mm8[:, 0:8], in_values=im[:], imm_value=-3e9), reads=[im, mm8], writes=[tm])
                    s.op("dve", lambda h: h.max(out=mm8[:, 8:16], in_=tm[:]), reads=[tm], writes=[mm8])
                    s.op("dve", lambda h: h.tensor_scalar(out=tm[:], in0=im[:], scalar1=mm8[:, 15:16], scalar2=NEG, op0=ALU.is_lt, op1=ALU.mult), reads=[im, mm8], writes=[tm])
                    pt = self.P[6 + j % 2]
                    self.tr(pt[0:32, 0:128], tm[:], self.ident32[:], [tm, self.ident32], [pt])
                    s.op("act", lambda h: h.copy(out=SELT[:, jc], in_=pt[0:32, 0:128]), reads=[pt], writes=[SELT])
                for j in range(NT):
                    jc = slice(j * 128, (j + 1) * 128)
                    accS, accW = self.P[4 + 2 * (j % 2)], self.P[5 + 2 * (j % 2)]
                    items = []
                    for i in range(0, j + 1):
                        ic = slice(i * 128, (i + 1) * 128)
                        qk = [(o3, KSL[:, ic], QG[:, :, jc], True, False, [KSL, QG]),
                              (o3, eblk[:, ic], b4(SELT[:, jc]), False, i != j, [eblk, SELT])]
                        if i == j:
                            qk.append((o3, ident[:], b4(mdiag[:]), False, True, [ident, mdiag]))

                        def pv(pT, i=i, j=j, acc=accS):
                            for g in range(4):
                                self.mm(acc[:, g * 65:(g + 1) * 65], pT[:, g * 128:(g + 1) * 128], VSL[:, i, :], i == 0 and g == 0, i == j, [pT, VSL], [acc], sig=(g == 3))
                        items.append(dict(kp=128, N=512, qk=qk, pv=pv))
                    i0 = max(0, j - 4)
                    for i in range(i0, j + 1):
                        ic = slice(i * 128, (i + 1) * 128)
                        masked = (i == j) or (i == j - 4)
                        qk = [(o3, KWN[:, ic], QG[:, :, jc], True, not masked, [KWN, QG])]
                        if masked:
                            mk = mdiag if i == j else medge
                            qk.append((o3, ident[:], b4(mk[:]), False, True, [ident, mk]))

                        def pv(pT, i=i, j=j, i0=i0, acc=accW):
                            for g in range(4):
                                self.mm(acc[:, g * 65:(g + 1) * 65], pT[:, g * 128:(g + 1) * 128], VWN[:, i, :], i == i0 and g == 0, i == j, [pT, VWN], [acc], sig=(g == 3))
                        items.append(dict(kp=128, N=512, qk=qk, pv=pv))
                    self.attn_stream(items, self.P[0:3], pts)
                    d, o, t, obf = dn[j % 2], oo[j % 2], ot[j % 2], ob[j % 2]
                    aS = accS[:, 0:260].rearrange("p (g e) -> p g e", e=65)
                    aW = accW[:, 0:260].rearrange("p (g e) -> p g e", e=65)
                    gv = GN[:, j, hk * 12:(hk + 1) * 12].rearrange("p (g r) -> p g r", r=3)
                    s.op("dve", lambda h: h.reciprocal(out=d[:, 4:8].unsqueeze(2), in_=aS[:, :, 64:65]), reads=[accS], writes=[d])
                    s.op("dve", lambda h: h.reciprocal(out=d[:, 8:12].unsqueeze(2), in_=aW[:, :, 64:65]), reads=[accW], writes=[d])
                    s.op("dve", lambda h: h.tensor_tensor(out=d[:, 4:8].unsqueeze(2), in0=d[:, 4:8].unsqueeze(2), in1=gv[:, :, 1:2], op=ALU.mult), reads=[d, GN], writes=[d])
                    s.op("dve", lambda h: h.tensor_tensor(out=d[:, 8:12].unsqueeze(2), in0=d[:, 8:12].unsqueeze(2), in1=gv[:, :, 2:3], op=ALU.mult), reads=[d, GN], writes=[d])
                    s.op("pool", lambda h: h.tensor_tensor(out=o[:], in0=OC[:, j], in1=gv[:, :, 0:1].broadcast_to([128, 4, 64]), op=ALU.mult), reads=[OC, GN], writes=[o])
                    s.op("dve", lambda h: h.tensor_tensor(out=t[:], in0=aS[:, :, 0:64], in1=d[:, 4:8].unsqueeze(2).broadcast_to([128, 4, 64]), op=ALU.mult), reads=[accS, d], writes=[t])
                    s.op("pool", lambda h: h.tensor_tensor(out=o[:], in0=o[:], in1=t[:], op=ALU.add), reads=[o, t], writes=[o])
                    s.op("dve", lambda h: h.tensor_tensor(out=t[:], in0=aW[:, :, 0:64], in1=d[:, 8:12].unsqueeze(2).broadcast_to([128, 4, 64]), op=ALU.mult), reads=[accW, d], writes=[t])
                    s.op("pool", lambda h: h.tensor_tensor(out=obf[:], in0=o[:], in1=t[:], op=ALU.add), reads=[o, t], writes=[obf])
                    s.dma("sp", Dr["O_d"][jc, 1024 + hk * 256:1024 + (hk + 1) * 256], obf[:].rearrange("p g d -> p (g d)"), reads=[obf], writes=[DR["O_d"]])

    def phase_merge(self, l, q):
        s, I, Dr, DR = self.s, self.I, self.Dr, self.DR
        xsrc, xres_r = self.xsrc(l, q)
        with ExitStack() as ps:
            wbr = s.sb([128, 12, D], BF16, "wbr", ps)
            wout = s.sb([128, 8, D], BF16, "wout", ps)
            for r in range(3):
                for hh in range(2):
                    s.dma("pool", wbr[:, r * 4:(r + 1) * 4, hh * 512:(hh + 1) * 512],
                          I["w_branch"][l, r].rearrange("(kc p) n -> p kc n", p=128)[:, :, hh * 512:(hh + 1) * 512], writes=[wbr])
            for k4 in range(2):
                for hh in range(2):
                    s.dma("pool", wout[:, k4 * 4:(k4 + 1) * 4, hh * 512:(hh + 1) * 512],
                          I["w_out"][l].rearrange("(kc p) n -> p kc n", p=128)[:, k4 * 4:(k4 + 1) * 4, hh * 512:(hh + 1) * 512], writes=[wout])
            g1 = s.sb([128, D], F32, "g1", ps)
            self.mod_bc(l, q, 2, g1)
            Ot = [s.sb([128, 1536], BF16, "Ot", ps) for _ in range(2)]
            GM = [s.sb([128, 3072], F32, "GMt", ps) for _ in range(2)]
            xt = [s.sb([128, D], F32, "xt", ps) for _ in range(2)]
            OT = [s.sb([128, 12, 128], BF16, "OT", ps) for _ in range(2)]
            MT = [s.sb([128, 8, 128], BF16, "MT", ps) for _ in range(2)]
            mg = s.sb([128, D], F32, "mg", ps)
            mgb = s.sb([128, D], BF16, "mgb", ps)
            tmp = [s.sb([128, 512], F32, "mtmp", ps) for _ in range(2)]
            nb = 0
            for tt in range(NT):
                rows = slice(tt * 128, (tt + 1) * 128)
                grow = slice(q * S + tt * 128, q * S + (tt + 1) * 128)
                O, G, x, ot, mt = Ot[tt % 2], GM[tt % 2], xt[tt % 2], OT[tt % 2], MT[tt % 2]
                s.dma("sp", O[:], Dr["O_d"][rows, :], reads=[DR["O_d"]], writes=[O])
                s.dma("sp", G[:], Dr["GM_d"][rows, :], reads=[DR["GM_d"]], writes=[G])
                s.dma("sp", x[:], xsrc[grow, :], reads=xres_r, writes=[x])
                pa, pb = self.P[0], self.P[1]
                pab, pbb = pa[:].bitcast(BF16), pb[:].bitcast(BF16)
                for kc in range(8):
                    self.tr(pab[:, kc * 128:(kc + 1) * 128], O[:, kc * 128:(kc + 1) * 128], self.ident[:], [O, self.ident], [pa], sig=(kc == 7))
                for kc in range(4):
                    self.tr(pbb[:, kc * 128:(kc + 1) * 128], O[:, (8 + kc) * 128:(9 + kc) * 128], self.ident[:], [O, self.ident], [pb], sig=(kc == 3))
                s.op("act", lambda h: h.copy(out=ot[:, 0:8, :], in_=pab.rearrange("p (k t) -> p k t", k=8)), reads=[pa], writes=[ot])
                s.op("act", lambda h: h.copy(out=ot[:, 8:12, :], in_=pbb[:, 0:512].rearrange("p (k t) -> p k t", k=4)), reads=[pb], writes=[ot])
                for r in range(3):
                    for half in range(2):
                        pm = self.P[2 + nb % 4]
                        tp = tmp[nb % 2]
                        nb += 1
                        hc = slice(half * 512, (half + 1) * 512)
                        for kc in range(4):
                            self.mm(pm[:, :], ot[:, r * 4 + kc, :], wbr[:, r * 4 + kc, hc], kc == 0, kc == 3, [ot, wbr], [pm], sig=(kc == 3))
                        gsl = G[:, r * 1024 + half * 512:r * 1024 + (half + 1) * 512]
                        if r == 0:
                            s.op("dve", lambda h: h.tensor_tensor(out=mg[:, hc], in0=pm[:, :], in1=gsl, op=ALU.mult), reads=[pm, G], writes=[mg])
                        else:
                            s.op("dve", lambda h: h.tensor_tensor(out=tp[:], in0=pm[:, :], in1=gsl, op=ALU.mult), reads=[pm, G], writes=[tp])
                            s.op("pool", lambda h: h.tensor_tensor(out=mg[:, hc], in0=mg[:, hc], in1=tp[:], op=ALU.add), reads=[mg, tp], writes=[mg])
                s.op("act", lambda h: h.copy(out=mgb[:], in_=mg[:]), reads=[mg], writes=[mgb])
                for kc in range(8):
                    self.tr(pab[:, kc * 128:(kc + 1) * 128], mgb[:, kc * 128:(kc + 1) * 128], self.ident[:], [mgb, self.ident], [pa], sig=(kc == 7))
                s.op("act", lambda h: h.copy(out=mt[:], in_=pab.rearrange("p (k t) -> p k t", k=8)), reads=[pa], writes=[mt])
                for half in range(2):
                    pm = self.P[2 + nb % 4]
                    tp = tmp[nb % 2]
                    nb += 1
                    hc = slice(half * 512, (half + 1) * 512)
                    for kc in range(8):
                        self.mm(pm[:, :], mt[:, kc, :], wout[:, kc, hc], kc == 0, kc == 7, [mt, wout], [pm], sig=(kc == 7))
                    s.op("dve", lambda h: h.tensor_tensor(out=tp[:], in0=pm[:, :], in1=g1[:, hc], op=ALU.mult), reads=[pm, g1], writes=[tp])
                    s.op("pool", lambda h: h.tensor_tensor(out=x[:, hc], in0=x[:, hc], in1=tp[:], op=ALU.add), reads=[x, tp], writes=[x])
                s.dma("sp", Dr["xres"][grow, :], x[:], reads=[x], writes=[DR["xres"]])

    def phase_moe(self, l, q):
        s, I, Dr, DR = self.s, self.I, self.Dr, self.DR
        last = (l == L_DEPTH - 1)
        with ExitStack() as ps:
            g2 = s.sb([128, D], F32, "g2", ps)
            self.mod_bc(l, q, 5, g2)
            H2T = s.sb([128, 8, S], BF16, "H2T", ps)
            ACC = s.sb([128, NT, D], F32, "ACC", ps)
            GATE = s.sb([128, NT, NE], F32, "GATE", ps)
            rw = s.sb([128, 8, NE], F32, "rw", ps)
            rb = s.sb([128, NE], F32, "rb", ps)
            b2 = s.sb([NE, D], F32, "b2", ps)
            b1 = s.sb([128, NE, 16], F32, "b1", ps)
            s.dma("sp", rw[:], I["router_w"][l].rearrange("(kc p) e -> p kc e", p=128), writes=[rw])
            s.dma("sp", rb[:], I["router_b"][l:l + 1, :].broadcast_to([128, NE]), writes=[rb])
            s.dma("sp", b2[:], I["exp_b2"][l], writes=[b2])
            s.dma("sp", b1[:], I["exp_b1T"][l], writes=[b1])
            with ExitStack() as p1:
                gsc, sh = self.norm_tiles(p1, l, q, "norm2_g", 4, 3)
                xt = [s.sb([128, D], F32, "xt", p1) for _ in range(2)]
                hf = [s.sb([128, D], F32, "hf", p1) for _ in range(2)]
                hb = [s.sb([128, D], BF16, "hb", p1) for _ in range(2)]
                hT32 = s.sb([128, 8, 128], F32, "hT32", p1)
                junk = s.sb([128, D], F32, "junk", p1)
                ss = [s.sb([128, 1], F32, "ss", p1) for _ in range(2)]
                lg = [s.sb([128, NE], F32, "lg", p1) for _ in range(2)]
                ex = [s.sb([128, NE], F32, "ex", p1) for _ in range(2)]
                m8 = [s.sb([128, 8], F32, "m8", p1) for _ in range(2)]
                sm = [s.sb([128, 2], F32, "sm", p1) for _ in range(2)]
                gT = [s.sb([NE, 128], F32, "gT", p1) for _ in range(2)]
                for tt in range(NT):
                    grow = slice(q * S + tt * 128, q * S + (tt + 1) * 128)
                    tc = slice(tt * 128, (tt + 1) * 128)
                    x, h32, h16, sq = xt[tt % 2], hf[tt % 2], hb[tt % 2], ss[tt % 2]
                    s.dma("sp", x[:], Dr["xres"][grow, :], reads=[DR["xres"]], writes=[x])
                    self.rstd_of((x, x[:]), (junk, junk[:]), sq)
                    s.op("dve", lambda hh: hh.scalar_tensor_tensor(out=x[:], in0=x[:], scalar=sq[:, 0:1], in1=gsc[:], op0=ALU.mult, op1=ALU.mult), reads=[x, sq, gsc], writes=[x])
                    s.op("pool", lambda hh: hh.tensor_tensor(out=h32[:], in0=x[:], in1=sh[:], op=ALU.add), reads=[x, sh], writes=[h32])
                    s.op("act", lambda hh: hh.copy(out=h16[:], in_=h32[:]), reads=[h32], writes=[h16])
                    pt = self.P[0]
                    ptb = pt[:].bitcast(BF16)
                    for kc in range(8):
                        self.tr(ptb[:, kc * 128:(kc + 1) * 128], h16[:, kc * 128:(kc + 1) * 128], self.ident[:], [h16, self.ident], [pt], sig=(kc == 7))
                    s.op("act", lambda hh: hh.copy(out=H2T[:, :, tc], in_=ptb.rearrange("p (k t) -> p k t", k=8)), reads=[pt], writes=[H2T])
                    for hh2 in range(2):
                        pf = self.P[1 + hh2]
                        for k4 in range(4):
                            kc = hh2 * 4 + k4
                            self.tr(pf[:, k4 * 128:(k4 + 1) * 128], h32[:, kc * 128:(kc + 1) * 128], self.ident32[:], [h32, self.ident32], [pf], sig=(k4 == 3))
                        s.op("dve", lambda hh: hh.tensor_copy(out=hT32[:, hh2 * 4:(hh2 + 1) * 4, :], in_=pf[:, :].rearrange("p (k t) -> p k t", k=4)), reads=[pf], writes=[hT32])
                    pl = self.P[3]
                    for kc in range(8):
                        self.mm(pl[:, 0:NE], hT32[:, kc, :], rw[:, kc, :], kc == 0, kc == 7, [hT32, rw], [pl], sig=(kc == 7))
                    lgt, et, mt, st, gt_ = lg[tt % 2], ex[tt % 2], m8[tt % 2], sm[tt % 2], gT[tt % 2]
                    s.op("dve", lambda hh: hh.tensor_tensor(out=lgt[:], in0=pl[:, 0:NE], in1=rb[:], op=ALU.add), reads=[pl, rb], writes=[lgt])
                    s.op("dve", lambda hh: hh.max(out=mt[:], in_=lgt[:]), reads=[lgt], writes=[mt])
                    s.op("dve", lambda hh: hh.tensor_scalar_mul(out=st[:, 0:1], in0=mt[:, 0:1], scalar1=-1.0), reads=[mt], writes=[st])
                    s.op("act", lambda hh: hh.activation(out=et[:], in_=lgt[:], func=AF.Exp, bias=st[:, 0:1]), reads=[lgt, st], writes=[et])
                    s.op("dve", lambda hh: hh.tensor_scalar(out=lgt[:], in0=lgt[:], scalar1=mt[:, 3:4], scalar2=None, op0=ALU.is_ge), reads=[lgt, mt], writes=[lgt])
                    s.op("dve", lambda hh: hh.tensor_tensor(out=et[:], in0=et[:], in1=lgt[:], op=ALU.mult), reads=[et, lgt], writes=[et])
                    s.op("dve", lambda hh: hh.reduce_sum(out=st[:, 1:2], in_=et[:], axis=AX.X), reads=[et], writes=[st])
                    s.op("dve", lambda hh: hh.reciprocal(out=st[:, 1:2], in_=st[:, 1:2]), reads=[st], writes=[st])
                    s.op("dve", lambda hh: hh.tensor_scalar_mul(out=GATE[:, tt, :], in0=et[:], scalar1=st[:, 1:2]), reads=[et, st], writes=[GATE])
                    pg = self.P[4]
                    self.tr(pg[0:NE, 0:128], GATE[:, tt, :], self.ident32[:], [GATE, self.ident32], [pg])
                    s.op("act", lambda hh: hh.copy(out=gt_[:], in_=pg[0:NE, 0:128]), reads=[pg], writes=[gt_])
                    for half in range(2):
                        pa = self.P[5 + half]
                        self.mm(pa[:, :], gt_[:], b2[:, half * 512:(half + 1) * 512], True, True, [gt_, b2], [pa])
                        s.op("act", lambda hh: hh.copy(out=ACC[:, tt, half * 512:(half + 1) * 512], in_=pa[:, :]), reads=[pa], writes=[ACC])
                s.barrier()
            with ExitStack() as p2:
                W1 = [s.sb([128, 8, 1024], BF16, "W1", p2) for _ in range(2)]
                W2 = [s.sb([128, 4, 1024], BF16, "W2", p2) for _ in range(2)]
                Ab = [s.sb([128, 4, 512], BF16, "Ab", p2) for _ in range(2)]
                Gt = [s.sb([128, 512], F32, "Gt", p2) for _ in range(2)]
                St = [s.sb([128, 512], F32, "St", p2) for _ in range(2)]
                Ut = [s.sb([128, 512], F32, "Ut", p2) for _ in range(2)]
                k = 0
                na = 0
                nf = 0
                ny = 0
                for e in range(NE):
                    w1src = I["exp_w1"][l, e].rearrange("(kc p) n -> p kc n", p=128)
                    w2src = I["exp_w2"][l, e].rearrange("(fc p) n -> p fc n", p=128)
                    for hf_ in range(2):
                        w1, w2 = W1[k % 2], W2[k % 2]
                        k += 1
                        s.dma("pool", w1[:, :, 0:512], w1src[:, :, hf_ * 512:(hf_ + 1) * 512], writes=[w1])
                        s.dma("pool", w1[:, :, 512:1024], w1src[:, :, 1024 + hf_ * 512:1024 + (hf_ + 1) * 512], writes=[w1])
                        for hh in range(2):
                            s.dma("pool", w2[:, :, hh * 512:(hh + 1) * 512], w2src[:, hf_ * 4:(hf_ + 1) * 4, hh * 512:(hh + 1) * 512], writes=[w2])
                        for tg in range(4):
                            A = Ab[na % 2]
                            na += 1
                            tgc = slice(tg * 512, (tg + 1) * 512)
                            for fc in range(4):
                                psG, psU = self.P[(nf % 2) * 2], self.P[(nf % 2) * 2 + 1]
                                G, Sg, U = Gt[nf % 2], St[nf % 2], Ut[nf % 2]
                                nf += 1
                                for kc in range(8):
                                    self.mm(psG[:, :], w1[:, kc, fc * 128:(fc + 1) * 128], H2T[:, kc, tgc], kc == 0, kc == 7, [w1, H2T], [psG], sig=(kc == 7))
                                for kc in range(8):
                                    self.mm(psU[:, :], w1[:, kc, 512 + fc * 128:512 + (fc + 1) * 128], H2T[:, kc, tgc], kc == 0, kc == 7, [w1, H2T], [psU], sig=(kc == 7))
                                ig = hf_ * 4 + fc
                                s.op("dve", lambda hh: hh.tensor_scalar(out=G[:], in0=psG[:, :], scalar1=b1[:, e, ig:ig + 1], scalar2=7.0, op0=ALU.add, op1=ALU.min), reads=[psG, b1], writes=[G])
                                s.op("act", lambda hh: hh.activation(out=Sg[:], in_=G[:], func=AF.Sigmoid, scale=1.702), reads=[G], writes=[Sg])
                                s.op("dve", lambda hh: hh.tensor_scalar(out=U[:], in0=psU[:, :], scalar1=b1[:, e, 8 + ig:8 + ig + 1], scalar2=7.0, op0=ALU.add, op1=ALU.min), reads=[psU, b1], writes=[U])
                                s.op("pool", lambda hh: hh.tensor_scalar(out=U[:], in0=U[:], scalar1=-7.0, scalar2=1.0, op0=ALU.max, op1=ALU.add), reads=[U], writes=[U])
                                s.op("pool", lambda hh: hh.tensor_tensor(out=G[:], in0=G[:], in1=Sg[:], op=ALU.mult), reads=[G, Sg], writes=[G])
                                s.op("pool", lambda hh: hh.tensor_tensor(out=A[:, fc, :], in0=G[:], in1=U[:], op=ALU.mult), reads=[G, U], writes=[A])
                            for t4 in range(4):
                                tt = tg * 4 + t4
                                for dh in range(2):
                                    psY = self.P[4 + ny % 4]
                                    ny += 1
                                    dc = slice(dh * 512, (dh + 1) * 512)
                                    for fc in range(4):
                                        self.mm(psY[:, :], A[:, fc, t4 * 128:(t4 + 1) * 128], w2[:, fc, dc], fc == 0, fc == 3, [A, w2], [psY], sig=(fc == 3))
                                    s.op("dve", lambda hh: hh.scalar_tensor_tensor(out=ACC[:, tt, dc], in0=psY[:, :], scalar=GATE[:, tt, e:e + 1], in1=ACC[:, tt, dc], op0=ALU.mult, op1=ALU.add),
                                         reads=[psY, GATE, ACC], writes=[ACC])
                s.barrier()
            with ExitStack() as p3:
                xt = [s.sb([128, D], F32, "xt", p3) for _ in range(2)]
                tmp = [s.sb([128, D], F32, "tmp3", p3) for _ in range(2)]
                junk = s.sb([128, D], F32, "junk", p3)
                ss = [s.sb([128, 1], F32, "ss", p3) for _ in range(2)]
                if last:
                    fg = s.sb([128, D], F32, "fg", p3)
                    s.dma("sp", fg[:], I["final_g"].broadcast_to([128, D]), writes=[fg])
                for tt in range(NT):
                    grow = slice(q * S + tt * 128, q * S + (tt + 1) * 128)
                    x, tp, sq = xt[tt % 2], tmp[tt % 2], ss[tt % 2]
                    s.dma("sp", x[:], Dr["xres"][grow, :], reads=[DR["xres"]], writes=[x])
                    s.op("dve", lambda hh: hh.tensor_tensor(out=tp[:], in0=ACC[:, tt, :], in1=g2[:], op=ALU.mult), reads=[ACC, g2], writes=[tp])
                    s.op("pool", lambda hh: hh.tensor_tensor(out=x[:], in0=x[:], in1=tp[:], op=ALU.add), reads=[x, tp], writes=[x])
                    if last:
                        self.rstd_of((x, x[:]), (junk, junk[:]), sq)
                        s.op("dve", lambda hh: hh.scalar_tensor_tensor(out=x[:], in0=x[:], scalar=sq[:, 0:1], in1=fg[:], op0=ALU.mult, op1=ALU.mult), reads=[x, sq, fg], writes=[x])
                        s.dma("sp", self.out[grow, :], x[:], reads=[x], writes=[R("out")])
                    else:
                        s.dma("sp", Dr["xres"][grow, :], x[:], reads=[x], writes=[DR["xres"]])
                s.barrier()


    def phase_moe2(self, l):
        s, I, Dr, DR = self.s, self.I, self.Dr, self.DR
        last = (l == L_DEPTH - 1)
        NTT, NB = self.ntt, self.nblk
        with ExitStack() as ps:
            g2 = []
            for q in range(self.nseq):
                t = s.sb([128, D], F32, "g2", ps)
                self.mod_bc(l, q, 5, t)
                g2.append(t)
            GATE = s.sb([128, NTT, NE], F32, "GATE", ps)
            GT = s.sb([NE, NTT, 128], F32, "GT", ps)
            IDX4 = s.sb([128, NTT, 4], I32, "IDX4", ps)
            G4 = s.sb([128, NTT, 4], F32, "G4", ps)
            IDXW = s.sb([128, NB, 2], I32, "IDXW", ps)
            IDXB = s.sb([128, NB], I32, "IDXB", ps)
            b2 = s.sb([NE, D], F32, "b2", ps)
            s.dma("sp", b2[:], I["exp_b2"][l], writes=[b2])
            with ExitStack() as p1:
                HB = s.sb([128, NTT, D], BF16, "HB", p1)
                MASK = s.sb([128, NTT, NE], BF16, "MASK", p1)
                DEST = s.sb([128, NTT, NE], F32, "DEST", p1)
                rw = s.sb([128, 8, NE], F32, "rw", p1)
                rb = s.sb([128, NE], F32, "rb", p1)
                s.dma("sp", rw[:], I["router_w"][l].rearrange("(kc p) e -> p kc e", p=128), writes=[rw])
                s.dma("sp", rb[:], I["router_b"][l:l + 1, :].broadcast_to([128, NE]), writes=[rb])
                ltri = self.load_const(p1, "ltri", [128, 128], BF16)
                ones = self.load_const(p1, "ones128", [128, 128], BF16)
                b512 = self.load_const(p1, "b512", [128, 64], F32)
                rowiota = self.load_const(p1, "rowiota", [128, 8], F32)
                piota = self.load_const(p1, "piota", [128, 1], F32)
                xt = [s.sb([128, D], F32, "xt", p1) for _ in range(2)]
                hf = [s.sb([128, D], F32, "hf", p1) for _ in range(2)]
                hT32 = s.sb([128, 8, 128], F32, "hT32", p1)
                junk = s.sb([128, D], F32, "junk", p1)
                ss = [s.sb([128, 1], F32, "ss", p1) for _ in range(2)]
                lg = [s.sb([128, NE], F32, "lg", p1) for _ in range(2)]
                ex = [s.sb([128, NE], F32, "ex", p1) for _ in range(2)]
                m8 = [s.sb([128, 8], F32, "m8", p1) for _ in range(2)]
                sm = [s.sb([128, 2], F32, "sm", p1) for _ in range(2)]
                gsc = sh = None
                for tt in range(NTT):
                    q = tt // NT
                    if tt % NT == 0:
                        gsc, sh = self.norm_tiles(p1, l, q, "norm2_g", 4, 3)
                    grow = slice(tt * 128, (tt + 1) * 128)
                    x, h32, sq = xt[tt % 2], hf[tt % 2], ss[tt % 2]
                    s.dma("sp", x[:], Dr["xres"][grow, :], reads=[DR["xres"]], writes=[x])
                    self.rstd_of((x, x[:]), (junk, junk[:]), sq)
                    s.op("dve", lambda hh: hh.scalar_tensor_tensor(out=x[:], in0=x[:], scalar=sq[:, 0:1], in1=gsc[:], op0=ALU.mult, op1=ALU.mult), reads=[x, sq, gsc], writes=[x])
                    s.op("pool", lambda hh: hh.tensor_tensor(out=h32[:], in0=x[:], in1=sh[:], op=ALU.add), reads=[x, sh], writes=[h32])
                    s.op("act", lambda hh: hh.copy(out=HB[:, tt, :], in_=h32[:]), reads=[h32], writes=[HB])
                    for hh2 in range(2):
                        pf = self.P[1 + hh2]
                        for k4 in range(4):
                            kc = hh2 * 4 + k4
                            self.tr(pf[:, k4 * 128:(k4 + 1) * 128], h32[:, kc * 128:(kc + 1) * 128], self.ident32[:], [h32, self.ident32], [pf], sig=(k4 == 3))
                        s.op("dve", lambda hh: hh.tensor_copy(out=hT32[:, hh2 * 4:(hh2 + 1) * 4, :], in_=pf[:, :].rearrange("p (k t) -> p k t", k=4)), reads=[pf], writes=[hT32])
                    pl = self.P[3]
                    for kc in range(8):
                        self.mm(pl[:, 0:NE], hT32[:, kc, :], rw[:, kc, :], kc == 0, kc == 7, [hT32, rw], [pl], sig=(kc == 7))
                    lgt, et, mt, st = lg[tt % 2], ex[tt % 2], m8[tt % 2], sm[tt % 2]
                    s.op("dve", lambda hh: hh.tensor_tensor(out=lgt[:], in0=pl[:, 0:NE], in1=rb[:], op=ALU.add), reads=[pl, rb], writes=[lgt])
                    s.op("dve", lambda hh: hh.max(out=mt[:], in_=lgt[:]), reads=[lgt], writes=[mt])
                    s.op("dve", lambda hh: hh.tensor_scalar_mul(out=st[:, 0:1], in0=mt[:, 0:1], scalar1=-1.0), reads=[mt], writes=[st])
                    s.op("act", lambda hh: hh.activation(out=et[:], in_=lgt[:], func=AF.Exp, bias=st[:, 0:1]), reads=[lgt, st], writes=[et])
                    s.op("dve", lambda hh: hh.tensor_scalar(out=lgt[:], in0=lgt[:], scalar1=mt[:, 3:4], scalar2=None, op0=ALU.is_ge), reads=[lgt, mt], writes=[lgt])
                    s.op("dve", lambda hh: hh.tensor_copy(out=MASK[:, tt, :], in_=lgt[:]), reads=[lgt], writes=[MASK])
                    s.op("dve", lambda hh: hh.tensor_tensor(out=et[:], in0=et[:], in1=lgt[:], op=ALU.mult), reads=[et, lgt], writes=[et])
                    s.op("dve", lambda hh: hh.reduce_sum(out=st[:, 1:2], in_=et[:], axis=AX.X), reads=[et], writes=[st])
                    s.op("dve", lambda hh: hh.reciprocal(out=st[:, 1:2], in_=st[:, 1:2]), reads=[st], writes=[st])
                    s.op("dve", lambda hh: hh.tensor_scalar_mul(out=GATE[:, tt, :], in0=et[:], scalar1=st[:, 1:2]), reads=[et, st], writes=[GATE])
                    pg = self.P[4 + tt % 2]
                    self.tr(pg[0:NE, 0:128], GATE[:, tt, :], self.ident32[:], [GATE, self.ident32], [pg])
                    s.op("act", lambda hh: hh.copy(out=GT[:, tt, :], in_=pg[0:NE, 0:128]), reads=[pg], writes=[GT])
                run = s.sb([128, NE], F32, "run", p1)
                s.op("dve", lambda hh: hh.memset(run[:], 0.0), writes=[run])
                for tt in range(NTT):
                    pr = self.P[6 + tt % 2]
                    self.mm(pr[:, 0:NE], ltri[:], MASK[:, tt, :], True, True, [ltri, MASK], [pr], sig=False)
                    self.mm(pr[:, NE:2 * NE], ones[:], MASK[:, tt, :], False, True, [ones, MASK], [pr])
                    s.op("dve", lambda hh: hh.tensor_tensor(out=DEST[:, tt, :], in0=pr[:, 0:NE], in1=run[:], op=ALU.add), reads=[pr, run], writes=[DEST])
                    s.op("dve", lambda hh: hh.tensor_tensor(out=run[:], in0=run[:], in1=pr[:, NE:2 * NE], op=ALU.add), reads=[pr, run], writes=[run])
                padded = s.sb([128, NE], F32, "padded", p1)
                tmpe = s.sb([128, NE], F32, "tmpe", p1)
                cum = [s.sb([128, NE], F32, "cum", p1) for _ in range(2)]
                s.op("dve", lambda hh: hh.memset(padded[:], 0.0), writes=[padded])
                for jb in range(NTT // 4):
                    s.op("dve", lambda hh, jb=jb: hh.scalar_tensor_tensor(out=padded[:], in0=run[:], scalar=512.0 * jb, in1=padded[:], op0=ALU.is_gt, op1=ALU.add), reads=[run, padded], writes=[padded])
                s.op("dve", lambda hh: hh.tensor_scalar_mul(out=padded[:], in0=padded[:], scalar1=512.0), reads=[padded], writes=[padded])
                s.op("dve", lambda hh: hh.tensor_copy(out=cum[0][:], in_=padded[:]), reads=[padded], writes=[cum[0]])
                ci = 0
                for shf in (1, 2, 4, 8, 16):
                    a, b = cum[ci], cum[1 - ci]
                    s.op("dve", lambda hh: hh.tensor_copy(out=b[:, 0:shf], in_=a[:, 0:shf]), reads=[a], writes=[b])
                    s.op("dve", lambda hh: hh.tensor_tensor(out=b[:, shf:NE], in0=a[:, shf:NE], in1=a[:, 0:NE - shf], op=ALU.add), reads=[a], writes=[b])
                    ci = 1 - ci
                pend = cum[ci]
                pstart = s.sb([128, NE], F32, "pstart", p1)
                s.op("dve", lambda hh: hh.tensor_tensor(out=pstart[:], in0=pend[:], in1=padded[:], op=ALU.subtract), reads=[pend, padded], writes=[pstart])
                s.op("dve", lambda hh: hh.tensor_tensor(out=DEST[:], in0=DEST[:], in1=pstart[:].unsqueeze(1).broadcast_to([128, NTT, NE]), op=ALU.add), reads=[DEST, pstart], writes=[DEST])
                s.op("dve", lambda hh: hh.scalar_tensor_tensor(out=DEST[:], in0=DEST[:], scalar=1.0, in1=MASK[:], op0=ALU.add, op1=ALU.mult), reads=[DEST, MASK], writes=[DEST])
                d4 = s.sb([128, NTT, 4], F32, "d4", p1)
                for tt in range(NTT):
                    mt, et = m8[tt % 2], ex[tt % 2]
                    s.op("dve", lambda hh: hh.max(out=mt[:], in_=DEST[:, tt, :]), reads=[DEST], writes=[mt])
                    s.op("dve", lambda hh: hh.tensor_scalar_add(out=d4[:, tt, :], in0=mt[:, 0:4], scalar1=-1.0), reads=[mt], writes=[d4])
                    for k in range(4):
                        s.op("dve", lambda hh: hh.scalar_tensor_tensor(out=et[:], in0=DEST[:, tt, :], scalar=mt[:, k:k + 1], in1=GATE[:, tt, :], op0=ALU.is_equal, op1=ALU.mult), reads=[DEST, mt, GATE], writes=[et])
                        s.op("dve", lambda hh: hh.reduce_sum(out=G4[:, tt, k:k + 1], in_=et[:], axis=AX.X), reads=[et], writes=[G4])
                s.op("dve", lambda hh: hh.tensor_copy(out=IDX4[:], in_=d4[:]), reads=[d4], writes=[IDX4])
                bexp = s.sb([128, 64], F32, "bexp", p1)
                s.op("dve", lambda hh: hh.memset(bexp[:], 0.0), writes=[bexp])
                for e in range(NE):
                    s.op("dve", lambda hh: hh.scalar_tensor_tensor(out=bexp[:], in0=b512[:], scalar=pend[:, e:e + 1], in1=bexp[:], op0=ALU.is_ge, op1=ALU.add), reads=[b512, pend, bexp], writes=[bexp])
                boob = s.sb([128, 64], F32, "boob", p1)
                s.op("dve", lambda hh: hh.tensor_scalar(out=boob[:], in0=bexp[:], scalar1=float(NE) - 0.5, scalar2=1.0e6, op0=ALU.is_ge, op1=ALU.mult), reads=[bexp], writes=[boob])
                s.op("dve", lambda hh: hh.tensor_scalar_min(out=bexp[:], in0=bexp[:], scalar1=float(NE - 1)), reads=[bexp], writes=[bexp])
                iwf = s.sb([128, NB, 2], F32, "iwf", p1)
                ibf = s.sb([128, NB], F32, "ibf", p1)
                e1k = s.sb([128, 64], F32, "e1k", p1)
                s.op("dve", lambda hh: hh.tensor_scalar(out=e1k[:], in0=bexp[:], scalar1=256.0, scalar2=float(l * NE * 256), op0=ALU.mult, op1=ALU.add), reads=[bexp], writes=[e1k])
                s.op("dve", lambda hh: hh.tensor_tensor(out=e1k[:], in0=e1k[:], in1=boob[:], op=ALU.add), reads=[e1k, boob], writes=[e1k])
                s.op("dve", lambda hh: hh.tensor_tensor(out=iwf[:], in0=rowiota[:, 0:2].unsqueeze(1).broadcast_to([128, NB, 2]), in1=e1k[:, 0:NB].unsqueeze(2).broadcast_to([128, NB, 2]), op=ALU.add), reads=[rowiota, e1k], writes=[iwf])
                s.op("dve", lambda hh: hh.tensor_copy(out=IDXW[:], in_=iwf[:]), reads=[iwf], writes=[IDXW])
                s.op("dve", lambda hh: hh.tensor_scalar(out=ibf[:], in0=bexp[:, 0:NB], scalar1=128.0, scalar2=piota[:, 0:1], op0=ALU.mult, op1=ALU.add), reads=[bexp, piota], writes=[ibf])
                s.op("dve", lambda hh: hh.tensor_scalar_add(out=ibf[:], in0=ibf[:], scalar1=float(l * NE * 128)), reads=[ibf], writes=[ibf])
                s.op("dve", lambda hh: hh.tensor_tensor(out=ibf[:], in0=ibf[:], in1=boob[:, 0:NB], op=ALU.add), reads=[ibf, boob], writes=[ibf])
                s.op("dve", lambda hh: hh.tensor_copy(out=IDXB[:], in_=ibf[:]), reads=[ibf], writes=[IDXB])
                if "dbg_d" in self.dbg:
                    dbt = s.sb([128, 4096], F32, "dbt", p1)
                    s.op("dve", lambda hh: hh.memset(dbt[:], 0.0), writes=[dbt])
                    s.op("dve", lambda hh: hh.tensor_copy(out=dbt[:, 0:32], in_=run[:]), reads=[run], writes=[dbt])
                    s.op("dve", lambda hh: hh.tensor_copy(out=dbt[:, 32:64], in_=pend[:]), reads=[pend], writes=[dbt])
                    s.op("dve", lambda hh: hh.tensor_copy(out=dbt[:, 64:128], in_=bexp[:]), reads=[bexp], writes=[dbt])
                    s.op("dve", lambda hh: hh.tensor_copy(out=dbt[:, 128:128 + NTT * 4], in_=d4[:].rearrange("p t k -> p (t k)")), reads=[d4], writes=[dbt])
                    s.op("dve", lambda hh: hh.tensor_copy(out=dbt[:, 512:512 + NTT * 4], in_=G4[:].rearrange("p t k -> p (t k)")), reads=[G4], writes=[dbt])
                    s.dma("sp", Dr["dbg_d"], dbt[:], reads=[dbt], writes=[DR["dbg_d"]])
                for tt in range(NTT):
                    for k in range(4):
                        s.idma(Dr["xs_d"][:, :], HB[:, tt, :], out_off=IDX4[:, tt, k:k + 1], reads=[HB, IDX4], writes=[])
                s.barrier()
            with ExitStack() as p2:
                W1 = [s.sb([128, 8, 2 * D], BF16, "W1", p2) for _ in range(2)]
                W2 = [s.sb([128, 8, D], BF16, "W2", p2) for _ in range(2)]
                B1 = [s.sb([128, 16], F32, "B1", p2) for _ in range(2)]
                XS = [s.sb([128, 4, D], BF16, "XS", p2) for _ in range(2)]
                XT = [s.sb([128, 8, 512], BF16, "XT", p2) for _ in range(2)]
                Ab = [s.sb([128, 8, 512], BF16, "Ab", p2) for _ in range(2)]
                Gt = [s.sb([128, 512], F32, "Gt", p2) for _ in range(2)]
                St = [s.sb([128, 512], F32, "St", p2) for _ in range(2)]
                Ut = [s.sb([128, 512], F32, "Ut", p2) for _ in range(2)]
                Yt = [s.sb([128, D], F32, "Yt", p2) for _ in range(2)]
                for wt_ in W1 + W2 + B1:
                    s.op("pool", lambda hh, wt_=wt_: hh.memset(wt_[:], 0.0), writes=[wt_])
                cnt = {"nf": 0, "ny": 0}

                def load_block(b):
                    w1, w2, b1, xs = W1[b % 2], W2[b % 2], B1[b % 2], XS[b % 2]
                    s.dma("sp", xs[:], Dr["xs_d"][b * 512:(b + 1) * 512, :].rearrange("(t p) d -> p t d", p=128), reads=[DR["xs_d"]], writes=[xs])
                    for j2 in range(2):
                        s.idma(w1[:, j2 * 4:(j2 + 1) * 4, :].rearrange("p a b -> p (a b)"), I["exp_w1"][:, :], in_off=IDXW[:, b, j2:j2 + 1], reads=[IDXW], writes=[w1], bounds=65535)
                    s.idma(b1[:, :], I["exp_b1E"][:, :], in_off=IDXB[:, b:b + 1], reads=[IDXB], writes=[b1], bounds=65535)

                def load_w2(b):
                    w2 = W2[b % 2]
                    s.idma(w2[:].rearrange("p a b -> p (a b)"), I["exp_w2"][:, :], in_off=IDXB[:, b:b + 1], reads=[IDXB], writes=[w2], bounds=65535)

                def transposes(b):
                    xs, xT = XS[b % 2], XT[b % 2]
                    for t4 in range(4):
                        pt = self.P[t4 % 2]
                        ptb = pt[:].bitcast(BF16)
                        for kc in range(8):
                            kb = (kc // 4) * 512 + (kc % 4)
                            self.tr(ptb[:, kc * 128:(kc + 1) * 128], xs[:, t4, kb:kb + 509:4], self.ident[:], [xs, self.ident], [pt], sig=(kc == 7))
                        if t4 % 2 == 0:
                            s.op("act", lambda hh: hh.copy(out=xT[:, :, t4 * 128:(t4 + 1) * 128], in_=ptb.rearrange("p (k t) -> p k t", k=8)), reads=[pt], writes=[xT])
                        else:
                            s.op("dve", lambda hh: hh.tensor_copy(out=xT[:, :, t4 * 128:(t4 + 1) * 128], in_=ptb.rearrange("p (k t) -> p k t", k=8)), reads=[pt], writes=[xT])

                def stage1_fc(b, fc):
                    w1, b1, xT, A = W1[b % 2], B1[b % 2], XT[b % 2], Ab[b % 2]
                    nf = cnt["nf"]
                    cnt["nf"] += 1
                    psG, psU = self.P[2 + (nf % 2) * 2], self.P[3 + (nf % 2) * 2]
                    G, Sg, U = Gt[nf % 2], St[nf % 2], Ut[nf % 2]
                    for kc in range(8):
                        self.mm(psG[:, :], w1[:, kc, fc:D:8], xT[:, kc, :], kc == 0, kc == 7, [w1, xT], [psG], sig=(kc == 7))
                    for kc in range(8):
                        self.mm(psU[:, :], w1[:, kc, D + fc:2 * D:8], xT[:, kc, :], kc == 0, kc == 7, [w1, xT], [psU], sig=(kc == 7))
                    s.op("dve", lambda hh: hh.tensor_scalar(out=G[:], in0=psG[:, :], scalar1=b1[:, fc:fc + 1], scalar2=7.0, op0=ALU.add, op1=ALU.min), reads=[psG, b1], writes=[G])
                    s.op("act", lambda hh: hh.activation(out=Sg[:], in_=G[:], func=AF.Sigmoid, scale=1.702), reads=[G], writes=[Sg])
                    s.op("act", lambda hh: hh.activation(out=U[:], in_=psU[:, :], func=AF.Identity, bias=b1[:, 8 + fc:8 + fc + 1]), reads=[psU, b1], writes=[U])
                    s.op("dve", lambda hh: hh.tensor_scalar(out=U[:], in0=U[:], scalar1=7.0, scalar2=-7.0, op0=ALU.min, op1=ALU.max), reads=[U], writes=[U])
                    s.op("dve", lambda hh: hh.tensor_tensor(out=G[:], in0=G[:], in1=Sg[:], op=ALU.mult), reads=[G, Sg], writes=[G])
                    s.op("dve", lambda hh: hh.scalar_tensor_tensor(out=A[:, fc, :], in0=U[:], scalar=1.0, in1=G[:], op0=ALU.add, op1=ALU.mult), reads=[U, G], writes=[A])

                def stage2_grp(b, g):
                    w2, A = W2[b % 2], Ab[b % 2]
                    t4, dh = g // 2, g % 2
                    ny = cnt["ny"]
                    y = Yt[(ny // 2) % 2]
                    psY = self.P[6 + ny % 2]
                    cnt["ny"] += 1
                    dc = slice(dh * 512, (dh + 1) * 512)
                    for fc in range(8):
                        self.mm(psY[:, :], A[:, fc, t4 * 128:(t4 + 1) * 128], w2[:, fc, dc], fc == 0, fc == 7, [A, w2], [psY], sig=(fc == 7))
                    s.op("act", lambda hh: hh.copy(out=y[:, dc], in_=psY[:, :]), reads=[psY], writes=[y])
                    if dh == 1:
                        r0 = b * 512 + t4 * 128
                        s.dma("sp", Dr["ys_d"][r0:r0 + 128, :], y[:], reads=[y], writes=[DR["ys_d"]])

                load_block(0)
                load_w2(0)
                load_block(1)
                load_w2(1)
                for b in range(NB):
                    if 1 <= b < NB - 1:
                        load_block(b + 1)
                    transposes(b)
                    for i in range(8):
                        stage1_fc(b, i)
                        if b >= 1:
                            stage2_grp(b - 1, i)
                    if 1 <= b < NB - 1:
                        load_w2(b + 1)
                for i in range(8):
                    stage2_grp(NB - 1, i)
                s.barrier()
            with ExitStack() as p3:
                xt = [s.sb([128, D], F32, "xt", p3) for _ in range(2)]
                acc = [s.sb([128, D], F32, "acc3", p3) for _ in range(2)]
                Yk = [s.sb([128, D], F32, "Yk", p3) for _ in range(4)]
                junk = s.sb([128, D], F32, "junk", p3)
                ss = [s.sb([128, 1], F32, "ss", p3) for _ in range(2)]
                if last:
                    fg = s.sb([128, D], F32, "fg", p3)
                    s.dma("sp", fg[:], I["final_g"].broadcast_to([128, D]), writes=[fg])
                nk = 0
                for tt in range(NTT):
                    q = tt // NT
                    grow = slice(tt * 128, (tt + 1) * 128)
                    x, ac, sq = xt[tt % 2], acc[tt % 2], ss[tt % 2]
                    s.dma("sp", x[:], Dr["xres"][grow, :], reads=[DR["xres"]], writes=[x])
                    pa = [self.P[(tt % 2) * 2], self.P[(tt % 2) * 2 + 1]]
                    for half in range(2):
                        self.mm(pa[half][:, :], GT[:, tt, :], b2[:, half * 512:(half + 1) * 512], True, True, [GT, b2], [pa[half]])
                    for k in range(4):
                        yk = Yk[nk % 4]
                        nk += 1
                        s.idma(yk[:, :], Dr["ys_d"][:, :], in_off=IDX4[:, tt, k:k + 1], reads=[IDX4, DR["ys_d"]], writes=[yk])
                        for half in range(2):
                            hc = slice(half * 512, (half + 1) * 512)
                            in1 = pa[half][:, :] if k == 0 else ac[:, hc]
                            rd = [yk, G4] + ([pa[half]] if k == 0 else [ac])
                            s.op("dve", lambda hh, in1=in1, hc=hc, yk=yk, k=k: hh.scalar_tensor_tensor(out=ac[:, hc], in0=yk[:, hc], scalar=G4[:, tt, k:k + 1], in1=in1, op0=ALU.mult, op1=ALU.add), reads=rd, writes=[ac])
                    s.op("pool", lambda hh: hh.tensor_tensor(out=ac[:], in0=ac[:], in1=g2[q][:], op=ALU.mult), reads=[ac, g2[q]], writes=[ac])
                    s.op("dve", lambda hh: hh.tensor_tensor(out=x[:], in0=x[:], in1=ac[:], op=ALU.add), reads=[x, ac], writes=[x])
                    if last:
                        self.rstd_of((x, x[:]), (junk, junk[:]), sq)
                        s.op("dve", lambda hh: hh.scalar_tensor_tensor(out=x[:], in0=x[:], scalar=sq[:, 0:1], in1=fg[:], op0=ALU.mult, op1=ALU.mult), reads=[x, sq, fg], writes=[x])
                        s.dma("sp", self.out[grow, :], x[:], reads=[x], writes=[R("out")])
                    else:
                        s.dma("sp", Dr["xres"][grow, :], x[:], reads=[x], writes=[DR["xres"]])
                s.barrier()


_CONSTS = None


def run(inputs, dbg=(), layers=L_DEPTH, nseq=NSEQ, stop_after=None, cores=NCORES, trace=False, only=None):
    global _CONSTS
    if _CONSTS is None:
        _CONSTS = host_consts()
    inp = {k: np.asarray(v) for k, v in inputs.items()}
    k = K(dbg=dbg, layers=layers, nseq=nseq, stop_after=stop_after, only=only)
    nc = k.build()
    in_maps = []
    for c in range(cores):
        m = host_inputs(inp, c)
        m.update(_CONSTS)
        in_maps.append(m)
    res = run_bass_kernel_spmd(nc, in_maps, core_ids=list(range(cores)), trace=trace)
    return res


def kernel(**inputs):
    res = run(inputs)
    out = np.concatenate([np.asarray(r["out"]).reshape(NSEQ, S, D) for r in res.results], axis=0)
    return out.astype(np.float32)
```

```python
import numpy as np
import ml_dtypes
from contextlib import ExitStack
import concourse.bass as bass
import concourse.mybir as mybir
from concourse.bass_utils import run_bass_kernel_spmd

F32 = mybir.dt.float32
BF16 = mybir.dt.bfloat16
I32 = mybir.dt.int32
AF = mybir.ActivationFunctionType
ALU = mybir.AluOpType
AX = mybir.AxisListType
NDSEM = 8


class R:
    __slots__ = ("name", "lw", "rd")

    def __init__(self, name=""):
        self.name = name
        self.lw = {}
        self.rd = {}


class T(R):
    __slots__ = ("t",)

    def __init__(self, t, name=""):
        super().__init__(name)
        self.t = t

    def __getitem__(self, k):
        return self.t[k]


class Eng:
    def __init__(self, name, h, sem, dsems):
        self.name = name
        self.h = h
        self.sem = sem
        self.count = 0
        self.known = {}
        self.dsems = dsems
        self.ndma = 0


class Sched:
    def __init__(self, nc, stack):
        self.nc = nc
        self.stack = stack
        self.E = {}
        for name, h, nd in (("pe", nc.tensor, 0), ("act", nc.scalar, NDSEM), ("dve", nc.vector, 0),
                            ("pool", nc.gpsimd, NDSEM), ("sp", nc.sync, NDSEM)):
            sem = stack.enter_context(nc.semaphore("s_" + name))
            ds = [stack.enter_context(nc.semaphore("d_%s%d" % (name, i))) for i in range(nd)]
            self.E[name] = Eng(name, h, sem, ds)
        self.dma_tokens = []
        self.nuniq = 0

    def sb(self, shape, dt, name=None, stack=None):
        self.nuniq += 1
        name = (name or "t") + "_%d" % self.nuniq
        t = (stack or self.stack).enter_context(self.nc.sbuf_tensor(name, list(shape), dt))
        return T(t, name)

    def ps(self, shape, dt=F32, name=None, stack=None):
        self.nuniq += 1
        name = (name or "p") + "_%d" % self.nuniq
        t = (stack or self.stack).enter_context(self.nc.psum_tensor(name, list(shape), dt))
        return T(t, name)

    def _wait(self, eng, tok):
        sem, val = tok
        if eng.known.get(sem, 0) >= val:
            return
        eng.h.wait_ge(sem, val)
        eng.known[sem] = val

    def _deps(self, eng, reads, writes):
        deps = []
        for r in reads:
            deps.extend(r.lw.items())
        for w in writes:
            deps.extend(w.lw.items())
            deps.extend(w.rd.items())
        for tok in deps:
            if tok[0] is eng.sem:
                if eng.name == "pe":
                    continue
                if tok[1] > eng.count:
                    continue
            self._wait(eng, tok)

    def _commit(self, tok, reads, writes):
        sem, val = tok
        for r in reads:
            if r.rd.get(sem, 0) < val:
                r.rd[sem] = val
        for w in writes:
            if w.lw.get(sem, 0) < val:
                w.lw[sem] = val

    def op(self, engname, fn, reads=(), writes=(), sig=True):
        eng = self.E[engname]
        self._deps(eng, reads, writes)
        ins = fn(eng.h)
        if sig:
            eng.count += 1
            ins.then_inc(eng.sem, 1)
            tok = (eng.sem, eng.count)
        else:
            tok = (eng.sem, eng.count + 1)
        self._commit(tok, reads, writes)
        return tok

    def dma(self, qname, out, in_, reads=(), writes=(), **kw):
        q = self.E[qname]
        i = q.ndma
        q.ndma += 1
        sem = q.dsems[i % NDSEM]
        val = 16 * (i // NDSEM + 1)
        if i >= NDSEM:
            self._wait(q, (sem, val - 16))
        self._deps(q, reads, writes)
        q.h.dma_start(out=out, in_=in_, **kw).then_inc(sem, 16)
        tok = (sem, val)
        self._commit(tok, reads, writes)
        self.dma_tokens.append(tok)
        return tok

    def idma(self, out, in_, out_off=None, in_off=None, reads=(), writes=(), bounds=None):
        q = self.E["pool"]
        i = q.ndma
        q.ndma += 1
        sem = q.dsems[i % NDSEM]
        val = 16 * (i // NDSEM + 1)
        if i >= NDSEM:
            self._wait(q, (sem, val - 16))
        self._deps(q, reads, writes)
        oo = bass.IndirectOffsetOnAxis(ap=out_off, axis=0) if out_off is not None else None
        io = bass.IndirectOffsetOnAxis(ap=in_off, axis=0) if in_off is not None else None
        if bounds is None:
            q.h.indirect_dma_start(out=out, out_offset=oo, in_=in_, in_offset=io).then_inc(sem, 16)
        else:
            if getattr(self, "bound_reg", None) is None:
                self.bound_reg = q.h.alloc_register("bnd")
                q.h.reg_mov(self.bound_reg, 65535)
            q.h.indirect_dma_start(out=out, out_offset=oo, in_=in_, in_offset=io, bounds_check=self.bound_reg, oob_is_err=False).then_inc(sem, 16)
        tok = (sem, val)
        self._commit(tok, reads, writes)
        self.dma_tokens.append(tok)
        return tok

    def barrier(self):
        toks = []
        for e in self.E.values():
            if e.count > 0:
                toks.append((e.sem, e.count))
            for j, s in enumerate(e.dsems):
                n = (e.ndma - j + NDSEM - 1) // NDSEM
                if n > 0:
                    toks.append((s, 16 * n))
        for e in self.E.values():
            for tok in toks:
                if tok[0] is e.sem:
                    continue
                self._wait(e, tok)

    def finish(self):
        sp = self.E["sp"]
        for e in self.E.values():
            for j, s in enumerate(e.dsems):
                n = (e.ndma - j + NDSEM - 1) // NDSEM
                if n > 0:
                    self._wait(sp, (s, 16 * n))


NCORES = 8
L_DEPTH = 2
D = 1024
S = 2048
NSEQ = 2
NT = S // 128
EPS = 1e-5
INW = 6680
SCALE = 0.125
NEG = -30000.0
NE = 32
SLOT_COL = ([0 + 64 * i for i in range(8)] + [512 + 64 * i for i in range(2)] + [768 + 64 * i for i in range(8)]
            + [1280 + 64 * i for i in range(8)] + [2304 + 64 * i for i in range(8)] + [2816 + 64 * i for i in range(2)]
            + [2944 + 64 * i for i in range(2)] + [3072 + 64 * i for i in range(2)] + [3328 + 64 * i for i in range(2)])
NSLOT = len(SLOT_COL)
SL_QA, SL_KA, SL_QB, SL_KB, SL_QC, SL_KCM, SL_VCM, SL_KSL, SL_KWN = 0, 8, 10, 18, 26, 34, 36, 38, 40
C_VA, C_VB, C_VSL, C_VWN, C_GN, C_GM = 640, 1792, 3200, 3456, 3584, 3608


def host_consts():
    bf = ml_dtypes.bfloat16
    c = {}
    t = np.arange(S)
    a_t, b_t = (t // 128).astype(np.float32), (t % 128).astype(np.float32)
    aug = np.zeros((NSLOT, 4, S), np.float32)
    kaug = np.stack([a_t, b_t, np.ones(S, np.float32), np.ones(S, np.float32)])

    def qaug(slope):
        return np.stack([np.full(S, 1024.0 * slope, np.float32), np.full(S, 8.0 * slope, np.float32),
                         -1024.0 * slope * a_t, -8.0 * slope * b_t])
    for i in range(8):
        aug[SL_QA + i] = qaug(2.0 ** -(i + 1))
        aug[SL_QC + i] = qaug(2.0 ** -(i + 1))
        aug[SL_QB + i] = qaug(2.0 ** (-2.0 * (i // 2 + 1)))
        aug[SL_KB + i] = kaug
    for i in range(2):
        aug[SL_KA + i] = kaug
        aug[SL_KSL + i] = kaug
        aug[SL_KWN + i] = kaug
    c["aug"] = aug.astype(bf)
    c["ident"] = np.eye(128, dtype=np.float32).astype(bf)
    c["ident32"] = np.eye(128, dtype=np.float32)
    sk = np.arange(128)[:, None]
    tq = np.arange(128)[None, :]
    c["mdiag"] = np.where(tq >= sk, 0.0, NEG).astype(bf)
    c["medge"] = np.where(tq < sk, 0.0, NEG).astype(bf)
    cc = np.arange(128)[:, None]
    c["cmaskT"] = np.where((16 * cc + 31 <= t[None, :]) & (cc < 127), 0.0, NEG).astype(bf)
    nb = np.arange(32)
    c["eblk"] = (t[None, :] // 64 == nb[:, None]).astype(np.float32).astype(bf)
    cur = t // 64
    forced = (nb[None, :] == 0) | (nb[None, :] == cur[:, None]) | (nb[None, :] == cur[:, None] - 1)
    causal = nb[None, :] <= cur[:, None]
    A = np.where(forced, 1e9 + 1e6 * nb[None, :], np.where(causal, 0.0, -1e9 - 1e6 * nb[None, :]))
    c["atab"] = A.astype(np.float32)
    cstart = np.arange(127) * 16
    sstart = nb * 64
    ov = np.clip(np.minimum(cstart[:, None] + 32, sstart[None, :] + 64) - np.maximum(cstart[:, None], sstart[None, :]), 0, None) / 32.0
    ovp = np.zeros((128, 32), np.float32)
    ovp[:127] = ov
    c["overlap"] = ovp.astype(bf)
    c["ltri"] = (np.arange(128)[:, None] < np.arange(128)[None, :]).astype(np.float32).astype(bf)
    c["ones128"] = np.ones((128, 128), np.float32).astype(bf)
    c["b512"] = np.broadcast_to((512.0 * np.arange(64, dtype=np.float32))[None, :], (128, 64)).copy()
    c["rowiota"] = (np.arange(8, dtype=np.float32)[None, :] * 128 + np.arange(128, dtype=np.float32)[:, None]).copy()
    c["piota"] = np.arange(128, dtype=np.float32).reshape(128, 1).copy()
    return c


def host_inputs(inp, core):
    b0 = core * NSEQ
    m = {}
    m["x"] = np.ascontiguousarray(inp["x"][b0:b0 + NSEQ].reshape(NSEQ * S, D))
    m["cT"] = np.ascontiguousarray(inp["c"][b0:b0 + NSEQ].T)
    for k in ("mod_w", "mod_b", "norm1_g", "norm2_g", "w_in", "b_in", "sinks", "diff_subln_g", "cmp_w1", "cmp_w2",
              "cmp_b2", "w_branch", "w_out", "router_w", "router_b", "exp_b2"):
        m[k] = inp[k]
    m["final_g"] = inp["final_g"].reshape(1, D)
    m["b_inT"] = np.ascontiguousarray(np.stack([inp["b_in"][:, SLOT_COL[2 * i]:SLOT_COL[2 * i] + 128] for i in range(NSLOT // 2)], axis=2))
    m["diff_lambda"] = inp["diff_lambda"].reshape(L_DEPTH, 256)
    m["cmp_posT"] = np.ascontiguousarray(inp["cmp_pos"].transpose(0, 1, 3, 2))
    m["cmp_b1T"] = np.ascontiguousarray(inp["cmp_b1"].reshape(L_DEPTH, 2, 2, 128).transpose(0, 1, 3, 2))
    m["cmp_b2T"] = np.ascontiguousarray(inp["cmp_b2"].reshape(L_DEPTH, 2, 64, 1))
    m["exp_b1E"] = np.ascontiguousarray(inp["exp_b1"].reshape(L_DEPTH, NE, 2, 128, 8).transpose(0, 1, 3, 2, 4)).reshape(L_DEPTH * NE * 128, 16)
    m["exp_w1"] = inp["exp_w1"].reshape(L_DEPTH * NE * 256, 4 * 2 * D)
    m["exp_w2"] = inp["exp_w2"].reshape(L_DEPTH * NE * 128, 8 * D)
    return m


IN_SHAPES = {
    "x": ([NSEQ * S, D], F32), "cT": ([D, NSEQ], F32), "mod_w": ([L_DEPTH, D, 6 * D], F32), "mod_b": ([L_DEPTH, 6 * D], F32),
    "norm1_g": ([L_DEPTH, D], F32), "norm2_g": ([L_DEPTH, D], F32), "w_in": ([L_DEPTH, D, INW], F32), "b_in": ([L_DEPTH, INW], F32),
    "sinks": ([L_DEPTH, 8], F32), "diff_subln_g": ([L_DEPTH, 128], F32), "cmp_w1": ([L_DEPTH, 2, 2048, 256], F32),
    "cmp_w2": ([L_DEPTH, 2, 256, 64], F32), "cmp_b2": ([L_DEPTH, 2, 64], F32), "w_branch": ([L_DEPTH, 3, 512, D], F32),
    "w_out": ([L_DEPTH, D, D], F32), "router_w": ([L_DEPTH, D, NE], F32), "router_b": ([L_DEPTH, NE], F32),
    "exp_w1": ([L_DEPTH * NE * 256, 8 * D], F32), "exp_w2": ([L_DEPTH * NE * 128, 8 * D], F32), "exp_b2": ([L_DEPTH, NE, D], F32),
    "final_g": ([1, D], F32), "b_inT": ([L_DEPTH, 128, NSLOT // 2], F32), "diff_lambda": ([L_DEPTH, 256], F32),
    "cmp_posT": ([L_DEPTH, 2, 64, 32], F32), "cmp_b1T": ([L_DEPTH, 2, 128, 2], F32), "cmp_b2T": ([L_DEPTH, 2, 64, 1], F32),
    "exp_b1E": ([L_DEPTH * NE * 128, 16], F32),
    "ltri": ([128, 128], BF16), "ones128": ([128, 128], BF16), "b512": ([128, 64], F32), "rowiota": ([128, 8], F32), "piota": ([128, 1], F32),
    "aug": ([NSLOT, 4, S], BF16), "ident": ([128, 128], BF16), "ident32": ([128, 128], F32), "mdiag": ([128, 128], BF16),
    "medge": ([128, 128], BF16), "cmaskT": ([128, S], BF16), "eblk": ([32, S], BF16), "atab": ([S, 32], F32),
    "overlap": ([128, 32], BF16),
}


def bcast_rows(ap1d_row, nparts):
    return ap1d_row.broadcast_to([nparts, ap1d_row.shape[-1]])


class K:
    def __init__(self, dbg=(), layers=L_DEPTH, nseq=NSEQ, stop_after=None, only=None):
        self.dbg = set(dbg)
        self.only = only
        self.layers = layers
        self.nseq = nseq
        self.stop_after = stop_after
        nc = self.nc = bass.Bass("TRN2", target_bir_lowering=False)
        self.I = {k: nc.dram_tensor(k, list(sh), dt, kind="ExternalInput").ap() for k, (sh, dt) in IN_SHAPES.items()}
        self.out = nc.dram_tensor("out", [NSEQ * S, D], F32, kind="ExternalOutput").ap()
        self.Dr = {}
        self.DR = {}

    def dram(self, name, shape, dt):
        kind = "ExternalOutput" if name in self.dbg else "Internal"
        self.Dr[name] = self.nc.dram_tensor(name, list(shape), dt, kind=kind).ap()
        self.DR[name] = R(name)
        return self.Dr[name]

    def mm(self, out, lhsT, rhs, start, stop, reads, writes, sig=True):
        return self.s.op("pe", lambda h: h.matmul(out, lhsT=lhsT, rhs=rhs, start=start, stop=stop), reads=reads, writes=writes, sig=sig)

    def tr(self, out, in_, ident, reads, writes, sig=True):
        return self.s.op("pe", lambda h: h.transpose(out=out, in_=in_, identity=ident), reads=reads, writes=writes, sig=sig)

    def rstd_of(self, xt, junk, ss, n=D):
        s = self.s
        s.op("act", lambda h: h.activation(out=junk[1], in_=xt[1], func=AF.Square, accum_out=ss[:]), reads=[xt[0]], writes=[junk[0], ss])
        s.op("dve", lambda h: h.tensor_scalar(out=ss[:], in0=ss[:], scalar1=1.0 / n, scalar2=EPS, op0=ALU.mult, op1=ALU.add), reads=[ss], writes=[ss])
        s.op("act", lambda h: h.sqrt(out=ss[:], in_=ss[:]), reads=[ss], writes=[ss])
        s.op("dve", lambda h: h.reciprocal(out=ss[:], in_=ss[:]), reads=[ss], writes=[ss])

    def build(self):
        nc = self.nc
        with ExitStack() as st:
            s = self.s = Sched(nc, st)
            self.P = [s.ps([128, 512], F32, "bank%d" % i) for i in range(8)]
            self.ident = s.sb([128, 128], BF16, "ident")
            self.ident32 = s.sb([128, 128], F32, "ident32")
            s.dma("sp", self.ident[:], self.I["ident"], writes=[self.ident])
            s.dma("sp", self.ident32[:], self.I["ident32"], writes=[self.ident32])
            self.dram("mod_d", [L_DEPTH, NSEQ, 6 * D], F32)
            self.dram("xres", [NSEQ * S, D], F32)
            self.dram("QT_d", [NSLOT, 64, S], BF16)
            self.dram("VA_d", [S, 2, 65], BF16)
            self.dram("VB_d", [S, 4, 129], BF16)
            self.dram("VSL_d", [S, 2, 65], BF16)
            self.dram("VWN_d", [S, 2, 65], BF16)
            self.dram("GN_d", [S, 24], F32)
            self.dram("GM_d", [S, 3072], F32)
            self.dram("KC_d", [2, 64, 128], BF16)
            self.dram("VC_d", [2, 128, 97], BF16)
            self.dram("O_d", [S, 1536], BF16)
            self.ntt = self.nseq * NT
            self.nblk = self.ntt + NE
            self.dram("xs_d", [self.nblk * 512, D], BF16)
            self.dram("ys_d", [self.nblk * 512, D], F32)
            self.dram("dbg_d", [128, 4096], F32)
            with ExitStack() as zs:
                zt = s.sb([128, 8192], BF16, "zt", zs)
                s.op("pool", lambda h: h.memset(zt[:], 0.0), writes=[zt])
                xv = self.Dr["xs_d"].rearrange("(a p r) d -> a p (r d)", p=128, r=8)
                for a in range(xv.shape[0]):
                    s.dma("sp", xv[a], zt[:], reads=[zt], writes=[self.DR["xs_d"]])
                s.barrier()
            try:
                self.body()
            except StopIteration:
                pass
            s.barrier()
            s.finish()
        return nc

    def phase_end(self, name):
        self.s.barrier()
        if self.stop_after == name:
            raise StopIteration

    def body(self):
        for l in range(self.layers):
            self.runp("mod%d" % l, self.phase_mod, l)
            for q in range(self.nseq):
                for nm, fn in (("inproj", self.phase_inproj), ("cmp", self.phase_cmp), ("swa", self.phase_swa), ("diff", self.phase_diff),
                               ("nsa", self.phase_nsa), ("merge", self.phase_merge)):
                    self.runp("%s%d_%d" % (nm, l, q), fn, l, q)
            self.runp("moe%d" % l, self.phase_moe2, l)

    def runp(self, name, fn, *args):
        if self.only is None or name in self.only:
            fn(*args)
        self.phase_end(name)

    def phase_mod(self, l):
        s, I = self.s, self.I
        with ExitStack() as ps:
            cT = s.sb([128, 8, NSEQ], F32, "cT", ps)
            cs = s.sb([128, 8, NSEQ], F32, "cs", ps)
            modb = s.sb([NSEQ, 6 * D], F32, "modb", ps)
            mods = s.sb([NSEQ, 6 * D], F32, "mods", ps)
            wb = [s.sb([128, 8, 512], F32, "modw", ps) for _ in range(2)]
            s.dma("sp", cT[:], I["cT"].rearrange("(kc p) b -> p kc b", p=128), writes=[cT])
            s.dma("sp", modb[:], I["mod_b"][l:l + 1, :].broadcast_to([NSEQ, 6 * D]), writes=[modb])
            s.op("act", lambda h: h.activation(out=cs[:], in_=cT[:], func=AF.Silu), reads=[cT], writes=[cs])
            wsrc = I["mod_w"][l].rearrange("(kc p) n -> p kc n", p=128)
            for cg in range(12):
                w = wb[cg % 2]
                s.dma("sp", w[:], wsrc[:, :, cg * 512:(cg + 1) * 512], writes=[w])
                pm = self.P[cg % 2]
                for kc in range(8):
                    self.mm(pm[0:NSEQ, :], cs[:, kc, :], w[:, kc, :], kc == 0, kc == 7, [cs, w], [pm], sig=(kc == 7))
                s.op("dve", lambda h: h.tensor_tensor(out=mods[:, cg * 512:(cg + 1) * 512], in0=pm[0:NSEQ, :], in1=modb[:, cg * 512:(cg + 1) * 512], op=ALU.add),
                     reads=[pm, modb], writes=[mods])
            for seg in (1, 4):
                s.op("dve", lambda h: h.tensor_scalar_add(out=mods[:, seg * D:(seg + 1) * D], in0=mods[:, seg * D:(seg + 1) * D], scalar1=1.0), reads=[mods], writes=[mods])
            s.dma("sp", self.Dr["mod_d"][l], mods[:], reads=[mods], writes=[self.DR["mod_d"]])

    def mod_bc(self, l, q, seg, tile):
        src = self.Dr["mod_d"][l, q:q + 1, seg * D:(seg + 1) * D].broadcast_to([128, D])
        self.s.dma("sp", tile[:], src, reads=[self.DR["mod_d"]], writes=[tile])

    def xsrc(self, l, q):
        return (self.I["x"] if l == 0 else self.Dr["xres"]), ([] if l == 0 else [self.DR["xres"]])

    def norm_tiles(self, ps, l, q, gname, seg_sc, seg_sh):
        s, I = self.s, self.I
        gsc = s.sb([128, D], F32, "gsc", ps)
        sh = s.sb([128, D], F32, "sh", ps)
        gt = s.sb([128, D], F32, "gt", ps)
        s.dma("sp", gt[:], I[gname][l:l + 1, :].broadcast_to([128, D]), writes=[gt])
        self.mod_bc(l, q, seg_sc, gsc)
        self.mod_bc(l, q, seg_sh, sh)
        s.op("dve", lambda h: h.tensor_tensor(out=gsc[:], in0=gsc[:], in1=gt[:], op=ALU.mult), reads=[gsc, gt], writes=[gsc])
        return gsc, sh

    def phase_inproj(self, l, q):
        s, I, Dr, DR = self.s, self.I, self.Dr, self.DR
        xsrc, xres_r = self.xsrc(l, q)
        with ExitStack() as ps:
            gsc, sh = self.norm_tiles(ps, l, q, "norm1_g", 1, 0)
            hT = s.sb([128, 8, S], BF16, "hT", ps)
            xt = [s.sb([128, D], F32, "xt", ps) for _ in range(2)]
            junk = s.sb([128, D], F32, "junk", ps)
            hb = [s.sb([128, D], BF16, "hb", ps) for _ in range(2)]
            ss = [s.sb([128, 1], F32, "ss", ps) for _ in range(2)]
            for tt in range(NT):
                x, h, sq = xt[tt % 2], hb[tt % 2], ss[tt % 2]
                r0 = q * S + tt * 128
                s.dma("sp", x[:], xsrc[r0:r0 + 128, :], reads=xres_r, writes=[x])
                self.rstd_of((x, x[:]), (junk, junk[:]), sq)
                s.op("dve", lambda hh: hh.scalar_tensor_tensor(out=x[:], in0=x[:], scalar=sq[:, 0:1], in1=gsc[:], op0=ALU.mult, op1=ALU.mult), reads=[x, sq, gsc], writes=[x])
                s.op("pool", lambda hh: hh.tensor_tensor(out=h[:], in0=x[:], in1=sh[:], op=ALU.add), reads=[x, sh], writes=[h])
                pt = self.P[tt % 2]
                ptb = pt[:].bitcast(BF16)
                for kc in range(8):
                    self.tr(ptb[:, kc * 128:(kc + 1) * 128], h[:, kc * 128:(kc + 1) * 128], self.ident[:], [h, self.ident], [pt], sig=(kc == 7))
                s.op("act", lambda hh: hh.copy(out=hT[:, :, tt * 128:(tt + 1) * 128], in_=ptb.rearrange("p (k t) -> p k t", k=8)), reads=[pt], writes=[hT])
            binT = s.sb([128, NSLOT // 2], F32, "binT", ps)
            s.dma("sp", binT[:], I["b_inT"][l], writes=[binT])
            wsrc = I["w_in"][l].rearrange("(kc p) n -> p kc n", p=128)
            wbuf = [s.sb([128, 8, 512], BF16, "wblk", ps) for _ in range(2)]
            stg = [s.sb([128, S], BF16, "stg", ps) for _ in range(2)]
            groups = [(SL_QA, 8), (SL_KA, 2), (SL_QB, 8), (SL_KB, 8), (SL_QC, 8), (SL_KCM, 4), (SL_KSL, 2), (SL_KWN, 2)]
            nw = 0
            nmm = 0
            for (s0, ns) in groups:
                w = wbuf[nw % 2]
                nw += 1
                c0 = SLOT_COL[s0]
                s.dma("pool", w[:, :, 0:ns * 64], wsrc[:, :, c0:c0 + ns * 64], writes=[w])
                for si in range(0, ns, 2):
                    slot = s0 + si
                    pr_ = slot // 2
                    sg = stg[pr_ % 2]
                    for tg in range(4):
                        pm = self.P[2 + nmm % 4]
                        nmm += 1
                        for kc in range(8):
                            self.mm(pm[:, :], w[:, kc, si * 64:(si + 2) * 64], hT[:, kc, tg * 512:(tg + 1) * 512], kc == 0, kc == 7, [w, hT], [pm], sig=(kc == 7))
                        s.op("act", lambda hh: hh.activation(out=sg[:, tg * 512:(tg + 1) * 512], in_=pm[:, :], func=AF.Identity, bias=binT[:, pr_:pr_ + 1]),
                             reads=[pm, binT], writes=[sg])
                    s.dma("sp", Dr["QT_d"][slot], sg[0:64, :], reads=[sg], writes=[DR["QT_d"]])
                    s.dma("sp", Dr["QT_d"][slot + 1], sg[64:128, :], reads=[sg], writes=[DR["QT_d"]])
            binb = s.sb([128, INW], F32, "binb", ps)
            s.dma("sp", binb[:], I["b_in"][l:l + 1, :].broadcast_to([128, INW]), writes=[binb])
            vt = {}
            for nm, nh, dv in (("VA_d", 2, 64), ("VB_d", 4, 128), ("VSL_d", 2, 64), ("VWN_d", 2, 64)):
                vt[nm] = [s.sb([128, nh, dv + 1], BF16, "vt", ps) for _ in range(2)]
                for v in vt[nm]:
                    s.op("pool", lambda hh: hh.memset(v[:, :, dv:dv + 1], 1.0), writes=[v])
            gnt = [s.sb([128, 24], F32, "gnt", ps) for _ in range(2)]
            gmt = [s.sb([128, 512], F32, "gmt", ps) for _ in range(2)]
            blocks = [("VA_d", C_VA, 128, 2, 64), ("VB_d", C_VB, 512, 4, 128), ("VSL_d", C_VSL, 128, 2, 64), ("VWN_d", C_VWN, 128, 2, 64),
                      ("GN_d", C_GN, 24, 0, 0)] + [("GM_d", C_GM + 512 * i, 512, i, 0) for i in range(6)]
            for (nm, c0, ncol, nh, dv) in blocks:
                w = wbuf[nw % 2]
                nw += 1
                s.dma("pool", w[:, :, 0:ncol], wsrc[:, :, c0:c0 + ncol], writes=[w])
                for tt in range(NT):
                    pm = self.P[2 + nmm % 4]
                    nmm += 1
                    for kc in range(8):
                        self.mm(pm[:, 0:ncol], hT[:, kc, tt * 128:(tt + 1) * 128], w[:, kc, 0:ncol], kc == 0, kc == 7, [w, hT], [pm], sig=(kc == 7))
                    rows = slice(tt * 128, (tt + 1) * 128)
                    if nm == "GN_d":
                        g = gnt[tt % 2]
                        s.op("dve", lambda hh: hh.tensor_tensor(out=g[:], in0=pm[:, 0:24], in1=binb[:, c0:c0 + 24], op=ALU.add), reads=[pm, binb], writes=[g])
                        s.op("act", lambda hh: hh.activation(out=g[:], in_=g[:], func=AF.Sigmoid), reads=[g], writes=[g])
                        s.dma("sp", Dr["GN_d"][rows, :], g[:], reads=[g], writes=[DR["GN_d"]])
                    elif nm == "GM_d":
                        g = gmt[tt % 2]
                        s.op("dve", lambda hh: hh.tensor_tensor(out=g[:], in0=pm[:, :], in1=binb[:, c0:c0 + 512], op=ALU.add), reads=[pm, binb], writes=[g])
                        s.op("act", lambda hh: hh.activation(out=g[:], in_=g[:], func=AF.Sigmoid), reads=[g], writes=[g])
                        s.dma("sp", Dr["GM_d"][rows, nh * 512:(nh + 1) * 512], g[:], reads=[g], writes=[DR["GM_d"]])
                    else:
                        v = vt[nm][tt % 2]
                        s.op("dve", lambda hh: hh.tensor_tensor(out=v[:, :, 0:dv], in0=pm[:, 0:ncol].rearrange("p (h d) -> p h d", d=dv),
                                                               in1=binb[:, c0:c0 + ncol].rearrange("p (h d) -> p h d", d=dv), op=ALU.add), reads=[pm, binb], writes=[v])
                        s.dma("sp", Dr[nm][rows], v[:], reads=[v], writes=[DR[nm]])

    def phase_cmp(self, l, q):
        s, I, Dr, DR = self.s, self.I, self.Dr, self.DR
        with ExitStack() as ps:
            ovl = self.load_const(ps, "overlap", [128, 32], BF16)
            w1 = s.sb([64, 32, 256], BF16, "cw1", ps)
            w2 = s.sb([128, 2, 64], BF16, "cw2", ps)
            posT = s.sb([64, 32], F32, "posT", ps)
            posb = s.sb([64, 32], BF16, "posb", ps)
            b1T = s.sb([128, 2], F32, "b1T", ps)
            bias = s.sb([128, 2], F32, "cbias", ps)
            b2T = s.sb([64, 1], F32, "b2T", ps)
            b2b = s.sb([128, 64], F32, "b2b", ps)
            xT = s.sb([64, S], BF16, "cxT", ps)
            hid = s.sb([128, 2, 128], BF16, "hid", ps)
            y = s.sb([128, 128], F32, "cy", ps)
            u = s.sb([128, 128], F32, "cu", ps)
            kct = s.sb([64, 128], BF16, "kct", ps)
            vct = s.sb([128, 97], BF16, "vct", ps)
            s.op("pool", lambda h: h.memset(kct[:], 0.0), writes=[kct])
            s.op("pool", lambda h: h.memset(vct[:], 0.0), writes=[vct])
            for which in range(2):
                s.dma("pool", w1[:], I["cmp_w1"][l, which].rearrange("(l d) f -> d l f", d=64), writes=[w1])
                s.dma("pool", w2[:], I["cmp_w2"][l, which].rearrange("(c p) d -> p c d", p=128), writes=[w2])
                s.dma("sp", posT[:], I["cmp_posT"][l, which], writes=[posT])
                s.dma("sp", b1T[:], I["cmp_b1T"][l, which], writes=[b1T])
                s.dma("sp", b2T[:], I["cmp_b2T"][l, which], writes=[b2T])
                s.dma("sp", b2b[:], I["cmp_b2"][l, which:which + 1, :].broadcast_to([128, 64]), writes=[b2b])
                s.op("act", lambda h: h.copy(out=posb[:], in_=posT[:]), reads=[posT], writes=[posb])
                for ch in range(2):
                    pm = self.P[ch]
                    for li in range(32):
                        self.mm(pm[:, 0:1], w1[:, li, ch * 128:(ch + 1) * 128], posb[:, li:li + 1], li == 0, li == 31, [w1, posb], [pm], sig=(li == 31))
                    s.op("dve", lambda h: h.tensor_tensor(out=bias[:, ch:ch + 1], in0=pm[:, 0:1], in1=b1T[:, ch:ch + 1], op=ALU.add), reads=[pm, b1T], writes=[bias])
                for hk in range(2):
                    s.dma("sp", xT[:], Dr["QT_d"][SL_KCM + which * 2 + hk], reads=[DR["QT_d"]], writes=[xT])
                    for ch in range(2):
                        pm = self.P[2 + ch]
                        for li in range(32):
                            self.mm(pm[:, 0:127], w1[:, li, ch * 128:(ch + 1) * 128], xT[:, li:li + 16 * 126 + 1:16], li == 0, li == 31, [w1, xT], [pm], sig=(li == 31))
                        s.op("act", lambda h: h.activation(out=y[:, 0:127], in_=pm[:, 0:127], func=AF.Identity, bias=bias[:, ch:ch + 1]), reads=[pm, bias], writes=[y])
                        s.op("dve", lambda h: h.tensor_tensor(out=u[:, 0:127], in0=y[:, 0:127], in1=y[:, 0:127], op=ALU.mult), reads=[y], writes=[u])
                        s.op("dve", lambda h: h.tensor_scalar(out=u[:, 0:127], in0=u[:, 0:127], scalar1=0.044715, scalar2=1.0, op0=ALU.mult, op1=ALU.add), reads=[u], writes=[u])
                        s.op("dve", lambda h: h.tensor_tensor(out=u[:, 0:127], in0=u[:, 0:127], in1=y[:, 0:127], op=ALU.mult), reads=[u, y], writes=[u])
                        s.op("act", lambda h: h.activation(out=u[:, 0:127], in_=u[:, 0:127], func=AF.Sigmoid, scale=1.5957691216057308), reads=[u], writes=[u])
                        s.op("dve", lambda h: h.tensor_tensor(out=hid[:, ch, 0:127], in0=u[:, 0:127], in1=y[:, 0:127], op=ALU.mult), reads=[u, y], writes=[hid])
                    pm2 = self.P[4 + hk]
                    if which == 0:
                        for ch in range(2):
                            self.mm(pm2[0:64, 0:127], w2[:, ch, :], hid[:, ch, 0:127], ch == 0, ch == 1, [w2, hid], [pm2], sig=(ch == 1))
                        s.op("act", lambda h: h.activation(out=kct[:, 0:127], in_=pm2[0:64, 0:127], func=AF.Identity, bias=b2T[:, 0:1]), reads=[pm2, b2T], writes=[kct])
                        s.dma("sp", Dr["KC_d"][hk], kct[:], reads=[kct], writes=[DR["KC_d"]])
                    else:
                        for ch in range(2):
                            self.mm(pm2[0:127, 0:64], hid[:, ch, 0:127], w2[:, ch, :], ch == 0, ch == 1, [w2, hid], [pm2], sig=(ch == 1))
                        s.op("dve", lambda h: h.tensor_tensor(out=vct[0:127, 0:64], in0=pm2[0:127, 0:64], in1=b2b[0:127, :], op=ALU.add), reads=[pm2, b2b], writes=[vct])
                        s.op("pool", lambda h: h.memset(vct[:, 64:65], 1.0), writes=[vct])
                        s.op("pool", lambda h: h.tensor_copy(out=vct[:, 65:97], in_=ovl[:]), reads=[ovl], writes=[vct])
                        s.dma("sp", Dr["VC_d"][hk], vct[:], reads=[vct], writes=[DR["VC_d"]])

    def load_heads(self, tile, slots, grouped):
        s = self.s
        for g, slot in enumerate(slots):
            dst0 = tile[0:64, g, :] if grouped else tile[0:64, :]
            dst1 = tile[64:68, g, :] if grouped else tile[64:68, :]
            s.dma("sp", dst0, self.Dr["QT_d"][slot], reads=[self.DR["QT_d"]], writes=[tile])
            s.dma("sp", dst1, self.I["aug"][slot], writes=[tile])

    def attn_stream(self, items, banks, pts):
        s = self.s
        n = len(items)

        def emit_qk(k):
            it = items[k]
            sc = banks[k % len(banks)]
            nq = len(it["qk"])
            for m, (outfn, lhsT, rhs, start, stop, reads) in enumerate(it["qk"]):
                self.mm(outfn(sc), lhsT, rhs, start, stop, reads, [sc], sig=(m == nq - 1))

        emit_qk(0)
        if n > 1:
            emit_qk(1)
        for k in range(n):
            if k + 2 < n:
                emit_qk(k + 2)
            it = items[k]
            sc = banks[k % len(banks)]
            pT = pts[k % len(pts)]
            kp, N = it["kp"], it["N"]
            s.op("act", lambda h: h.activation(out=pT[0:kp, 0:N], in_=sc[0:kp, 0:N], func=AF.Exp, scale=SCALE), reads=[sc], writes=[pT])
            it["pv"](pT)

    def load_const(self, ps, name, shape, dt):
        t = self.s.sb(shape, dt, name, ps)
        self.s.dma("sp", t[:], self.I[name], writes=[t])
        return t

    def phase_swa(self, l, q):
        s, I, Dr, DR = self.s, self.I, self.Dr, self.DR
        ident = self.ident
        with ExitStack() as ps:
            mdiag = self.load_const(ps, "mdiag", [128, 128], BF16)
            medge = self.load_const(ps, "medge", [128, 128], BF16)
            esink = s.sb([128, 8], F32, "esink", ps)
            s.dma("sp", esink[:], I["sinks"][l:l + 1, :].broadcast_to([128, 8]), writes=[esink])
            s.op("act", lambda h: h.activation(out=esink[:], in_=esink[:], func=AF.Exp), reads=[esink], writes=[esink])
            QG = s.sb([68, 4, S], BF16, "QG", ps)
            KT = s.sb([68, S], BF16, "KT", ps)
            V = s.sb([128, NT, 65], BF16, "V", ps)
            pts = [s.sb([128, 512], BF16, "pT", ps) for _ in range(3)]
            ot = [s.sb([128, 4, 64], BF16, "ot", ps) for _ in range(2)]
            den = [s.sb([128, 4], F32, "den", ps) for _ in range(2)]
            for hk in range(2):
                self.load_heads(QG, [SL_QA + hk * 4 + g for g in range(4)], True)
                self.load_heads(KT, [SL_KA + hk], False)
                s.dma("sp", V[:], Dr["VA_d"][:, hk, :].rearrange("(n p) e -> p n e", p=128), reads=[DR["VA_d"]], writes=[V])
                for j in range(NT):
                    acc = self.P[4 + j % 2]
                    tiles = [i for i in (j - 1, j) if i >= 0]
                    items = []
                    for idx, i in enumerate(tiles):
                        mask = mdiag if i == j else medge
                        o3 = lambda sc: sc[:, :].rearrange("p (g t) -> p g t", g=4)
                        qk = [(o3, KT[:, i * 128:(i + 1) * 128], QG[:, :, j * 128:(j + 1) * 128], True, False, [KT, QG]),
                              (o3, ident[:], mask[:].unsqueeze(1).broadcast_to([128, 4, 128]), False, True, [ident, mask])]

                        def pv(pT, i=i, idx=idx, acc=acc, last=len(tiles) - 1):
                            for g in range(4):
                                self.mm(acc[:, g * 65:(g + 1) * 65], pT[:, g * 128:(g + 1) * 128], V[:, i, :], idx == 0 and g == 0, idx == last, [pT, V], [acc], sig=(g == 3))
                        items.append(dict(kp=128, N=512, qk=qk, pv=pv))
                    self.attn_stream(items, self.P[0:3], pts)
                    a3 = acc[:, 0:260].rearrange("p (g e) -> p g e", e=65)
                    dn, o = den[j % 2], ot[j % 2]
                    s.op("dve", lambda h: h.tensor_tensor(out=dn[:].unsqueeze(2), in0=a3[:, :, 64:65], in1=esink[:, hk * 4:(hk + 1) * 4].unsqueeze(2), op=ALU.add), reads=[acc, esink], writes=[dn])
                    s.op("dve", lambda h: h.reciprocal(out=dn[:], in_=dn[:]), reads=[dn], writes=[dn])
                    s.op("dve", lambda h: h.tensor_tensor(out=o[:], in0=a3[:, :, 0:64], in1=dn[:].unsqueeze(2).broadcast_to([128, 4, 64]), op=ALU.mult), reads=[acc, dn], writes=[o])
                    s.dma("sp", Dr["O_d"][j * 128:(j + 1) * 128, hk * 256:(hk + 1) * 256], o[:].rearrange("p g d -> p (g d)"), reads=[o], writes=[DR["O_d"]])

    def phase_diff(self, l, q):
        s, I, Dr, DR = self.s, self.I, self.Dr, self.DR
        ident = self.ident
        lam_init = 0.8 - 0.6 * float(np.exp(-0.3 * l))
        with ExitStack() as ps:
            mdiag = self.load_const(ps, "mdiag", [128, 128], BF16)
            dl = s.sb([128, 256], F32, "dl", ps)
            s.dma("sp", dl[:], I["diff_lambda"][l:l + 1, :].broadcast_to([128, 256]), writes=[dl])
            pr = s.sb([128, 2, 64], F32, "pr", ps)
            d4 = dl[:].rearrange("p (a b d) -> p a b d", a=2, b=2)
            s.op("dve", lambda h: h.tensor_tensor(out=pr[:], in0=d4[:, :, 0, :], in1=d4[:, :, 1, :], op=ALU.mult), reads=[dl], writes=[pr])
            e2 = s.sb([128, 2], F32, "e2", ps)
            s.op("dve", lambda h: h.reduce_sum(out=e2[:], in_=pr[:], axis=AX.X), reads=[pr], writes=[e2])
            s.op("act", lambda h: h.activation(out=e2[:], in_=e2[:], func=AF.Exp), reads=[e2], writes=[e2])
            nlam = s.sb([128, 1], F32, "nlam", ps)
            s.op("dve", lambda h: h.scalar_tensor_tensor(out=nlam[:], in0=e2[:, 0:1], scalar=lam_init, in1=e2[:, 1:2], op0=ALU.add, op1=ALU.subtract), reads=[e2], writes=[nlam])
            s.op("dve", lambda h: h.tensor_scalar_mul(out=nlam[:], in0=nlam[:], scalar1=-1.0), reads=[nlam], writes=[nlam])
            gsub = s.sb([128, 128], F32, "gsub", ps)
            s.dma("sp", gsub[:], I["diff_subln_g"][l:l + 1, :].broadcast_to([128, 128]), writes=[gsub])
            s.op("dve", lambda h: h.tensor_scalar_mul(out=gsub[:], in0=gsub[:], scalar1=1.0 - lam_init), reads=[gsub], writes=[gsub])
            KT = [s.sb([68, S], BF16, "KTb", ps) for _ in range(2)]
            QT = [s.sb([68, S], BF16, "QTb", ps) for _ in range(2)]
            V = s.sb([128, NT, 129], BF16, "Vb", ps)
            pts = [s.sb([128, 512], BF16, "pT", ps) for _ in range(3)]
            t0 = [s.sb([128, 128], F32, "t0", ps) for _ in range(2)]
            junk = s.sb([128, 128], F32, "junkb", ps)
            rr = [s.sb([128, 4], F32, "rr", ps) for _ in range(2)]
            ob = [s.sb([128, 128], BF16, "ob", ps) for _ in range(2)]
            nfin = 0
            for hd in range(4):
                for c in range(2):
                    self.load_heads(KT[c], [SL_KB + 2 * hd + c], False)
                    self.load_heads(QT[c], [SL_QB + 2 * hd + c], False)
                s.dma("sp", V[:], Dr["VB_d"][:, hd, :].rearrange("(n p) e -> p n e", p=128), reads=[DR["VB_d"]], writes=[V])
                for G in range(4):
                    def accap(c, jj):
                        return self.P[4 + c * 2 + jj // 2], (jj % 2) * 129
                    items = []
                    for c in range(2):
                        for i in range(0, 4 * G + 4):
                            j0 = max(i, 4 * G)
                            N = (4 * G + 4 - j0) * 128
                            kt = KT[c][:, i * 128:(i + 1) * 128]
                            qk = []
                            if i >= 4 * G:
                                qk.append((lambda sc: sc[:, 0:128], kt, QT[c][:, j0 * 128:(j0 + 1) * 128], True, False, [KT[c], QT[c]]))
                                qk.append((lambda sc: sc[:, 0:128], ident[:], mdiag[:], False, True, [ident, mdiag]))
                                if N > 128:
                                    qk.append((lambda sc, N=N: sc[:, 128:N], kt, QT[c][:, (j0 + 1) * 128:(4 * G + 4) * 128], True, True, [KT[c], QT[c]]))
                            else:
                                qk.append((lambda sc, N=N: sc[:, 0:N], kt, QT[c][:, j0 * 128:(4 * G + 4) * 128], True, True, [KT[c], QT[c]]))

                            def pv(pT, c=c, i=i, j0=j0, G=G):
                                for jj in range(j0, 4 * G + 4):
                                    bank, off = accap(c, jj - 4 * G)
                                    self.mm(bank[:, off:off + 129], pT[:, (jj - j0) * 128:(jj - j0 + 1) * 128], V[:, i, :], i == 0 and (jj - 4 * G) % 2 == 0, i == jj, [pT, V], [bank], sig=(jj == 4 * G + 3))
                            items.append(dict(kp=128, N=N, qk=qk, pv=pv))
                    self.attn_stream(items, self.P[0:3], pts)
                    for jj in range(4):
                        b0, o0 = accap(0, jj)
                        b1, o1 = accap(1, jj)
                        r, t, o = rr[nfin % 2], t0[nfin % 2], ob[nfin % 2]
                        nfin += 1
                        s.op("dve", lambda h: h.reciprocal(out=r[:, 0:1], in_=b0[:, o0 + 128:o0 + 129]), reads=[b0], writes=[r])
                        s.op("dve", lambda h: h.reciprocal(out=r[:, 1:2], in_=b1[:, o1 + 128:o1 + 129]), reads=[b1], writes=[r])
                        s.op("dve", lambda h: h.tensor_tensor(out=r[:, 1:2], in0=r[:, 1:2], in1=nlam[:], op=ALU.mult), reads=[r, nlam], writes=[r])
                        s.op("dve", lambda h: h.tensor_scalar_mul(out=t[:], in0=b0[:, o0:o0 + 128], scalar1=r[:, 0:1]), reads=[b0, r], writes=[t])
                        s.op("dve", lambda h: h.scalar_tensor_tensor(out=t[:], in0=b1[:, o1:o1 + 128], scalar=r[:, 1:2], in1=t[:], op0=ALU.mult, op1=ALU.add), reads=[b1, r, t], writes=[t])
                        s.op("act", lambda h: h.activation(out=junk[:], in_=t[:], func=AF.Square, accum_out=r[:, 2:3]), reads=[t], writes=[junk, r])
                        s.op("dve", lambda h: h.tensor_scalar(out=r[:, 2:3], in0=r[:, 2:3], scalar1=1.0 / 128, scalar2=EPS, op0=ALU.mult, op1=ALU.add), reads=[r], writes=[r])
                        s.op("act", lambda h: h.sqrt(out=r[:, 2:3], in_=r[:, 2:3]), reads=[r], writes=[r])
                        s.op("dve", lambda h: h.reciprocal(out=r[:, 2:3], in_=r[:, 2:3]), reads=[r], writes=[r])
                        s.op("dve", lambda h: h.scalar_tensor_tensor(out=o[:], in0=t[:], scalar=r[:, 2:3], in1=gsub[:], op0=ALU.mult, op1=ALU.mult), reads=[t, r, gsub], writes=[o])
                        row0 = (4 * G + jj) * 128
                        s.dma("sp", Dr["O_d"][row0:row0 + 128, 512 + hd * 128:512 + (hd + 1) * 128], o[:], reads=[o], writes=[DR["O_d"]])

    def phase_nsa(self, l, q):
        s, I, Dr, DR = self.s, self.I, self.Dr, self.DR
        ident = self.ident
        with ExitStack() as ps:
            mdiag = self.load_const(ps, "mdiag", [128, 128], BF16)
            medge = self.load_const(ps, "medge", [128, 128], BF16)
            cmaskT = self.load_const(ps, "cmaskT", [128, S], BF16)
            eblk = self.load_const(ps, "eblk", [32, S], BF16)
            atab = s.sb([128, NT, 32], F32, "atab", ps)
            s.dma("sp", atab[:], I["atab"].rearrange("(n p) b -> p n b", p=128), writes=[atab])
            GN = s.sb([128, NT, 24], F32, "GN", ps)
            s.dma("sp", GN[:], Dr["GN_d"].rearrange("(n p) c -> p n c", p=128), reads=[DR["GN_d"]], writes=[GN])
            QG = s.sb([68, 4, S], BF16, "QGc", ps)
            KSL = s.sb([68, S], BF16, "KSL", ps)
            KWN = s.sb([68, S], BF16, "KWN", ps)
            KCT = s.sb([64, 128], BF16, "KCT", ps)
            VC = s.sb([128, 97], BF16, "VC", ps)
            VSL = s.sb([128, NT, 65], BF16, "VSL", ps)
            VWN = s.sb([128, NT, 65], BF16, "VWN", ps)
            OC = s.sb([128, NT, 4, 64], F32, "OC", ps)
            SELT = s.sb([32, S], BF16, "SELT", ps)
            pts = [s.sb([128, 512], BF16, "pT", ps) for _ in range(3)]
            dn = [s.sb([128, 12], F32, "dn", ps) for _ in range(2)]
            imp = [s.sb([128, 32], F32, "imp", ps) for _ in range(2)]
            tmp32 = [s.sb([128, 32], F32, "tmp32", ps) for _ in range(2)]
            m8 = [s.sb([128, 16], F32, "m8", ps) for _ in range(2)]
            oo = [s.sb([128, 4, 64], F32, "oo", ps) for _ in range(2)]
            ot = [s.sb([128, 4, 64], F32, "otmp", ps) for _ in range(2)]
            ob = [s.sb([128, 4, 64], BF16, "obf", ps) for _ in range(2)]
            o3 = lambda sc: sc[:, :].rearrange("p (g t) -> p g t", g=4)
            o3c = lambda sc: sc[0:127, :].rearrange("p (g t) -> p g t", g=4)
            b4 = lambda ap: ap.unsqueeze(1).broadcast_to([ap.shape[0], 4, 128])
            for hk in range(2):
                self.load_heads(QG, [SL_QC + hk * 4 + g for g in range(4)], True)
                self.load_heads(KSL, [SL_KSL + hk], False)
                self.load_heads(KWN, [SL_KWN + hk], False)
                s.dma("sp", KCT[:], Dr["KC_d"][hk], reads=[DR["KC_d"]], writes=[KCT])
                s.dma("sp", VC[:], Dr["VC_d"][hk], reads=[DR["VC_d"]], writes=[VC])
                s.dma("sp", VSL[:], Dr["VSL_d"][:, hk, :].rearrange("(n p) e -> p n e", p=128), reads=[DR["VSL_d"]], writes=[VSL])
                s.dma("sp", VWN[:], Dr["VWN_d"][:, hk, :].rearrange("(n p) e -> p n e", p=128), reads=[DR["VWN_d"]], writes=[VWN])
                for j in range(NT):
                    acc = self.P[4 + j % 2]
                    jc = slice(j * 128, (j + 1) * 128)
                    qk = [(o3c, KCT[:, 0:127], QG[0:64, :, jc], True, False, [KCT, QG]),
                          (o3c, ident[0:127, 0:127], b4(cmaskT[0:127, jc]), False, True, [ident, cmaskT])]

                    def pv(pT, acc=acc):
                        for g in range(4):
                            self.mm(acc[:, g * 97:(g + 1) * 97], pT[0:127, g * 128:(g + 1) * 128], VC[0:127, :], g == 0, True, [pT, VC], [acc], sig=(g == 3))
                    self.attn_stream([dict(kp=127, N=512, qk=qk, pv=pv)], self.P[0:3], pts)
                    a3 = acc[:, 0:388].rearrange("p (g e) -> p g e", e=97)
                    d, im, tm, mm8 = dn[j % 2], imp[j % 2], tmp32[j % 2], m8[j % 2]
                    s.op("dve", lambda h: h.tensor_scalar_max(out=d[:, 0:4].unsqueeze(2), in0=a3[:, :, 64:65], scalar1=1e-30), reads=[acc], writes=[d])
                    s.op("dve", lambda h: h.reciprocal(out=d[:, 0:4], in_=d[:, 0:4]), reads=[d], writes=[d])
                    s.op("dve", lambda h: h.tensor_tensor(out=OC[:, j], in0=a3[:, :, 0:64], in1=d[:, 0:4].unsqueeze(2).broadcast_to([128, 4, 64]), op=ALU.mult), reads=[acc, d], writes=[OC])
                    s.op("dve", lambda h: h.tensor_scalar_mul(out=im[:], in0=a3[:, 0, 65:97], scalar1=d[:, 0:1]), reads=[acc, d], writes=[im])
                    for g in range(1, 4):
                        s.op("dve", lambda h, g=g: h.scalar_tensor_tensor(out=im[:], in0=a3[:, g, 65:97], scalar=d[:, g:g + 1], in1=im[:], op0=ALU.mult, op1=ALU.add), reads=[acc, d, im], writes=[im])
                    s.op("dve", lambda h: h.tensor_tensor(out=im[:], in0=im[:], in1=atab[:, j, :], op=ALU.add), reads=[im, atab], writes=[im])
                    s.op("dve", lambda h: h.max(out=mm8[:, 0:8], in_=im[:]), reads=[im], writes=[mm8])
                    s.op("dve", lambda h: h.match_replace(out=tm[:], in_to_replace=mm8[:, 0:8], in_values=im[:], imm_value=-3e9), reads=[im, mm8], writes=[tm])
                    s.op("dve", lambda h: h.max(out=mm8[:, 8:16], in_=tm[:]), reads=[tm], writes=[mm8])
                    s.op("dve", lambda h: h.tensor_scalar(out=tm[:], in0=im[:], scalar1=mm8[:, 15:16], scalar2=NEG, op0=ALU.is_lt, op1=ALU.mult), reads=[im, mm8], writes=[tm])
                    pt = self.P[6 + j % 2]
                    self.tr(pt[0:32, 0:128], tm[:], self.ident32[:], [tm, self.ident32], [pt])
                    s.op("act", lambda h: h.copy(out=SELT[:, jc], in_=pt[0:32, 0:128]), reads=[pt], writes=[SELT])
                for j in range(NT):
                    jc = slice(j * 128, (j + 1) * 128)
                    accS, accW = self.P[4 + 2 * (j % 2)], self.P[5 + 2 * (j % 2)]
                    items = []
                    for i in range(0, j + 1):
                        ic = slice(i * 128, (i + 1) * 128)
                        qk = [(o3, KSL[:, ic], QG[:, :, jc], True, False, [KSL, QG]),
                              (o3, eblk[:, ic], b4(SELT[:, jc]), False, i != j, [eblk, SELT])]
                        if i == j:
                            qk.append((o3, ident[:], b4(mdiag[:]), False, True, [ident, mdiag]))

                        def pv(pT, i=i, j=j, acc=accS):
                            for g in range(4):
                                self.mm(acc[:, g * 65:(g + 1) * 65], pT[:, g * 128:(g + 1) * 128], VSL[:, i, :], i == 0 and g == 0, i == j, [pT, VSL], [acc], sig=(g == 3))
                        items.append(dict(kp=128, N=512, qk=qk, pv=pv))
                    i0 = max(0, j - 4)
                    for i in range(i0, j + 1):
                        ic = slice(i * 128, (i + 1) * 128)
                        masked = (i == j) or (i == j - 4)
                        qk = [(o3, KWN[:, ic], QG[:, :, jc], True, not masked, [KWN, QG])]
                        if masked:
                            mk = mdiag if i == j else medge
                            qk.append((o3, ident[:], b4(mk[:]), False, True, [ident, mk]))

                        def pv(pT, i=i, j=j, i0=i0, acc=accW):
                            for g in range(4):
                                self.mm(acc[:, g * 65:(g + 1) * 65], pT[:, g * 128:(g + 1) * 128], VWN[:, i, :], i == i0 and g == 0, i == j, [pT, VWN], [acc], sig=(g == 3))
                        items.append(dict(kp=128, N=512, qk=qk, pv=pv))
                    self.attn_stream(items, self.P[0:3], pts)
                    d, o, t, obf = dn[j % 2], oo[j % 2], ot[j % 2], ob[j % 2]
                    aS = accS[:, 0:260].rearrange("p (g e) -> p g e", e=65)
                    aW = accW[:, 0:260].rearrange("p (g e) -> p g e", e=65)
                    gv = GN[:, j, hk * 12:(hk + 1) * 12].rearrange("p (g r) -> p g r", r=3)
                    s.op("dve", lambda h: h.reciprocal(out=d[:, 4:8].unsqueeze(2), in_=aS[:, :, 64:65]), reads=[accS], writes=[d])
                    s.op("dve", lambda h: h.reciprocal(out=d[:, 8:12].unsqueeze(2), in_=aW[:, :, 64:65]), reads=[accW], writes=[d])
                    s.op("dve", lambda h: h.tensor_tensor(out=d[:, 4:8].unsqueeze(2), in0=d[:, 4:8].unsqueeze(2), in1=gv[:, :, 1:2], op=ALU.mult), reads=[d, GN], writes=[d])
                    s.op("dve", lambda h: h.tensor_tensor(out=d[:, 8:12].unsqueeze(2), in0=d[:, 8:12].unsqueeze(2), in1=gv[:, :, 2:3], op=ALU.mult), reads=[d, GN], writes=[d])
                    s.op("pool", lambda h: h.tensor_tensor(out=o[:], in0=OC[:, j], in1=gv[:, :, 0:1].broadcast_to([128, 4, 64]), op=ALU.mult), reads=[OC, GN], writes=[o])
                    s.op("dve", lambda h: h.tensor_tensor(out=t[:], in0=aS[:, :, 0:64], in1=d[:, 4:8].unsqueeze(2).broadcast_to([128, 4, 64]), op=ALU.mult), reads=[accS, d], writes=[t])
                    s.op("pool", lambda h: h.tensor_tensor(out=o[:], in0=o[:], in1=t[:], op=ALU.add), reads=[o, t], writes=[o])
                    s.op("dve", lambda h: h.tensor_tensor(out=t[:], in0=aW[:, :, 0:64], in1=d[:, 8:12].unsqueeze(2).broadcast_to([128, 4, 64]), op=ALU.mult), reads=[accW, d], writes=[t])
                    s.op("pool", lambda h: h.tensor_tensor(out=obf[:], in0=o[:], in1=t[:], op=ALU.add), reads=[o, t], writes=[obf])
                    s.dma("sp", Dr["O_d"][jc, 1024 + hk * 256:1024 + (hk + 1) * 256], obf[:].rearrange("p g d -> p (g d)"), reads=[obf], writes=[DR["O_d"]])

    def phase_merge(self, l, q):
        s, I, Dr, DR = self.s, self.I, self.Dr, self.DR
        xsrc, xres_r = self.xsrc(l, q)
        with ExitStack() as ps:
            wbr = s.sb([128, 12, D], BF16, "wbr", ps)
            wout = s.sb([128, 8, D], BF16, "wout", ps)
            for r in range(3):
                for hh in range(2):
                    s.dma("pool", wbr[:, r * 4:(r + 1) * 4, hh * 512:(hh + 1) * 512],
                          I["w_branch"][l, r].rearrange("(kc p) n -> p kc n", p=128)[:, :, hh * 512:(hh + 1) * 512], writes=[wbr])
            for k4 in range(2):
                for hh in range(2):
                    s.dma("pool", wout[:, k4 * 4:(k4 + 1) * 4, hh * 512:(hh + 1) * 512],
                          I["w_out"][l].rearrange("(kc p) n -> p kc n", p=128)[:, k4 * 4:(k4 + 1) * 4, hh * 512:(hh + 1) * 512], writes=[wout])
            g1 = s.sb([128, D], F32, "g1", ps)
            self.mod_bc(l, q, 2, g1)
            Ot = [s.sb([128, 1536], BF16, "Ot", ps) for _ in range(2)]
            GM = [s.sb([128, 3072], F32, "GMt", ps) for _ in range(2)]
            xt = [s.sb([128, D], F32, "xt", ps) for _ in range(2)]
            OT = [s.sb([128, 12, 128], BF16, "OT", ps) for _ in range(2)]
            MT = [s.sb([128, 8, 128], BF16, "MT", ps) for _ in range(2)]
            mg = s.sb([128, D], F32, "mg", ps)
            mgb = s.sb([128, D], BF16, "mgb", ps)
            tmp = [s.sb([128, 512], F32, "mtmp", ps) for _ in range(2)]
            nb = 0
            for tt in range(NT):
                rows = slice(tt * 128, (tt + 1) * 128)
                grow = slice(q * S + tt * 128, q * S + (tt + 1) * 128)
                O, G, x, ot, mt = Ot[tt % 2], GM[tt % 2], xt[tt % 2], OT[tt % 2], MT[tt % 2]
                s.dma("sp", O[:], Dr["O_d"][rows, :], reads=[DR["O_d"]], writes=[O])
                s.dma("sp", G[:], Dr["GM_d"][rows, :], reads=[DR["GM_d"]], writes=[G])
                s.dma("sp", x[:], xsrc[grow, :], reads=xres_r, writes=[x])
                pa, pb = self.P[0], self.P[1]
                pab, pbb = pa[:].bitcast(BF16), pb[:].bitcast(BF16)
                for kc in range(8):
                    self.tr(pab[:, kc * 128:(kc + 1) * 128], O[:, kc * 128:(kc + 1) * 128], self.ident[:], [O, self.ident], [pa], sig=(kc == 7))
                for kc in range(4):
                    self.tr(pbb[:, kc * 128:(kc + 1) * 128], O[:, (8 + kc) * 128:(9 + kc) * 128], self.ident[:], [O, self.ident], [pb], sig=(kc == 3))
                s.op("act", lambda h: h.copy(out=ot[:, 0:8, :], in_=pab.rearrange("p (k t) -> p k t", k=8)), reads=[pa], writes=[ot])
                s.op("act", lambda h: h.copy(out=ot[:, 8:12, :], in_=pbb[:, 0:512].rearrange("p (k t) -> p k t", k=4)), reads=[pb], writes=[ot])
                for r in range(3):
                    for half in range(2):
                        pm = self.P[2 + nb % 4]
                        tp = tmp[nb % 2]
                        nb += 1
                        hc = slice(half * 512, (half + 1) * 512)
                        for kc in range(4):
                            self.mm(pm[:, :], ot[:, r * 4 + kc, :], wbr[:, r * 4 + kc, hc], kc == 0, kc == 3, [ot, wbr], [pm], sig=(kc == 3))
                        gsl = G[:, r * 1024 + half * 512:r * 1024 + (half + 1) * 512]
                        if r == 0:
                            s.op("dve", lambda h: h.tensor_tensor(out=mg[:, hc], in0=pm[:, :], in1=gsl, op=ALU.mult), reads=[pm, G], writes=[mg])
                        else:
                            s.op("dve", lambda h: h.tensor_tensor(out=tp[:], in0=pm[:, :], in1=gsl, op=ALU.mult), reads=[pm, G], writes=[tp])
                            s.op("pool", lambda h: h.tensor_tensor(out=mg[:, hc], in0=mg[:, hc], in1=tp[:], op=ALU.add), reads=[mg, tp], writes=[mg])
                s.op("act", lambda h: h.copy(out=mgb[:], in_=mg[:]), reads=[mg], writes=[mgb])
                for kc in range(8):
                    self.tr(pab[:, kc * 128:(kc + 1) * 128], mgb[:, kc * 128:(kc + 1) * 128], self.ident[:], [mgb, self.ident], [pa], sig=(kc == 7))
                s.op("act", lambda h: h.copy(out=mt[:], in_=pab.rearrange("p (k t) -> p k t", k=8)), reads=[pa], writes=[mt])
                for half in range(2):
                    pm = self.P[2 + nb % 4]
                    tp = tmp[nb % 2]
                    nb += 1
                    hc = slice(half * 512, (half + 1) * 512)
                    for kc in range(8):
                        self.mm(pm[:, :], mt[:, kc, :], wout[:, kc, hc], kc == 0, kc == 7, [mt, wout], [pm], sig=(kc == 7))
                    s.op("dve", lambda h: h.tensor_tensor(out=tp[:], in0=pm[:, :], in1=g1[:, hc], op=ALU.mult), reads=[pm, g1], writes=[tp])
                    s.op("pool", lambda h: h.tensor_tensor(out=x[:, hc], in0=x[:, hc], in1=tp[:], op=ALU.add), reads=[x, tp], writes=[x])
                s.dma("sp", Dr["xres"][grow, :], x[:], reads=[x], writes=[DR["xres"]])

    def phase_moe2(self, l):
        s, I, Dr, DR = self.s, self.I, self.Dr, self.DR
        last = (l == L_DEPTH - 1)
        NTT, NB = self.ntt, self.nblk
        with ExitStack() as ps:
            g2 = []
            for q in range(self.nseq):
                t = s.sb([128, D], F32, "g2", ps)
                self.mod_bc(l, q, 5, t)
                g2.append(t)
            GATE = s.sb([128, NTT, NE], F32, "GATE", ps)
            GT = s.sb([NE, NTT, 128], F32, "GT", ps)
            IDX4 = s.sb([128, NTT, 4], I32, "IDX4", ps)
            G4 = s.sb([128, NTT, 4], F32, "G4", ps)
            IDXW = s.sb([128, NB, 2], I32, "IDXW", ps)
            IDXB = s.sb([128, NB], I32, "IDXB", ps)
            b2 = s.sb([NE, D], F32, "b2", ps)
            s.dma("sp", b2[:], I["exp_b2"][l], writes=[b2])
            with ExitStack() as p1:
                HB = s.sb([128, NTT, D], BF16, "HB", p1)
                MASK = s.sb([128, NTT, NE], BF16, "MASK", p1)
                DEST = s.sb([128, NTT, NE], F32, "DEST", p1)
                rw = s.sb([128, 8, NE], F32, "rw", p1)
                rb = s.sb([128, NE], F32, "rb", p1)
                s.dma("sp", rw[:], I["router_w"][l].rearrange("(kc p) e -> p kc e", p=128), writes=[rw])
                s.dma("sp", rb[:], I["router_b"][l:l + 1, :].broadcast_to([128, NE]), writes=[rb])
                ltri = self.load_const(p1, "ltri", [128, 128], BF16)
                ones = self.load_const(p1, "ones128", [128, 128], BF16)
                b512 = self.load_const(p1, "b512", [128, 64], F32)
                rowiota = self.load_const(p1, "rowiota", [128, 8], F32)
                piota = self.load_const(p1, "piota", [128, 1], F32)
                xt = [s.sb([128, D], F32, "xt", p1) for _ in range(2)]
                hf = [s.sb([128, D], F32, "hf", p1) for _ in range(2)]
                hT32s = [s.sb([128, 8, 128], F32, "hT32", p1) for _ in range(2)]
                junk = s.sb([128, D], F32, "junk", p1)
                ss = [s.sb([128, 1], F32, "ss", p1) for _ in range(2)]
                lg = [s.sb([128, NE], F32, "lg", p1) for _ in range(2)]
                ex = [s.sb([128, NE], F32, "ex", p1) for _ in range(2)]
                m8 = [s.sb([128, 8], F32, "m8", p1) for _ in range(2)]
                sm = [s.sb([128, 2], F32, "sm", p1) for _ in range(2)]
                gsc = sh = None
                for tt in range(NTT):
                    q = tt // NT
                    if tt % NT == 0:
                        gsc, sh = self.norm_tiles(p1, l, q, "norm2_g", 4, 3)
                    grow = slice(tt * 128, (tt + 1) * 128)
                    x, h32, sq = xt[tt % 2], hf[tt % 2], ss[tt % 2]
                    s.dma("sp", x[:], Dr["xres"][grow, :], reads=[DR["xres"]], writes=[x])
                    self.rstd_of((x, x[:]), (junk, junk[:]), sq)
                    s.op("dve", lambda hh: hh.scalar_tensor_tensor(out=x[:], in0=x[:], scalar=sq[:, 0:1], in1=gsc[:], op0=ALU.mult, op1=ALU.mult), reads=[x, sq, gsc], writes=[x])
                    s.op("pool", lambda hh: hh.tensor_tensor(out=h32[:], in0=x[:], in1=sh[:], op=ALU.add), reads=[x, sh], writes=[h32])
                    s.op("act", lambda hh: hh.copy(out=HB[:, tt, :], in_=h32[:]), reads=[h32], writes=[HB])
                    hT32 = hT32s[tt % 2]
                    for hh2 in range(2):
                        pf = self.P[(tt % 2) * 2 + hh2]
                        for k4 in range(4):
                            kc = hh2 * 4 + k4
                            self.tr(pf[:, k4 * 128:(k4 + 1) * 128], h32[:, kc * 128:(kc + 1) * 128], self.ident32[:], [h32, self.ident32], [pf], sig=(k4 == 3))
                        s.op("dve", lambda hh: hh.tensor_copy(out=hT32[:, hh2 * 4:(hh2 + 1) * 4, :], in_=pf[:, :].rearrange("p (k t) -> p k t", k=4)), reads=[pf], writes=[hT32])
                    pl = self.P[4 + tt % 2]
                    for kc in range(8):
                        self.mm(pl[:, 0:NE], hT32[:, kc, :], rw[:, kc, :], kc == 0, kc == 7, [hT32, rw], [pl], sig=(kc == 7))
                    lgt, et, mt, st = lg[tt % 2], ex[tt % 2], m8[tt % 2], sm[tt % 2]
                    s.op("dve", lambda hh: hh.tensor_tensor(out=lgt[:], in0=pl[:, 0:NE], in1=rb[:], op=ALU.add), reads=[pl, rb], writes=[lgt])
                    s.op("dve", lambda hh: hh.max(out=mt[:], in_=lgt[:]), reads=[lgt], writes=[mt])
                    s.op("dve", lambda hh: hh.tensor_scalar_mul(out=st[:, 0:1], in0=mt[:, 0:1], scalar1=-1.0), reads=[mt], writes=[st])
                    s.op("act", lambda hh: hh.activation(out=et[:], in_=lgt[:], func=AF.Exp, bias=st[:, 0:1]), reads=[lgt, st], writes=[et])
                    s.op("dve", lambda hh: hh.tensor_scalar(out=lgt[:], in0=lgt[:], scalar1=mt[:, 3:4], scalar2=None, op0=ALU.is_ge), reads=[lgt, mt], writes=[lgt])
                    s.op("dve", lambda hh: hh.tensor_copy(out=MASK[:, tt, :], in_=lgt[:]), reads=[lgt], writes=[MASK])
                    s.op("dve", lambda hh: hh.tensor_tensor(out=et[:], in0=et[:], in1=lgt[:], op=ALU.mult), reads=[et, lgt], writes=[et])
                    s.op("dve", lambda hh: hh.reduce_sum(out=st[:, 1:2], in_=et[:], axis=AX.X), reads=[et], writes=[st])
                    s.op("dve", lambda hh: hh.reciprocal(out=st[:, 1:2], in_=st[:, 1:2]), reads=[st], writes=[st])
                    s.op("dve", lambda hh: hh.tensor_scalar_mul(out=GATE[:, tt, :], in0=et[:], scalar1=st[:, 1:2]), reads=[et, st], writes=[GATE])
                    pg = self.P[6 + tt % 2]
                    self.tr(pg[0:NE, 0:128], GATE[:, tt, :], self.ident32[:], [GATE, self.ident32], [pg])
                    s.op("act", lambda hh: hh.copy(out=GT[:, tt, :], in_=pg[0:NE, 0:128]), reads=[pg], writes=[GT])
                run = s.sb([128, NE], F32, "run", p1)
                s.op("dve", lambda hh: hh.memset(run[:], 0.0), writes=[run])
                for tt in range(NTT):
                    pr = self.P[6 + tt % 2]
                    self.mm(pr[:, 0:NE], ltri[:], MASK[:, tt, :], True, True, [ltri, MASK], [pr], sig=False)
                    self.mm(pr[:, NE:2 * NE], ones[:], MASK[:, tt, :], False, True, [ones, MASK], [pr])
                    s.op("dve", lambda hh: hh.tensor_tensor(out=DEST[:, tt, :], in0=pr[:, 0:NE], in1=run[:], op=ALU.add), reads=[pr, run], writes=[DEST])
                    s.op("dve", lambda hh: hh.tensor_tensor(out=run[:], in0=run[:], in1=pr[:, NE:2 * NE], op=ALU.add), reads=[pr, run], writes=[run])
                padded = s.sb([128, NE], F32, "padded", p1)
                tmpe = s.sb([128, NE], F32, "tmpe", p1)
                cum = [s.sb([128, NE], F32, "cum", p1) for _ in range(2)]
                s.op("dve", lambda hh: hh.memset(padded[:], 0.0), writes=[padded])
                for jb in range(NTT // 4):
                    s.op("dve", lambda hh, jb=jb: hh.scalar_tensor_tensor(out=padded[:], in0=run[:], scalar=512.0 * jb, in1=padded[:], op0=ALU.is_gt, op1=ALU.add), reads=[run, padded], writes=[padded])
                s.op("dve", lambda hh: hh.tensor_scalar_mul(out=padded[:], in0=padded[:], scalar1=512.0), reads=[padded], writes=[padded])
                s.op("dve", lambda hh: hh.tensor_copy(out=cum[0][:], in_=padded[:]), reads=[padded], writes=[cum[0]])
                ci = 0
                for shf in (1, 2, 4, 8, 16):
                    a, b = cum[ci], cum[1 - ci]
                    s.op("dve", lambda hh: hh.tensor_copy(out=b[:, 0:shf], in_=a[:, 0:shf]), reads=[a], writes=[b])
                    s.op("dve", lambda hh: hh.tensor_tensor(out=b[:, shf:NE], in0=a[:, shf:NE], in1=a[:, 0:NE - shf], op=ALU.add), reads=[a], writes=[b])
                    ci = 1 - ci
                pend = cum[ci]
                pstart = s.sb([128, NE], F32, "pstart", p1)
                s.op("dve", lambda hh: hh.tensor_tensor(out=pstart[:], in0=pend[:], in1=padded[:], op=ALU.subtract), reads=[pend, padded], writes=[pstart])
                s.op("dve", lambda hh: hh.tensor_tensor(out=DEST[:], in0=DEST[:], in1=pstart[:].unsqueeze(1).broadcast_to([128, NTT, NE]), op=ALU.add), reads=[DEST, pstart], writes=[DEST])
                s.op("dve", lambda hh: hh.scalar_tensor_tensor(out=DEST[:], in0=DEST[:], scalar=1.0, in1=MASK[:], op0=ALU.add, op1=ALU.mult), reads=[DEST, MASK], writes=[DEST])
                d4 = s.sb([128, NTT, 4], F32, "d4", p1)
                for tt in range(NTT):
                    mt, et = m8[tt % 2], ex[tt % 2]
                    s.op("dve", lambda hh: hh.max(out=mt[:], in_=DEST[:, tt, :]), reads=[DEST], writes=[mt])
                    s.op("dve", lambda hh: hh.tensor_scalar_add(out=d4[:, tt, :], in0=mt[:, 0:4], scalar1=-1.0), reads=[mt], writes=[d4])
                    for k in range(4):
                        s.op("dve", lambda hh: hh.scalar_tensor_tensor(out=et[:], in0=DEST[:, tt, :], scalar=mt[:, k:k + 1], in1=GATE[:, tt, :], op0=ALU.is_equal, op1=ALU.mult), reads=[DEST, mt, GATE], writes=[et])
                        s.op("dve", lambda hh: hh.reduce_sum(out=G4[:, tt, k:k + 1], in_=et[:], axis=AX.X), reads=[et], writes=[G4])
                s.op("dve", lambda hh: hh.tensor_copy(out=IDX4[:], in_=d4[:]), reads=[d4], writes=[IDX4])
                bexp = s.sb([128, 64], F32, "bexp", p1)
                s.op("dve", lambda hh: hh.memset(bexp[:], 0.0), writes=[bexp])
                for e in range(NE):
                    s.op("dve", lambda hh: hh.scalar_tensor_tensor(out=bexp[:], in0=b512[:], scalar=pend[:, e:e + 1], in1=bexp[:], op0=ALU.is_ge, op1=ALU.add), reads=[b512, pend, bexp], writes=[bexp])
                boob = s.sb([128, 64], F32, "boob", p1)
                s.op("dve", lambda hh: hh.tensor_scalar(out=boob[:], in0=bexp[:], scalar1=float(NE) - 0.5, scalar2=1.0e6, op0=ALU.is_ge, op1=ALU.mult), reads=[bexp], writes=[boob])
                s.op("dve", lambda hh: hh.tensor_scalar_min(out=bexp[:], in0=bexp[:], scalar1=float(NE - 1)), reads=[bexp], writes=[bexp])
                iwf = s.sb([128, NB, 2], F32, "iwf", p1)
                ibf = s.sb([128, NB], F32, "ibf", p1)
                e1k = s.sb([128, 64], F32, "e1k", p1)
                s.op("dve", lambda hh: hh.tensor_scalar(out=e1k[:], in0=bexp[:], scalar1=256.0, scalar2=float(l * NE * 256), op0=ALU.mult, op1=ALU.add), reads=[bexp], writes=[e1k])
                s.op("dve", lambda hh: hh.tensor_tensor(out=e1k[:], in0=e1k[:], in1=boob[:], op=ALU.add), reads=[e1k, boob], writes=[e1k])
                s.op("dve", lambda hh: hh.tensor_tensor(out=iwf[:], in0=rowiota[:, 0:2].unsqueeze(1).broadcast_to([128, NB, 2]), in1=e1k[:, 0:NB].unsqueeze(2).broadcast_to([128, NB, 2]), op=ALU.add), reads=[rowiota, e1k], writes=[iwf])
                s.op("dve", lambda hh: hh.tensor_copy(out=IDXW[:], in_=iwf[:]), reads=[iwf], writes=[IDXW])
                s.op("dve", lambda hh: hh.tensor_scalar(out=ibf[:], in0=bexp[:, 0:NB], scalar1=128.0, scalar2=piota[:, 0:1], op0=ALU.mult, op1=ALU.add), reads=[bexp, piota], writes=[ibf])
                s.op("dve", lambda hh: hh.tensor_scalar_add(out=ibf[:], in0=ibf[:], scalar1=float(l * NE * 128)), reads=[ibf], writes=[ibf])
                s.op("dve", lambda hh: hh.tensor_tensor(out=ibf[:], in0=ibf[:], in1=boob[:, 0:NB], op=ALU.add), reads=[ibf, boob], writes=[ibf])
                s.op("dve", lambda hh: hh.tensor_copy(out=IDXB[:], in_=ibf[:]), reads=[ibf], writes=[IDXB])
                if "dbg_d" in self.dbg:
                    dbt = s.sb([128, 4096], F32, "dbt", p1)
                    s.op("dve", lambda hh: hh.memset(dbt[:], 0.0), writes=[dbt])
                    s.op("dve", lambda hh: hh.tensor_copy(out=dbt[:, 0:32], in_=run[:]), reads=[run], writes=[dbt])
                    s.op("dve", lambda hh: hh.tensor_copy(out=dbt[:, 32:64], in_=pend[:]), reads=[pend], writes=[dbt])
                    s.op("dve", lambda hh: hh.tensor_copy(out=dbt[:, 64:128], in_=bexp[:]), reads=[bexp], writes=[dbt])
                    s.op("dve", lambda hh: hh.tensor_copy(out=dbt[:, 128:128 + NTT * 4], in_=d4[:].rearrange("p t k -> p (t k)")), reads=[d4], writes=[dbt])
                    s.op("dve", lambda hh: hh.tensor_copy(out=dbt[:, 512:512 + NTT * 4], in_=G4[:].rearrange("p t k -> p (t k)")), reads=[G4], writes=[dbt])
                    s.dma("sp", Dr["dbg_d"], dbt[:], reads=[dbt], writes=[DR["dbg_d"]])
                for tt in range(NTT):
                    for k in range(4):
                        s.idma(Dr["xs_d"][:, :], HB[:, tt, :], out_off=IDX4[:, tt, k:k + 1], reads=[HB, IDX4], writes=[])
                s.barrier()
            with ExitStack() as p2:
                W1 = [s.sb([128, 8, 2 * D], BF16, "W1", p2) for _ in range(2)]
                W2 = [s.sb([128, 8, D], BF16, "W2", p2) for _ in range(2)]
                B1 = [s.sb([128, 16], F32, "B1", p2) for _ in range(2)]
                XS = [s.sb([128, 4, D], BF16, "XS", p2) for _ in range(2)]
                XT = [s.sb([128, 8, 512], BF16, "XT", p2) for _ in range(2)]
                Ab = [s.sb([128, 8, 512], BF16, "Ab", p2) for _ in range(2)]
                Gt = [s.sb([128, 512], F32, "Gt", p2) for _ in range(2)]
                St = [s.sb([128, 512], F32, "St", p2) for _ in range(2)]
                Ut = [s.sb([128, 512], F32, "Ut", p2) for _ in range(2)]
                Yt = [s.sb([128, D], F32, "Yt", p2) for _ in range(2)]
                for wt_ in W1 + W2 + B1:
                    s.op("pool", lambda hh, wt_=wt_: hh.memset(wt_[:], 0.0), writes=[wt_])
                cnt = {"nf": 0, "ny": 0}

                def load_block(b):
                    w1, w2, b1, xs = W1[b % 2], W2[b % 2], B1[b % 2], XS[b % 2]
                    s.dma("sp", xs[:], Dr["xs_d"][b * 512:(b + 1) * 512, :].rearrange("(t p) d -> p t d", p=128), reads=[DR["xs_d"]], writes=[xs])
                    for j2 in range(2):
                        s.idma(w1[:, j2 * 4:(j2 + 1) * 4, :].rearrange("p a b -> p (a b)"), I["exp_w1"][:, :], in_off=IDXW[:, b, j2:j2 + 1], reads=[IDXW], writes=[w1], bounds=65535)
                    s.idma(b1[:, :], I["exp_b1E"][:, :], in_off=IDXB[:, b:b + 1], reads=[IDXB], writes=[b1], bounds=65535)

                def load_w2(b):
                    w2 = W2[b % 2]
                    s.idma(w2[:].rearrange("p a b -> p (a b)"), I["exp_w2"][:, :], in_off=IDXB[:, b:b + 1], reads=[IDXB], writes=[w2], bounds=65535)

                def transposes(b):
                    xs, xT = XS[b % 2], XT[b % 2]
                    for t4 in range(4):
                        pt = self.P[t4 % 2]
                        ptb = pt[:].bitcast(BF16)
                        for kc in range(8):
                            kb = (kc // 4) * 512 + (kc % 4)
                            self.tr(ptb[:, kc * 128:(kc + 1) * 128], xs[:, t4, kb:kb + 509:4], self.ident[:], [xs, self.ident], [pt], sig=(kc == 7))
                        if t4 % 2 == 0:
                            s.op("act", lambda hh: hh.copy(out=xT[:, :, t4 * 128:(t4 + 1) * 128], in_=ptb.rearrange("p (k t) -> p k t", k=8)), reads=[pt], writes=[xT])
                        else:
                            s.op("dve", lambda hh: hh.tensor_copy(out=xT[:, :, t4 * 128:(t4 + 1) * 128], in_=ptb.rearrange("p (k t) -> p k t", k=8)), reads=[pt], writes=[xT])

                def stage1_fc(b, fc):
                    w1, b1, xT, A = W1[b % 2], B1[b % 2], XT[b % 2], Ab[b % 2]
                    nf = cnt["nf"]
                    cnt["nf"] += 1
                    psG, psU = self.P[2 + (nf % 2) * 2], self.P[3 + (nf % 2) * 2]
                    G, Sg, U = Gt[nf % 2], St[nf % 2], Ut[nf % 2]
                    for kc in range(8):
                        self.mm(psG[:, :], w1[:, kc, fc:D:8], xT[:, kc, :], kc == 0, kc == 7, [w1, xT], [psG], sig=(kc == 7))
                    for kc in range(8):
                        self.mm(psU[:, :], w1[:, kc, D + fc:2 * D:8], xT[:, kc, :], kc == 0, kc == 7, [w1, xT], [psU], sig=(kc == 7))
                    s.op("dve", lambda hh: hh.tensor_scalar(out=G[:], in0=psG[:, :], scalar1=b1[:, fc:fc + 1], scalar2=7.0, op0=ALU.add, op1=ALU.min), reads=[psG, b1], writes=[G])
                    s.op("act", lambda hh: hh.activation(out=Sg[:], in_=G[:], func=AF.Sigmoid, scale=1.702), reads=[G], writes=[Sg])
                    s.op("act", lambda hh: hh.activation(out=U[:], in_=psU[:, :], func=AF.Identity, bias=b1[:, 8 + fc:8 + fc + 1]), reads=[psU, b1], writes=[U])
                    s.op("dve", lambda hh: hh.tensor_scalar(out=U[:], in0=U[:], scalar1=7.0, scalar2=-7.0, op0=ALU.min, op1=ALU.max), reads=[U], writes=[U])
                    s.op("dve", lambda hh: hh.tensor_tensor(out=G[:], in0=G[:], in1=Sg[:], op=ALU.mult), reads=[G, Sg], writes=[G])
                    s.op("dve", lambda hh: hh.scalar_tensor_tensor(out=A[:, fc, :], in0=U[:], scalar=1.0, in1=G[:], op0=ALU.add, op1=ALU.mult), reads=[U, G], writes=[A])

                def stage2_grp(b, g):
                    w2, A = W2[b % 2], Ab[b % 2]
                    t4, dh = g // 2, g % 2
                    ny = cnt["ny"]
                    y = Yt[(ny // 2) % 2]
                    psY = self.P[6 + ny % 2]
                    cnt["ny"] += 1
                    dc = slice(dh * 512, (dh + 1) * 512)
                    for fc in range(8):
                        self.mm(psY[:, :], A[:, fc, t4 * 128:(t4 + 1) * 128], w2[:, fc, dc], fc == 0, fc == 7, [A, w2], [psY], sig=(fc == 7))
                    s.op("act", lambda hh: hh.copy(out=y[:, dc], in_=psY[:, :]), reads=[psY], writes=[y])
                    if dh == 1:
                        r0 = b * 512 + t4 * 128
                        s.dma("sp", Dr["ys_d"][r0:r0 + 128, :], y[:], reads=[y], writes=[DR["ys_d"]])

                load_block(0)
                load_w2(0)
                load_block(1)
                load_w2(1)
                for b in range(NB):
                    if 1 <= b < NB - 1:
                        load_block(b + 1)
                    transposes(b)
                    for i in range(8):
                        stage1_fc(b, i)
                        if b >= 1:
                            stage2_grp(b - 1, i)
                    if 1 <= b < NB - 1:
                        load_w2(b + 1)
                for i in range(8):
                    stage2_grp(NB - 1, i)
                s.barrier()
            with ExitStack() as p3:
                xt = [s.sb([128, D], F32, "xt", p3) for _ in range(2)]
                acc = [s.sb([128, D], F32, "acc3", p3) for _ in range(2)]
                Yk = [s.sb([128, D], F32, "Yk", p3) for _ in range(4)]
                junk = s.sb([128, D], F32, "junk", p3)
                ss = [s.sb([128, 1], F32, "ss", p3) for _ in range(2)]
                if last:
                    fg = s.sb([128, D], F32, "fg", p3)
                    s.dma("sp", fg[:], I["final_g"].broadcast_to([128, D]), writes=[fg])
                nk = 0
                for tt in range(NTT):
                    q = tt // NT
                    grow = slice(tt * 128, (tt + 1) * 128)
                    x, ac, sq = xt[tt % 2], acc[tt % 2], ss[tt % 2]
                    s.dma("sp", x[:], Dr["xres"][grow, :], reads=[DR["xres"]], writes=[x])
                    pa = [self.P[(tt % 2) * 2], self.P[(tt % 2) * 2 + 1]]
                    for half in range(2):
                        self.mm(pa[half][:, :], GT[:, tt, :], b2[:, half * 512:(half + 1) * 512], True, True, [GT, b2], [pa[half]])
                    for k in range(4):
                        yk = Yk[nk % 4]
                        nk += 1
                        s.idma(yk[:, :], Dr["ys_d"][:, :], in_off=IDX4[:, tt, k:k + 1], reads=[IDX4, DR["ys_d"]], writes=[yk])
                        for half in range(2):
                            hc = slice(half * 512, (half + 1) * 512)
                            in1 = pa[half][:, :] if k == 0 else ac[:, hc]
                            rd = [yk, G4] + ([pa[half]] if k == 0 else [ac])
                            s.op("dve", lambda hh, in1=in1, hc=hc, yk=yk, k=k: hh.scalar_tensor_tensor(out=ac[:, hc], in0=yk[:, hc], scalar=G4[:, tt, k:k + 1], in1=in1, op0=ALU.mult, op1=ALU.add), reads=rd, writes=[ac])
                    s.op("pool", lambda hh: hh.tensor_tensor(out=ac[:], in0=ac[:], in1=g2[q][:], op=ALU.mult), reads=[ac, g2[q]], writes=[ac])
                    s.op("dve", lambda hh: hh.tensor_tensor(out=x[:], in0=x[:], in1=ac[:], op=ALU.add), reads=[x, ac], writes=[x])
                    if last:
                        self.rstd_of((x, x[:]), (junk, junk[:]), sq)
                        s.op("dve", lambda hh: hh.scalar_tensor_tensor(out=x[:], in0=x[:], scalar=sq[:, 0:1], in1=fg[:], op0=ALU.mult, op1=ALU.mult), reads=[x, sq, fg], writes=[x])
                        s.dma("sp", self.out[grow, :], x[:], reads=[x], writes=[R("out")])
                    else:
                        s.dma("sp", Dr["xres"][grow, :], x[:], reads=[x], writes=[DR["xres"]])
                s.barrier()


_CONSTS = None


def run(inputs, dbg=(), layers=L_DEPTH, nseq=NSEQ, stop_after=None, cores=NCORES, trace=False, only=None):
    global _CONSTS
    if _CONSTS is None:
        _CONSTS = host_consts()
    inp = {k: np.asarray(v) for k, v in inputs.items()}
    k = K(dbg=dbg, layers=layers, nseq=nseq, stop_after=stop_after, only=only)
    nc = k.build()
    in_maps = []
    for c in range(cores):
        m = host_inputs(inp, c)
        m.update(_CONSTS)
        in_maps.append(m)
    res = run_bass_kernel_spmd(nc, in_maps, core_ids=list(range(cores)), trace=trace)
    return res


def kernel(**inputs):
    res = run(inputs)
    out = np.concatenate([np.asarray(r["out"]).reshape(NSEQ, S, D) for r in res.results], axis=0)
    return out.astype(np.float32)
```

```python
import numpy as np
import ml_dtypes
from contextlib import ExitStack
import concourse.bass as bass
import concourse.mybir as mybir
from concourse.bass_utils import run_bass_kernel_spmd

F32 = mybir.dt.float32
BF16 = mybir.dt.bfloat16
I32 = mybir.dt.int32
AF = mybir.ActivationFunctionType
ALU = mybir.AluOpType
AX = mybir.AxisListType
NDSEM = 8


class R:
    __slots__ = ("name", "lw", "rd")

    def __init__(self, name=""):
        self.name = name
        self.lw = {}
        self.rd = {}


class T(R):
    __slots__ = ("t",)

    def __init__(self, t, name=""):
        super().__init__(name)
        self.t = t

    def __getitem__(self, k):
        return self.t[k]


class Eng:
    def __init__(self, name, h, sem, dsems):
        self.name = name
        self.h = h
        self.sem = sem
        self.count = 0
        self.known = {}
        self.dsems = dsems
        self.ndma = 0


class Sched:
    def __init__(self, nc, stack):
        self.nc = nc
        self.stack = stack
        self.E = {}
        for name, h, nd in (("pe", nc.tensor, 0), ("act", nc.scalar, NDSEM), ("dve", nc.vector, 0),
                            ("pool", nc.gpsimd, NDSEM), ("sp", nc.sync, NDSEM)):
            sem = stack.enter_context(nc.semaphore("s_" + name))
            ds = [stack.enter_context(nc.semaphore("d_%s%d" % (name, i))) for i in range(nd)]
            self.E[name] = Eng(name, h, sem, ds)
        self.dma_tokens = []
        self.nuniq = 0

    def sb(self, shape, dt, name=None, stack=None):
        self.nuniq += 1
        name = (name or "t") + "_%d" % self.nuniq
        t = (stack or self.stack).enter_context(self.nc.sbuf_tensor(name, list(shape), dt))
        return T(t, name)

    def ps(self, shape, dt=F32, name=None, stack=None):
        self.nuniq += 1
        name = (name or "p") + "_%d" % self.nuniq
        t = (stack or self.stack).enter_context(self.nc.psum_tensor(name, list(shape), dt))
        return T(t, name)

    def _wait(self, eng, tok):
        sem, val = tok
        if eng.known.get(sem, 0) >= val:
            return
        eng.h.wait_ge(sem, val)
        eng.known[sem] = val

    def _deps(self, eng, reads, writes):
        deps = []
        for r in reads:
            deps.extend(r.lw.items())
        for w in writes:
            deps.extend(w.lw.items())
            deps.extend(w.rd.items())
        for tok in deps:
            if tok[0] is eng.sem:
                if eng.name == "pe":
                    continue
                if tok[1] > eng.count:
                    continue
            self._wait(eng, tok)

    def _commit(self, tok, reads, writes):
        sem, val = tok
        for r in reads:
            if r.rd.get(sem, 0) < val:
                r.rd[sem] = val
        for w in writes:
            if w.lw.get(sem, 0) < val:
                w.lw[sem] = val

    def op(self, engname, fn, reads=(), writes=(), sig=True):
        eng = self.E[engname]
        self._deps(eng, reads, writes)
        ins = fn(eng.h)
        if sig:
            eng.count += 1
            ins.then_inc(eng.sem, 1)
            tok = (eng.sem, eng.count)
        else:
            tok = (eng.sem, eng.count + 1)
        self._commit(tok, reads, writes)
        return tok

    def dma(self, qname, out, in_, reads=(), writes=(), **kw):
        q = self.E[qname]
        i = q.ndma
        q.ndma += 1
        sem = q.dsems[i % NDSEM]
        val = 16 * (i // NDSEM + 1)
        if i >= NDSEM:
            self._wait(q, (sem, val - 16))
        self._deps(q, reads, writes)
        q.h.dma_start(out=out, in_=in_, **kw).then_inc(sem, 16)
        tok = (sem, val)
        self._commit(tok, reads, writes)
        self.dma_tokens.append(tok)
        return tok

    def idma(self, out, in_, out_off=None, in_off=None, reads=(), writes=(), bounds=None):
        q = self.E["pool"]
        i = q.ndma
        q.ndma += 1
        sem = q.dsems[i % NDSEM]
        val = 16 * (i // NDSEM + 1)
        if i >= NDSEM:
            self._wait(q, (sem, val - 16))
        self._deps(q, reads, writes)
        oo = bass.IndirectOffsetOnAxis(ap=out_off, axis=0) if out_off is not None else None
        io = bass.IndirectOffsetOnAxis(ap=in_off, axis=0) if in_off is not None else None
        if bounds is None:
            q.h.indirect_dma_start(out=out, out_offset=oo, in_=in_, in_offset=io).then_inc(sem, 16)
        else:
            if getattr(self, "bound_reg", None) is None:
                self.bound_reg = q.h.alloc_register("bnd")
                q.h.reg_mov(self.bound_reg, 65535)
            q.h.indirect_dma_start(out=out, out_offset=oo, in_=in_, in_offset=io, bounds_check=self.bound_reg, oob_is_err=False).then_inc(sem, 16)
        tok = (sem, val)
        self._commit(tok, reads, writes)
        self.dma_tokens.append(tok)
        return tok

    def barrier(self):
        toks = []
        for e in self.E.values():
            if e.count > 0:
                toks.append((e.sem, e.count))
            for j, s in enumerate(e.dsems):
                n = (e.ndma - j + NDSEM - 1) // NDSEM
                if n > 0:
                    toks.append((s, 16 * n))
        for e in self.E.values():
            for tok in toks:
                if tok[0] is e.sem:
                    continue
                self._wait(e, tok)

    def finish(self):
        sp = self.E["sp"]
        for e in self.E.values():
            for j, s in enumerate(e.dsems):
                n = (e.ndma - j + NDSEM - 1) // NDSEM
                if n > 0:
                    self._wait(sp, (s, 16 * n))


NCORES = 8
L_DEPTH = 2
D = 1024
S = 2048
NSEQ = 2
NT = S // 128
EPS = 1e-5
INW = 6680
SCALE = 0.125
NEG = -30000.0
NE = 32
SLOT_COL = ([0 + 64 * i for i in range(8)] + [512 + 64 * i for i in range(2)] + [768 + 64 * i for i in range(8)]
            + [1280 + 64 * i for i in range(8)] + [2304 + 64 * i for i in range(8)] + [2816 + 64 * i for i in range(2)]
            + [2944 + 64 * i for i in range(2)] + [3072 + 64 * i for i in range(2)] + [3328 + 64 * i for i in range(2)])
NSLOT = len(SLOT_COL)
SL_QA, SL_KA, SL_QB, SL_KB, SL_QC, SL_KCM, SL_VCM, SL_KSL, SL_KWN = 0, 8, 10, 18, 26, 34, 36, 38, 40
C_VA, C_VB, C_VSL, C_VWN, C_GN, C_GM = 640, 1792, 3200, 3456, 3584, 3608


def host_consts():
    bf = ml_dtypes.bfloat16
    c = {}
    t = np.arange(S)
    a_t, b_t = (t // 128).astype(np.float32), (t % 128).astype(np.float32)
    aug = np.zeros((NSLOT, 4, S), np.float32)
    kaug = np.stack([a_t, b_t, np.ones(S, np.float32), np.ones(S, np.float32)])

    def qaug(slope):
        return np.stack([np.full(S, 1024.0 * slope, np.float32), np.full(S, 8.0 * slope, np.float32),
                         -1024.0 * slope * a_t, -8.0 * slope * b_t])
    for i in range(8):
        aug[SL_QA + i] = qaug(2.0 ** -(i + 1))
        aug[SL_QC + i] = qaug(2.0 ** -(i + 1))
        aug[SL_QB + i] = qaug(2.0 ** (-2.0 * (i // 2 + 1)))
        aug[SL_KB + i] = kaug
    for i in range(2):
        aug[SL_KA + i] = kaug
        aug[SL_KSL + i] = kaug
        aug[SL_KWN + i] = kaug
    c["aug"] = aug.astype(bf)
    c["ident"] = np.eye(128, dtype=np.float32).astype(bf)
    c["ident32"] = np.eye(128, dtype=np.float32)
    sk = np.arange(128)[:, None]
    tq = np.arange(128)[None, :]
    c["mdiag"] = np.where(tq >= sk, 0.0, NEG).astype(bf)
    c["medge"] = np.where(tq < sk, 0.0, NEG).astype(bf)
    cc = np.arange(128)[:, None]
    c["cmaskT"] = np.where((16 * cc + 31 <= t[None, :]) & (cc < 127), 0.0, NEG).astype(bf)
    nb = np.arange(32)
    c["eblk"] = (t[None, :] // 64 == nb[:, None]).astype(np.float32).astype(bf)
    cur = t // 64
    forced = (nb[None, :] == 0) | (nb[None, :] == cur[:, None]) | (nb[None, :] == cur[:, None] - 1)
    causal = nb[None, :] <= cur[:, None]
    A = np.where(forced, 1e9 + 1e6 * nb[None, :], np.where(causal, 0.0, -1e9 - 1e6 * nb[None, :]))
    c["atab"] = A.astype(np.float32)
    cstart = np.arange(127) * 16
    sstart = nb * 64
    ov = np.clip(np.minimum(cstart[:, None] + 32, sstart[None, :] + 64) - np.maximum(cstart[:, None], sstart[None, :]), 0, None) / 32.0
    ovp = np.zeros((128, 32), np.float32)
    ovp[:127] = ov
    c["overlap"] = ovp.astype(bf)
    c["ltri"] = (np.arange(128)[:, None] < np.arange(128)[None, :]).astype(np.float32).astype(bf)
    c["ones128"] = np.ones((128, 128), np.float32).astype(bf)
    c["b512"] = np.broadcast_to((512.0 * np.arange(64, dtype=np.float32))[None, :], (128, 64)).copy()
    c["rowiota"] = (np.arange(8, dtype=np.float32)[None, :] * 128 + np.arange(128, dtype=np.float32)[:, None]).copy()
    c["piota"] = np.arange(128, dtype=np.float32).reshape(128, 1).copy()
    return c


def host_inputs(inp, core):
    b0 = core * NSEQ
    m = {}
    m["x"] = np.ascontiguousarray(inp["x"][b0:b0 + NSEQ].reshape(NSEQ * S, D))
    m["cT"] = np.ascontiguousarray(inp["c"][b0:b0 + NSEQ].T)
    for k in ("mod_w", "mod_b", "norm1_g", "norm2_g", "w_in", "b_in", "sinks", "diff_subln_g", "cmp_w1", "cmp_w2",
              "cmp_b2", "w_branch", "w_out", "router_w", "router_b", "exp_b2"):
        m[k] = inp[k]
    m["final_g"] = inp["final_g"].reshape(1, D)
    m["b_inT"] = np.ascontiguousarray(np.stack([inp["b_in"][:, SLOT_COL[2 * i]:SLOT_COL[2 * i] + 128] for i in range(NSLOT // 2)], axis=2))
    m["diff_lambda"] = inp["diff_lambda"].reshape(L_DEPTH, 256)
    m["cmp_posT"] = np.ascontiguousarray(inp["cmp_pos"].transpose(0, 1, 3, 2))
    m["cmp_b1T"] = np.ascontiguousarray(inp["cmp_b1"].reshape(L_DEPTH, 2, 2, 128).transpose(0, 1, 3, 2))
    m["cmp_b2T"] = np.ascontiguousarray(inp["cmp_b2"].reshape(L_DEPTH, 2, 64, 1))
    m["exp_b1E"] = np.ascontiguousarray(inp["exp_b1"].reshape(L_DEPTH, NE, 2, 128, 8).transpose(0, 1, 3, 2, 4)).reshape(L_DEPTH * NE * 128, 16)
    m["exp_w1"] = inp["exp_w1"].reshape(L_DEPTH * NE * 256, 4 * 2 * D)
    m["exp_w2"] = inp["exp_w2"].reshape(L_DEPTH * NE * 128, 8 * D)
    return m


IN_SHAPES = {
    "x": ([NSEQ * S, D], F32), "cT": ([D, NSEQ], F32), "mod_w": ([L_DEPTH, D, 6 * D], F32), "mod_b": ([L_DEPTH, 6 * D], F32),
    "norm1_g": ([L_DEPTH, D], F32), "norm2_g": ([L_DEPTH, D], F32), "w_in": ([L_DEPTH, D, INW], F32), "b_in": ([L_DEPTH, INW], F32),
    "sinks": ([L_DEPTH, 8], F32), "diff_subln_g": ([L_DEPTH, 128], F32), "cmp_w1": ([L_DEPTH, 2, 2048, 256], F32),
    "cmp_w2": ([L_DEPTH, 2, 256, 64], F32), "cmp_b2": ([L_DEPTH, 2, 64], F32), "w_branch": ([L_DEPTH, 3, 512, D], F32),
    "w_out": ([L_DEPTH, D, D], F32), "router_w": ([L_DEPTH, D, NE], F32), "router_b": ([L_DEPTH, NE], F32),
    "exp_w1": ([L_DEPTH * NE * 256, 8 * D], F32), "exp_w2": ([L_DEPTH * NE * 128, 8 * D], F32), "exp_b2": ([L_DEPTH, NE, D], F32),
    "final_g": ([1, D], F32), "b_inT": ([L_DEPTH, 128, NSLOT // 2], F32), "diff_lambda": ([L_DEPTH, 256], F32),
    "cmp_posT": ([L_DEPTH, 2, 64, 32], F32), "cmp_b1T": ([L_DEPTH, 2, 128, 2], F32), "cmp_b2T": ([L_DEPTH, 2, 64, 1], F32),
    "exp_b1E": ([L_DEPTH * NE * 128, 16], F32),
    "ltri": ([128, 128], BF16), "ones128": ([128, 128], BF16), "b512": ([128, 64], F32), "rowiota": ([128, 8], F32), "piota": ([128, 1], F32),
    "aug": ([NSLOT, 4, S], BF16), "ident": ([128, 128], BF16), "ident32": ([128, 128], F32), "mdiag": ([128, 128], BF16),
    "medge": ([128, 128], BF16), "cmaskT": ([128, S], BF16), "eblk": ([32, S], BF16), "atab": ([S, 32], F32),
    "overlap": ([128, 32], BF16),
}


def bcast_rows(ap1d_row, nparts):
    return ap1d_row.broadcast_to([nparts, ap1d_row.shape[-1]])


class K:
    def __init__(self, dbg=(), layers=L_DEPTH, nseq=NSEQ, stop_after=None, only=None):
        self.dbg = set(dbg)
        self.only = only
        self.layers = layers
        self.nseq = nseq
        self.stop_after = stop_after
        nc = self.nc = bass.Bass("TRN2", target_bir_lowering=False)
        self.I = {k: nc.dram_tensor(k, list(sh), dt, kind="ExternalInput").ap() for k, (sh, dt) in IN_SHAPES.items()}
        self.out = nc.dram_tensor("out", [NSEQ * S, D], F32, kind="ExternalOutput").ap()
        self.Dr = {}
        self.DR = {}

    def dram(self, name, shape, dt):
        kind = "ExternalOutput" if name in self.dbg else "Internal"
        self.Dr[name] = self.nc.dram_tensor(name, list(shape), dt, kind=kind).ap()
        self.DR[name] = R(name)
        return self.Dr[name]

    def mm(self, out, lhsT, rhs, start, stop, reads, writes, sig=True):
        return self.s.op("pe", lambda h: h.matmul(out, lhsT=lhsT, rhs=rhs, start=start, stop=stop), reads=reads, writes=writes, sig=sig)

    def tr(self, out, in_, ident, reads, writes, sig=True):
        return self.s.op("pe", lambda h: h.transpose(out=out, in_=in_, identity=ident), reads=reads, writes=writes, sig=sig)

    def rstd_of(self, xt, junk, ss, n=D):
        s = self.s
        s.op("act", lambda h: h.activation(out=junk[1], in_=xt[1], func=AF.Square, accum_out=ss[:]), reads=[xt[0]], writes=[junk[0], ss])
        s.op("dve", lambda h: h.tensor_scalar(out=ss[:], in0=ss[:], scalar1=1.0 / n, scalar2=EPS, op0=ALU.mult, op1=ALU.add), reads=[ss], writes=[ss])
        s.op("act", lambda h: h.sqrt(out=ss[:], in_=ss[:]), reads=[ss], writes=[ss])
        s.op("dve", lambda h: h.reciprocal(out=ss[:], in_=ss[:]), reads=[ss], writes=[ss])

    def build(self):
        nc = self.nc
        with ExitStack() as st:
            s = self.s = Sched(nc, st)
            self.P = [s.ps([128, 512], F32, "bank%d" % i) for i in range(8)]
            self.ident = s.sb([128, 128], BF16, "ident")
            self.ident32 = s.sb([128, 128], F32, "ident32")
            s.dma("sp", self.ident[:], self.I["ident"], writes=[self.ident])
            s.dma("sp", self.ident32[:], self.I["ident32"], writes=[self.ident32])
            self.dram("mod_d", [L_DEPTH, NSEQ, 6 * D], F32)
            self.dram("xres", [NSEQ * S, D], F32)
            self.dram("QT_d", [NSLOT, 64, S], BF16)
            self.dram("VA_d", [S, 2, 65], BF16)
            self.dram("VB_d", [S, 4, 129], BF16)
            self.dram("VSL_d", [S, 2, 65], BF16)
            self.dram("VWN_d", [S, 2, 65], BF16)
            self.dram("GN_d", [S, 24], F32)
            self.dram("GM_d", [S, 3072], F32)
            self.dram("KC_d", [2, 64, 128], BF16)
            self.dram("VC_d", [2, 128, 97], BF16)
            self.dram("O_d", [S, 1536], BF16)
            self.ntt = self.nseq * NT
            self.nblk = self.ntt + NE
            self.dram("xs_d", [self.nblk * 512, D], BF16)
            self.dram("ys_d", [self.nblk * 512, D], F32)
            self.dram("dbg_d", [128, 4096], F32)
            with ExitStack() as zs:
                zt = s.sb([128, 8192], BF16, "zt", zs)
                s.op("pool", lambda h: h.memset(zt[:], 0.0), writes=[zt])
                xv = self.Dr["xs_d"].rearrange("(a p r) d -> a p (r d)", p=128, r=8)
                for a in range(xv.shape[0]):
                    s.dma("sp", xv[a], zt[:], reads=[zt], writes=[self.DR["xs_d"]])
                s.barrier()
            try:
                self.body()
            except StopIteration:
                pass
            s.barrier()
            s.finish()
        return nc

    def phase_end(self, name):
        self.s.barrier()
        if self.stop_after == name:
            raise StopIteration

    def body(self):
        for l in range(self.layers):
            self.runp("mod%d" % l, self.phase_mod, l)
            for q in range(self.nseq):
                for nm, fn in (("inproj", self.phase_inproj), ("cmp", self.phase_cmp), ("swa", self.phase_swa), ("diff", self.phase_diff),
                               ("nsa", self.phase_nsa), ("merge", self.phase_merge)):
                    self.runp("%s%d_%d" % (nm, l, q), fn, l, q)
            self.runp("moe%d" % l, self.phase_moe2, l)

    def runp(self, name, fn, *args):
        if self.only is None or name in self.only:
            fn(*args)
        self.phase_end(name)

    def phase_mod(self, l):
        s, I = self.s, self.I
        with ExitStack() as ps:
            cT = s.sb([128, 8, NSEQ], F32, "cT", ps)
            cs = s.sb([128, 8, NSEQ], F32, "cs", ps)
            modb = s.sb([NSEQ, 6 * D], F32, "modb", ps)
            mods = s.sb([NSEQ, 6 * D], F32, "mods", ps)
            wb = [s.sb([128, 8, 512], F32, "modw", ps) for _ in range(2)]
            s.dma("sp", cT[:], I["cT"].rearrange("(kc p) b -> p kc b", p=128), writes=[cT])
            s.dma("sp", modb[:], I["mod_b"][l:l + 1, :].broadcast_to([NSEQ, 6 * D]), writes=[modb])
            s.op("act", lambda h: h.activation(out=cs[:], in_=cT[:], func=AF.Silu), reads=[cT], writes=[cs])
            wsrc = I["mod_w"][l].rearrange("(kc p) n -> p kc n", p=128)
            for cg in range(12):
                w = wb[cg % 2]
                s.dma("sp", w[:], wsrc[:, :, cg * 512:(cg + 1) * 512], writes=[w])
                pm = self.P[cg % 2]
                for kc in range(8):
                    self.mm(pm[0:NSEQ, :], cs[:, kc, :], w[:, kc, :], kc == 0, kc == 7, [cs, w], [pm], sig=(kc == 7))
                s.op("dve", lambda h: h.tensor_tensor(out=mods[:, cg * 512:(cg + 1) * 512], in0=pm[0:NSEQ, :], in1=modb[:, cg * 512:(cg + 1) * 512], op=ALU.add),
                     reads=[pm, modb], writes=[mods])
            for seg in (1, 4):
                s.op("dve", lambda h: h.tensor_scalar_add(out=mods[:, seg * D:(seg + 1) * D], in0=mods[:, seg * D:(seg + 1) * D], scalar1=1.0), reads=[mods], writes=[mods])
            s.dma("sp", self.Dr["mod_d"][l], mods[:], reads=[mods], writes=[self.DR["mod_d"]])

    def mod_bc(self, l, q, seg, tile):
        src = self.Dr["mod_d"][l, q:q + 1, seg * D:(seg + 1) * D].broadcast_to([128, D])
        self.s.dma("sp", tile[:], src, reads=[self.DR["mod_d"]], writes=[tile])

    def xsrc(self, l, q):
        return (self.I["x"] if l == 0 else self.Dr["xres"]), ([] if l == 0 else [self.DR["xres"]])

    def norm_tiles(self, ps, l, q, gname, seg_sc, seg_sh):
        s, I = self.s, self.I
        gsc = s.sb([128, D], F32, "gsc", ps)
        sh = s.sb([128, D], F32, "sh", ps)
        gt = s.sb([128, D], F32, "gt", ps)
        s.dma("sp", gt[:], I[gname][l:l + 1, :].broadcast_to([128, D]), writes=[gt])
        self.mod_bc(l, q, seg_sc, gsc)
        self.mod_bc(l, q, seg_sh, sh)
        s.op("dve", lambda h: h.tensor_tensor(out=gsc[:], in0=gsc[:], in1=gt[:], op=ALU.mult), reads=[gsc, gt], writes=[gsc])
        return gsc, sh

    def phase_inproj(self, l, q):
        s, I, Dr, DR = self.s, self.I, self.Dr, self.DR
        xsrc, xres_r = self.xsrc(l, q)
        with ExitStack() as ps:
            gsc, sh = self.norm_tiles(ps, l, q, "norm1_g", 1, 0)
            hT = s.sb([128, 8, S], BF16, "hT", ps)
            xt = [s.sb([128, D], F32, "xt", ps) for _ in range(2)]
            junk = s.sb([128, D], F32, "junk", ps)
            hb = [s.sb([128, D], BF16, "hb", ps) for _ in range(2)]
            ss = [s.sb([128, 1], F32, "ss", ps) for _ in range(2)]
            for tt in range(NT):
                x, h, sq = xt[tt % 2], hb[tt % 2], ss[tt % 2]
                r0 = q * S + tt * 128
                s.dma("sp", x[:], xsrc[r0:r0 + 128, :], reads=xres_r, writes=[x])
                self.rstd_of((x, x[:]), (junk, junk[:]), sq)
                s.op("dve", lambda hh: hh.scalar_tensor_tensor(out=x[:], in0=x[:], scalar=sq[:, 0:1], in1=gsc[:], op0=ALU.mult, op1=ALU.mult), reads=[x, sq, gsc], writes=[x])
                s.op("pool", lambda hh: hh.tensor_tensor(out=h[:], in0=x[:], in1=sh[:], op=ALU.add), reads=[x, sh], writes=[h])
                pt = self.P[tt % 2]
                ptb = pt[:].bitcast(BF16)
                for kc in range(8):
                    self.tr(ptb[:, kc * 128:(kc + 1) * 128], h[:, kc * 128:(kc + 1) * 128], self.ident[:], [h, self.ident], [pt], sig=(kc == 7))
                s.op("act", lambda hh: hh.copy(out=hT[:, :, tt * 128:(tt + 1) * 128], in_=ptb.rearrange("p (k t) -> p k t", k=8)), reads=[pt], writes=[hT])
            binT = s.sb([128, NSLOT // 2], F32, "binT", ps)
            s.dma("sp", binT[:], I["b_inT"][l], writes=[binT])
            wsrc = I["w_in"][l].rearrange("(kc p) n -> p kc n", p=128)
            wbuf = [s.sb([128, 8, 512], BF16, "wblk", ps) for _ in range(2)]
            stg = [s.sb([128, S], BF16, "stg", ps) for _ in range(2)]
            groups = [(SL_QA, 8), (SL_KA, 2), (SL_QB, 8), (SL_KB, 8), (SL_QC, 8), (SL_KCM, 4), (SL_KSL, 2), (SL_KWN, 2)]
            nw = 0
            nmm = 0
            for (s0, ns) in groups:
                w = wbuf[nw % 2]
                nw += 1
                c0 = SLOT_COL[s0]
                s.dma("pool", w[:, :, 0:ns * 64], wsrc[:, :, c0:c0 + ns * 64], writes=[w])
                for si in range(0, ns, 2):
                    slot = s0 + si
                    pr_ = slot // 2
                    sg = stg[pr_ % 2]
                    for tg in range(4):
                        pm = self.P[2 + nmm % 4]
                        nmm += 1
                        for kc in range(8):
                            self.mm(pm[:, :], w[:, kc, si * 64:(si + 2) * 64], hT[:, kc, tg * 512:(tg + 1) * 512], kc == 0, kc == 7, [w, hT], [pm], sig=(kc == 7))
                        s.op("act", lambda hh: hh.activation(out=sg[:, tg * 512:(tg + 1) * 512], in_=pm[:, :], func=AF.Identity, bias=binT[:, pr_:pr_ + 1]),
                             reads=[pm, binT], writes=[sg])
                    s.dma("sp", Dr["QT_d"][slot], sg[0:64, :], reads=[sg], writes=[DR["QT_d"]])
                    s.dma("sp", Dr["QT_d"][slot + 1], sg[64:128, :], reads=[sg], writes=[DR["QT_d"]])
            binb = s.sb([128, INW], F32, "binb", ps)
            s.dma("sp", binb[:], I["b_in"][l:l + 1, :].broadcast_to([128, INW]), writes=[binb])
            vt = {}
            for nm, nh, dv in (("VA_d", 2, 64), ("VB_d", 4, 128), ("VSL_d", 2, 64), ("VWN_d", 2, 64)):
                vt[nm] = [s.sb([128, nh, dv + 1], BF16, "vt", ps) for _ in range(2)]
                for v in vt[nm]:
                    s.op("pool", lambda hh: hh.memset(v[:, :, dv:dv + 1], 1.0), writes=[v])
            gnt = [s.sb([128, 24], F32, "gnt", ps) for _ in range(2)]
            gmt = [s.sb([128, 512], F32, "gmt", ps) for _ in range(2)]
            blocks = [("VA_d", C_VA, 128, 2, 64), ("VB_d", C_VB, 512, 4, 128), ("VSL_d", C_VSL, 128, 2, 64), ("VWN_d", C_VWN, 128, 2, 64),
                      ("GN_d", C_GN, 24, 0, 0)] + [("GM_d", C_GM + 512 * i, 512, i, 0) for i in range(6)]
            for (nm, c0, ncol, nh, dv) in blocks:
                w = wbuf[nw % 2]
                nw += 1
                s.dma("pool", w[:, :, 0:ncol], wsrc[:, :, c0:c0 + ncol], writes=[w])
                for tt in range(NT):
                    pm = self.P[2 + nmm % 4]
                    nmm += 1
                    for kc in range(8):
                        self.mm(pm[:, 0:ncol], hT[:, kc, tt * 128:(tt + 1) * 128], w[:, kc, 0:ncol], kc == 0, kc == 7, [w, hT], [pm], sig=(kc == 7))
                    rows = slice(tt * 128, (tt + 1) * 128)
                    if nm == "GN_d":
                        g = gnt[tt % 2]
                        s.op("dve", lambda hh: hh.tensor_tensor(out=g[:], in0=pm[:, 0:24], in1=binb[:, c0:c0 + 24], op=ALU.add), reads=[pm, binb], writes=[g])
                        s.op("act", lambda hh: hh.activation(out=g[:], in_=g[:], func=AF.Sigmoid), reads=[g], writes=[g])
                        s.dma("sp", Dr["GN_d"][rows, :], g[:], reads=[g], writes=[DR["GN_d"]])
                    elif nm == "GM_d":
                        g = gmt[tt % 2]
                        s.op("dve", lambda hh: hh.tensor_tensor(out=g[:], in0=pm[:, :], in1=binb[:, c0:c0 + 512], op=ALU.add), reads=[pm, binb], writes=[g])
                        s.op("act", lambda hh: hh.activation(out=g[:], in_=g[:], func=AF.Sigmoid), reads=[g], writes=[g])
                        s.dma("sp", Dr["GM_d"][rows, nh * 512:(nh + 1) * 512], g[:], reads=[g], writes=[DR["GM_d"]])
                    else:
                        v = vt[nm][tt % 2]
                        s.op("dve", lambda hh: hh.tensor_tensor(out=v[:, :, 0:dv], in0=pm[:, 0:ncol].rearrange("p (h d) -> p h d", d=dv),
                                                               in1=binb[:, c0:c0 + ncol].rearrange("p (h d) -> p h d", d=dv), op=ALU.add), reads=[pm, binb], writes=[v])
                        s.dma("sp", Dr[nm][rows], v[:], reads=[v], writes=[DR[nm]])

    def phase_cmp(self, l, q):
        s, I, Dr, DR = self.s, self.I, self.Dr, self.DR
        with ExitStack() as ps:
            ovl = self.load_const(ps, "overlap", [128, 32], BF16)
            w1 = s.sb([64, 32, 256], BF16, "cw1", ps)
            w2 = s.sb([128, 2, 64], BF16, "cw2", ps)
            posT = s.sb([64, 32], F32, "posT", ps)
            posb = s.sb([64, 32], BF16, "posb", ps)
            b1T = s.sb([128, 2], F32, "b1T", ps)
            bias = s.sb([128, 2], F32, "cbias", ps)
            b2T = s.sb([64, 1], F32, "b2T", ps)
            b2b = s.sb([128, 64], F32, "b2b", ps)
            xT = s.sb([64, S], BF16, "cxT", ps)
            hid = s.sb([128, 2, 128], BF16, "hid", ps)
            y = s.sb([128, 128], F32, "cy", ps)
            u = s.sb([128, 128], F32, "cu", ps)
            kct = s.sb([64, 128], BF16, "kct", ps)
            vct = s.sb([128, 97], BF16, "vct", ps)
            s.op("pool", lambda h: h.memset(kct[:], 0.0), writes=[kct])
            s.op("pool", lambda h: h.memset(vct[:], 0.0), writes=[vct])
            for which in range(2):
                s.dma("pool", w1[:], I["cmp_w1"][l, which].rearrange("(l d) f -> d l f", d=64), writes=[w1])
                s.dma("pool", w2[:], I["cmp_w2"][l, which].rearrange("(c p) d -> p c d", p=128), writes=[w2])
                s.dma("sp", posT[:], I["cmp_posT"][l, which], writes=[posT])
                s.dma("sp", b1T[:], I["cmp_b1T"][l, which], writes=[b1T])
                s.dma("sp", b2T[:], I["cmp_b2T"][l, which], writes=[b2T])
                s.dma("sp", b2b[:], I["cmp_b2"][l, which:which + 1, :].broadcast_to([128, 64]), writes=[b2b])
                s.op("act", lambda h: h.copy(out=posb[:], in_=posT[:]), reads=[posT], writes=[posb])
                for ch in range(2):
                    pm = self.P[ch]
                    for li in range(32):
                        self.mm(pm[:, 0:1], w1[:, li, ch * 128:(ch + 1) * 128], posb[:, li:li + 1], li == 0, li == 31, [w1, posb], [pm], sig=(li == 31))
                    s.op("dve", lambda h: h.tensor_tensor(out=bias[:, ch:ch + 1], in0=pm[:, 0:1], in1=b1T[:, ch:ch + 1], op=ALU.add), reads=[pm, b1T], writes=[bias])
                for hk in range(2):
                    s.dma("sp", xT[:], Dr["QT_d"][SL_KCM + which * 2 + hk], reads=[DR["QT_d"]], writes=[xT])
                    for ch in range(2):
                        pm = self.P[2 + ch]
                        for li in range(32):
                            self.mm(pm[:, 0:127], w1[:, li, ch * 128:(ch + 1) * 128], xT[:, li:li + 16 * 126 + 1:16], li == 0, li == 31, [w1, xT], [pm], sig=(li == 31))
                        s.op("act", lambda h: h.activation(out=y[:, 0:127], in_=pm[:, 0:127], func=AF.Identity, bias=bias[:, ch:ch + 1]), reads=[pm, bias], writes=[y])
                        s.op("dve", lambda h: h.tensor_tensor(out=u[:, 0:127], in0=y[:, 0:127], in1=y[:, 0:127], op=ALU.mult), reads=[y], writes=[u])
                        s.op("dve", lambda h: h.tensor_scalar(out=u[:, 0:127], in0=u[:, 0:127], scalar1=0.044715, scalar2=1.0, op0=ALU.mult, op1=ALU.add), reads=[u], writes=[u])
                        s.op("dve", lambda h: h.tensor_tensor(out=u[:, 0:127], in0=u[:, 0:127], in1=y[:, 0:127], op=ALU.mult), reads=[u, y], writes=[u])
                        s.op("act", lambda h: h.activation(out=u[:, 0:127], in_=u[:, 0:127], func=AF.Sigmoid, scale=1.5957691216057308), reads=[u], writes=[u])
                        s.op("dve", lambda h: h.tensor_tensor(out=hid[:, ch, 0:127], in0=u[:, 0:127], in1=y[:, 0:127], op=ALU.mult), reads=[u, y], writes=[hid])
                    pm2 = self.P[4 + hk]
                    if which == 0:
                        for ch in range(2):
                            self.mm(pm2[0:64, 0:127], w2[:, ch, :], hid[:, ch, 0:127], ch == 0, ch == 1, [w2, hid], [pm2], sig=(ch == 1))
                        s.op("act", lambda h: h.activation(out=kct[:, 0:127], in_=pm2[0:64, 0:127], func=AF.Identity, bias=b2T[:, 0:1]), reads=[pm2, b2T], writes=[kct])
                        s.dma("sp", Dr["KC_d"][hk], kct[:], reads=[kct], writes=[DR["KC_d"]])
                    else:
                        for ch in range(2):
                            self.mm(pm2[0:127, 0:64], hid[:, ch, 0:127], w2[:, ch, :], ch == 0, ch == 1, [w2, hid], [pm2], sig=(ch == 1))
                        s.op("dve", lambda h: h.tensor_tensor(out=vct[0:127, 0:64], in0=pm2[0:127, 0:64], in1=b2b[0:127, :], op=ALU.add), reads=[pm2, b2b], writes=[vct])
                        s.op("pool", lambda h: h.memset(vct[:, 64:65], 1.0), writes=[vct])
                        s.op("pool", lambda h: h.tensor_copy(out=vct[:, 65:97], in_=ovl[:]), reads=[ovl], writes=[vct])
                        s.dma("sp", Dr["VC_d"][hk], vct[:], reads=[vct], writes=[DR["VC_d"]])

    def load_heads(self, tile, slots, grouped):
        s = self.s
        for g, slot in enumerate(slots):
            dst0 = tile[0:64, g, :] if grouped else tile[0:64, :]
            dst1 = tile[64:68, g, :] if grouped else tile[64:68, :]
            s.dma("sp", dst0, self.Dr["QT_d"][slot], reads=[self.DR["QT_d"]], writes=[tile])
            s.dma("sp", dst1, self.I["aug"][slot], writes=[tile])

    def attn_stream(self, items, banks, pts):
        s = self.s
        n = len(items)

        def emit_qk(k):
            it = items[k]
            sc = banks[k % len(banks)]
            nq = len(it["qk"])
            for m, (outfn, lhsT, rhs, start, stop, reads) in enumerate(it["qk"]):
                self.mm(outfn(sc), lhsT, rhs, start, stop, reads, [sc], sig=(m == nq - 1))

        emit_qk(0)
        if n > 1:
            emit_qk(1)
        for k in range(n):
            if k + 2 < n:
                emit_qk(k + 2)
            it = items[k]
            sc = banks[k % len(banks)]
            pT = pts[k % len(pts)]
            kp, N = it["kp"], it["N"]
            s.op("act", lambda h: h.activation(out=pT[0:kp, 0:N], in_=sc[0:kp, 0:N], func=AF.Exp, scale=SCALE), reads=[sc], writes=[pT])
            it["pv"](pT)

    def load_const(self, ps, name, shape, dt):
        t = self.s.sb(shape, dt, name, ps)
        self.s.dma("sp", t[:], self.I[name], writes=[t])
        return t

    def phase_swa(self, l, q):
        s, I, Dr, DR = self.s, self.I, self.Dr, self.DR
        ident = self.ident
        with ExitStack() as ps:
            mdiag = self.load_const(ps, "mdiag", [128, 128], BF16)
            medge = self.load_const(ps, "medge", [128, 128], BF16)
            esink = s.sb([128, 8], F32, "esink", ps)
            s.dma("sp", esink[:], I["sinks"][l:l + 1, :].broadcast_to([128, 8]), writes=[esink])
            s.op("act", lambda h: h.activation(out=esink[:], in_=esink[:], func=AF.Exp), reads=[esink], writes=[esink])
            QG = s.sb([68, 4, S], BF16, "QG", ps)
            KT = s.sb([68, S], BF16, "KT", ps)
            V = s.sb([128, NT, 65], BF16, "V", ps)
            pts = [s.sb([128, 512], BF16, "pT", ps) for _ in range(3)]
            ot = [s.sb([128, 4, 64], BF16, "ot", ps) for _ in range(2)]
            den = [s.sb([128, 4], F32, "den", ps) for _ in range(2)]
            for hk in range(2):
                self.load_heads(QG, [SL_QA + hk * 4 + g for g in range(4)], True)
                self.load_heads(KT, [SL_KA + hk], False)
                s.dma("sp", V[:], Dr["VA_d"][:, hk, :].rearrange("(n p) e -> p n e", p=128), reads=[DR["VA_d"]], writes=[V])
                for j in range(NT):
                    acc = self.P[4 + j % 2]
                    tiles = [i for i in (j - 1, j) if i >= 0]
                    items = []
                    for idx, i in enumerate(tiles):
                        mask = mdiag if i == j else medge
                        o3 = lambda sc: sc[:, :].rearrange("p (g t) -> p g t", g=4)
                        qk = [(o3, KT[:, i * 128:(i + 1) * 128], QG[:, :, j * 128:(j + 1) * 128], True, False, [KT, QG]),
                              (o3, ident[:], mask[:].unsqueeze(1).broadcast_to([128, 4, 128]), False, True, [ident, mask])]

                        def pv(pT, i=i, idx=idx, acc=acc, last=len(tiles) - 1):
                            for g in range(4):
                                self.mm(acc[:, g * 65:(g + 1) * 65], pT[:, g * 128:(g + 1) * 128], V[:, i, :], idx == 0 and g == 0, idx == last, [pT, V], [acc], sig=(g == 3))
                        items.append(dict(kp=128, N=512, qk=qk, pv=pv))
                    self.attn_stream(items, self.P[0:3], pts)
                    a3 = acc[:, 0:260].rearrange("p (g e) -> p g e", e=65)
                    dn, o = den[j % 2], ot[j % 2]
                    s.op("dve", lambda h: h.tensor_tensor(out=dn[:].unsqueeze(2), in0=a3[:, :, 64:65], in1=esink[:, hk * 4:(hk + 1) * 4].unsqueeze(2), op=ALU.add), reads=[acc, esink], writes=[dn])
                    s.op("dve", lambda h: h.reciprocal(out=dn[:], in_=dn[:]), reads=[dn], writes=[dn])
                    s.op("dve", lambda h: h.tensor_tensor(out=o[:], in0=a3[:, :, 0:64], in1=dn[:].unsqueeze(2).broadcast_to([128, 4, 64]), op=ALU.mult), reads=[acc, dn], writes=[o])
                    s.dma("sp", Dr["O_d"][j * 128:(j + 1) * 128, hk * 256:(hk + 1) * 256], o[:].rearrange("p g d -> p (g d)"), reads=[o], writes=[DR["O_d"]])

    def phase_diff(self, l, q):
        s, I, Dr, DR = self.s, self.I, self.Dr, self.DR
        ident = self.ident
        lam_init = 0.8 - 0.6 * float(np.exp(-0.3 * l))
        with ExitStack() as ps:
            mdiag = self.load_const(ps, "mdiag", [128, 128], BF16)
            dl = s.sb([128, 256], F32, "dl", ps)
            s.dma("sp", dl[:], I["diff_lambda"][l:l + 1, :].broadcast_to([128, 256]), writes=[dl])
            pr = s.sb([128, 2, 64], F32, "pr", ps)
            d4 = dl[:].rearrange("p (a b d) -> p a b d", a=2, b=2)
            s.op("dve", lambda h: h.tensor_tensor(out=pr[:], in0=d4[:, :, 0, :], in1=d4[:, :, 1, :], op=ALU.mult), reads=[dl], writes=[pr])
            e2 = s.sb([128, 2], F32, "e2", ps)
            s.op("dve", lambda h: h.reduce_sum(out=e2[:], in_=pr[:], axis=AX.X), reads=[pr], writes=[e2])
            s.op("act", lambda h: h.activation(out=e2[:], in_=e2[:], func=AF.Exp), reads=[e2], writes=[e2])
            nlam = s.sb([128, 1], F32, "nlam", ps)
            s.op("dve", lambda h: h.scalar_tensor_tensor(out=nlam[:], in0=e2[:, 0:1], scalar=lam_init, in1=e2[:, 1:2], op0=ALU.add, op1=ALU.subtract), reads=[e2], writes=[nlam])
            s.op("dve", lambda h: h.tensor_scalar_mul(out=nlam[:], in0=nlam[:], scalar1=-1.0), reads=[nlam], writes=[nlam])
            gsub = s.sb([128, 128], F32, "gsub", ps)
            s.dma("sp", gsub[:], I["diff_subln_g"][l:l + 1, :].broadcast_to([128, 128]), writes=[gsub])
            s.op("dve", lambda h: h.tensor_scalar_mul(out=gsub[:], in0=gsub[:], scalar1=1.0 - lam_init), reads=[gsub], writes=[gsub])
            KT = [s.sb([68, S], BF16, "KTb", ps) for _ in range(2)]
            QT = [s.sb([68, S], BF16, "QTb", ps) for _ in range(2)]
            V = s.sb([128, NT, 129], BF16, "Vb", ps)
            pts = [s.sb([128, 512], BF16, "pT", ps) for _ in range(3)]
            t0 = [s.sb([128, 128], F32, "t0", ps) for _ in range(2)]
            junk = s.sb([128, 128], F32, "junkb", ps)
            rr = [s.sb([128, 4], F32, "rr", ps) for _ in range(2)]
            ob = [s.sb([128, 128], BF16, "ob", ps) for _ in range(2)]
            accc = [[s.sb([128, 258], F32, "accc", ps) for _ in range(4)] for _ in range(2)]
            nfin = 0
            for hd in range(4):
                for c in range(2):
                    self.load_heads(KT[c], [SL_KB + 2 * hd + c], False)
                    self.load_heads(QT[c], [SL_QB + 2 * hd + c], False)
                s.dma("sp", V[:], Dr["VB_d"][:, hd, :].rearrange("(n p) e -> p n e", p=128), reads=[DR["VB_d"]], writes=[V])
                for G in range(4):
                    def accap(c, jj):
                        return self.P[4 + c * 2 + jj // 2], (jj % 2) * 129
                    items = []
                    for c in range(2):
                        for i in range(0, 4 * G + 4):
                            j0 = max(i, 4 * G)
                            N = (4 * G + 4 - j0) * 128
                            kt = KT[c][:, i * 128:(i + 1) * 128]
                            qk = []
                            if i >= 4 * G:
                                qk.append((lambda sc: sc[:, 0:128], kt, QT[c][:, j0 * 128:(j0 + 1) * 128], True, False, [KT[c], QT[c]]))
                                qk.append((lambda sc: sc[:, 0:128], ident[:], mdiag[:], False, True, [ident, mdiag]))
                                if N > 128:
                                    qk.append((lambda sc, N=N: sc[:, 128:N], kt, QT[c][:, (j0 + 1) * 128:(4 * G + 4) * 128], True, True, [KT[c], QT[c]]))
                            else:
                                qk.append((lambda sc, N=N: sc[:, 0:N], kt, QT[c][:, j0 * 128:(4 * G + 4) * 128], True, True, [KT[c], QT[c]]))

                            def pv(pT, c=c, i=i, j0=j0, G=G):
                                for jj in range(j0, 4 * G + 4):
                                    bank, off = accap(c, jj - 4 * G)
                                    self.mm(bank[:, off:off + 129], pT[:, (jj - j0) * 128:(jj - j0 + 1) * 128], V[:, i, :], i == 0 and (jj - 4 * G) % 2 == 0, i == jj, [pT, V], [bank], sig=(jj == 4 * G + 3))
                            items.append(dict(kp=128, N=N, qk=qk, pv=pv))
                    self.attn_stream(items, self.P[0:3], pts)
                    cps = accc[G % 2]
                    for bi in range(4):
                        bank = self.P[4 + bi]
                        if bi % 2 == 0:
                            s.op("act", lambda h, bank=bank, bi=bi: h.copy(out=cps[bi][:], in_=bank[:, 0:258]), reads=[bank], writes=[cps[bi]])
                        else:
                            s.op("dve", lambda h, bank=bank, bi=bi: h.tensor_copy(out=cps[bi][:], in_=bank[:, 0:258]), reads=[bank], writes=[cps[bi]])
                    for jj in range(4):
                        b0, o0 = cps[0 * 2 + jj // 2], (jj % 2) * 129
                        b1, o1 = cps[1 * 2 + jj // 2], (jj % 2) * 129
                        r, t, o = rr[nfin % 2], t0[nfin % 2], ob[nfin % 2]
                        nfin += 1
                        s.op("dve", lambda h: h.reciprocal(out=r[:, 0:1], in_=b0[:, o0 + 128:o0 + 129]), reads=[b0], writes=[r])
                        s.op("dve", lambda h: h.reciprocal(out=r[:, 1:2], in_=b1[:, o1 + 128:o1 + 129]), reads=[b1], writes=[r])
                        s.op("dve", lambda h: h.tensor_tensor(out=r[:, 1:2], in0=r[:, 1:2], in1=nlam[:], op=ALU.mult), reads=[r, nlam], writes=[r])
                        s.op("dve", lambda h: h.tensor_scalar_mul(out=t[:], in0=b0[:, o0:o0 + 128], scalar1=r[:, 0:1]), reads=[b0, r], writes=[t])
                        s.op("dve", lambda h: h.scalar_tensor_tensor(out=t[:], in0=b1[:, o1:o1 + 128], scalar=r[:, 1:2], in1=t[:], op0=ALU.mult, op1=ALU.add), reads=[b1, r, t], writes=[t])
                        s.op("act", lambda h: h.activation(out=junk[:], in_=t[:], func=AF.Square, accum_out=r[:, 2:3]), reads=[t], writes=[junk, r])
                        s.op("dve", lambda h: h.tensor_scalar(out=r[:, 2:3], in0=r[:, 2:3], scalar1=1.0 / 128, scalar2=EPS, op0=ALU.mult, op1=ALU.add), reads=[r], writes=[r])
                        s.op("act", lambda h: h.sqrt(out=r[:, 2:3], in_=r[:, 2:3]), reads=[r], writes=[r])
                        s.op("dve", lambda h: h.reciprocal(out=r[:, 2:3], in_=r[:, 2:3]), reads=[r], writes=[r])
                        s.op("dve", lambda h: h.scalar_tensor_tensor(out=o[:], in0=t[:], scalar=r[:, 2:3], in1=gsub[:], op0=ALU.mult, op1=ALU.mult), reads=[t, r, gsub], writes=[o])
                        row0 = (4 * G + jj) * 128
                        s.dma("sp", Dr["O_d"][row0:row0 + 128, 512 + hd * 128:512 + (hd + 1) * 128], o[:], reads=[o], writes=[DR["O_d"]])

    def phase_nsa(self, l, q):
        s, I, Dr, DR = self.s, self.I, self.Dr, self.DR
        ident = self.ident
        with ExitStack() as ps:
            mdiag = self.load_const(ps, "mdiag", [128, 128], BF16)
            medge = self.load_const(ps, "medge", [128, 128], BF16)
            cmaskT = self.load_const(ps, "cmaskT", [128, S], BF16)
            eblk = self.load_const(ps, "eblk", [32, S], BF16)
            atab = s.sb([128, NT, 32], F32, "atab", ps)
            s.dma("sp", atab[:], I["atab"].rearrange("(n p) b -> p n b", p=128), writes=[atab])
            GN = s.sb([128, NT, 24], F32, "GN", ps)
            s.dma("sp", GN[:], Dr["GN_d"].rearrange("(n p) c -> p n c", p=128), reads=[DR["GN_d"]], writes=[GN])
            QG = s.sb([68, 4, S], BF16, "QGc", ps)
            KSL = s.sb([68, S], BF16, "KSL", ps)
            KWN = s.sb([68, S], BF16, "KWN", ps)
            KCT = s.sb([64, 128], BF16, "KCT", ps)
            VC = s.sb([128, 97], BF16, "VC", ps)
            VSL = s.sb([128, NT, 65], BF16, "VSL", ps)
            VWN = s.sb([128, NT, 65], BF16, "VWN", ps)
            OC = s.sb([128, NT, 4, 64], F32, "OC", ps)
            SELT = s.sb([32, S], BF16, "SELT", ps)
            pts = [s.sb([128, 512], BF16, "pT", ps) for _ in range(3)]
            dn = [s.sb([128, 12], F32, "dn", ps) for _ in range(2)]
            imp = [s.sb([128, 32], F32, "imp", ps) for _ in range(2)]
            tmp32 = [s.sb([128, 32], F32, "tmp32", ps) for _ in range(2)]
            m8 = [s.sb([128, 16], F32, "m8", ps) for _ in range(2)]
            oo = [s.sb([128, 4, 64], F32, "oo", ps) for _ in range(2)]
            ot = [s.sb([128, 4, 64], F32, "otmp", ps) for _ in range(2)]
            ob = [s.sb([128, 4, 64], BF16, "obf", ps) for _ in range(2)]
            o3 = lambda sc: sc[:, :].rearrange("p (g t) -> p g t", g=4)
            o3c = lambda sc: sc[0:127, :].rearrange("p (g t) -> p g t", g=4)
            b4 = lambda ap: ap.unsqueeze(1).broadcast_to([ap.shape[0], 4, 128])
            for hk in range(2):
                self.load_heads(QG, [SL_QC + hk * 4 + g for g in range(4)], True)
                self.load_heads(KSL, [SL_KSL + hk], False)
                self.load_heads(KWN, [SL_KWN + hk], False)
                s.dma("sp", KCT[:], Dr["KC_d"][hk], reads=[DR["KC_d"]], writes=[KCT])
                s.dma("sp", VC[:], Dr["VC_d"][hk], reads=[DR["VC_d"]], writes=[VC])
                s.dma("sp", VSL[:], Dr["VSL_d"][:, hk, :].rearrange("(n p) e -> p n e", p=128), reads=[DR["VSL_d"]], writes=[VSL])
                s.dma("sp", VWN[:], Dr["VWN_d"][:, hk, :].rearrange("(n p) e -> p n e", p=128), reads=[DR["VWN_d"]], writes=[VWN])
                for j in range(NT):
                    acc = self.P[4 + j % 2]
                    jc = slice(j * 128, (j + 1) * 128)
                    qk = [(o3c, KCT[:, 0:127], QG[0:64, :, jc], True, False, [KCT, QG]),
                          (o3c, ident[0:127, 0:127], b4(cmaskT[0:127, jc]), False, True, [ident, cmaskT])]

                    def pv(pT, acc=acc):
                        for g in range(4):
                            self.mm(acc[:, g * 97:(g + 1) * 97], pT[0:127, g * 128:(g + 1) * 128], VC[0:127, :], g == 0, True, [pT, VC], [acc], sig=(g == 3))
                    self.attn_stream([dict(kp=127, N=512, qk=qk, pv=pv)], self.P[0:3], pts)
                    a3 = acc[:, 0:388].rearrange("p (g e) -> p g e", e=97)
                    d, im, tm, mm8 = dn[j % 2], imp[j % 2], tmp32[j % 2], m8[j % 2]
                    s.op("dve", lambda h: h.tensor_scalar_max(out=d[:, 0:4].unsqueeze(2), in0=a3[:, :, 64:65], scalar1=1e-30), reads=[acc], writes=[d])
                    s.op("dve", lambda h: h.reciprocal(out=d[:, 0:4], in_=d[:, 0:4]), reads=[d], writes=[d])
                    s.op("dve", lambda h: h.tensor_tensor(out=OC[:, j], in0=a3[:, :, 0:64], in1=d[:, 0:4].unsqueeze(2).broadcast_to([128, 4, 64]), op=ALU.mult), reads=[acc, d], writes=[OC])
                    s.op("dve", lambda h: h.tensor_scalar_mul(out=im[:], in0=a3[:, 0, 65:97], scalar1=d[:, 0:1]), reads=[acc, d], writes=[im])
                    for g in range(1, 4):
                        s.op("dve", lambda h, g=g: h.scalar_tensor_tensor(out=im[:], in0=a3[:, g, 65:97], scalar=d[:, g:g + 1], in1=im[:], op0=ALU.mult, op1=ALU.add), reads=[acc, d, im], writes=[im])
                    s.op("dve", lambda h: h.tensor_tensor(out=im[:], in0=im[:], in1=atab[:, j, :], op=ALU.add), reads=[im, atab], writes=[im])
                    s.op("dve", lambda h: h.max(out=mm8[:, 0:8], in_=im[:]), reads=[im], writes=[mm8])
                    s.op("dve", lambda h: h.match_replace(out=tm[:], in_to_replace=mm8[:, 0:8], in_values=im[:], imm_value=-3e9), reads=[im, mm8], writes=[tm])
                    s.op("dve", lambda h: h.max(out=mm8[:, 8:16], in_=tm[:]), reads=[tm], writes=[mm8])
                    s.op("dve", lambda h: h.tensor_scalar(out=tm[:], in0=im[:], scalar1=mm8[:, 15:16], scalar2=NEG, op0=ALU.is_lt, op1=ALU.mult), reads=[im, mm8], writes=[tm])
                    pt = self.P[6 + j % 2]
                    self.tr(pt[0:32, 0:128], tm[:], self.ident32[:], [tm, self.ident32], [pt])
                    s.op("act", lambda h: h.copy(out=SELT[:, jc], in_=pt[0:32, 0:128]), reads=[pt], writes=[SELT])
                for j in range(NT):
                    jc = slice(j * 128, (j + 1) * 128)
                    accS, accW = self.P[4 + 2 * (j % 2)], self.P[5 + 2 * (j % 2)]
                    items = []
                    for i in range(0, j + 1):
                        ic = slice(i * 128, (i + 1) * 128)
                        qk = [(o3, KSL[:, ic], QG[:, :, jc], True, False, [KSL, QG]),
                              (o3, eblk[:, ic], b4(SELT[:, jc]), False, i != j, [eblk, SELT])]
                        if i == j:
                            qk.append((o3, ident[:], b4(mdiag[:]), False, True, [ident, mdiag]))

                        def pv(pT, i=i, j=j, acc=accS):
                            for g in range(4):
                                self.mm(acc[:, g * 65:(g + 1) * 65], pT[:, g * 128:(g + 1) * 128], VSL[:, i, :], i == 0 and g == 0, i == j, [pT, VSL], [acc], sig=(g == 3))
                        items.append(dict(kp=128, N=512, qk=qk, pv=pv))
                    i0 = max(0, j - 4)
                    for i in range(i0, j + 1):
                        ic = slice(i * 128, (i + 1) * 128)
                        masked = (i == j) or (i == j - 4)
                        qk = [(o3, KWN[:, ic], QG[:, :, jc], True, not masked, [KWN, QG])]
                        if masked:
                            mk = mdiag if i == j else medge
                            qk.append((o3, ident[:], b4(mk[:]), False, True, [ident, mk]))

                        def pv(pT, i=i, j=j, i0=i0, acc=accW):
                            for g in range(4):
                                self.mm(acc[:, g * 65:(g + 1) * 65], pT[:, g * 128:(g + 1) * 128], VWN[:, i, :], i == i0 and g == 0, i == j, [pT, VWN], [acc], sig=(g == 3))
                        items.append(dict(kp=128, N=512, qk=qk, pv=pv))
                    self.attn_stream(items, self.P[0:3], pts)
                    d, o, t, obf = dn[j % 2], oo[j % 2], ot[j % 2], ob[j % 2]
                    aS = accS[:, 0:260].rearrange("p (g e) -> p g e", e=65)
                    aW = accW[:, 0:260].rearrange("p (g e) -> p g e", e=65)
                    gv = GN[:, j, hk * 12:(hk + 1) * 12].rearrange("p (g r) -> p g r", r=3)
                    s.op("dve", lambda h: h.reciprocal(out=d[:, 4:8].unsqueeze(2), in_=aS[:, :, 64:65]), reads=[accS], writes=[d])
                    s.op("dve", lambda h: h.reciprocal(out=d[:, 8:12].unsqueeze(2), in_=aW[:, :, 64:65]), reads=[accW], writes=[d])
                    s.op("dve", lambda h: h.tensor_tensor(out=d[:, 4:8].unsqueeze(2), in0=d[:, 4:8].unsqueeze(2), in1=gv[:, :, 1:2], op=ALU.mult), reads=[d, GN], writes=[d])
                    s.op("dve", lambda h: h.tensor_tensor(out=d[:, 8:12].unsqueeze(2), in0=d[:, 8:12].unsqueeze(2), in1=gv[:, :, 2:3], op=ALU.mult), reads=[d, GN], writes=[d])
                    s.op("pool", lambda h: h.tensor_tensor(out=o[:], in0=OC[:, j], in1=gv[:, :, 0:1].broadcast_to([128, 4, 64]), op=ALU.mult), reads=[OC, GN], writes=[o])
                    s.op("dve", lambda h: h.tensor_tensor(out=t[:], in0=aS[:, :, 0:64], in1=d[:, 4:8].unsqueeze(2).broadcast_to([128, 4, 64]), op=ALU.mult), reads=[accS, d], writes=[t])
                    s.op("pool", lambda h: h.tensor_tensor(out=o[:], in0=o[:], in1=t[:], op=ALU.add), reads=[o, t], writes=[o])
                    s.op("dve", lambda h: h.tensor_tensor(out=t[:], in0=aW[:, :, 0:64], in1=d[:, 8:12].unsqueeze(2).broadcast_to([128, 4, 64]), op=ALU.mult), reads=[accW, d], writes=[t])
                    s.op("pool", lambda h: h.tensor_tensor(out=obf[:], in0=o[:], in1=t[:], op=ALU.add), reads=[o, t], writes=[obf])
                    s.dma("sp", Dr["O_d"][jc, 1024 + hk * 256:1024 + (hk + 1) * 256], obf[:].rearrange("p g d -> p (g d)"), reads=[obf], writes=[DR["O_d"]])

    def phase_merge(self, l, q):
        s, I, Dr, DR = self.s, self.I, self.Dr, self.DR
        xsrc, xres_r = self.xsrc(l, q)
        with ExitStack() as ps:
            wbr = s.sb([128, 12, D], BF16, "wbr", ps)
            wout = s.sb([128, 8, D], BF16, "wout", ps)
            for r in range(3):
                for hh in range(2):
                    s.dma("pool", wbr[:, r * 4:(r + 1) * 4, hh * 512:(hh + 1) * 512],
                          I["w_branch"][l, r].rearrange("(kc p) n -> p kc n", p=128)[:, :, hh * 512:(hh + 1) * 512], writes=[wbr])
            for k4 in range(2):
                for hh in range(2):
                    s.dma("pool", wout[:, k4 * 4:(k4 + 1) * 4, hh * 512:(hh + 1) * 512],
                          I["w_out"][l].rearrange("(kc p) n -> p kc n", p=128)[:, k4 * 4:(k4 + 1) * 4, hh * 512:(hh + 1) * 512], writes=[wout])
            g1 = s.sb([128, D], F32, "g1", ps)
            self.mod_bc(l, q, 2, g1)
            Ot = [s.sb([128, 1536], BF16, "Ot", ps) for _ in range(2)]
            GM = [s.sb([128, 3072], F32, "GMt", ps) for _ in range(2)]
            xt = [s.sb([128, D], F32, "xt", ps) for _ in range(2)]
            OT = [s.sb([128, 12, 128], BF16, "OT", ps) for _ in range(2)]
            MT = [s.sb([128, 8, 128], BF16, "MT", ps) for _ in range(2)]
            mg = s.sb([128, D], F32, "mg", ps)
            mgb = s.sb([128, D], BF16, "mgb", ps)
            tmp = [s.sb([128, 512], F32, "mtmp", ps) for _ in range(2)]
            nb = 0
            for tt in range(NT):
                rows = slice(tt * 128, (tt + 1) * 128)
                grow = slice(q * S + tt * 128, q * S + (tt + 1) * 128)
                O, G, x, ot, mt = Ot[tt % 2], GM[tt % 2], xt[tt % 2], OT[tt % 2], MT[tt % 2]
                s.dma("sp", O[:], Dr["O_d"][rows, :], reads=[DR["O_d"]], writes=[O])
                s.dma("sp", G[:], Dr["GM_d"][rows, :], reads=[DR["GM_d"]], writes=[G])
                s.dma("sp", x[:], xsrc[grow, :], reads=[], writes=[x])
                pa, pb = self.P[0], self.P[1]
                pab, pbb = pa[:].bitcast(BF16), pb[:].bitcast(BF16)
                for kc in range(8):
                    self.tr(pab[:, kc * 128:(kc + 1) * 128], O[:, kc * 128:(kc + 1) * 128], self.ident[:], [O, self.ident], [pa], sig=(kc == 7))
                for kc in range(4):
                    self.tr(pbb[:, kc * 128:(kc + 1) * 128], O[:, (8 + kc) * 128:(9 + kc) * 128], self.ident[:], [O, self.ident], [pb], sig=(kc == 3))
                s.op("act", lambda h: h.copy(out=ot[:, 0:8, :], in_=pab.rearrange("p (k t) -> p k t", k=8)), reads=[pa], writes=[ot])
                s.op("act", lambda h: h.copy(out=ot[:, 8:12, :], in_=pbb[:, 0:512].rearrange("p (k t) -> p k t", k=4)), reads=[pb], writes=[ot])
                for r in range(3):
                    for half in range(2):
                        pm = self.P[2 + nb % 4]
                        tp = tmp[nb % 2]
                        nb += 1
                        hc = slice(half * 512, (half + 1) * 512)
                        for kc in range(4):
                            self.mm(pm[:, :], ot[:, r * 4 + kc, :], wbr[:, r * 4 + kc, hc], kc == 0, kc == 3, [ot, wbr], [pm], sig=(kc == 3))
                        gsl = G[:, r * 1024 + half * 512:r * 1024 + (half + 1) * 512]
                        if r == 0:
                            s.op("dve", lambda h: h.tensor_tensor(out=mg[:, hc], in0=pm[:, :], in1=gsl, op=ALU.mult), reads=[pm, G], writes=[mg])
                        else:
                            s.op("dve", lambda h: h.tensor_tensor(out=tp[:], in0=pm[:, :], in1=gsl, op=ALU.mult), reads=[pm, G], writes=[tp])
                            s.op("pool", lambda h: h.tensor_tensor(out=mg[:, hc], in0=mg[:, hc], in1=tp[:], op=ALU.add), reads=[mg, tp], writes=[mg])
                s.op("act", lambda h: h.copy(out=mgb[:], in_=mg[:]), reads=[mg], writes=[mgb])
                for kc in range(8):
                    self.tr(pab[:, kc * 128:(kc + 1) * 128], mgb[:, kc * 128:(kc + 1) * 128], self.ident[:], [mgb, self.ident], [pa], sig=(kc == 7))
                s.op("act", lambda h: h.copy(out=mt[:], in_=pab.rearrange("p (k t) -> p k t", k=8)), reads=[pa], writes=[mt])
                for half in range(2):
                    pm = self.P[2 + nb % 4]
                    tp = tmp[nb % 2]
                    nb += 1
                    hc = slice(half * 512, (half + 1) * 512)
                    for kc in range(8):
                        self.mm(pm[:, :], mt[:, kc, :], wout[:, kc, hc], kc == 0, kc == 7, [mt, wout], [pm], sig=(kc == 7))
                    s.op("dve", lambda h: h.tensor_tensor(out=tp[:], in0=pm[:, :], in1=g1[:, hc], op=ALU.mult), reads=[pm, g1], writes=[tp])
                    s.op("pool", lambda h: h.tensor_tensor(out=x[:, hc], in0=x[:, hc], in1=tp[:], op=ALU.add), reads=[x, tp], writes=[x])
                s.dma("pool", Dr["xres"][grow, :], x[:], reads=[x], writes=[DR["xres"]])

    def phase_moe2(self, l):
        s, I, Dr, DR = self.s, self.I, self.Dr, self.DR
        last = (l == L_DEPTH - 1)
        NTT, NB = self.ntt, self.nblk
        with ExitStack() as ps:
            g2 = []
            for q in range(self.nseq):
                t = s.sb([128, D], F32, "g2", ps)
                self.mod_bc(l, q, 5, t)
                g2.append(t)
            GATE = s.sb([128, NTT, NE], F32, "GATE", ps)
            GT = s.sb([NE, NTT, 128], F32, "GT", ps)
            IDX4 = s.sb([128, NTT, 4], I32, "IDX4", ps)
            G4 = s.sb([128, NTT, 4], F32, "G4", ps)
            IDXW = s.sb([128, NB, 2], I32, "IDXW", ps)
            IDXB = s.sb([128, NB], I32, "IDXB", ps)
            b2 = s.sb([NE, D], F32, "b2", ps)
            s.dma("sp", b2[:], I["exp_b2"][l], writes=[b2])
            with ExitStack() as p1:
                HB = s.sb([128, NTT, D], BF16, "HB", p1)
                MASK = s.sb([128, NTT, NE], BF16, "MASK", p1)
                DEST = s.sb([128, NTT, NE], F32, "DEST", p1)
                rw = s.sb([128, 8, NE], F32, "rw", p1)
                rb = s.sb([128, NE], F32, "rb", p1)
                s.dma("sp", rw[:], I["router_w"][l].rearrange("(kc p) e -> p kc e", p=128), writes=[rw])
                s.dma("sp", rb[:], I["router_b"][l:l + 1, :].broadcast_to([128, NE]), writes=[rb])
                ltri = self.load_const(p1, "ltri", [128, 128], BF16)
                ones = self.load_const(p1, "ones128", [128, 128], BF16)
                b512 = self.load_const(p1, "b512", [128, 64], F32)
                rowiota = self.load_const(p1, "rowiota", [128, 8], F32)
                piota = self.load_const(p1, "piota", [128, 1], F32)
                xt = [s.sb([128, D], F32, "xt", p1) for _ in range(2)]
                hf = [s.sb([128, D], F32, "hf", p1) for _ in range(2)]
                hT32s = [s.sb([128, 8, 128], F32, "hT32", p1) for _ in range(2)]
                junk = s.sb([128, D], F32, "junk", p1)
                ss = [s.sb([128, 1], F32, "ss", p1) for _ in range(2)]
                lg = [s.sb([128, NE], F32, "lg", p1) for _ in range(2)]
                ex = [s.sb([128, NE], F32, "ex", p1) for _ in range(2)]
                m8 = [s.sb([128, 8], F32, "m8", p1) for _ in range(2)]
                sm = [s.sb([128, 2], F32, "sm", p1) for _ in range(2)]
                gsc = sh = None
                for tt in range(NTT):
                    q = tt // NT
                    if tt % NT == 0:
                        gsc, sh = self.norm_tiles(p1, l, q, "norm2_g", 4, 3)
                    grow = slice(tt * 128, (tt + 1) * 128)
                    x, h32, sq = xt[tt % 2], hf[tt % 2], ss[tt % 2]
                    s.dma("sp", x[:], Dr["xres"][grow, :], reads=[DR["xres"]], writes=[x])
                    self.rstd_of((x, x[:]), (junk, junk[:]), sq)
                    s.op("dve", lambda hh: hh.scalar_tensor_tensor(out=x[:], in0=x[:], scalar=sq[:, 0:1], in1=gsc[:], op0=ALU.mult, op1=ALU.mult), reads=[x, sq, gsc], writes=[x])
                    s.op("pool", lambda hh: hh.tensor_tensor(out=h32[:], in0=x[:], in1=sh[:], op=ALU.add), reads=[x, sh], writes=[h32])
                    s.op("act", lambda hh: hh.copy(out=HB[:, tt, :], in_=h32[:]), reads=[h32], writes=[HB])
                    hT32 = hT32s[tt % 2]
                    for hh2 in range(2):
                        pf = self.P[(tt % 2) * 2 + hh2]
                        for k4 in range(4):
                            kc = hh2 * 4 + k4
                            self.tr(pf[:, k4 * 128:(k4 + 1) * 128], h32[:, kc * 128:(kc + 1) * 128], self.ident32[:], [h32, self.ident32], [pf], sig=(k4 == 3))
                        s.op("dve", lambda hh: hh.tensor_copy(out=hT32[:, hh2 * 4:(hh2 + 1) * 4, :], in_=pf[:, :].rearrange("p (k t) -> p k t", k=4)), reads=[pf], writes=[hT32])
                    pl = self.P[4 + tt % 2]
                    for kc in range(8):
                        self.mm(pl[:, 0:NE], hT32[:, kc, :], rw[:, kc, :], kc == 0, kc == 7, [hT32, rw], [pl], sig=(kc == 7))
                    lgt, et, mt, st = lg[tt % 2], ex[tt % 2], m8[tt % 2], sm[tt % 2]
                    s.op("dve", lambda hh: hh.tensor_tensor(out=lgt[:], in0=pl[:, 0:NE], in1=rb[:], op=ALU.add), reads=[pl, rb], writes=[lgt])
                    s.op("dve", lambda hh: hh.max(out=mt[:], in_=lgt[:]), reads=[lgt], writes=[mt])
                    s.op("dve", lambda hh: hh.tensor_scalar_mul(out=st[:, 0:1], in0=mt[:, 0:1], scalar1=-1.0), reads=[mt], writes=[st])
                    s.op("act", lambda hh: hh.activation(out=et[:], in_=lgt[:], func=AF.Exp, bias=st[:, 0:1]), reads=[lgt, st], writes=[et])
                    s.op("dve", lambda hh: hh.tensor_scalar(out=lgt[:], in0=lgt[:], scalar1=mt[:, 3:4], scalar2=None, op0=ALU.is_ge), reads=[lgt, mt], writes=[lgt])
                    s.op("dve", lambda hh: hh.tensor_copy(out=MASK[:, tt, :], in_=lgt[:]), reads=[lgt], writes=[MASK])
                    s.op("dve", lambda hh: hh.tensor_tensor(out=et[:], in0=et[:], in1=lgt[:], op=ALU.mult), reads=[et, lgt], writes=[et])
                    s.op("dve", lambda hh: hh.reduce_sum(out=st[:, 1:2], in_=et[:], axis=AX.X), reads=[et], writes=[st])
                    s.op("dve", lambda hh: hh.reciprocal(out=st[:, 1:2], in_=st[:, 1:2]), reads=[st], writes=[st])
                    s.op("dve", lambda hh: hh.tensor_scalar_mul(out=GATE[:, tt, :], in0=et[:], scalar1=st[:, 1:2]), reads=[et, st], writes=[GATE])
                    pg = self.P[6 + tt % 2]
                    self.tr(pg[0:NE, 0:128], GATE[:, tt, :], self.ident32[:], [GATE, self.ident32], [pg])
                    s.op("act", lambda hh: hh.copy(out=GT[:, tt, :], in_=pg[0:NE, 0:128]), reads=[pg], writes=[GT])
                run = s.sb([128, NE], F32, "run", p1)
                s.op("dve", lambda hh: hh.memset(run[:], 0.0), writes=[run])
                for tt in range(NTT):
                    pr = self.P[6 + tt % 2]
                    self.mm(pr[:, 0:NE], ltri[:], MASK[:, tt, :], True, True, [ltri, MASK], [pr], sig=False)
                    self.mm(pr[:, NE:2 * NE], ones[:], MASK[:, tt, :], False, True, [ones, MASK], [pr])
                    s.op("dve", lambda hh: hh.tensor_tensor(out=DEST[:, tt, :], in0=pr[:, 0:NE], in1=run[:], op=ALU.add), reads=[pr, run], writes=[DEST])
                    s.op("dve", lambda hh: hh.tensor_tensor(out=run[:], in0=run[:], in1=pr[:, NE:2 * NE], op=ALU.add), reads=[pr, run], writes=[run])
                padded = s.sb([128, NE], F32, "padded", p1)
                tmpe = s.sb([128, NE], F32, "tmpe", p1)
                cum = [s.sb([128, NE], F32, "cum", p1) for _ in range(2)]
                s.op("dve", lambda hh: hh.memset(padded[:], 0.0), writes=[padded])
                for jb in range(NTT // 4):
                    s.op("dve", lambda hh, jb=jb: hh.scalar_tensor_tensor(out=padded[:], in0=run[:], scalar=512.0 * jb, in1=padded[:], op0=ALU.is_gt, op1=ALU.add), reads=[run, padded], writes=[padded])
                s.op("dve", lambda hh: hh.tensor_scalar_mul(out=padded[:], in0=padded[:], scalar1=512.0), reads=[padded], writes=[padded])
                s.op("dve", lambda hh: hh.tensor_copy(out=cum[0][:], in_=padded[:]), reads=[padded], writes=[cum[0]])
                ci = 0
                for shf in (1, 2, 4, 8, 16):
                    a, b = cum[ci], cum[1 - ci]
                    s.op("dve", lambda hh: hh.tensor_copy(out=b[:, 0:shf], in_=a[:, 0:shf]), reads=[a], writes=[b])
                    s.op("dve", lambda hh: hh.tensor_tensor(out=b[:, shf:NE], in0=a[:, shf:NE], in1=a[:, 0:NE - shf], op=ALU.add), reads=[a], writes=[b])
                    ci = 1 - ci
                pend = cum[ci]
                pstart = s.sb([128, NE], F32, "pstart", p1)
                s.op("dve", lambda hh: hh.tensor_tensor(out=pstart[:], in0=pend[:], in1=padded[:], op=ALU.subtract), reads=[pend, padded], writes=[pstart])
                s.op("dve", lambda hh: hh.tensor_tensor(out=DEST[:], in0=DEST[:], in1=pstart[:].unsqueeze(1).broadcast_to([128, NTT, NE]), op=ALU.add), reads=[DEST, pstart], writes=[DEST])
                s.op("dve", lambda hh: hh.scalar_tensor_tensor(out=DEST[:], in0=DEST[:], scalar=1.0, in1=MASK[:], op0=ALU.add, op1=ALU.mult), reads=[DEST, MASK], writes=[DEST])
                d4 = s.sb([128, NTT, 4], F32, "d4", p1)
                for tt in range(NTT):
                    mt, et = m8[tt % 2], ex[tt % 2]
                    s.op("dve", lambda hh: hh.max(out=mt[:], in_=DEST[:, tt, :]), reads=[DEST], writes=[mt])
                    s.op("dve", lambda hh: hh.tensor_scalar_add(out=d4[:, tt, :], in0=mt[:, 0:4], scalar1=-1.0), reads=[mt], writes=[d4])
                    for k in range(4):
                        s.op("dve", lambda hh: hh.scalar_tensor_tensor(out=et[:], in0=DEST[:, tt, :], scalar=mt[:, k:k + 1], in1=GATE[:, tt, :], op0=ALU.is_equal, op1=ALU.mult), reads=[DEST, mt, GATE], writes=[et])
                        s.op("dve", lambda hh: hh.reduce_sum(out=G4[:, tt, k:k + 1], in_=et[:], axis=AX.X), reads=[et], writes=[G4])
                s.op("dve", lambda hh: hh.tensor_copy(out=IDX4[:], in_=d4[:]), reads=[d4], writes=[IDX4])
                bexp = s.sb([128, 64], F32, "bexp", p1)
                s.op("dve", lambda hh: hh.memset(bexp[:], 0.0), writes=[bexp])
                for e in range(NE):
                    s.op("dve", lambda hh: hh.scalar_tensor_tensor(out=bexp[:], in0=b512[:], scalar=pend[:, e:e + 1], in1=bexp[:], op0=ALU.is_ge, op1=ALU.add), reads=[b512, pend, bexp], writes=[bexp])
                boob = s.sb([128, 64], F32, "boob", p1)
                s.op("dve", lambda hh: hh.tensor_scalar(out=boob[:], in0=bexp[:], scalar1=float(NE) - 0.5, scalar2=1.0e6, op0=ALU.is_ge, op1=ALU.mult), reads=[bexp], writes=[boob])
                s.op("dve", lambda hh: hh.tensor_scalar_min(out=bexp[:], in0=bexp[:], scalar1=float(NE - 1)), reads=[bexp], writes=[bexp])
                iwf = s.sb([128, NB, 2], F32, "iwf", p1)
                ibf = s.sb([128, NB], F32, "ibf", p1)
                e1k = s.sb([128, 64], F32, "e1k", p1)
                s.op("dve", lambda hh: hh.tensor_scalar(out=e1k[:], in0=bexp[:], scalar1=256.0, scalar2=float(l * NE * 256), op0=ALU.mult, op1=ALU.add), reads=[bexp], writes=[e1k])
                s.op("dve", lambda hh: hh.tensor_tensor(out=e1k[:], in0=e1k[:], in1=boob[:], op=ALU.add), reads=[e1k, boob], writes=[e1k])
                s.op("dve", lambda hh: hh.tensor_tensor(out=iwf[:], in0=rowiota[:, 0:2].unsqueeze(1).broadcast_to([128, NB, 2]), in1=e1k[:, 0:NB].unsqueeze(2).broadcast_to([128, NB, 2]), op=ALU.add), reads=[rowiota, e1k], writes=[iwf])
                s.op("dve", lambda hh: hh.tensor_copy(out=IDXW[:], in_=iwf[:]), reads=[iwf], writes=[IDXW])
                s.op("dve", lambda hh: hh.tensor_scalar(out=ibf[:], in0=bexp[:, 0:NB], scalar1=128.0, scalar2=piota[:, 0:1], op0=ALU.mult, op1=ALU.add), reads=[bexp, piota], writes=[ibf])
                s.op("dve", lambda hh: hh.tensor_scalar_add(out=ibf[:], in0=ibf[:], scalar1=float(l * NE * 128)), reads=[ibf], writes=[ibf])
                s.op("dve", lambda hh: hh.tensor_tensor(out=ibf[:], in0=ibf[:], in1=boob[:, 0:NB], op=ALU.add), reads=[ibf, boob], writes=[ibf])
                s.op("dve", lambda hh: hh.tensor_copy(out=IDXB[:], in_=ibf[:]), reads=[ibf], writes=[IDXB])
                if "dbg_d" in self.dbg:
                    dbt = s.sb([128, 4096], F32, "dbt", p1)
                    s.op("dve", lambda hh: hh.memset(dbt[:], 0.0), writes=[dbt])
                    s.op("dve", lambda hh: hh.tensor_copy(out=dbt[:, 0:32], in_=run[:]), reads=[run], writes=[dbt])
                    s.op("dve", lambda hh: hh.tensor_copy(out=dbt[:, 32:64], in_=pend[:]), reads=[pend], writes=[dbt])
                    s.op("dve", lambda hh: hh.tensor_copy(out=dbt[:, 64:128], in_=bexp[:]), reads=[bexp], writes=[dbt])
                    s.op("dve", lambda hh: hh.tensor_copy(out=dbt[:, 128:128 + NTT * 4], in_=d4[:].rearrange("p t k -> p (t k)")), reads=[d4], writes=[dbt])
                    s.op("dve", lambda hh: hh.tensor_copy(out=dbt[:, 512:512 + NTT * 4], in_=G4[:].rearrange("p t k -> p (t k)")), reads=[G4], writes=[dbt])
                    s.dma("sp", Dr["dbg_d"], dbt[:], reads=[dbt], writes=[DR["dbg_d"]])
                for tt in range(NTT):
                    for k in range(4):
                        s.idma(Dr["xs_d"][:, :], HB[:, tt, :], out_off=IDX4[:, tt, k:k + 1], reads=[HB, IDX4], writes=[])
                s.barrier()
            with ExitStack() as p2:
                W1 = [s.sb([128, 8, 2 * D], BF16, "W1", p2) for _ in range(2)]
                W2 = [s.sb([128, 8, D], BF16, "W2", p2) for _ in range(2)]
                B1 = [s.sb([128, 16], F32, "B1", p2) for _ in range(2)]
                XS = [s.sb([128, 4, D], BF16, "XS", p2) for _ in range(2)]
                XT = [s.sb([128, 8, 512], BF16, "XT", p2) for _ in range(2)]
                Ab = [s.sb([128, 8, 512], BF16, "Ab", p2) for _ in range(2)]
                Gt = [s.sb([128, 512], F32, "Gt", p2) for _ in range(2)]
                St = [s.sb([128, 512], F32, "St", p2) for _ in range(2)]
                Ut = [s.sb([128, 512], F32, "Ut", p2) for _ in range(2)]
                Yt = [s.sb([128, D], F32, "Yt", p2) for _ in range(2)]
                for wt_ in W1 + W2 + B1:
                    s.op("pool", lambda hh, wt_=wt_: hh.memset(wt_[:], 0.0), writes=[wt_])
                cnt = {"nf": 0, "ny": 0}

                def load_block(b):
                    w1, w2, b1, xs = W1[b % 2], W2[b % 2], B1[b % 2], XS[b % 2]
                    s.dma("sp", xs[:], Dr["xs_d"][b * 512:(b + 1) * 512, :].rearrange("(t p) d -> p t d", p=128), reads=[DR["xs_d"]], writes=[xs])
                    for j2 in range(2):
                        s.idma(w1[:, j2 * 4:(j2 + 1) * 4, :].rearrange("p a b -> p (a b)"), I["exp_w1"][:, :], in_off=IDXW[:, b, j2:j2 + 1], reads=[IDXW], writes=[w1], bounds=65535)
                    s.idma(b1[:, :], I["exp_b1E"][:, :], in_off=IDXB[:, b:b + 1], reads=[IDXB], writes=[b1], bounds=65535)

                def load_w2(b):
                    w2 = W2[b % 2]
                    s.idma(w2[:].rearrange("p a b -> p (a b)"), I["exp_w2"][:, :], in_off=IDXB[:, b:b + 1], reads=[IDXB], writes=[w2], bounds=65535)

                def transposes(b):
                    xs, xT = XS[b % 2], XT[b % 2]
                    for t4 in range(4):
                        pt = self.P[t4 % 2]
                        ptb = pt[:].bitcast(BF16)
                        for kc in range(8):
                            kb = (kc // 4) * 512 + (kc % 4)
                            self.tr(ptb[:, kc * 128:(kc + 1) * 128], xs[:, t4, kb:kb + 509:4], self.ident[:], [xs, self.ident], [pt], sig=(kc == 7))
                        if t4 % 2 == 0:
                            s.op("act", lambda hh: hh.copy(out=xT[:, :, t4 * 128:(t4 + 1) * 128], in_=ptb.rearrange("p (k t) -> p k t", k=8)), reads=[pt], writes=[xT])
                        else:
                            s.op("dve", lambda hh: hh.tensor_copy(out=xT[:, :, t4 * 128:(t4 + 1) * 128], in_=ptb.rearrange("p (k t) -> p k t", k=8)), reads=[pt], writes=[xT])

                def stage1_fc(b, fc):
                    w1, b1, xT, A = W1[b % 2], B1[b % 2], XT[b % 2], Ab[b % 2]
                    nf = cnt["nf"]
                    cnt["nf"] += 1
                    psG, psU = self.P[2 + (nf % 2) * 2], self.P[3 + (nf % 2) * 2]
                    G, Sg, U = Gt[nf % 2], St[nf % 2], Ut[nf % 2]
                    for kc in range(8):
                        self.mm(psG[:, :], w1[:, kc, fc:D:8], xT[:, kc, :], kc == 0, kc == 7, [w1, xT], [psG], sig=(kc == 7))
                    for kc in range(8):
                        self.mm(psU[:, :], w1[:, kc, D + fc:2 * D:8], xT[:, kc, :], kc == 0, kc == 7, [w1, xT], [psU], sig=(kc == 7))
                    s.op("dve", lambda hh: hh.tensor_scalar(out=G[:], in0=psG[:, :], scalar1=b1[:, fc:fc + 1], scalar2=7.0, op0=ALU.add, op1=ALU.min), reads=[psG, b1], writes=[G])
                    s.op("act", lambda hh: hh.activation(out=Sg[:], in_=G[:], func=AF.Sigmoid, scale=1.702), reads=[G], writes=[Sg])
                    s.op("act", lambda hh: hh.activation(out=U[:], in_=psU[:, :], func=AF.Identity, bias=b1[:, 8 + fc:8 + fc + 1]), reads=[psU, b1], writes=[U])
                    s.op("dve", lambda hh: hh.tensor_scalar(out=U[:], in0=U[:], scalar1=7.0, scalar2=-7.0, op0=ALU.min, op1=ALU.max), reads=[U], writes=[U])
                    s.op("dve", lambda hh: hh.tensor_tensor(out=G[:], in0=G[:], in1=Sg[:], op=ALU.mult), reads=[G, Sg], writes=[G])
                    s.op("dve", lambda hh: hh.scalar_tensor_tensor(out=A[:, fc, :], in0=U[:], scalar=1.0, in1=G[:], op0=ALU.add, op1=ALU.mult), reads=[U, G], writes=[A])

                def stage2_grp(b, g):
                    w2, A = W2[b % 2], Ab[b % 2]
                    t4, dh = g // 2, g % 2
                    ny = cnt["ny"]
                    y = Yt[(ny // 2) % 2]
                    psY = self.P[6 + ny % 2]
                    cnt["ny"] += 1
                    dc = slice(dh * 512, (dh + 1) * 512)
                    for fc in range(8):
                        self.mm(psY[:, :], A[:, fc, t4 * 128:(t4 + 1) * 128], w2[:, fc, dc], fc == 0, fc == 7, [A, w2], [psY], sig=(fc == 7))
                    s.op("act", lambda hh: hh.copy(out=y[:, dc], in_=psY[:, :]), reads=[psY], writes=[y])
                    if dh == 1:
                        r0 = b * 512 + t4 * 128
                        s.dma("sp", Dr["ys_d"][r0:r0 + 128, :], y[:], reads=[y], writes=[DR["ys_d"]])

                load_block(0)
                load_w2(0)
                load_block(1)
                load_w2(1)
                for b in range(NB):
                    if 1 <= b < NB - 1:
                        load_block(b + 1)
                    transposes(b)
                    for i in range(8):
                        stage1_fc(b, i)
                        if b >= 1:
                            stage2_grp(b - 1, i)
                    if 1 <= b < NB - 1:
                        load_w2(b + 1)
                for i in range(8):
                    stage2_grp(NB - 1, i)
                s.barrier()
            with ExitStack() as p3:
                xt = [s.sb([128, D], F32, "xt", p3) for _ in range(2)]
                acc = [s.sb([128, D], F32, "acc3", p3) for _ in range(2)]
                Yk = [s.sb([128, D], F32, "Yk", p3) for _ in range(4)]
                junk = s.sb([128, D], F32, "junk", p3)
                ss = [s.sb([128, 1], F32, "ss", p3) for _ in range(2)]
                if last:
                    fg = s.sb([128, D], F32, "fg", p3)
                    s.dma("sp", fg[:], I["final_g"].broadcast_to([128, D]), writes=[fg])
                nk = 0
                for tt in range(NTT):
                    q = tt // NT
                    grow = slice(tt * 128, (tt + 1) * 128)
                    x, ac, sq = xt[tt % 2], acc[tt % 2], ss[tt % 2]
                    if tt == 0:
                        s.dma("sp", x[:], Dr["xres"][grow, :], reads=[], writes=[x])
                    if tt + 1 < NTT:
                        s.dma("sp", xt[(tt + 1) % 2][:], Dr["xres"][(tt + 1) * 128:(tt + 2) * 128, :], reads=[], writes=[xt[(tt + 1) % 2]])
                    pa = [self.P[(tt % 2) * 2], self.P[(tt % 2) * 2 + 1]]
                    for half in range(2):
                        self.mm(pa[half][:, :], GT[:, tt, :], b2[:, half * 512:(half + 1) * 512], True, True, [GT, b2], [pa[half]])
                    for k in range(4):
                        yk = Yk[nk % 4]
                        nk += 1
                        s.idma(yk[:, :], Dr["ys_d"][:, :], in_off=IDX4[:, tt, k:k + 1], reads=[IDX4, DR["ys_d"]], writes=[yk])
                        for half in range(2):
                            hc = slice(half * 512, (half + 1) * 512)
                            in1 = pa[half][:, :] if k == 0 else ac[:, hc]
                            rd = [yk, G4] + ([pa[half]] if k == 0 else [ac])
                            s.op("dve", lambda hh, in1=in1, hc=hc, yk=yk, k=k: hh.scalar_tensor_tensor(out=ac[:, hc], in0=yk[:, hc], scalar=G4[:, tt, k:k + 1], in1=in1, op0=ALU.mult, op1=ALU.add), reads=rd, writes=[ac])
                    s.op("pool", lambda hh: hh.tensor_tensor(out=ac[:], in0=ac[:], in1=g2[q][:], op=ALU.mult), reads=[ac, g2[q]], writes=[ac])
                    s.op("dve", lambda hh: hh.tensor_tensor(out=x[:], in0=x[:], in1=ac[:], op=ALU.add), reads=[x, ac], writes=[x])
                    if last:
                        self.rstd_of((x, x[:]), (junk, junk[:]), sq)
                        s.op("dve", lambda hh: hh.scalar_tensor_tensor(out=x[:], in0=x[:], scalar=sq[:, 0:1], in1=fg[:], op0=ALU.mult, op1=ALU.mult), reads=[x, sq, fg], writes=[x])
                        s.dma("sp", self.out[grow, :], x[:], reads=[x], writes=[R("out")])
                    else:
                        s.dma("sp", Dr["xres"][grow, :], x[:], reads=[x], writes=[DR["xres"]])
                s.barrier()


_CONSTS = None


def run(inputs, dbg=(), layers=L_DEPTH, nseq=NSEQ, stop_after=None, cores=NCORES, trace=False, only=None):
    global _CONSTS
    if _CONSTS is None:
        _CONSTS = host_consts()
    inp = {k: np.asarray(v) for k, v in inputs.items()}
    k = K(dbg=dbg, layers=layers, nseq=nseq, stop_after=stop_after, only=only)
    nc = k.build()
    in_maps = []
    for c in range(cores):
        m = host_inputs(inp, c)
        m.update(_CONSTS)
        in_maps.append(m)
    res = run_bass_kernel_spmd(nc, in_maps, core_ids=list(range(cores)), trace=trace)
    return res


def kernel(**inputs):
    res = run(inputs)
    out = np.concatenate([np.asarray(r["out"]).reshape(NSEQ, S, D) for r in res.results], axis=0)
    return out.astype(np.float32)
```

```python
import numpy as np
import ml_dtypes
from contextlib import ExitStack
import concourse.bass as bass
import concourse.mybir as mybir
from concourse.bass_utils import run_bass_kernel_spmd

F32 = mybir.dt.float32
BF16 = mybir.dt.bfloat16
I32 = mybir.dt.int32
AF = mybir.ActivationFunctionType
ALU = mybir.AluOpType
AX = mybir.AxisListType
NDSEM = 8


class R:
    __slots__ = ("name", "lw", "rd")

    def __init__(self, name=""):
        self.name = name
        self.lw = {}
        self.rd = {}


class T(R):
    __slots__ = ("t",)

    def __init__(self, t, name=""):
        super().__init__(name)
        self.t = t

    def __getitem__(self, k):
        return self.t[k]


class Eng:
    def __init__(self, name, h, sem, dsems):
        self.name = name
        self.h = h
        self.sem = sem
        self.count = 0
        self.known = {}
        self.dsems = dsems
        self.ndma = 0


class Sched:
    def __init__(self, nc, stack):
        self.nc = nc
        self.stack = stack
        self.E = {}
        for name, h, nd in (("pe", nc.tensor, 0), ("act", nc.scalar, NDSEM), ("dve", nc.vector, 0),
                            ("pool", nc.gpsimd, NDSEM), ("sp", nc.sync, NDSEM)):
            sem = stack.enter_context(nc.semaphore("s_" + name))
            ds = [stack.enter_context(nc.semaphore("d_%s%d" % (name, i))) for i in range(nd)]
            self.E[name] = Eng(name, h, sem, ds)
        self.dma_tokens = []
        self.nuniq = 0

    def sb(self, shape, dt, name=None, stack=None):
        self.nuniq += 1
        name = (name or "t") + "_%d" % self.nuniq
        t = (stack or self.stack).enter_context(self.nc.sbuf_tensor(name, list(shape), dt))
        return T(t, name)

    def ps(self, shape, dt=F32, name=None, stack=None):
        self.nuniq += 1
        name = (name or "p") + "_%d" % self.nuniq
        t = (stack or self.stack).enter_context(self.nc.psum_tensor(name, list(shape), dt))
        return T(t, name)

    def _wait(self, eng, tok):
        sem, val = tok
        if eng.known.get(sem, 0) >= val:
            return
        eng.h.wait_ge(sem, val)
        eng.known[sem] = val

    def _deps(self, eng, reads, writes):
        deps = []
        for r in reads:
            deps.extend(r.lw.items())
        for w in writes:
            deps.extend(w.lw.items())
            deps.extend(w.rd.items())
        for tok in deps:
            if tok[0] is eng.sem:
                if eng.name == "pe":
                    continue
                if tok[1] > eng.count:
                    continue
            self._wait(eng, tok)

    def _commit(self, tok, reads, writes):
        sem, val = tok
        for r in reads:
            if r.rd.get(sem, 0) < val:
                r.rd[sem] = val
        for w in writes:
            if w.lw.get(sem, 0) < val:
                w.lw[sem] = val

    def op(self, engname, fn, reads=(), writes=(), sig=True):
        eng = self.E[engname]
        self._deps(eng, reads, writes)
        ins = fn(eng.h)
        if sig:
            eng.count += 1
            ins.then_inc(eng.sem, 1)
            tok = (eng.sem, eng.count)
        else:
            tok = (eng.sem, eng.count + 1)
        self._commit(tok, reads, writes)
        return tok

    def dma(self, qname, out, in_, reads=(), writes=(), **kw):
        q = self.E[qname]
        i = q.ndma
        q.ndma += 1
        sem = q.dsems[i % NDSEM]
        val = 16 * (i // NDSEM + 1)
        if i >= NDSEM:
            self._wait(q, (sem, val - 16))
        self._deps(q, reads, writes)
        q.h.dma_start(out=out, in_=in_, **kw).then_inc(sem, 16)
        tok = (sem, val)
        self._commit(tok, reads, writes)
        self.dma_tokens.append(tok)
        return tok

    def idma(self, out, in_, out_off=None, in_off=None, reads=(), writes=(), bounds=None):
        q = self.E["pool"]
        i = q.ndma
        q.ndma += 1
        sem = q.dsems[i % NDSEM]
        val = 16 * (i // NDSEM + 1)
        if i >= NDSEM:
            self._wait(q, (sem, val - 16))
        self._deps(q, reads, writes)
        oo = bass.IndirectOffsetOnAxis(ap=out_off, axis=0) if out_off is not None else None
        io = bass.IndirectOffsetOnAxis(ap=in_off, axis=0) if in_off is not None else None
        if bounds is None:
            q.h.indirect_dma_start(out=out, out_offset=oo, in_=in_, in_offset=io).then_inc(sem, 16)
        else:
            if getattr(self, "bound_reg", None) is None:
                self.bound_reg = q.h.alloc_register("bnd")
                q.h.reg_mov(self.bound_reg, 65535)
            q.h.indirect_dma_start(out=out, out_offset=oo, in_=in_, in_offset=io, bounds_check=self.bound_reg, oob_is_err=False).then_inc(sem, 16)
        tok = (sem, val)
        self._commit(tok, reads, writes)
        self.dma_tokens.append(tok)
        return tok

    def barrier(self):
        toks = []
        for e in self.E.values():
            if e.count > 0:
                toks.append((e.sem, e.count))
            for j, s in enumerate(e.dsems):
                n = (e.ndma - j + NDSEM - 1) // NDSEM
                if n > 0:
                    toks.append((s, 16 * n))
        for e in self.E.values():
            for tok in toks:
                if tok[0] is e.sem:
                    continue
                self._wait(e, tok)

    def finish(self):
        sp = self.E["sp"]
        for e in self.E.values():
            for j, s in enumerate(e.dsems):
                n = (e.ndma - j + NDSEM - 1) // NDSEM
                if n > 0:
                    self._wait(sp, (s, 16 * n))


NCORES = 8
L_DEPTH = 2
D = 1024
S = 2048
NSEQ = 2
NT = S // 128
EPS = 1e-5
INW = 6680
SCALE = 0.125
NEG = -30000.0
NE = 32
SLOT_COL = ([0 + 64 * i for i in range(8)] + [512 + 64 * i for i in range(2)] + [768 + 64 * i for i in range(8)]
            + [1280 + 64 * i for i in range(8)] + [2304 + 64 * i for i in range(8)] + [2816 + 64 * i for i in range(2)]
            + [2944 + 64 * i for i in range(2)] + [3072 + 64 * i for i in range(2)] + [3328 + 64 * i for i in range(2)])
NSLOT = len(SLOT_COL)
SL_QA, SL_KA, SL_QB, SL_KB, SL_QC, SL_KCM, SL_VCM, SL_KSL, SL_KWN = 0, 8, 10, 18, 26, 34, 36, 38, 40
C_VA, C_VB, C_VSL, C_VWN, C_GN, C_GM = 640, 1792, 3200, 3456, 3584, 3608


def host_consts():
    bf = ml_dtypes.bfloat16
    c = {}
    t = np.arange(S)
    a_t, b_t = (t // 128).astype(np.float32), (t % 128).astype(np.float32)
    aug = np.zeros((NSLOT, 4, S), np.float32)
    kaug = np.stack([a_t, b_t, np.ones(S, np.float32), np.ones(S, np.float32)])

    def qaug(slope):
        return np.stack([np.full(S, 1024.0 * slope, np.float32), np.full(S, 8.0 * slope, np.float32),
                         -1024.0 * slope * a_t, -8.0 * slope * b_t])
    for i in range(8):
        aug[SL_QA + i] = qaug(2.0 ** -(i + 1))
        aug[SL_QC + i] = qaug(2.0 ** -(i + 1))
        aug[SL_QB + i] = qaug(2.0 ** (-2.0 * (i // 2 + 1)))
        aug[SL_KB + i] = kaug
    for i in range(2):
        aug[SL_KA + i] = kaug
        aug[SL_KSL + i] = kaug
        aug[SL_KWN + i] = kaug
    c["aug"] = aug.astype(bf)
    c["ident"] = np.eye(128, dtype=np.float32).astype(bf)
    c["ident32"] = np.eye(128, dtype=np.float32)
    sk = np.arange(128)[:, None]
    tq = np.arange(128)[None, :]
    c["mdiag"] = np.where(tq >= sk, 0.0, NEG).astype(bf)
    c["medge"] = np.where(tq < sk, 0.0, NEG).astype(bf)
    cc = np.arange(128)[:, None]
    c["cmaskT"] = np.where((16 * cc + 31 <= t[None, :]) & (cc < 127), 0.0, NEG).astype(bf)
    nb = np.arange(32)
    c["eblk"] = (t[None, :] // 64 == nb[:, None]).astype(np.float32).astype(bf)
    cur = t // 64
    forced = (nb[None, :] == 0) | (nb[None, :] == cur[:, None]) | (nb[None, :] == cur[:, None] - 1)
    causal = nb[None, :] <= cur[:, None]
    A = np.where(forced, 1e9 + 1e6 * nb[None, :], np.where(causal, 0.0, -1e9 - 1e6 * nb[None, :]))
    c["atab"] = A.astype(np.float32)
    cstart = np.arange(127) * 16
    sstart = nb * 64
    ov = np.clip(np.minimum(cstart[:, None] + 32, sstart[None, :] + 64) - np.maximum(cstart[:, None], sstart[None, :]), 0, None) / 32.0
    ovp = np.zeros((128, 32), np.float32)
    ovp[:127] = ov
    c["overlap"] = ovp.astype(bf)
    c["ltri"] = (np.arange(128)[:, None] < np.arange(128)[None, :]).astype(np.float32).astype(bf)
    c["ones128"] = np.ones((128, 128), np.float32).astype(bf)
    c["b512"] = np.broadcast_to((512.0 * np.arange(64, dtype=np.float32))[None, :], (128, 64)).copy()
    c["rowiota"] = (np.arange(8, dtype=np.float32)[None, :] * 128 + np.arange(128, dtype=np.float32)[:, None]).copy()
    c["piota"] = np.arange(128, dtype=np.float32).reshape(128, 1).copy()
    return c


def host_inputs(inp, core):
    b0 = core * NSEQ
    m = {}
    m["x"] = np.ascontiguousarray(inp["x"][b0:b0 + NSEQ].reshape(NSEQ * S, D))
    m["cT"] = np.ascontiguousarray(inp["c"][b0:b0 + NSEQ].T)
    for k in ("mod_w", "mod_b", "norm1_g", "norm2_g", "w_in", "b_in", "sinks", "diff_subln_g", "cmp_w1", "cmp_w2",
              "cmp_b2", "w_branch", "w_out", "router_w", "router_b", "exp_b2"):
        m[k] = inp[k]
    m["final_g"] = inp["final_g"].reshape(1, D)
    m["b_inT"] = np.ascontiguousarray(np.stack([inp["b_in"][:, SLOT_COL[2 * i]:SLOT_COL[2 * i] + 128] for i in range(NSLOT // 2)], axis=2))
    m["diff_lambda"] = inp["diff_lambda"].reshape(L_DEPTH, 256)
    m["cmp_posT"] = np.ascontiguousarray(inp["cmp_pos"].transpose(0, 1, 3, 2))
    m["cmp_b1T"] = np.ascontiguousarray(inp["cmp_b1"].reshape(L_DEPTH, 2, 2, 128).transpose(0, 1, 3, 2))
    m["cmp_b2T"] = np.ascontiguousarray(inp["cmp_b2"].reshape(L_DEPTH, 2, 64, 1))
    m["exp_b1E"] = np.ascontiguousarray(inp["exp_b1"].reshape(L_DEPTH, NE, 2, 128, 8).transpose(0, 1, 3, 2, 4)).reshape(L_DEPTH * NE * 128, 16)
    m["exp_w1"] = inp["exp_w1"].reshape(L_DEPTH * NE * 256, 4 * 2 * D)
    m["exp_w2"] = inp["exp_w2"].reshape(L_DEPTH * NE * 128, 8 * D)
    return m


IN_SHAPES = {
    "x": ([NSEQ * S, D], F32), "cT": ([D, NSEQ], F32), "mod_w": ([L_DEPTH, D, 6 * D], F32), "mod_b": ([L_DEPTH, 6 * D], F32),
    "norm1_g": ([L_DEPTH, D], F32), "norm2_g": ([L_DEPTH, D], F32), "w_in": ([L_DEPTH, D, INW], F32), "b_in": ([L_DEPTH, INW], F32),
    "sinks": ([L_DEPTH, 8], F32), "diff_subln_g": ([L_DEPTH, 128], F32), "cmp_w1": ([L_DEPTH, 2, 2048, 256], F32),
    "cmp_w2": ([L_DEPTH, 2, 256, 64], F32), "cmp_b2": ([L_DEPTH, 2, 64], F32), "w_branch": ([L_DEPTH, 3, 512, D], F32),
    "w_out": ([L_DEPTH, D, D], F32), "router_w": ([L_DEPTH, D, NE], F32), "router_b": ([L_DEPTH, NE], F32),
    "exp_w1": ([L_DEPTH * NE * 256, 8 * D], F32), "exp_w2": ([L_DEPTH * NE * 128, 8 * D], F32), "exp_b2": ([L_DEPTH, NE, D], F32),
    "final_g": ([1, D], F32), "b_inT": ([L_DEPTH, 128, NSLOT // 2], F32), "diff_lambda": ([L_DEPTH, 256], F32),
    "cmp_posT": ([L_DEPTH, 2, 64, 32], F32), "cmp_b1T": ([L_DEPTH, 2, 128, 2], F32), "cmp_b2T": ([L_DEPTH, 2, 64, 1], F32),
    "exp_b1E": ([L_DEPTH * NE * 128, 16], F32),
    "ltri": ([128, 128], BF16), "ones128": ([128, 128], BF16), "b512": ([128, 64], F32), "rowiota": ([128, 8], F32), "piota": ([128, 1], F32),
    "aug": ([NSLOT, 4, S], BF16), "ident": ([128, 128], BF16), "ident32": ([128, 128], F32), "mdiag": ([128, 128], BF16),
    "medge": ([128, 128], BF16), "cmaskT": ([128, S], BF16), "eblk": ([32, S], BF16), "atab": ([S, 32], F32),
    "overlap": ([128, 32], BF16),
}


def bcast_rows(ap1d_row, nparts):
    return ap1d_row.broadcast_to([nparts, ap1d_row.shape[-1]])


class K:
    def __init__(self, dbg=(), layers=L_DEPTH, nseq=NSEQ, stop_after=None, only=None):
        self.dbg = set(dbg)
        self.only = only
        self.layers = layers
        self.nseq = nseq
        self.stop_after = stop_after
        nc = self.nc = bass.Bass("TRN2", target_bir_lowering=False)
        self.I = {k: nc.dram_tensor(k, list(sh), dt, kind="ExternalInput").ap() for k, (sh, dt) in IN_SHAPES.items()}
        self.out = nc.dram_tensor("out", [NSEQ * S, D], F32, kind="ExternalOutput").ap()
        self.Dr = {}
        self.DR = {}

    def dram(self, name, shape, dt):
        kind = "ExternalOutput" if name in self.dbg else "Internal"
        self.Dr[name] = self.nc.dram_tensor(name, list(shape), dt, kind=kind).ap()
        self.DR[name] = R(name)
        return self.Dr[name]

    def mm(self, out, lhsT, rhs, start, stop, reads, writes, sig=True):
        return self.s.op("pe", lambda h: h.matmul(out, lhsT=lhsT, rhs=rhs, start=start, stop=stop), reads=reads, writes=writes, sig=sig)

    def tr(self, out, in_, ident, reads, writes, sig=True):
        return self.s.op("pe", lambda h: h.transpose(out=out, in_=in_, identity=ident), reads=reads, writes=writes, sig=sig)

    def rstd_of(self, xt, junk, ss, n=D):
        s = self.s
        s.op("act", lambda h: h.activation(out=junk[1], in_=xt[1], func=AF.Square, accum_out=ss[:]), reads=[xt[0]], writes=[junk[0], ss])
        s.op("dve", lambda h: h.tensor_scalar(out=ss[:], in0=ss[:], scalar1=1.0 / n, scalar2=EPS, op0=ALU.mult, op1=ALU.add), reads=[ss], writes=[ss])
        s.op("act", lambda h: h.sqrt(out=ss[:], in_=ss[:]), reads=[ss], writes=[ss])
        s.op("dve", lambda h: h.reciprocal(out=ss[:], in_=ss[:]), reads=[ss], writes=[ss])

    def build(self):
        nc = self.nc
        with ExitStack() as st:
            s = self.s = Sched(nc, st)
            self.P = [s.ps([128, 512], F32, "bank%d" % i) for i in range(8)]
            self.ident = s.sb([128, 128], BF16, "ident")
            self.ident32 = s.sb([128, 128], F32, "ident32")
            s.dma("sp", self.ident[:], self.I["ident"], writes=[self.ident])
            s.dma("sp", self.ident32[:], self.I["ident32"], writes=[self.ident32])
            self.dram("mod_d", [L_DEPTH, NSEQ, 6 * D], F32)
            self.dram("xres", [NSEQ * S, D], F32)
            self.dram("QT_d", [NSLOT, 64, S], BF16)
            self.dram("VA_d", [S, 2, 65], BF16)
            self.dram("VB_d", [S, 4, 129], BF16)
            self.dram("VSL_d", [S, 2, 65], BF16)
            self.dram("VWN_d", [S, 2, 65], BF16)
            self.dram("GN_d", [S, 24], F32)
            self.dram("GM_d", [S, 3072], F32)
            self.dram("KC_d", [2, 64, 128], BF16)
            self.dram("VC_d", [2, 128, 97], BF16)
            self.dram("O_d", [S, 1536], BF16)
            self.ntt = self.nseq * NT
            self.nblk = self.ntt + NE
            self.dram("xs_d", [self.nblk * 512, D], BF16)
            self.dram("ys_d", [self.nblk * 512, D], F32)
            self.dram("dbg_d", [128, 4096], F32)
            zt = s.sb([128, 2048], BF16, "zt")
            s.op("pool", lambda h: h.memset(zt[:], 0.0), writes=[zt])
            xv = self.Dr["xs_d"].rearrange("(a p r) d -> a p (r d)", p=128, r=2)
            for a in range(xv.shape[0]):
                s.dma("pool", xv[a], zt[:], reads=[zt], writes=[])
            try:
                self.body()
            except StopIteration:
                pass
            s.barrier()
            s.finish()
        return nc

    def phase_end(self, name):
        self.s.barrier()
        if self.stop_after == name:
            raise StopIteration

    def body(self):
        for l in range(self.layers):
            self.runp("mod%d" % l, self.phase_mod, l)
            for q in range(self.nseq):
                for nm, fn in (("inproj", self.phase_inproj), ("cmp", self.phase_cmp), ("swa", self.phase_swa), ("diff", self.phase_diff),
                               ("nsa", self.phase_nsa), ("merge", self.phase_merge)):
                    self.runp("%s%d_%d" % (nm, l, q), fn, l, q)
            self.runp("moe%d" % l, self.phase_moe2, l)

    def runp(self, name, fn, *args):
        if self.only is None or name in self.only:
            fn(*args)
        self.phase_end(name)

    def phase_mod(self, l):
        s, I = self.s, self.I
        with ExitStack() as ps:
            cT = s.sb([128, 8, NSEQ], F32, "cT", ps)
            cs = s.sb([128, 8, NSEQ], F32, "cs", ps)
            modb = s.sb([NSEQ, 6 * D], F32, "modb", ps)
            mods = s.sb([NSEQ, 6 * D], F32, "mods", ps)
            wb = [s.sb([128, 8, 512], F32, "modw", ps) for _ in range(2)]
            s.dma("sp", cT[:], I["cT"].rearrange("(kc p) b -> p kc b", p=128), writes=[cT])
            s.dma("sp", modb[:], I["mod_b"][l:l + 1, :].broadcast_to([NSEQ, 6 * D]), writes=[modb])
            s.op("act", lambda h: h.activation(out=cs[:], in_=cT[:], func=AF.Silu), reads=[cT], writes=[cs])
            wsrc = I["mod_w"][l].rearrange("(kc p) n -> p kc n", p=128)
            for cg in range(12):
                w = wb[cg % 2]
                s.dma("sp", w[:], wsrc[:, :, cg * 512:(cg + 1) * 512], writes=[w])
                pm = self.P[cg % 2]
                for kc in range(8):
                    self.mm(pm[0:NSEQ, :], cs[:, kc, :], w[:, kc, :], kc == 0, kc == 7, [cs, w], [pm], sig=(kc == 7))
                s.op("dve", lambda h: h.tensor_tensor(out=mods[:, cg * 512:(cg + 1) * 512], in0=pm[0:NSEQ, :], in1=modb[:, cg * 512:(cg + 1) * 512], op=ALU.add),
                     reads=[pm, modb], writes=[mods])
            for seg in (1, 4):
                s.op("dve", lambda h: h.tensor_scalar_add(out=mods[:, seg * D:(seg + 1) * D], in0=mods[:, seg * D:(seg + 1) * D], scalar1=1.0), reads=[mods], writes=[mods])
            s.dma("sp", self.Dr["mod_d"][l], mods[:], reads=[mods], writes=[self.DR["mod_d"]])

    def mod_bc(self, l, q, seg, tile):
        src = self.Dr["mod_d"][l, q:q + 1, seg * D:(seg + 1) * D].broadcast_to([128, D])
        self.s.dma("sp", tile[:], src, reads=[self.DR["mod_d"]], writes=[tile])

    def xsrc(self, l, q):
        return (self.I["x"] if l == 0 else self.Dr["xres"]), ([] if l == 0 else [self.DR["xres"]])

    def norm_tiles(self, ps, l, q, gname, seg_sc, seg_sh):
        s, I = self.s, self.I
        gsc = s.sb([128, D], F32, "gsc", ps)
        sh = s.sb([128, D], F32, "sh", ps)
        gt = s.sb([128, D], F32, "gt", ps)
        s.dma("sp", gt[:], I[gname][l:l + 1, :].broadcast_to([128, D]), writes=[gt])
        self.mod_bc(l, q, seg_sc, gsc)
        self.mod_bc(l, q, seg_sh, sh)
        s.op("dve", lambda h: h.tensor_tensor(out=gsc[:], in0=gsc[:], in1=gt[:], op=ALU.mult), reads=[gsc, gt], writes=[gsc])
        return gsc, sh

    def phase_inproj(self, l, q):
        s, I, Dr, DR = self.s, self.I, self.Dr, self.DR
        xsrc, xres_r = self.xsrc(l, q)
        with ExitStack() as ps:
            gsc, sh = self.norm_tiles(ps, l, q, "norm1_g", 1, 0)
            hT = s.sb([128, 8, S], BF16, "hT", ps)
            hTr = [R("hT%d" % i) for i in range(4)]
            xt = [s.sb([128, D], F32, "xt", ps) for _ in range(2)]
            junk = s.sb([128, D], F32, "junk", ps)
            hb = [s.sb([128, D], BF16, "hb", ps) for _ in range(2)]
            ss = [s.sb([128, 1], F32, "ss", ps) for _ in range(2)]
            for tt in range(NT):
                x, h, sq = xt[tt % 2], hb[tt % 2], ss[tt % 2]
                r0 = q * S + tt * 128
                s.dma("sp", x[:], xsrc[r0:r0 + 128, :], reads=xres_r, writes=[x])
                self.rstd_of((x, x[:]), (junk, junk[:]), sq)
                s.op("dve", lambda hh: hh.scalar_tensor_tensor(out=x[:], in0=x[:], scalar=sq[:, 0:1], in1=gsc[:], op0=ALU.mult, op1=ALU.mult), reads=[x, sq, gsc], writes=[x])
                s.op("pool", lambda hh: hh.tensor_tensor(out=h[:], in0=x[:], in1=sh[:], op=ALU.add), reads=[x, sh], writes=[h])
                pt = self.P[tt % 2]
                ptb = pt[:].bitcast(BF16)
                for kc in range(8):
                    self.tr(ptb[:, kc * 128:(kc + 1) * 128], h[:, kc * 128:(kc + 1) * 128], self.ident[:], [h, self.ident], [pt], sig=(kc == 7))
                s.op("act", lambda hh: hh.copy(out=hT[:, :, tt * 128:(tt + 1) * 128], in_=ptb.rearrange("p (k t) -> p k t", k=8)), reads=[pt], writes=[hTr[tt // 4]])
            binT = s.sb([128, NSLOT // 2], F32, "binT", ps)
            s.dma("sp", binT[:], I["b_inT"][l], writes=[binT])
            wsrc = I["w_in"][l].rearrange("(kc p) n -> p kc n", p=128)
            wbuf = [s.sb([128, 8, 512], BF16, "wblk", ps) for _ in range(2)]
            stg = [s.sb([128, S], BF16, "stg", ps) for _ in range(2)]
            groups = [(SL_QA, 8), (SL_KA, 2), (SL_QB, 8), (SL_KB, 8), (SL_QC, 8), (SL_KCM, 4), (SL_KSL, 2), (SL_KWN, 2)]
            nw = 0
            nmm = 0
            for (s0, ns) in groups:
                w = wbuf[nw % 2]
                nw += 1
                c0 = SLOT_COL[s0]
                s.dma("pool", w[:, :, 0:ns * 64], wsrc[:, :, c0:c0 + ns * 64], writes=[w])
                for si in range(0, ns, 2):
                    slot = s0 + si
                    pr_ = slot // 2
                    sg = stg[pr_ % 2]
                    for tg in range(4):
                        pm = self.P[2 + nmm % 4]
                        nmm += 1
                        for kc in range(8):
                            self.mm(pm[:, :], w[:, kc, si * 64:(si + 2) * 64], hT[:, kc, tg * 512:(tg + 1) * 512], kc == 0, kc == 7, [w, hTr[tg]], [pm], sig=(kc == 7))
                        s.op("act", lambda hh: hh.activation(out=sg[:, tg * 512:(tg + 1) * 512], in_=pm[:, :], func=AF.Identity, bias=binT[:, pr_:pr_ + 1]),
                             reads=[pm, binT], writes=[sg])
                    s.dma("sp", Dr["QT_d"][slot], sg[0:64, :], reads=[sg], writes=[DR["QT_d"]])
                    s.dma("sp", Dr["QT_d"][slot + 1], sg[64:128, :], reads=[sg], writes=[DR["QT_d"]])
            binb = s.sb([128, INW], F32, "binb", ps)
            s.dma("sp", binb[:], I["b_in"][l:l + 1, :].broadcast_to([128, INW]), writes=[binb])
            vt = {}
            for nm, nh, dv in (("VA_d", 2, 64), ("VB_d", 4, 128), ("VSL_d", 2, 64), ("VWN_d", 2, 64)):
                vt[nm] = [s.sb([128, nh, dv + 1], BF16, "vt", ps) for _ in range(2)]
                for v in vt[nm]:
                    s.op("pool", lambda hh: hh.memset(v[:, :, dv:dv + 1], 1.0), writes=[v])
            gnt = [s.sb([128, 24], F32, "gnt", ps) for _ in range(2)]
            gmt = [s.sb([128, 512], F32, "gmt", ps) for _ in range(2)]
            blocks = [("VA_d", C_VA, 128, 2, 64), ("VB_d", C_VB, 512, 4, 128), ("VSL_d", C_VSL, 128, 2, 64), ("VWN_d", C_VWN, 128, 2, 64),
                      ("GN_d", C_GN, 24, 0, 0)] + [("GM_d", C_GM + 512 * i, 512, i, 0) for i in range(6)]
            for (nm, c0, ncol, nh, dv) in blocks:
                w = wbuf[nw % 2]
                nw += 1
                s.dma("pool", w[:, :, 0:ncol], wsrc[:, :, c0:c0 + ncol], writes=[w])
                for tt in range(NT):
                    pm = self.P[2 + nmm % 4]
                    nmm += 1
                    for kc in range(8):
                        self.mm(pm[:, 0:ncol], hT[:, kc, tt * 128:(tt + 1) * 128], w[:, kc, 0:ncol], kc == 0, kc == 7, [w, hTr[tt // 4]], [pm], sig=(kc == 7))
                    rows = slice(tt * 128, (tt + 1) * 128)
                    if nm == "GN_d":
                        g = gnt[tt % 2]
                        s.op("dve", lambda hh: hh.tensor_tensor(out=g[:], in0=pm[:, 0:24], in1=binb[:, c0:c0 + 24], op=ALU.add), reads=[pm, binb], writes=[g])
                        s.op("act", lambda hh: hh.activation(out=g[:], in_=g[:], func=AF.Sigmoid), reads=[g], writes=[g])
                        s.dma("sp", Dr["GN_d"][rows, :], g[:], reads=[g], writes=[DR["GN_d"]])
                    elif nm == "GM_d":
                        g = gmt[tt % 2]
                        s.op("dve", lambda hh: hh.tensor_tensor(out=g[:], in0=pm[:, :], in1=binb[:, c0:c0 + 512], op=ALU.add), reads=[pm, binb], writes=[g])
                        s.op("act", lambda hh: hh.activation(out=g[:], in_=g[:], func=AF.Sigmoid), reads=[g], writes=[g])
                        s.dma("sp", Dr["GM_d"][rows, nh * 512:(nh + 1) * 512], g[:], reads=[g], writes=[DR["GM_d"]])
                    else:
                        v = vt[nm][tt % 2]
                        s.op("dve", lambda hh: hh.tensor_tensor(out=v[:, :, 0:dv], in0=pm[:, 0:ncol].rearrange("p (h d) -> p h d", d=dv),
                                                               in1=binb[:, c0:c0 + ncol].rearrange("p (h d) -> p h d", d=dv), op=ALU.add), reads=[pm, binb], writes=[v])
                        s.dma("sp", Dr[nm][rows], v[:], reads=[v], writes=[DR[nm]])

    def phase_cmp(self, l, q):
        s, I, Dr, DR = self.s, self.I, self.Dr, self.DR
        with ExitStack() as ps:
            ovl = self.load_const(ps, "overlap", [128, 32], BF16)
            w1 = s.sb([64, 32, 256], BF16, "cw1", ps)
            w2 = s.sb([128, 2, 64], BF16, "cw2", ps)
            posT = s.sb([64, 32], F32, "posT", ps)
            posb = s.sb([64, 32], BF16, "posb", ps)
            b1T = s.sb([128, 2], F32, "b1T", ps)
            bias = s.sb([128, 2], F32, "cbias", ps)
            b2T = s.sb([64, 1], F32, "b2T", ps)
            b2b = s.sb([128, 64], F32, "b2b", ps)
            xT = s.sb([64, S], BF16, "cxT", ps)
            hid = s.sb([128, 2, 128], BF16, "hid", ps)
            y = s.sb([128, 128], F32, "cy", ps)
            u = s.sb([128, 128], F32, "cu", ps)
            kct = s.sb([64, 128], BF16, "kct", ps)
            vct = s.sb([128, 97], BF16, "vct", ps)
            s.op("pool", lambda h: h.memset(kct[:], 0.0), writes=[kct])
            s.op("pool", lambda h: h.memset(vct[:], 0.0), writes=[vct])
            for which in range(2):
                s.dma("pool", w1[:], I["cmp_w1"][l, which].rearrange("(l d) f -> d l f", d=64), writes=[w1])
                s.dma("pool", w2[:], I["cmp_w2"][l, which].rearrange("(c p) d -> p c d", p=128), writes=[w2])
                s.dma("sp", posT[:], I["cmp_posT"][l, which], writes=[posT])
                s.dma("sp", b1T[:], I["cmp_b1T"][l, which], writes=[b1T])
                s.dma("sp", b2T[:], I["cmp_b2T"][l, which], writes=[b2T])
                s.dma("sp", b2b[:], I["cmp_b2"][l, which:which + 1, :].broadcast_to([128, 64]), writes=[b2b])
                s.op("act", lambda h: h.copy(out=posb[:], in_=posT[:]), reads=[posT], writes=[posb])
                for ch in range(2):
                    pm = self.P[ch]
                    for li in range(32):
                        self.mm(pm[:, 0:1], w1[:, li, ch * 128:(ch + 1) * 128], posb[:, li:li + 1], li == 0, li == 31, [w1, posb], [pm], sig=(li == 31))
                    s.op("dve", lambda h: h.tensor_tensor(out=bias[:, ch:ch + 1], in0=pm[:, 0:1], in1=b1T[:, ch:ch + 1], op=ALU.add), reads=[pm, b1T], writes=[bias])
                for hk in range(2):
                    s.dma("sp", xT[:], Dr["QT_d"][SL_KCM + which * 2 + hk], reads=[DR["QT_d"]], writes=[xT])
                    for ch in range(2):
                        pm = self.P[2 + ch]
                        for li in range(32):
                            self.mm(pm[:, 0:127], w1[:, li, ch * 128:(ch + 1) * 128], xT[:, li:li + 16 * 126 + 1:16], li == 0, li == 31, [w1, xT], [pm], sig=(li == 31))
                        s.op("act", lambda h: h.activation(out=y[:, 0:127], in_=pm[:, 0:127], func=AF.Identity, bias=bias[:, ch:ch + 1]), reads=[pm, bias], writes=[y])
                        s.op("dve", lambda h: h.tensor_tensor(out=u[:, 0:127], in0=y[:, 0:127], in1=y[:, 0:127], op=ALU.mult), reads=[y], writes=[u])
                        s.op("dve", lambda h: h.tensor_scalar(out=u[:, 0:127], in0=u[:, 0:127], scalar1=0.044715, scalar2=1.0, op0=ALU.mult, op1=ALU.add), reads=[u], writes=[u])
                        s.op("dve", lambda h: h.tensor_tensor(out=u[:, 0:127], in0=u[:, 0:127], in1=y[:, 0:127], op=ALU.mult), reads=[u, y], writes=[u])
                        s.op("act", lambda h: h.activation(out=u[:, 0:127], in_=u[:, 0:127], func=AF.Sigmoid, scale=1.5957691216057308), reads=[u], writes=[u])
                        s.op("dve", lambda h: h.tensor_tensor(out=hid[:, ch, 0:127], in0=u[:, 0:127], in1=y[:, 0:127], op=ALU.mult), reads=[u, y], writes=[hid])
                    pm2 = self.P[4 + hk]
                    if which == 0:
                        for ch in range(2):
                            self.mm(pm2[0:64, 0:127], w2[:, ch, :], hid[:, ch, 0:127], ch == 0, ch == 1, [w2, hid], [pm2], sig=(ch == 1))
                        s.op("act", lambda h: h.activation(out=kct[:, 0:127], in_=pm2[0:64, 0:127], func=AF.Identity, bias=b2T[:, 0:1]), reads=[pm2, b2T], writes=[kct])
                        s.dma("sp", Dr["KC_d"][hk], kct[:], reads=[kct], writes=[DR["KC_d"]])
                    else:
                        for ch in range(2):
                            self.mm(pm2[0:127, 0:64], hid[:, ch, 0:127], w2[:, ch, :], ch == 0, ch == 1, [w2, hid], [pm2], sig=(ch == 1))
                        s.op("dve", lambda h: h.tensor_tensor(out=vct[0:127, 0:64], in0=pm2[0:127, 0:64], in1=b2b[0:127, :], op=ALU.add), reads=[pm2, b2b], writes=[vct])
                        s.op("pool", lambda h: h.memset(vct[:, 64:65], 1.0), writes=[vct])
                        s.op("pool", lambda h: h.tensor_copy(out=vct[:, 65:97], in_=ovl[:]), reads=[ovl], writes=[vct])
                        s.dma("sp", Dr["VC_d"][hk], vct[:], reads=[vct], writes=[DR["VC_d"]])

    def load_heads(self, tile, slots, grouped):
        s = self.s
        for g, slot in enumerate(slots):
            dst0 = tile[0:64, g, :] if grouped else tile[0:64, :]
            dst1 = tile[64:68, g, :] if grouped else tile[64:68, :]
            s.dma("sp", dst0, self.Dr["QT_d"][slot], reads=[self.DR["QT_d"]], writes=[tile])
            s.dma("sp", dst1, self.I["aug"][slot], writes=[tile])

    def attn_stream(self, items, banks, pts):
        s = self.s
        n = len(items)

        def emit_qk(k):
            it = items[k]
            sc = banks[k % len(banks)]
            nq = len(it["qk"])
            for m, (outfn, lhsT, rhs, start, stop, reads) in enumerate(it["qk"]):
                self.mm(outfn(sc), lhsT, rhs, start, stop, reads, [sc], sig=(m == nq - 1))

        emit_qk(0)
        if n > 1:
            emit_qk(1)
        for k in range(n):
            if k + 2 < n:
                emit_qk(k + 2)
            it = items[k]
            sc = banks[k % len(banks)]
            pT = pts[k % len(pts)]
            kp, N = it["kp"], it["N"]
            s.op("act", lambda h: h.activation(out=pT[0:kp, 0:N], in_=sc[0:kp, 0:N], func=AF.Exp, scale=SCALE), reads=[sc], writes=[pT])
            it["pv"](pT)

    def load_const(self, ps, name, shape, dt):
        t = self.s.sb(shape, dt, name, ps)
        self.s.dma("sp", t[:], self.I[name], writes=[t])
        return t

    def phase_swa(self, l, q):
        s, I, Dr, DR = self.s, self.I, self.Dr, self.DR
        ident = self.ident
        with ExitStack() as ps:
            mdiag = self.load_const(ps, "mdiag", [128, 128], BF16)
            medge = self.load_const(ps, "medge", [128, 128], BF16)
            esink = s.sb([128, 8], F32, "esink", ps)
            s.dma("sp", esink[:], I["sinks"][l:l + 1, :].broadcast_to([128, 8]), writes=[esink])
            s.op("act", lambda h: h.activation(out=esink[:], in_=esink[:], func=AF.Exp), reads=[esink], writes=[esink])
            QG = s.sb([68, 4, S], BF16, "QG", ps)
            KT = s.sb([68, S], BF16, "KT", ps)
            V = s.sb([128, NT, 65], BF16, "V", ps)
            pts = [s.sb([128, 512], BF16, "pT", ps) for _ in range(3)]
            ot = [s.sb([128, 4, 64], BF16, "ot", ps) for _ in range(2)]
            den = [s.sb([128, 4], F32, "den", ps) for _ in range(2)]
            for hk in range(2):
                self.load_heads(QG, [SL_QA + hk * 4 + g for g in range(4)], True)
                self.load_heads(KT, [SL_KA + hk], False)
                s.dma("sp", V[:], Dr["VA_d"][:, hk, :].rearrange("(n p) e -> p n e", p=128), reads=[DR["VA_d"]], writes=[V])
                for j in range(NT):
                    acc = self.P[4 + j % 2]
                    tiles = [i for i in (j - 1, j) if i >= 0]
                    items = []
                    for idx, i in enumerate(tiles):
                        mask = mdiag if i == j else medge
                        o3 = lambda sc: sc[:, :].rearrange("p (g t) -> p g t", g=4)
                        qk = [(o3, KT[:, i * 128:(i + 1) * 128], QG[:, :, j * 128:(j + 1) * 128], True, False, [KT, QG]),
                              (o3, ident[:], mask[:].unsqueeze(1).broadcast_to([128, 4, 128]), False, True, [ident, mask])]

                        def pv(pT, i=i, idx=idx, acc=acc, last=len(tiles) - 1):
                            for g in range(4):
                                self.mm(acc[:, g * 65:(g + 1) * 65], pT[:, g * 128:(g + 1) * 128], V[:, i, :], idx == 0 and g == 0, idx == last, [pT, V], [acc], sig=(g == 3))
                        items.append(dict(kp=128, N=512, qk=qk, pv=pv))
                    self.attn_stream(items, self.P[0:3], pts)
                    a3 = acc[:, 0:260].rearrange("p (g e) -> p g e", e=65)
                    dn, o = den[j % 2], ot[j % 2]
                    s.op("dve", lambda h: h.tensor_tensor(out=dn[:].unsqueeze(2), in0=a3[:, :, 64:65], in1=esink[:, hk * 4:(hk + 1) * 4].unsqueeze(2), op=ALU.add), reads=[acc, esink], writes=[dn])
                    s.op("dve", lambda h: h.reciprocal(out=dn[:], in_=dn[:]), reads=[dn], writes=[dn])
                    s.op("dve", lambda h: h.tensor_tensor(out=o[:], in0=a3[:, :, 0:64], in1=dn[:].unsqueeze(2).broadcast_to([128, 4, 64]), op=ALU.mult), reads=[acc, dn], writes=[o])
                    s.dma("sp", Dr["O_d"][j * 128:(j + 1) * 128, hk * 256:(hk + 1) * 256], o[:].rearrange("p g d -> p (g d)"), reads=[o], writes=[DR["O_d"]])

    def phase_diff(self, l, q):
        s, I, Dr, DR = self.s, self.I, self.Dr, self.DR
        ident = self.ident
        lam_init = 0.8 - 0.6 * float(np.exp(-0.3 * l))
        with ExitStack() as ps:
            mdiag = self.load_const(ps, "mdiag", [128, 128], BF16)
            dl = s.sb([128, 256], F32, "dl", ps)
            s.dma("sp", dl[:], I["diff_lambda"][l:l + 1, :].broadcast_to([128, 256]), writes=[dl])
            pr = s.sb([128, 2, 64], F32, "pr", ps)
            d4 = dl[:].rearrange("p (a b d) -> p a b d", a=2, b=2)
            s.op("dve", lambda h: h.tensor_tensor(out=pr[:], in0=d4[:, :, 0, :], in1=d4[:, :, 1, :], op=ALU.mult), reads=[dl], writes=[pr])
            e2 = s.sb([128, 2], F32, "e2", ps)
            s.op("dve", lambda h: h.reduce_sum(out=e2[:], in_=pr[:], axis=AX.X), reads=[pr], writes=[e2])
            s.op("act", lambda h: h.activation(out=e2[:], in_=e2[:], func=AF.Exp), reads=[e2], writes=[e2])
            nlam = s.sb([128, 1], F32, "nlam", ps)
            s.op("dve", lambda h: h.scalar_tensor_tensor(out=nlam[:], in0=e2[:, 0:1], scalar=lam_init, in1=e2[:, 1:2], op0=ALU.add, op1=ALU.subtract), reads=[e2], writes=[nlam])
            s.op("dve", lambda h: h.tensor_scalar_mul(out=nlam[:], in0=nlam[:], scalar1=-1.0), reads=[nlam], writes=[nlam])
            gsub = s.sb([128, 128], F32, "gsub", ps)
            s.dma("sp", gsub[:], I["diff_subln_g"][l:l + 1, :].broadcast_to([128, 128]), writes=[gsub])
            s.op("dve", lambda h: h.tensor_scalar_mul(out=gsub[:], in0=gsub[:], scalar1=1.0 - lam_init), reads=[gsub], writes=[gsub])
            KT = [s.sb([68, S], BF16, "KTb", ps) for _ in range(2)]
            QT = [s.sb([68, S], BF16, "QTb", ps) for _ in range(2)]
            V = s.sb([128, NT, 129], BF16, "Vb", ps)
            pts = [s.sb([128, 512], BF16, "pT", ps) for _ in range(3)]
            t0 = [s.sb([128, 128], F32, "t0", ps) for _ in range(2)]
            junk = s.sb([128, 128], F32, "junkb", ps)
            rr = [s.sb([128, 4], F32, "rr", ps) for _ in range(2)]
            ob = [s.sb([128, 128], BF16, "ob", ps) for _ in range(2)]
            accc = [[s.sb([128, 258], F32, "accc", ps) for _ in range(4)] for _ in range(2)]
            nfin = 0
            for hd in range(4):
                for c in range(2):
                    self.load_heads(KT[c], [SL_KB + 2 * hd + c], False)
                    self.load_heads(QT[c], [SL_QB + 2 * hd + c], False)
                s.dma("sp", V[:], Dr["VB_d"][:, hd, :].rearrange("(n p) e -> p n e", p=128), reads=[DR["VB_d"]], writes=[V])
                for G in range(4):
                    def accap(c, jj):
                        return self.P[4 + c * 2 + jj // 2], (jj % 2) * 129
                    items = []
                    for c in range(2):
                        for i in range(0, 4 * G + 4):
                            j0 = max(i, 4 * G)
                            N = (4 * G + 4 - j0) * 128
                            kt = KT[c][:, i * 128:(i + 1) * 128]
                            qk = []
                            if i >= 4 * G:
                                qk.append((lambda sc: sc[:, 0:128], kt, QT[c][:, j0 * 128:(j0 + 1) * 128], True, False, [KT[c], QT[c]]))
                                qk.append((lambda sc: sc[:, 0:128], ident[:], mdiag[:], False, True, [ident, mdiag]))
                                if N > 128:
                                    qk.append((lambda sc, N=N: sc[:, 128:N], kt, QT[c][:, (j0 + 1) * 128:(4 * G + 4) * 128], True, True, [KT[c], QT[c]]))
                            else:
                                qk.append((lambda sc, N=N: sc[:, 0:N], kt, QT[c][:, j0 * 128:(4 * G + 4) * 128], True, True, [KT[c], QT[c]]))

                            def pv(pT, c=c, i=i, j0=j0, G=G):
                                for jj in range(j0, 4 * G + 4):
                                    bank, off = accap(c, jj - 4 * G)
                                    self.mm(bank[:, off:off + 129], pT[:, (jj - j0) * 128:(jj - j0 + 1) * 128], V[:, i, :], i == 0 and (jj - 4 * G) % 2 == 0, i == jj, [pT, V], [bank], sig=(jj == 4 * G + 3))
                            items.append(dict(kp=128, N=N, qk=qk, pv=pv))
                    self.attn_stream(items, self.P[0:3], pts)
                    cps = accc[G % 2]
                    for bi in range(4):
                        bank = self.P[4 + bi]
                        if bi % 2 == 0:
                            s.op("act", lambda h, bank=bank, bi=bi: h.copy(out=cps[bi][:], in_=bank[:, 0:258]), reads=[bank], writes=[cps[bi]])
                        else:
                            s.op("dve", lambda h, bank=bank, bi=bi: h.tensor_copy(out=cps[bi][:], in_=bank[:, 0:258]), reads=[bank], writes=[cps[bi]])
                    for jj in range(4):
                        b0, o0 = cps[0 * 2 + jj // 2], (jj % 2) * 129
                        b1, o1 = cps[1 * 2 + jj // 2], (jj % 2) * 129
                        r, t, o = rr[nfin % 2], t0[nfin % 2], ob[nfin % 2]
                        nfin += 1
                        s.op("dve", lambda h: h.reciprocal(out=r[:, 0:1], in_=b0[:, o0 + 128:o0 + 129]), reads=[b0], writes=[r])
                        s.op("dve", lambda h: h.reciprocal(out=r[:, 1:2], in_=b1[:, o1 + 128:o1 + 129]), reads=[b1], writes=[r])
                        s.op("dve", lambda h: h.tensor_tensor(out=r[:, 1:2], in0=r[:, 1:2], in1=nlam[:], op=ALU.mult), reads=[r, nlam], writes=[r])
                        s.op("dve", lambda h: h.tensor_scalar_mul(out=t[:], in0=b0[:, o0:o0 + 128], scalar1=r[:, 0:1]), reads=[b0, r], writes=[t])
                        s.op("dve", lambda h: h.scalar_tensor_tensor(out=t[:], in0=b1[:, o1:o1 + 128], scalar=r[:, 1:2], in1=t[:], op0=ALU.mult, op1=ALU.add), reads=[b1, r, t], writes=[t])
                        s.op("act", lambda h: h.activation(out=junk[:], in_=t[:], func=AF.Square, accum_out=r[:, 2:3]), reads=[t], writes=[junk, r])
                        s.op("dve", lambda h: h.tensor_scalar(out=r[:, 2:3], in0=r[:, 2:3], scalar1=1.0 / 128, scalar2=EPS, op0=ALU.mult, op1=ALU.add), reads=[r], writes=[r])
                        s.op("act", lambda h: h.sqrt(out=r[:, 2:3], in_=r[:, 2:3]), reads=[r], writes=[r])
                        s.op("dve", lambda h: h.reciprocal(out=r[:, 2:3], in_=r[:, 2:3]), reads=[r], writes=[r])
                        s.op("dve", lambda h: h.scalar_tensor_tensor(out=o[:], in0=t[:], scalar=r[:, 2:3], in1=gsub[:], op0=ALU.mult, op1=ALU.mult), reads=[t, r, gsub], writes=[o])
                        row0 = (4 * G + jj) * 128
                        s.dma("sp", Dr["O_d"][row0:row0 + 128, 512 + hd * 128:512 + (hd + 1) * 128], o[:], reads=[o], writes=[DR["O_d"]])

    def phase_nsa(self, l, q):
        s, I, Dr, DR = self.s, self.I, self.Dr, self.DR
        ident = self.ident
        with ExitStack() as ps:
            mdiag = self.load_const(ps, "mdiag", [128, 128], BF16)
            medge = self.load_const(ps, "medge", [128, 128], BF16)
            cmaskT = self.load_const(ps, "cmaskT", [128, S], BF16)
            eblk = self.load_const(ps, "eblk", [32, S], BF16)
            atab = s.sb([128, NT, 32], F32, "atab", ps)
            s.dma("sp", atab[:], I["atab"].rearrange("(n p) b -> p n b", p=128), writes=[atab])
            GN = s.sb([128, NT, 24], F32, "GN", ps)
            s.dma("sp", GN[:], Dr["GN_d"].rearrange("(n p) c -> p n c", p=128), reads=[DR["GN_d"]], writes=[GN])
            QG = s.sb([68, 4, S], BF16, "QGc", ps)
            KSL = s.sb([68, S], BF16, "KSL", ps)
            KWN = s.sb([68, S], BF16, "KWN", ps)
            KCT = s.sb([64, 128], BF16, "KCT", ps)
            VC = s.sb([128, 97], BF16, "VC", ps)
            VSL = s.sb([128, NT, 65], BF16, "VSL", ps)
            VWN = s.sb([128, NT, 65], BF16, "VWN", ps)
            OC = s.sb([128, NT, 4, 64], F32, "OC", ps)
            SELT = s.sb([32, S], BF16, "SELT", ps)
            pts = [s.sb([128, 512], BF16, "pT", ps) for _ in range(3)]
            dn = [s.sb([128, 12], F32, "dn", ps) for _ in range(2)]
            imp = [s.sb([128, 32], F32, "imp", ps) for _ in range(2)]
            tmp32 = [s.sb([128, 32], F32, "tmp32", ps) for _ in range(2)]
            m8 = [s.sb([128, 16], F32, "m8", ps) for _ in range(2)]
            oo = [s.sb([128, 4, 64], F32, "oo", ps) for _ in range(2)]
            ot = [s.sb([128, 4, 64], F32, "otmp", ps) for _ in range(2)]
            ob = [s.sb([128, 4, 64], BF16, "obf", ps) for _ in range(2)]
            o3 = lambda sc: sc[:, :].rearrange("p (g t) -> p g t", g=4)
            o3c = lambda sc: sc[0:127, :].rearrange("p (g t) -> p g t", g=4)
            b4 = lambda ap: ap.unsqueeze(1).broadcast_to([ap.shape[0], 4, 128])
            for hk in range(2):
                self.load_heads(QG, [SL_QC + hk * 4 + g for g in range(4)], True)
                self.load_heads(KSL, [SL_KSL + hk], False)
                self.load_heads(KWN, [SL_KWN + hk], False)
                s.dma("sp", KCT[:], Dr["KC_d"][hk], reads=[DR["KC_d"]], writes=[KCT])
                s.dma("sp", VC[:], Dr["VC_d"][hk], reads=[DR["VC_d"]], writes=[VC])
                s.dma("sp", VSL[:], Dr["VSL_d"][:, hk, :].rearrange("(n p) e -> p n e", p=128), reads=[DR["VSL_d"]], writes=[VSL])
                s.dma("sp", VWN[:], Dr["VWN_d"][:, hk, :].rearrange("(n p) e -> p n e", p=128), reads=[DR["VWN_d"]], writes=[VWN])
                for j in range(NT):
                    acc = self.P[4 + j % 2]
                    jc = slice(j * 128, (j + 1) * 128)
                    qk = [(o3c, KCT[:, 0:127], QG[0:64, :, jc], True, False, [KCT, QG]),
                          (o3c, ident[0:127, 0:127], b4(cmaskT[0:127, jc]), False, True, [ident, cmaskT])]

                    def pv(pT, acc=acc):
                        for g in range(4):
                            self.mm(acc[:, g * 97:(g + 1) * 97], pT[0:127, g * 128:(g + 1) * 128], VC[0:127, :], g == 0, True, [pT, VC], [acc], sig=(g == 3))
                    self.attn_stream([dict(kp=127, N=512, qk=qk, pv=pv)], self.P[0:3], pts)
                    a3 = acc[:, 0:388].rearrange("p (g e) -> p g e", e=97)
                    d, im, tm, mm8 = dn[j % 2], imp[j % 2], tmp32[j % 2], m8[j % 2]
                    s.op("dve", lambda h: h.tensor_scalar_max(out=d[:, 0:4].unsqueeze(2), in0=a3[:, :, 64:65], scalar1=1e-30), reads=[acc], writes=[d])
                    s.op("dve", lambda h: h.reciprocal(out=d[:, 0:4], in_=d[:, 0:4]), reads=[d], writes=[d])
                    s.op("dve", lambda h: h.tensor_tensor(out=OC[:, j], in0=a3[:, :, 0:64], in1=d[:, 0:4].unsqueeze(2).broadcast_to([128, 4, 64]), op=ALU.mult), reads=[acc, d], writes=[OC])
                    s.op("dve", lambda h: h.tensor_scalar_mul(out=im[:], in0=a3[:, 0, 65:97], scalar1=d[:, 0:1]), reads=[acc, d], writes=[im])
                    for g in range(1, 4):
                        s.op("dve", lambda h, g=g: h.scalar_tensor_tensor(out=im[:], in0=a3[:, g, 65:97], scalar=d[:, g:g + 1], in1=im[:], op0=ALU.mult, op1=ALU.add), reads=[acc, d, im], writes=[im])
                    s.op("dve", lambda h: h.tensor_tensor(out=im[:], in0=im[:], in1=atab[:, j, :], op=ALU.add), reads=[im, atab], writes=[im])
                    s.op("dve", lambda h: h.max(out=mm8[:, 0:8], in_=im[:]), reads=[im], writes=[mm8])
                    s.op("dve", lambda h: h.match_replace(out=tm[:], in_to_replace=mm8[:, 0:8], in_values=im[:], imm_value=-3e9), reads=[im, mm8], writes=[tm])
                    s.op("dve", lambda h: h.max(out=mm8[:, 8:16], in_=tm[:]), reads=[tm], writes=[mm8])
                    s.op("dve", lambda h: h.tensor_scalar(out=tm[:], in0=im[:], scalar1=mm8[:, 15:16], scalar2=NEG, op0=ALU.is_lt, op1=ALU.mult), reads=[im, mm8], writes=[tm])
                    pt = self.P[6 + j % 2]
                    self.tr(pt[0:32, 0:128], tm[:], self.ident32[:], [tm, self.ident32], [pt])
                    s.op("act", lambda h: h.copy(out=SELT[:, jc], in_=pt[0:32, 0:128]), reads=[pt], writes=[SELT])
                for j in range(NT):
                    jc = slice(j * 128, (j + 1) * 128)
                    accS, accW = self.P[4 + 2 * (j % 2)], self.P[5 + 2 * (j % 2)]
                    items = []
                    for i in range(0, j + 1):
                        ic = slice(i * 128, (i + 1) * 128)
                        qk = [(o3, KSL[:, ic], QG[:, :, jc], True, False, [KSL, QG]),
                              (o3, eblk[:, ic], b4(SELT[:, jc]), False, i != j, [eblk, SELT])]
                        if i == j:
                            qk.append((o3, ident[:], b4(mdiag[:]), False, True, [ident, mdiag]))

                        def pv(pT, i=i, j=j, acc=accS):
                            for g in range(4):
                                self.mm(acc[:, g * 65:(g + 1) * 65], pT[:, g * 128:(g + 1) * 128], VSL[:, i, :], i == 0 and g == 0, i == j, [pT, VSL], [acc], sig=(g == 3))
                        items.append(dict(kp=128, N=512, qk=qk, pv=pv))
                    i0 = max(0, j - 4)
                    for i in range(i0, j + 1):
                        ic = slice(i * 128, (i + 1) * 128)
                        masked = (i == j) or (i == j - 4)
                        qk = [(o3, KWN[:, ic], QG[:, :, jc], True, not masked, [KWN, QG])]
                        if masked:
                            mk = mdiag if i == j else medge
                            qk.append((o3, ident[:], b4(mk[:]), False, True, [ident, mk]))

                        def pv(pT, i=i, j=j, i0=i0, acc=accW):
                            for g in range(4):
                                self.mm(acc[:, g * 65:(g + 1) * 65], pT[:, g * 128:(g + 1) * 128], VWN[:, i, :], i == i0 and g == 0, i == j, [pT, VWN], [acc], sig=(g == 3))
                        items.append(dict(kp=128, N=512, qk=qk, pv=pv))
                    self.attn_stream(items, self.P[0:3], pts)
                    d, o, t, obf = dn[j % 2], oo[j % 2], ot[j % 2], ob[j % 2]
                    aS = accS[:, 0:260].rearrange("p (g e) -> p g e", e=65)
                    aW = accW[:, 0:260].rearrange("p (g e) -> p g e", e=65)
                    gv = GN[:, j, hk * 12:(hk + 1) * 12].rearrange("p (g r) -> p g r", r=3)
                    s.op("dve", lambda h: h.reciprocal(out=d[:, 4:8].unsqueeze(2), in_=aS[:, :, 64:65]), reads=[accS], writes=[d])
                    s.op("dve", lambda h: h.reciprocal(out=d[:, 8:12].unsqueeze(2), in_=aW[:, :, 64:65]), reads=[accW], writes=[d])
                    s.op("dve", lambda h: h.tensor_tensor(out=d[:, 4:8].unsqueeze(2), in0=d[:, 4:8].unsqueeze(2), in1=gv[:, :, 1:2], op=ALU.mult), reads=[d, GN], writes=[d])
                    s.op("dve", lambda h: h.tensor_tensor(out=d[:, 8:12].unsqueeze(2), in0=d[:, 8:12].unsqueeze(2), in1=gv[:, :, 2:3], op=ALU.mult), reads=[d, GN], writes=[d])
                    s.op("pool", lambda h: h.tensor_tensor(out=o[:], in0=OC[:, j], in1=gv[:, :, 0:1].broadcast_to([128, 4, 64]), op=ALU.mult), reads=[OC, GN], writes=[o])
                    s.op("dve", lambda h: h.tensor_tensor(out=t[:], in0=aS[:, :, 0:64], in1=d[:, 4:8].unsqueeze(2).broadcast_to([128, 4, 64]), op=ALU.mult), reads=[accS, d], writes=[t])
                    s.op("pool", lambda h: h.tensor_tensor(out=o[:], in0=o[:], in1=t[:], op=ALU.add), reads=[o, t], writes=[o])
                    s.op("dve", lambda h: h.tensor_tensor(out=t[:], in0=aW[:, :, 0:64], in1=d[:, 8:12].unsqueeze(2).broadcast_to([128, 4, 64]), op=ALU.mult), reads=[accW, d], writes=[t])
                    s.op("pool", lambda h: h.tensor_tensor(out=obf[:], in0=o[:], in1=t[:], op=ALU.add), reads=[o, t], writes=[obf])
                    s.dma("sp", Dr["O_d"][jc, 1024 + hk * 256:1024 + (hk + 1) * 256], obf[:].rearrange("p g d -> p (g d)"), reads=[obf], writes=[DR["O_d"]])

    def phase_merge(self, l, q):
        s, I, Dr, DR = self.s, self.I, self.Dr, self.DR
        xsrc, xres_r = self.xsrc(l, q)
        with ExitStack() as ps:
            wbr = s.sb([128, 12, D], BF16, "wbr", ps)
            wout = s.sb([128, 8, D], BF16, "wout", ps)
            for r in range(3):
                for hh in range(2):
                    s.dma("pool", wbr[:, r * 4:(r + 1) * 4, hh * 512:(hh + 1) * 512],
                          I["w_branch"][l, r].rearrange("(kc p) n -> p kc n", p=128)[:, :, hh * 512:(hh + 1) * 512], writes=[wbr])
            for k4 in range(2):
                for hh in range(2):
                    s.dma("pool", wout[:, k4 * 4:(k4 + 1) * 4, hh * 512:(hh + 1) * 512],
                          I["w_out"][l].rearrange("(kc p) n -> p kc n", p=128)[:, k4 * 4:(k4 + 1) * 4, hh * 512:(hh + 1) * 512], writes=[wout])
            g1 = s.sb([128, D], F32, "g1", ps)
            self.mod_bc(l, q, 2, g1)
            Ot = [s.sb([128, 1536], BF16, "Ot", ps) for _ in range(2)]
            GM = [s.sb([128, 3072], F32, "GMt", ps) for _ in range(2)]
            xt = [s.sb([128, D], F32, "xt", ps) for _ in range(2)]
            OT = [s.sb([128, 12, 128], BF16, "OT", ps) for _ in range(2)]
            MT = [s.sb([128, 8, 128], BF16, "MT", ps) for _ in range(2)]
            mg = s.sb([128, D], F32, "mg", ps)
            mgb = s.sb([128, D], BF16, "mgb", ps)
            tmp = [s.sb([128, 512], F32, "mtmp", ps) for _ in range(2)]
            nb = 0
            for tt in range(NT):
                rows = slice(tt * 128, (tt + 1) * 128)
                grow = slice(q * S + tt * 128, q * S + (tt + 1) * 128)
                O, G, x, ot, mt = Ot[tt % 2], GM[tt % 2], xt[tt % 2], OT[tt % 2], MT[tt % 2]
                s.dma("sp", O[:], Dr["O_d"][rows, :], reads=[DR["O_d"]], writes=[O])
                s.dma("sp", G[:], Dr["GM_d"][rows, :], reads=[DR["GM_d"]], writes=[G])
                s.dma("sp", x[:], xsrc[grow, :], reads=[], writes=[x])
                pa, pb = self.P[0], self.P[1]
                pab, pbb = pa[:].bitcast(BF16), pb[:].bitcast(BF16)
                for kc in range(8):
                    self.tr(pab[:, kc * 128:(kc + 1) * 128], O[:, kc * 128:(kc + 1) * 128], self.ident[:], [O, self.ident], [pa], sig=(kc == 7))
                for kc in range(4):
                    self.tr(pbb[:, kc * 128:(kc + 1) * 128], O[:, (8 + kc) * 128:(9 + kc) * 128], self.ident[:], [O, self.ident], [pb], sig=(kc == 3))
                s.op("act", lambda h: h.copy(out=ot[:, 0:8, :], in_=pab.rearrange("p (k t) -> p k t", k=8)), reads=[pa], writes=[ot])
                s.op("act", lambda h: h.copy(out=ot[:, 8:12, :], in_=pbb[:, 0:512].rearrange("p (k t) -> p k t", k=4)), reads=[pb], writes=[ot])
                for r in range(3):
                    for half in range(2):
                        pm = self.P[2 + nb % 4]
                        tp = tmp[nb % 2]
                        nb += 1
                        hc = slice(half * 512, (half + 1) * 512)
                        for kc in range(4):
                            self.mm(pm[:, :], ot[:, r * 4 + kc, :], wbr[:, r * 4 + kc, hc], kc == 0, kc == 3, [ot, wbr], [pm], sig=(kc == 3))
                        gsl = G[:, r * 1024 + half * 512:r * 1024 + (half + 1) * 512]
                        if r == 0:
                            s.op("dve", lambda h: h.tensor_tensor(out=mg[:, hc], in0=pm[:, :], in1=gsl, op=ALU.mult), reads=[pm, G], writes=[mg])
                        else:
                            s.op("dve", lambda h: h.tensor_tensor(out=tp[:], in0=pm[:, :], in1=gsl, op=ALU.mult), reads=[pm, G], writes=[tp])
                            s.op("pool", lambda h: h.tensor_tensor(out=mg[:, hc], in0=mg[:, hc], in1=tp[:], op=ALU.add), reads=[mg, tp], writes=[mg])
                s.op("act", lambda h: h.copy(out=mgb[:], in_=mg[:]), reads=[mg], writes=[mgb])
                for kc in range(8):
                    self.tr(pab[:, kc * 128:(kc + 1) * 128], mgb[:, kc * 128:(kc + 1) * 128], self.ident[:], [mgb, self.ident], [pa], sig=(kc == 7))
                s.op("act", lambda h: h.copy(out=mt[:], in_=pab.rearrange("p (k t) -> p k t", k=8)), reads=[pa], writes=[mt])
                for half in range(2):
                    pm = self.P[2 + nb % 4]
                    tp = tmp[nb % 2]
                    nb += 1
                    hc = slice(half * 512, (half + 1) * 512)
                    for kc in range(8):
                        self.mm(pm[:, :], mt[:, kc, :], wout[:, kc, hc], kc == 0, kc == 7, [mt, wout], [pm], sig=(kc == 7))
                    s.op("dve", lambda h: h.tensor_tensor(out=tp[:], in0=pm[:, :], in1=g1[:, hc], op=ALU.mult), reads=[pm, g1], writes=[tp])
                    s.op("pool", lambda h: h.tensor_tensor(out=x[:, hc], in0=x[:, hc], in1=tp[:], op=ALU.add), reads=[x, tp], writes=[x])
                s.dma("pool", Dr["xres"][grow, :], x[:], reads=[x], writes=[DR["xres"]])

    def phase_moe2(self, l):
        s, I, Dr, DR = self.s, self.I, self.Dr, self.DR
        last = (l == L_DEPTH - 1)
        NTT, NB = self.ntt, self.nblk
        with ExitStack() as ps:
            g2 = []
            for q in range(self.nseq):
                t = s.sb([128, D], F32, "g2", ps)
                self.mod_bc(l, q, 5, t)
                g2.append(t)
            GATE = s.sb([128, NTT, NE], F32, "GATE", ps)
            GT = s.sb([NE, NTT, 128], F32, "GT", ps)
            IDX4 = s.sb([128, NTT, 4], I32, "IDX4", ps)
            G4 = s.sb([128, NTT, 4], F32, "G4", ps)
            IDXW = s.sb([128, NB, 2], I32, "IDXW", ps)
            IDXB = s.sb([128, NB], I32, "IDXB", ps)
            b2 = s.sb([NE, D], F32, "b2", ps)
            s.dma("sp", b2[:], I["exp_b2"][l], writes=[b2])
            with ExitStack() as p1:
                HB = s.sb([128, NTT, D], BF16, "HB", p1)
                MASK = s.sb([128, NTT, NE], BF16, "MASK", p1)
                DEST = s.sb([128, NTT, NE], F32, "DEST", p1)
                rw = s.sb([128, 8, NE], F32, "rw", p1)
                rb = s.sb([128, NE], F32, "rb", p1)
                s.dma("sp", rw[:], I["router_w"][l].rearrange("(kc p) e -> p kc e", p=128), writes=[rw])
                s.dma("sp", rb[:], I["router_b"][l:l + 1, :].broadcast_to([128, NE]), writes=[rb])
                ltri = self.load_const(p1, "ltri", [128, 128], BF16)
                ones = self.load_const(p1, "ones128", [128, 128], BF16)
                b512 = self.load_const(p1, "b512", [128, 64], F32)
                rowiota = self.load_const(p1, "rowiota", [128, 8], F32)
                piota = self.load_const(p1, "piota", [128, 1], F32)
                xt = [s.sb([128, D], F32, "xt", p1) for _ in range(2)]
                hf = [s.sb([128, D], F32, "hf", p1) for _ in range(2)]
                hT32s = [s.sb([128, 8, 128], F32, "hT32", p1) for _ in range(2)]
                junk = s.sb([128, D], F32, "junk", p1)
                ss = [s.sb([128, 1], F32, "ss", p1) for _ in range(2)]
                lg = [s.sb([128, NE], F32, "lg", p1) for _ in range(2)]
                ex = [s.sb([128, NE], F32, "ex", p1) for _ in range(2)]
                m8 = [s.sb([128, 8], F32, "m8", p1) for _ in range(2)]
                sm = [s.sb([128, 2], F32, "sm", p1) for _ in range(2)]
                gsc = sh = None
                for tt in range(NTT):
                    q = tt // NT
                    if tt % NT == 0:
                        gsc, sh = self.norm_tiles(p1, l, q, "norm2_g", 4, 3)
                    grow = slice(tt * 128, (tt + 1) * 128)
                    x, h32, sq = xt[tt % 2], hf[tt % 2], ss[tt % 2]
                    s.dma("sp", x[:], Dr["xres"][grow, :], reads=[DR["xres"]], writes=[x])
                    self.rstd_of((x, x[:]), (junk, junk[:]), sq)
                    s.op("dve", lambda hh: hh.scalar_tensor_tensor(out=x[:], in0=x[:], scalar=sq[:, 0:1], in1=gsc[:], op0=ALU.mult, op1=ALU.mult), reads=[x, sq, gsc], writes=[x])
                    s.op("pool", lambda hh: hh.tensor_tensor(out=h32[:], in0=x[:], in1=sh[:], op=ALU.add), reads=[x, sh], writes=[h32])
                    s.op("act", lambda hh: hh.copy(out=HB[:, tt, :], in_=h32[:]), reads=[h32], writes=[HB])
                    hT32 = hT32s[tt % 2]
                    for hh2 in range(2):
                        pf = self.P[(tt % 2) * 2 + hh2]
                        for k4 in range(4):
                            kc = hh2 * 4 + k4
                            self.tr(pf[:, k4 * 128:(k4 + 1) * 128], h32[:, kc * 128:(kc + 1) * 128], self.ident32[:], [h32, self.ident32], [pf], sig=(k4 == 3))
                        s.op("dve", lambda hh: hh.tensor_copy(out=hT32[:, hh2 * 4:(hh2 + 1) * 4, :], in_=pf[:, :].rearrange("p (k t) -> p k t", k=4)), reads=[pf], writes=[hT32])
                    pl = self.P[4 + tt % 2]
                    for kc in range(8):
                        self.mm(pl[:, 0:NE], hT32[:, kc, :], rw[:, kc, :], kc == 0, kc == 7, [hT32, rw], [pl], sig=(kc == 7))
                    lgt, et, mt, st = lg[tt % 2], ex[tt % 2], m8[tt % 2], sm[tt % 2]
                    s.op("dve", lambda hh: hh.tensor_tensor(out=lgt[:], in0=pl[:, 0:NE], in1=rb[:], op=ALU.add), reads=[pl, rb], writes=[lgt])
                    s.op("dve", lambda hh: hh.max(out=mt[:], in_=lgt[:]), reads=[lgt], writes=[mt])
                    s.op("dve", lambda hh: hh.tensor_scalar_mul(out=st[:, 0:1], in0=mt[:, 0:1], scalar1=-1.0), reads=[mt], writes=[st])
                    s.op("act", lambda hh: hh.activation(out=et[:], in_=lgt[:], func=AF.Exp, bias=st[:, 0:1]), reads=[lgt, st], writes=[et])
                    s.op("dve", lambda hh: hh.tensor_scalar(out=lgt[:], in0=lgt[:], scalar1=mt[:, 3:4], scalar2=None, op0=ALU.is_ge), reads=[lgt, mt], writes=[lgt])
                    s.op("dve", lambda hh: hh.tensor_copy(out=MASK[:, tt, :], in_=lgt[:]), reads=[lgt], writes=[MASK])
                    s.op("dve", lambda hh: hh.tensor_tensor(out=et[:], in0=et[:], in1=lgt[:], op=ALU.mult), reads=[et, lgt], writes=[et])
                    s.op("dve", lambda hh: hh.reduce_sum(out=st[:, 1:2], in_=et[:], axis=AX.X), reads=[et], writes=[st])
                    s.op("dve", lambda hh: hh.reciprocal(out=st[:, 1:2], in_=st[:, 1:2]), reads=[st], writes=[st])
                    s.op("dve", lambda hh: hh.tensor_scalar_mul(out=GATE[:, tt, :], in0=et[:], scalar1=st[:, 1:2]), reads=[et, st], writes=[GATE])
                    pg = self.P[6 + tt % 2]
                    self.tr(pg[0:NE, 0:128], GATE[:, tt, :], self.ident32[:], [GATE, self.ident32], [pg])
                    s.op("act", lambda hh: hh.copy(out=GT[:, tt, :], in_=pg[0:NE, 0:128]), reads=[pg], writes=[GT])
                run = s.sb([128, NE], F32, "run", p1)
                s.op("dve", lambda hh: hh.memset(run[:], 0.0), writes=[run])
                for tt in range(NTT):
                    pr = self.P[6 + tt % 2]
                    self.mm(pr[:, 0:NE], ltri[:], MASK[:, tt, :], True, True, [ltri, MASK], [pr], sig=False)
                    self.mm(pr[:, NE:2 * NE], ones[:], MASK[:, tt, :], False, True, [ones, MASK], [pr])
                    s.op("dve", lambda hh: hh.tensor_tensor(out=DEST[:, tt, :], in0=pr[:, 0:NE], in1=run[:], op=ALU.add), reads=[pr, run], writes=[DEST])
                    s.op("dve", lambda hh: hh.tensor_tensor(out=run[:], in0=run[:], in1=pr[:, NE:2 * NE], op=ALU.add), reads=[pr, run], writes=[run])
                padded = s.sb([128, NE], F32, "padded", p1)
                tmpe = s.sb([128, NE], F32, "tmpe", p1)
                cum = [s.sb([128, NE], F32, "cum", p1) for _ in range(2)]
                s.op("dve", lambda hh: hh.memset(padded[:], 0.0), writes=[padded])
                for jb in range(NTT // 4):
                    s.op("dve", lambda hh, jb=jb: hh.scalar_tensor_tensor(out=padded[:], in0=run[:], scalar=512.0 * jb, in1=padded[:], op0=ALU.is_gt, op1=ALU.add), reads=[run, padded], writes=[padded])
                s.op("dve", lambda hh: hh.tensor_scalar_mul(out=padded[:], in0=padded[:], scalar1=512.0), reads=[padded], writes=[padded])
                s.op("dve", lambda hh: hh.tensor_copy(out=cum[0][:], in_=padded[:]), reads=[padded], writes=[cum[0]])
                ci = 0
                for shf in (1, 2, 4, 8, 16):
                    a, b = cum[ci], cum[1 - ci]
                    s.op("dve", lambda hh: hh.tensor_copy(out=b[:, 0:shf], in_=a[:, 0:shf]), reads=[a], writes=[b])
                    s.op("dve", lambda hh: hh.tensor_tensor(out=b[:, shf:NE], in0=a[:, shf:NE], in1=a[:, 0:NE - shf], op=ALU.add), reads=[a], writes=[b])
                    ci = 1 - ci
                pend = cum[ci]
                pstart = s.sb([128, NE], F32, "pstart", p1)
                s.op("dve", lambda hh: hh.tensor_tensor(out=pstart[:], in0=pend[:], in1=padded[:], op=ALU.subtract), reads=[pend, padded], writes=[pstart])
                s.op("dve", lambda hh: hh.tensor_tensor(out=DEST[:], in0=DEST[:], in1=pstart[:].unsqueeze(1).broadcast_to([128, NTT, NE]), op=ALU.add), reads=[DEST, pstart], writes=[DEST])
                s.op("dve", lambda hh: hh.scalar_tensor_tensor(out=DEST[:], in0=DEST[:], scalar=1.0, in1=MASK[:], op0=ALU.add, op1=ALU.mult), reads=[DEST, MASK], writes=[DEST])
                d4 = s.sb([128, NTT, 4], F32, "d4", p1)
                for tt in range(NTT):
                    mt, et = m8[tt % 2], ex[tt % 2]
                    s.op("dve", lambda hh: hh.max(out=mt[:], in_=DEST[:, tt, :]), reads=[DEST], writes=[mt])
                    s.op("dve", lambda hh: hh.tensor_scalar_add(out=d4[:, tt, :], in0=mt[:, 0:4], scalar1=-1.0), reads=[mt], writes=[d4])
                    for k in range(4):
                        s.op("dve", lambda hh: hh.scalar_tensor_tensor(out=et[:], in0=DEST[:, tt, :], scalar=mt[:, k:k + 1], in1=GATE[:, tt, :], op0=ALU.is_equal, op1=ALU.mult), reads=[DEST, mt, GATE], writes=[et])
                        s.op("dve", lambda hh: hh.reduce_sum(out=G4[:, tt, k:k + 1], in_=et[:], axis=AX.X), reads=[et], writes=[G4])
                s.op("dve", lambda hh: hh.tensor_copy(out=IDX4[:], in_=d4[:]), reads=[d4], writes=[IDX4])
                bexp = s.sb([128, 64], F32, "bexp", p1)
                s.op("dve", lambda hh: hh.memset(bexp[:], 0.0), writes=[bexp])
                for e in range(NE):
                    s.op("dve", lambda hh: hh.scalar_tensor_tensor(out=bexp[:], in0=b512[:], scalar=pend[:, e:e + 1], in1=bexp[:], op0=ALU.is_ge, op1=ALU.add), reads=[b512, pend, bexp], writes=[bexp])
                boob = s.sb([128, 64], F32, "boob", p1)
                s.op("dve", lambda hh: hh.tensor_scalar(out=boob[:], in0=bexp[:], scalar1=float(NE) - 0.5, scalar2=1.0e6, op0=ALU.is_ge, op1=ALU.mult), reads=[bexp], writes=[boob])
                s.op("dve", lambda hh: hh.tensor_scalar_min(out=bexp[:], in0=bexp[:], scalar1=float(NE - 1)), reads=[bexp], writes=[bexp])
                iwf = s.sb([128, NB, 2], F32, "iwf", p1)
                ibf = s.sb([128, NB], F32, "ibf", p1)
                e1k = s.sb([128, 64], F32, "e1k", p1)
                s.op("dve", lambda hh: hh.tensor_scalar(out=e1k[:], in0=bexp[:], scalar1=256.0, scalar2=float(l * NE * 256), op0=ALU.mult, op1=ALU.add), reads=[bexp], writes=[e1k])
                s.op("dve", lambda hh: hh.tensor_tensor(out=e1k[:], in0=e1k[:], in1=boob[:], op=ALU.add), reads=[e1k, boob], writes=[e1k])
                s.op("dve", lambda hh: hh.tensor_tensor(out=iwf[:], in0=rowiota[:, 0:2].unsqueeze(1).broadcast_to([128, NB, 2]), in1=e1k[:, 0:NB].unsqueeze(2).broadcast_to([128, NB, 2]), op=ALU.add), reads=[rowiota, e1k], writes=[iwf])
                s.op("dve", lambda hh: hh.tensor_copy(out=IDXW[:], in_=iwf[:]), reads=[iwf], writes=[IDXW])
                s.op("dve", lambda hh: hh.tensor_scalar(out=ibf[:], in0=bexp[:, 0:NB], scalar1=128.0, scalar2=piota[:, 0:1], op0=ALU.mult, op1=ALU.add), reads=[bexp, piota], writes=[ibf])
                s.op("dve", lambda hh: hh.tensor_scalar_add(out=ibf[:], in0=ibf[:], scalar1=float(l * NE * 128)), reads=[ibf], writes=[ibf])
                s.op("dve", lambda hh: hh.tensor_tensor(out=ibf[:], in0=ibf[:], in1=boob[:, 0:NB], op=ALU.add), reads=[ibf, boob], writes=[ibf])
                s.op("dve", lambda hh: hh.tensor_copy(out=IDXB[:], in_=ibf[:]), reads=[ibf], writes=[IDXB])
                if "dbg_d" in self.dbg:
                    dbt = s.sb([128, 4096], F32, "dbt", p1)
                    s.op("dve", lambda hh: hh.memset(dbt[:], 0.0), writes=[dbt])
                    s.op("dve", lambda hh: hh.tensor_copy(out=dbt[:, 0:32], in_=run[:]), reads=[run], writes=[dbt])
                    s.op("dve", lambda hh: hh.tensor_copy(out=dbt[:, 32:64], in_=pend[:]), reads=[pend], writes=[dbt])
                    s.op("dve", lambda hh: hh.tensor_copy(out=dbt[:, 64:128], in_=bexp[:]), reads=[bexp], writes=[dbt])
                    s.op("dve", lambda hh: hh.tensor_copy(out=dbt[:, 128:128 + NTT * 4], in_=d4[:].rearrange("p t k -> p (t k)")), reads=[d4], writes=[dbt])
                    s.op("dve", lambda hh: hh.tensor_copy(out=dbt[:, 512:512 + NTT * 4], in_=G4[:].rearrange("p t k -> p (t k)")), reads=[G4], writes=[dbt])
                    s.dma("sp", Dr["dbg_d"], dbt[:], reads=[dbt], writes=[DR["dbg_d"]])
                for tt in range(NTT):
                    for k in range(4):
                        s.idma(Dr["xs_d"][:, :], HB[:, tt, :], out_off=IDX4[:, tt, k:k + 1], reads=[HB, IDX4], writes=[])
                s.barrier()
            with ExitStack() as p2:
                W1 = [s.sb([128, 8, 2 * D], BF16, "W1", p2) for _ in range(2)]
                W2 = [s.sb([128, 8, D], BF16, "W2", p2) for _ in range(2)]
                B1 = [s.sb([128, 16], F32, "B1", p2) for _ in range(2)]
                XS = [s.sb([128, 4, D], BF16, "XS", p2) for _ in range(2)]
                XT = [s.sb([128, 8, 512], BF16, "XT", p2) for _ in range(2)]
                Ab = [s.sb([128, 8, 512], BF16, "Ab", p2) for _ in range(2)]
                Gt = [s.sb([128, 512], F32, "Gt", p2) for _ in range(2)]
                St = [s.sb([128, 512], F32, "St", p2) for _ in range(2)]
                Ut = [s.sb([128, 512], F32, "Ut", p2) for _ in range(2)]
                Yt = [s.sb([128, D], F32, "Yt", p2) for _ in range(2)]
                for wt_ in W1 + W2 + B1:
                    s.op("pool", lambda hh, wt_=wt_: hh.memset(wt_[:], 0.0), writes=[wt_])
                cnt = {"nf": 0, "ny": 0}

                def load_block(b):
                    w1, w2, b1, xs = W1[b % 2], W2[b % 2], B1[b % 2], XS[b % 2]
                    s.dma("sp", xs[:], Dr["xs_d"][b * 512:(b + 1) * 512, :].rearrange("(t p) d -> p t d", p=128), reads=[DR["xs_d"]], writes=[xs])
                    for j2 in range(2):
                        s.idma(w1[:, j2 * 4:(j2 + 1) * 4, :].rearrange("p a b -> p (a b)"), I["exp_w1"][:, :], in_off=IDXW[:, b, j2:j2 + 1], reads=[IDXW], writes=[w1], bounds=65535)
                    s.idma(b1[:, :], I["exp_b1E"][:, :], in_off=IDXB[:, b:b + 1], reads=[IDXB], writes=[b1], bounds=65535)

                def load_w2(b):
                    w2 = W2[b % 2]
                    s.idma(w2[:].rearrange("p a b -> p (a b)"), I["exp_w2"][:, :], in_off=IDXB[:, b:b + 1], reads=[IDXB], writes=[w2], bounds=65535)

                def transposes(b):
                    xs, xT = XS[b % 2], XT[b % 2]
                    for t4 in range(4):
                        pt = self.P[t4 % 2]
                        ptb = pt[:].bitcast(BF16)
                        for kc in range(8):
                            kb = (kc // 4) * 512 + (kc % 4)
                            self.tr(ptb[:, kc * 128:(kc + 1) * 128], xs[:, t4, kb:kb + 509:4], self.ident[:], [xs, self.ident], [pt], sig=(kc == 7))
                        if t4 % 2 == 0:
                            s.op("act", lambda hh: hh.copy(out=xT[:, :, t4 * 128:(t4 + 1) * 128], in_=ptb.rearrange("p (k t) -> p k t", k=8)), reads=[pt], writes=[xT])
                        else:
                            s.op("dve", lambda hh: hh.tensor_copy(out=xT[:, :, t4 * 128:(t4 + 1) * 128], in_=ptb.rearrange("p (k t) -> p k t", k=8)), reads=[pt], writes=[xT])

                def stage1_fc(b, fc):
                    w1, b1, xT, A = W1[b % 2], B1[b % 2], XT[b % 2], Ab[b % 2]
                    nf = cnt["nf"]
                    cnt["nf"] += 1
                    psG, psU = self.P[2 + (nf % 2) * 2], self.P[3 + (nf % 2) * 2]
                    G, Sg, U = Gt[nf % 2], St[nf % 2], Ut[nf % 2]
                    for kc in range(8):
                        self.mm(psG[:, :], w1[:, kc, fc:D:8], xT[:, kc, :], kc == 0, kc == 7, [w1, xT], [psG], sig=(kc == 7))
                    for kc in range(8):
                        self.mm(psU[:, :], w1[:, kc, D + fc:2 * D:8], xT[:, kc, :], kc == 0, kc == 7, [w1, xT], [psU], sig=(kc == 7))
                    s.op("dve", lambda hh: hh.tensor_scalar(out=G[:], in0=psG[:, :], scalar1=b1[:, fc:fc + 1], scalar2=7.0, op0=ALU.add, op1=ALU.min), reads=[psG, b1], writes=[G])
                    s.op("act", lambda hh: hh.activation(out=Sg[:], in_=G[:], func=AF.Sigmoid, scale=1.702), reads=[G], writes=[Sg])
                    s.op("act", lambda hh: hh.activation(out=U[:], in_=psU[:, :], func=AF.Identity, bias=b1[:, 8 + fc:8 + fc + 1]), reads=[psU, b1], writes=[U])
                    s.op("dve", lambda hh: hh.tensor_scalar(out=U[:], in0=U[:], scalar1=7.0, scalar2=-7.0, op0=ALU.min, op1=ALU.max), reads=[U], writes=[U])
                    s.op("dve", lambda hh: hh.tensor_tensor(out=G[:], in0=G[:], in1=Sg[:], op=ALU.mult), reads=[G, Sg], writes=[G])
                    s.op("dve", lambda hh: hh.scalar_tensor_tensor(out=A[:, fc, :], in0=U[:], scalar=1.0, in1=G[:], op0=ALU.add, op1=ALU.mult), reads=[U, G], writes=[A])

                def stage2_grp(b, g):
                    w2, A = W2[b % 2], Ab[b % 2]
                    t4, dh = g // 2, g % 2
                    ny = cnt["ny"]
                    y = Yt[(ny // 2) % 2]
                    psY = self.P[6 + ny % 2]
                    cnt["ny"] += 1
                    dc = slice(dh * 512, (dh + 1) * 512)
                    for fc in range(8):
                        self.mm(psY[:, :], A[:, fc, t4 * 128:(t4 + 1) * 128], w2[:, fc, dc], fc == 0, fc == 7, [A, w2], [psY], sig=(fc == 7))
                    s.op("act", lambda hh: hh.copy(out=y[:, dc], in_=psY[:, :]), reads=[psY], writes=[y])
                    if dh == 1:
                        r0 = b * 512 + t4 * 128
                        s.dma("sp", Dr["ys_d"][r0:r0 + 128, :], y[:], reads=[y], writes=[DR["ys_d"]])

                load_block(0)
                load_w2(0)
                load_block(1)
                load_w2(1)
                for b in range(NB):
                    if 1 <= b < NB - 1:
                        load_block(b + 1)
                    transposes(b)
                    for i in range(8):
                        stage1_fc(b, i)
                        if b >= 1:
                            stage2_grp(b - 1, i)
                    if 1 <= b < NB - 1:
                        load_w2(b + 1)
                for i in range(8):
                    stage2_grp(NB - 1, i)
                s.barrier()
            with ExitStack() as p3:
                xt = [s.sb([128, D], F32, "xt", p3) for _ in range(2)]
                acc = [s.sb([128, D], F32, "acc3", p3) for _ in range(2)]
                Yk = [s.sb([128, D], F32, "Yk", p3) for _ in range(4)]
                junk = s.sb([128, D], F32, "junk", p3)
                ss = [s.sb([128, 1], F32, "ss", p3) for _ in range(2)]
                if last:
                    fg = s.sb([128, D], F32, "fg", p3)
                    s.dma("sp", fg[:], I["final_g"].broadcast_to([128, D]), writes=[fg])
                nk = 0
                for tt in range(NTT):
                    q = tt // NT
                    grow = slice(tt * 128, (tt + 1) * 128)
                    x, ac, sq = xt[tt % 2], acc[tt % 2], ss[tt % 2]
                    if tt == 0:
                        s.dma("sp", x[:], Dr["xres"][grow, :], reads=[], writes=[x])
                    if tt + 1 < NTT:
                        s.dma("sp", xt[(tt + 1) % 2][:], Dr["xres"][(tt + 1) * 128:(tt + 2) * 128, :], reads=[], writes=[xt[(tt + 1) % 2]])
                    pa = [self.P[(tt % 2) * 2], self.P[(tt % 2) * 2 + 1]]
                    for half in range(2):
                        self.mm(pa[half][:, :], GT[:, tt, :], b2[:, half * 512:(half + 1) * 512], True, True, [GT, b2], [pa[half]])
                    for k in range(4):
                        yk = Yk[nk % 4]
                        nk += 1
                        s.idma(yk[:, :], Dr["ys_d"][:, :], in_off=IDX4[:, tt, k:k + 1], reads=[IDX4, DR["ys_d"]], writes=[yk])
                        for half in range(2):
                            hc = slice(half * 512, (half + 1) * 512)
                            in1 = pa[half][:, :] if k == 0 else ac[:, hc]
                            rd = [yk, G4] + ([pa[half]] if k == 0 else [ac])
                            s.op("dve", lambda hh, in1=in1, hc=hc, yk=yk, k=k: hh.scalar_tensor_tensor(out=ac[:, hc], in0=yk[:, hc], scalar=G4[:, tt, k:k + 1], in1=in1, op0=ALU.mult, op1=ALU.add), reads=rd, writes=[ac])
                    s.op("pool", lambda hh: hh.tensor_tensor(out=ac[:], in0=ac[:], in1=g2[q][:], op=ALU.mult), reads=[ac, g2[q]], writes=[ac])
                    s.op("dve", lambda hh: hh.tensor_tensor(out=x[:], in0=x[:], in1=ac[:], op=ALU.add), reads=[x, ac], writes=[x])
                    if last:
                        self.rstd_of((x, x[:]), (junk, junk[:]), sq)
                        s.op("dve", lambda hh: hh.scalar_tensor_tensor(out=x[:], in0=x[:], scalar=sq[:, 0:1], in1=fg[:], op0=ALU.mult, op1=ALU.mult), reads=[x, sq, fg], writes=[x])
                        s.dma("sp", self.out[grow, :], x[:], reads=[x], writes=[R("out")])
                    else:
                        s.dma("sp", Dr["xres"][grow, :], x[:], reads=[x], writes=[DR["xres"]])
                s.barrier()


_CONSTS = None


def run(inputs, dbg=(), layers=L_DEPTH, nseq=NSEQ, stop_after=None, cores=NCORES, trace=False, only=None):
    global _CONSTS
    if _CONSTS is None:
        _CONSTS = host_consts()
    inp = {k: np.asarray(v) for k, v in inputs.items()}
    k = K(dbg=dbg, layers=layers, nseq=nseq, stop_after=stop_after, only=only)
    nc = k.build()
    in_maps = []
    for c in range(cores):
        m = host_inputs(inp, c)
        m.update(_CONSTS)
        in_maps.append(m)
    res = run_bass_kernel_spmd(nc, in_maps, core_ids=list(range(cores)), trace=trace)
    return res


def kernel(**inputs):
    res = run(inputs)
    out = np.concatenate([np.asarray(r["out"]).reshape(NSEQ, S, D) for r in res.results], axis=0)
    return out.astype(np.float32)
```

```python
import numpy as np
import ml_dtypes
from contextlib import ExitStack
import concourse.bass as bass
import concourse.mybir as mybir
from concourse.bass_utils import run_bass_kernel_spmd

F32 = mybir.dt.float32
BF16 = mybir.dt.bfloat16
I32 = mybir.dt.int32
AF = mybir.ActivationFunctionType
ALU = mybir.AluOpType
AX = mybir.AxisListType
NDSEM = 8


class R:
    __slots__ = ("name", "lw", "rd")

    def __init__(self, name=""):
        self.name = name
        self.lw = {}
        self.rd = {}


class T(R):
    __slots__ = ("t",)

    def __init__(self, t, name=""):
        super().__init__(name)
        self.t = t

    def __getitem__(self, k):
        return self.t[k]


class Eng:
    def __init__(self, name, h, sem, dsems):
        self.name = name
        self.h = h
        self.sem = sem
        self.count = 0
        self.known = {}
        self.dsems = dsems
        self.ndma = 0


class Sched:
    def __init__(self, nc, stack):
        self.nc = nc
        self.stack = stack
        self.E = {}
        for name, h, nd in (("pe", nc.tensor, 0), ("act", nc.scalar, NDSEM), ("dve", nc.vector, 0),
                            ("pool", nc.gpsimd, NDSEM), ("sp", nc.sync, NDSEM)):
            sem = stack.enter_context(nc.semaphore("s_" + name))
            ds = [stack.enter_context(nc.semaphore("d_%s%d" % (name, i))) for i in range(nd)]
            self.E[name] = Eng(name, h, sem, ds)
        self.dma_tokens = []
        self.nuniq = 0

    def sb(self, shape, dt, name=None, stack=None):
        self.nuniq += 1
        name = (name or "t") + "_%d" % self.nuniq
        t = (stack or self.stack).enter_context(self.nc.sbuf_tensor(name, list(shape), dt))
        return T(t, name)

    def ps(self, shape, dt=F32, name=None, stack=None):
        self.nuniq += 1
        name = (name or "p") + "_%d" % self.nuniq
        t = (stack or self.stack).enter_context(self.nc.psum_tensor(name, list(shape), dt))
        return T(t, name)

    def _wait(self, eng, tok):
        sem, val = tok
        if eng.known.get(sem, 0) >= val:
            return
        eng.h.wait_ge(sem, val)
        eng.known[sem] = val

    def _deps(self, eng, reads, writes):
        deps = []
        for r in reads:
            deps.extend(r.lw.items())
        for w in writes:
            deps.extend(w.lw.items())
            deps.extend(w.rd.items())
        for tok in deps:
            if tok[0] is eng.sem:
                if eng.name == "pe":
                    continue
                if tok[1] > eng.count:
                    continue
            self._wait(eng, tok)

    def _commit(self, tok, reads, writes):
        sem, val = tok
        for r in reads:
            if r.rd.get(sem, 0) < val:
                r.rd[sem] = val
        for w in writes:
            if w.lw.get(sem, 0) < val:
                w.lw[sem] = val

    def op(self, engname, fn, reads=(), writes=(), sig=True):
        eng = self.E[engname]
        self._deps(eng, reads, writes)
        ins = fn(eng.h)
        if sig:
            eng.count += 1
            ins.then_inc(eng.sem, 1)
            tok = (eng.sem, eng.count)
        else:
            tok = (eng.sem, eng.count + 1)
        self._commit(tok, reads, writes)
        return tok

    def dma(self, qname, out, in_, reads=(), writes=(), **kw):
        q = self.E[qname]
        i = q.ndma
        q.ndma += 1
        sem = q.dsems[i % NDSEM]
        val = 16 * (i // NDSEM + 1)
        if i >= NDSEM:
            self._wait(q, (sem, val - 16))
        self._deps(q, reads, writes)
        q.h.dma_start(out=out, in_=in_, **kw).then_inc(sem, 16)
        tok = (sem, val)
        self._commit(tok, reads, writes)
        self.dma_tokens.append(tok)
        return tok

    def idma(self, out, in_, out_off=None, in_off=None, reads=(), writes=(), bounds=None):
        q = self.E["pool"]
        i = q.ndma
        q.ndma += 1
        sem = q.dsems[i % NDSEM]
        val = 16 * (i // NDSEM + 1)
        if i >= NDSEM:
            self._wait(q, (sem, val - 16))
        self._deps(q, reads, writes)
        oo = bass.IndirectOffsetOnAxis(ap=out_off, axis=0) if out_off is not None else None
        io = bass.IndirectOffsetOnAxis(ap=in_off, axis=0) if in_off is not None else None
        if bounds is None:
            q.h.indirect_dma_start(out=out, out_offset=oo, in_=in_, in_offset=io).then_inc(sem, 16)
        else:
            if getattr(self, "bound_reg", None) is None:
                self.bound_reg = q.h.alloc_register("bnd")
                q.h.reg_mov(self.bound_reg, 65535)
            q.h.indirect_dma_start(out=out, out_offset=oo, in_=in_, in_offset=io, bounds_check=self.bound_reg, oob_is_err=False).then_inc(sem, 16)
        tok = (sem, val)
        self._commit(tok, reads, writes)
        self.dma_tokens.append(tok)
        return tok

    def barrier(self):
        toks = []
        for e in self.E.values():
            if e.count > 0:
                toks.append((e.sem, e.count))
            for j, s in enumerate(e.dsems):
                n = (e.ndma - j + NDSEM - 1) // NDSEM
                if n > 0:
                    toks.append((s, 16 * n))
        for e in self.E.values():
            for tok in toks:
                if tok[0] is e.sem:
                    continue
                self._wait(e, tok)

    def finish(self):
        sp = self.E["sp"]
        for e in self.E.values():
            for j, s in enumerate(e.dsems):
                n = (e.ndma - j + NDSEM - 1) // NDSEM
                if n > 0:
                    self._wait(sp, (s, 16 * n))


NCORES = 8
L_DEPTH = 2
D = 1024
S = 2048
NSEQ = 2
NT = S // 128
EPS = 1e-5
INW = 6680
SCALE = 0.125
NEG = -30000.0
NE = 32
SLOT_COL = ([0 + 64 * i for i in range(8)] + [512 + 64 * i for i in range(2)] + [768 + 64 * i for i in range(8)]
            + [1280 + 64 * i for i in range(8)] + [2304 + 64 * i for i in range(8)] + [2816 + 64 * i for i in range(2)]
            + [2944 + 64 * i for i in range(2)] + [3072 + 64 * i for i in range(2)] + [3328 + 64 * i for i in range(2)])
NSLOT = len(SLOT_COL)
SL_QA, SL_KA, SL_QB, SL_KB, SL_QC, SL_KCM, SL_VCM, SL_KSL, SL_KWN = 0, 8, 10, 18, 26, 34, 36, 38, 40
C_VA, C_VB, C_VSL, C_VWN, C_GN, C_GM = 640, 1792, 3200, 3456, 3584, 3608


def host_consts():
    bf = ml_dtypes.bfloat16
    c = {}
    t = np.arange(S)
    a_t, b_t = (t // 128).astype(np.float32), (t % 128).astype(np.float32)
    aug = np.zeros((NSLOT, 4, S), np.float32)
    kaug = np.stack([a_t, b_t, np.ones(S, np.float32), np.ones(S, np.float32)])

    def qaug(slope):
        return np.stack([np.full(S, 1024.0 * slope, np.float32), np.full(S, 8.0 * slope, np.float32),
                         -1024.0 * slope * a_t, -8.0 * slope * b_t])
    for i in range(8):
        aug[SL_QA + i] = qaug(2.0 ** -(i + 1))
        aug[SL_QC + i] = qaug(2.0 ** -(i + 1))
        aug[SL_QB + i] = qaug(2.0 ** (-2.0 * (i // 2 + 1)))
        aug[SL_KB + i] = kaug
    for i in range(2):
        aug[SL_KA + i] = kaug
        aug[SL_KSL + i] = kaug
        aug[SL_KWN + i] = kaug
    c["aug"] = aug.astype(bf)
    c["ident"] = np.eye(128, dtype=np.float32).astype(bf)
    c["ident32"] = np.eye(128, dtype=np.float32)
    sk = np.arange(128)[:, None]
    tq = np.arange(128)[None, :]
    c["mdiag"] = np.where(tq >= sk, 0.0, NEG).astype(bf)
    c["medge"] = np.where(tq < sk, 0.0, NEG).astype(bf)
    cc = np.arange(128)[:, None]
    c["cmaskT"] = np.where((16 * cc + 31 <= t[None, :]) & (cc < 127), 0.0, NEG).astype(bf)
    nb = np.arange(32)
    c["eblk"] = (t[None, :] // 64 == nb[:, None]).astype(np.float32).astype(bf)
    cur = t // 64
    forced = (nb[None, :] == 0) | (nb[None, :] == cur[:, None]) | (nb[None, :] == cur[:, None] - 1)
    causal = nb[None, :] <= cur[:, None]
    A = np.where(forced, 1e9 + 1e6 * nb[None, :], np.where(causal, 0.0, -1e9 - 1e6 * nb[None, :]))
    c["atab"] = A.astype(np.float32)
    cstart = np.arange(127) * 16
    sstart = nb * 64
    ov = np.clip(np.minimum(cstart[:, None] + 32, sstart[None, :] + 64) - np.maximum(cstart[:, None], sstart[None, :]), 0, None) / 32.0
    ovp = np.zeros((128, 32), np.float32)
    ovp[:127] = ov
    c["overlap"] = ovp.astype(bf)
    c["ltri"] = (np.arange(128)[:, None] < np.arange(128)[None, :]).astype(np.float32).astype(bf)
    c["ones128"] = np.ones((128, 128), np.float32).astype(bf)
    c["b512"] = np.broadcast_to((512.0 * np.arange(64, dtype=np.float32))[None, :], (128, 64)).copy()
    c["rowiota"] = (np.arange(8, dtype=np.float32)[None, :] * 128 + np.arange(128, dtype=np.float32)[:, None]).copy()
    c["piota"] = np.arange(128, dtype=np.float32).reshape(128, 1).copy()
    return c


def host_inputs(inp, core):
    b0 = core * NSEQ
    m = {}
    m["x"] = np.ascontiguousarray(inp["x"][b0:b0 + NSEQ].reshape(NSEQ * S, D))
    m["cT"] = np.ascontiguousarray(inp["c"][b0:b0 + NSEQ].T)
    for k in ("mod_w", "mod_b", "norm1_g", "norm2_g", "w_in", "b_in", "sinks", "diff_subln_g", "cmp_w1", "cmp_w2",
              "cmp_b2", "w_branch", "w_out", "router_w", "router_b", "exp_b2"):
        m[k] = inp[k]
    m["final_g"] = inp["final_g"].reshape(1, D)
    m["b_inT"] = np.ascontiguousarray(np.stack([inp["b_in"][:, SLOT_COL[2 * i]:SLOT_COL[2 * i] + 128] for i in range(NSLOT // 2)], axis=2))
    m["diff_lambda"] = inp["diff_lambda"].reshape(L_DEPTH, 256)
    m["cmp_posT"] = np.ascontiguousarray(inp["cmp_pos"].transpose(0, 1, 3, 2))
    m["cmp_b1T"] = np.ascontiguousarray(inp["cmp_b1"].reshape(L_DEPTH, 2, 2, 128).transpose(0, 1, 3, 2))
    m["cmp_b2T"] = np.ascontiguousarray(inp["cmp_b2"].reshape(L_DEPTH, 2, 64, 1))
    m["exp_b1E"] = np.ascontiguousarray(inp["exp_b1"].reshape(L_DEPTH, NE, 2, 128, 8).transpose(0, 1, 3, 2, 4)).reshape(L_DEPTH * NE * 128, 16)
    m["exp_w1"] = inp["exp_w1"].reshape(L_DEPTH * NE * 256, 4 * 2 * D)
    m["exp_w2"] = inp["exp_w2"].reshape(L_DEPTH * NE * 128, 8 * D)
    return m


IN_SHAPES = {
    "x": ([NSEQ * S, D], F32), "cT": ([D, NSEQ], F32), "mod_w": ([L_DEPTH, D, 6 * D], F32), "mod_b": ([L_DEPTH, 6 * D], F32),
    "norm1_g": ([L_DEPTH, D], F32), "norm2_g": ([L_DEPTH, D], F32), "w_in": ([L_DEPTH, D, INW], F32), "b_in": ([L_DEPTH, INW], F32),
    "sinks": ([L_DEPTH, 8], F32), "diff_subln_g": ([L_DEPTH, 128], F32), "cmp_w1": ([L_DEPTH, 2, 2048, 256], F32),
    "cmp_w2": ([L_DEPTH, 2, 256, 64], F32), "cmp_b2": ([L_DEPTH, 2, 64], F32), "w_branch": ([L_DEPTH, 3, 512, D], F32),
    "w_out": ([L_DEPTH, D, D], F32), "router_w": ([L_DEPTH, D, NE], F32), "router_b": ([L_DEPTH, NE], F32),
    "exp_w1": ([L_DEPTH * NE * 256, 8 * D], F32), "exp_w2": ([L_DEPTH * NE * 128, 8 * D], F32), "exp_b2": ([L_DEPTH, NE, D], F32),
    "final_g": ([1, D], F32), "b_inT": ([L_DEPTH, 128, NSLOT // 2], F32), "diff_lambda": ([L_DEPTH, 256], F32),
    "cmp_posT": ([L_DEPTH, 2, 64, 32], F32), "cmp_b1T": ([L_DEPTH, 2, 128, 2], F32), "cmp_b2T": ([L_DEPTH, 2, 64, 1], F32),
    "exp_b1E": ([L_DEPTH * NE * 128, 16], F32),
    "ltri": ([128, 128], BF16), "ones128": ([128, 128], BF16), "b512": ([128, 64], F32), "rowiota": ([128, 8], F32), "piota": ([128, 1], F32),
    "aug": ([NSLOT, 4, S], BF16), "ident": ([128, 128], BF16), "ident32": ([128, 128], F32), "mdiag": ([128, 128], BF16),
    "medge": ([128, 128], BF16), "cmaskT": ([128, S], BF16), "eblk": ([32, S], BF16), "atab": ([S, 32], F32),
    "overlap": ([128, 32], BF16),
}


def bcast_rows(ap1d_row, nparts):
    return ap1d_row.broadcast_to([nparts, ap1d_row.shape[-1]])


class K:
    def __init__(self, dbg=(), layers=L_DEPTH, nseq=NSEQ, stop_after=None, only=None):
        self.dbg = set(dbg)
        self.only = only
        self.layers = layers
        self.nseq = nseq
        self.stop_after = stop_after
        nc = self.nc = bass.Bass("TRN2", target_bir_lowering=False)
        self.I = {k: nc.dram_tensor(k, list(sh), dt, kind="ExternalInput").ap() for k, (sh, dt) in IN_SHAPES.items()}
        self.out = nc.dram_tensor("out", [NSEQ * S, D], F32, kind="ExternalOutput").ap()
        self.Dr = {}
        self.DR = {}

    def dram(self, name, shape, dt):
        kind = "ExternalOutput" if name in self.dbg else "Internal"
        self.Dr[name] = self.nc.dram_tensor(name, list(shape), dt, kind=kind).ap()
        self.DR[name] = R(name)
        return self.Dr[name]

    def mm(self, out, lhsT, rhs, start, stop, reads, writes, sig=True):
        return self.s.op("pe", lambda h: h.matmul(out, lhsT=lhsT, rhs=rhs, start=start, stop=stop), reads=reads, writes=writes, sig=sig)

    def tr(self, out, in_, ident, reads, writes, sig=True):
        return self.s.op("pe", lambda h: h.transpose(out=out, in_=in_, identity=ident), reads=reads, writes=writes, sig=sig)

    def rstd_of(self, xt, junk, ss, n=D):
        s = self.s
        s.op("act", lambda h: h.activation(out=junk[1], in_=xt[1], func=AF.Square, accum_out=ss[:]), reads=[xt[0]], writes=[junk[0], ss])
        s.op("dve", lambda h: h.tensor_scalar(out=ss[:], in0=ss[:], scalar1=1.0 / n, scalar2=EPS, op0=ALU.mult, op1=ALU.add), reads=[ss], writes=[ss])
        s.op("act", lambda h: h.sqrt(out=ss[:], in_=ss[:]), reads=[ss], writes=[ss])
        s.op("dve", lambda h: h.reciprocal(out=ss[:], in_=ss[:]), reads=[ss], writes=[ss])

    def build(self):
        nc = self.nc
        with ExitStack() as st:
            s = self.s = Sched(nc, st)
            self.P = [s.ps([128, 512], F32, "bank%d" % i) for i in range(8)]
            self.ident = s.sb([128, 128], BF16, "ident")
            self.ident32 = s.sb([128, 128], F32, "ident32")
            s.dma("sp", self.ident[:], self.I["ident"], writes=[self.ident])
            s.dma("sp", self.ident32[:], self.I["ident32"], writes=[self.ident32])
            self.dram("mod_d", [L_DEPTH, NSEQ, 6 * D], F32)
            self.dram("xres", [NSEQ * S, D], F32)
            self.dram("QT_d", [NSLOT, 64, S], BF16)
            self.dram("VA_d", [S, 2, 65], BF16)
            self.dram("VB_d", [S, 4, 129], BF16)
            self.dram("VSL_d", [S, 2, 65], BF16)
            self.dram("VWN_d", [S, 2, 65], BF16)
            self.dram("GN_d", [S, 24], F32)
            self.dram("GM_d", [S, 3072], F32)
            self.dram("KC_d", [2, 64, 128], BF16)
            self.dram("VC_d", [2, 128, 97], BF16)
            self.dram("O_d", [S, 1536], BF16)
            self.ntt = self.nseq * NT
            self.nblk = self.ntt + NE
            self.dram("xs_d", [self.nblk * 512, D], BF16)
            self.dram("ys_d", [self.nblk * 512, D], F32)
            self.dram("dbg_d", [128, 4096], F32)
            zt = s.sb([128, 2048], BF16, "zt")
            s.op("pool", lambda h: h.memset(zt[:], 0.0), writes=[zt])
            xv = self.Dr["xs_d"].rearrange("(a p r) d -> a p (r d)", p=128, r=2)
            for a in range(xv.shape[0]):
                s.dma("pool", xv[a], zt[:], reads=[zt], writes=[])
            try:
                self.body()
            except StopIteration:
                pass
            s.barrier()
            s.finish()
        return nc

    def phase_end(self, name):
        self.s.barrier()
        if self.stop_after == name:
            raise StopIteration

    def body(self):
        for l in range(self.layers):
            self.runp("mod%d" % l, self.phase_mod, l)
            for q in range(self.nseq):
                for nm, fn in (("inproj", self.phase_inproj), ("cmp", self.phase_cmp), ("swa", self.phase_swa), ("diff", self.phase_diff),
                               ("nsa", self.phase_nsa), ("merge", self.phase_merge)):
                    self.runp("%s%d_%d" % (nm, l, q), fn, l, q)
            self.runp("moe%d" % l, self.phase_moe2, l)

    def runp(self, name, fn, *args):
        if self.only is None or name in self.only:
            fn(*args)
        self.phase_end(name)

    def phase_mod(self, l):
        s, I = self.s, self.I
        with ExitStack() as ps:
            cT = s.sb([128, 8, NSEQ], F32, "cT", ps)
            cs = s.sb([128, 8, NSEQ], F32, "cs", ps)
            modb = s.sb([NSEQ, 6 * D], F32, "modb", ps)
            mods = s.sb([NSEQ, 6 * D], F32, "mods", ps)
            wb = [s.sb([128, 8, 512], F32, "modw", ps) for _ in range(2)]
            s.dma("sp", cT[:], I["cT"].rearrange("(kc p) b -> p kc b", p=128), writes=[cT])
            s.dma("sp", modb[:], I["mod_b"][l:l + 1, :].broadcast_to([NSEQ, 6 * D]), writes=[modb])
            s.op("act", lambda h: h.activation(out=cs[:], in_=cT[:], func=AF.Silu), reads=[cT], writes=[cs])
            wsrc = I["mod_w"][l].rearrange("(kc p) n -> p kc n", p=128)
            for cg in range(12):
                w = wb[cg % 2]
                s.dma("sp", w[:], wsrc[:, :, cg * 512:(cg + 1) * 512], writes=[w])
                pm = self.P[cg % 2]
                for kc in range(8):
                    self.mm(pm[0:NSEQ, :], cs[:, kc, :], w[:, kc, :], kc == 0, kc == 7, [cs, w], [pm], sig=(kc == 7))
                s.op("dve", lambda h: h.tensor_tensor(out=mods[:, cg * 512:(cg + 1) * 512], in0=pm[0:NSEQ, :], in1=modb[:, cg * 512:(cg + 1) * 512], op=ALU.add),
                     reads=[pm, modb], writes=[mods])
            for seg in (1, 4):
                s.op("dve", lambda h: h.tensor_scalar_add(out=mods[:, seg * D:(seg + 1) * D], in0=mods[:, seg * D:(seg + 1) * D], scalar1=1.0), reads=[mods], writes=[mods])
            s.dma("sp", self.Dr["mod_d"][l], mods[:], reads=[mods], writes=[self.DR["mod_d"]])

    def mod_bc(self, l, q, seg, tile):
        src = self.Dr["mod_d"][l, q:q + 1, seg * D:(seg + 1) * D].broadcast_to([128, D])
        self.s.dma("sp", tile[:], src, reads=[self.DR["mod_d"]], writes=[tile])

    def xsrc(self, l, q):
        return (self.I["x"] if l == 0 else self.Dr["xres"]), ([] if l == 0 else [self.DR["xres"]])

    def norm_tiles(self, ps, l, q, gname, seg_sc, seg_sh):
        s, I = self.s, self.I
        gsc = s.sb([128, D], F32, "gsc", ps)
        sh = s.sb([128, D], F32, "sh", ps)
        gt = s.sb([128, D], F32, "gt", ps)
        s.dma("sp", gt[:], I[gname][l:l + 1, :].broadcast_to([128, D]), writes=[gt])
        self.mod_bc(l, q, seg_sc, gsc)
        self.mod_bc(l, q, seg_sh, sh)
        s.op("dve", lambda h: h.tensor_tensor(out=gsc[:], in0=gsc[:], in1=gt[:], op=ALU.mult), reads=[gsc, gt], writes=[gsc])
        return gsc, sh

    def phase_inproj(self, l, q):
        s, I, Dr, DR = self.s, self.I, self.Dr, self.DR
        xsrc, xres_r = self.xsrc(l, q)
        with ExitStack() as ps:
            gsc, sh = self.norm_tiles(ps, l, q, "norm1_g", 1, 0)
            hT = s.sb([128, 8, S], BF16, "hT", ps)
            hTr = [R("hT%d" % i) for i in range(4)]
            xt = [s.sb([128, D], F32, "xt", ps) for _ in range(2)]
            junk = s.sb([128, D], F32, "junk", ps)
            hb = [s.sb([128, D], BF16, "hb", ps) for _ in range(2)]
            ss = [s.sb([128, 1], F32, "ss", ps) for _ in range(2)]
            for tt in range(NT):
                x, h, sq = xt[tt % 2], hb[tt % 2], ss[tt % 2]
                r0 = q * S + tt * 128
                s.dma("sp", x[:], xsrc[r0:r0 + 128, :], reads=xres_r, writes=[x])
                self.rstd_of((x, x[:]), (junk, junk[:]), sq)
                s.op("dve", lambda hh: hh.scalar_tensor_tensor(out=x[:], in0=x[:], scalar=sq[:, 0:1], in1=gsc[:], op0=ALU.mult, op1=ALU.mult), reads=[x, sq, gsc], writes=[x])
                s.op("pool", lambda hh: hh.tensor_tensor(out=h[:], in0=x[:], in1=sh[:], op=ALU.add), reads=[x, sh], writes=[h])
                pt = self.P[tt % 2]
                ptb = pt[:].bitcast(BF16)
                for kc in range(8):
                    self.tr(ptb[:, kc * 128:(kc + 1) * 128], h[:, kc * 128:(kc + 1) * 128], self.ident[:], [h, self.ident], [pt], sig=(kc == 7))
                s.op("act", lambda hh: hh.copy(out=hT[:, :, tt * 128:(tt + 1) * 128], in_=ptb.rearrange("p (k t) -> p k t", k=8)), reads=[pt], writes=[hTr[tt // 4]])
            binT = s.sb([128, NSLOT // 2], F32, "binT", ps)
            s.dma("sp", binT[:], I["b_inT"][l], writes=[binT])
            wsrc = I["w_in"][l].rearrange("(kc p) n -> p kc n", p=128)
            wbuf = [s.sb([128, 8, 512], BF16, "wblk", ps) for _ in range(2)]
            stg = [s.sb([128, S], BF16, "stg", ps) for _ in range(2)]
            groups = [(SL_QA, 8), (SL_KA, 2), (SL_QB, 8), (SL_KB, 8), (SL_QC, 8), (SL_KCM, 4), (SL_KSL, 2), (SL_KWN, 2)]
            nw = 0
            nmm = 0
            for (s0, ns) in groups:
                w = wbuf[nw % 2]
                nw += 1
                c0 = SLOT_COL[s0]
                s.dma("pool", w[:, :, 0:ns * 64], wsrc[:, :, c0:c0 + ns * 64], writes=[w])
                for si in range(0, ns, 2):
                    slot = s0 + si
                    pr_ = slot // 2
                    sg = stg[pr_ % 2]
                    for tg in range(4):
                        pm = self.P[2 + nmm % 4]
                        nmm += 1
                        for kc in range(8):
                            self.mm(pm[:, :], w[:, kc, si * 64:(si + 2) * 64], hT[:, kc, tg * 512:(tg + 1) * 512], kc == 0, kc == 7, [w, hTr[tg]], [pm], sig=(kc == 7))
                        s.op("act", lambda hh: hh.activation(out=sg[:, tg * 512:(tg + 1) * 512], in_=pm[:, :], func=AF.Identity, bias=binT[:, pr_:pr_ + 1]),
                             reads=[pm, binT], writes=[sg])
                    s.dma("sp", Dr["QT_d"][slot], sg[0:64, :], reads=[sg], writes=[DR["QT_d"]])
                    s.dma("sp", Dr["QT_d"][slot + 1], sg[64:128, :], reads=[sg], writes=[DR["QT_d"]])
            binb = s.sb([128, INW], F32, "binb", ps)
            s.dma("sp", binb[:], I["b_in"][l:l + 1, :].broadcast_to([128, INW]), writes=[binb])
            vt = {}
            for nm, nh, dv in (("VA_d", 2, 64), ("VB_d", 4, 128), ("VSL_d", 2, 64), ("VWN_d", 2, 64)):
                vt[nm] = [s.sb([128, nh, dv + 1], BF16, "vt", ps) for _ in range(2)]
                for v in vt[nm]:
                    s.op("pool", lambda hh: hh.memset(v[:, :, dv:dv + 1], 1.0), writes=[v])
            gnt = [s.sb([128, 24], F32, "gnt", ps) for _ in range(2)]
            gmt = [s.sb([128, 512], F32, "gmt", ps) for _ in range(2)]
            blocks = [("VA_d", C_VA, 128, 2, 64), ("VB_d", C_VB, 512, 4, 128), ("VSL_d", C_VSL, 128, 2, 64), ("VWN_d", C_VWN, 128, 2, 64),
                      ("GN_d", C_GN, 24, 0, 0)] + [("GM_d", C_GM + 512 * i, 512, i, 0) for i in range(6)]
            for (nm, c0, ncol, nh, dv) in blocks:
                w = wbuf[nw % 2]
                nw += 1
                s.dma("pool", w[:, :, 0:ncol], wsrc[:, :, c0:c0 + ncol], writes=[w])
                for tt in range(NT):
                    pm = self.P[2 + nmm % 4]
                    nmm += 1
                    for kc in range(8):
                        self.mm(pm[:, 0:ncol], hT[:, kc, tt * 128:(tt + 1) * 128], w[:, kc, 0:ncol], kc == 0, kc == 7, [w, hTr[tt // 4]], [pm], sig=(kc == 7))
                    rows = slice(tt * 128, (tt + 1) * 128)
                    if nm == "GN_d":
                        g = gnt[tt % 2]
                        s.op("dve", lambda hh: hh.tensor_tensor(out=g[:], in0=pm[:, 0:24], in1=binb[:, c0:c0 + 24], op=ALU.add), reads=[pm, binb], writes=[g])
                        s.op("act", lambda hh: hh.activation(out=g[:], in_=g[:], func=AF.Sigmoid), reads=[g], writes=[g])
                        s.dma("sp", Dr["GN_d"][rows, :], g[:], reads=[g], writes=[DR["GN_d"]])
                    elif nm == "GM_d":
                        g = gmt[tt % 2]
                        s.op("dve", lambda hh: hh.tensor_tensor(out=g[:], in0=pm[:, :], in1=binb[:, c0:c0 + 512], op=ALU.add), reads=[pm, binb], writes=[g])
                        s.op("act", lambda hh: hh.activation(out=g[:], in_=g[:], func=AF.Sigmoid), reads=[g], writes=[g])
                        s.dma("sp", Dr["GM_d"][rows, nh * 512:(nh + 1) * 512], g[:], reads=[g], writes=[DR["GM_d"]])
                    else:
                        v = vt[nm][tt % 2]
                        s.op("dve", lambda hh: hh.tensor_tensor(out=v[:, :, 0:dv], in0=pm[:, 0:ncol].rearrange("p (h d) -> p h d", d=dv),
                                                               in1=binb[:, c0:c0 + ncol].rearrange("p (h d) -> p h d", d=dv), op=ALU.add), reads=[pm, binb], writes=[v])
                        s.dma("sp", Dr[nm][rows], v[:], reads=[v], writes=[DR[nm]])

    def phase_cmp(self, l, q):
        s, I, Dr, DR = self.s, self.I, self.Dr, self.DR
        with ExitStack() as ps:
            ovl = self.load_const(ps, "overlap", [128, 32], BF16)
            w1 = s.sb([64, 32, 256], BF16, "cw1", ps)
            w2 = s.sb([128, 2, 64], BF16, "cw2", ps)
            posT = s.sb([64, 32], F32, "posT", ps)
            posb = s.sb([64, 32], BF16, "posb", ps)
            b1T = s.sb([128, 2], F32, "b1T", ps)
            bias = s.sb([128, 2], F32, "cbias", ps)
            b2T = s.sb([64, 1], F32, "b2T", ps)
            b2b = s.sb([128, 64], F32, "b2b", ps)
            xT = s.sb([64, S], BF16, "cxT", ps)
            hid = s.sb([128, 2, 128], BF16, "hid", ps)
            y = s.sb([128, 128], F32, "cy", ps)
            u = s.sb([128, 128], F32, "cu", ps)
            kct = s.sb([64, 128], BF16, "kct", ps)
            vct = s.sb([128, 97], BF16, "vct", ps)
            s.op("pool", lambda h: h.memset(kct[:], 0.0), writes=[kct])
            s.op("pool", lambda h: h.memset(vct[:], 0.0), writes=[vct])
            for which in range(2):
                s.dma("pool", w1[:], I["cmp_w1"][l, which].rearrange("(l d) f -> d l f", d=64), writes=[w1])
                s.dma("pool", w2[:], I["cmp_w2"][l, which].rearrange("(c p) d -> p c d", p=128), writes=[w2])
                s.dma("sp", posT[:], I["cmp_posT"][l, which], writes=[posT])
                s.dma("sp", b1T[:], I["cmp_b1T"][l, which], writes=[b1T])
                s.dma("sp", b2T[:], I["cmp_b2T"][l, which], writes=[b2T])
                s.dma("sp", b2b[:], I["cmp_b2"][l, which:which + 1, :].broadcast_to([128, 64]), writes=[b2b])
                s.op("act", lambda h: h.copy(out=posb[:], in_=posT[:]), reads=[posT], writes=[posb])
                for ch in range(2):
                    pm = self.P[ch]
                    for li in range(32):
                        self.mm(pm[:, 0:1], w1[:, li, ch * 128:(ch + 1) * 128], posb[:, li:li + 1], li == 0, li == 31, [w1, posb], [pm], sig=(li == 31))
                    s.op("dve", lambda h: h.tensor_tensor(out=bias[:, ch:ch + 1], in0=pm[:, 0:1], in1=b1T[:, ch:ch + 1], op=ALU.add), reads=[pm, b1T], writes=[bias])
                for hk in range(2):
                    s.dma("sp", xT[:], Dr["QT_d"][SL_KCM + which * 2 + hk], reads=[DR["QT_d"]], writes=[xT])
                    for ch in range(2):
                        pm = self.P[2 + ch]
                        for li in range(32):
                            self.mm(pm[:, 0:127], w1[:, li, ch * 128:(ch + 1) * 128], xT[:, li:li + 16 * 126 + 1:16], li == 0, li == 31, [w1, xT], [pm], sig=(li == 31))
                        s.op("act", lambda h: h.activation(out=y[:, 0:127], in_=pm[:, 0:127], func=AF.Identity, bias=bias[:, ch:ch + 1]), reads=[pm, bias], writes=[y])
                        s.op("dve", lambda h: h.tensor_tensor(out=u[:, 0:127], in0=y[:, 0:127], in1=y[:, 0:127], op=ALU.mult), reads=[y], writes=[u])
                        s.op("dve", lambda h: h.tensor_scalar(out=u[:, 0:127], in0=u[:, 0:127], scalar1=0.044715, scalar2=1.0, op0=ALU.mult, op1=ALU.add), reads=[u], writes=[u])
                        s.op("dve", lambda h: h.tensor_tensor(out=u[:, 0:127], in0=u[:, 0:127], in1=y[:, 0:127], op=ALU.mult), reads=[u, y], writes=[u])
                        s.op("act", lambda h: h.activation(out=u[:, 0:127], in_=u[:, 0:127], func=AF.Sigmoid, scale=1.5957691216057308), reads=[u], writes=[u])
                        s.op("dve", lambda h: h.tensor_tensor(out=hid[:, ch, 0:127], in0=u[:, 0:127], in1=y[:, 0:127], op=ALU.mult), reads=[u, y], writes=[hid])
                    pm2 = self.P[4 + hk]
                    if which == 0:
                        for ch in range(2):
                            self.mm(pm2[0:64, 0:127], w2[:, ch, :], hid[:, ch, 0:127], ch == 0, ch == 1, [w2, hid], [pm2], sig=(ch == 1))
                        s.op("act", lambda h: h.activation(out=kct[:, 0:127], in_=pm2[0:64, 0:127], func=AF.Identity, bias=b2T[:, 0:1]), reads=[pm2, b2T], writes=[kct])
                        s.dma("sp", Dr["KC_d"][hk], kct[:], reads=[kct], writes=[DR["KC_d"]])
                    else:
                        for ch in range(2):
                            self.mm(pm2[0:127, 0:64], hid[:, ch, 0:127], w2[:, ch, :], ch == 0, ch == 1, [w2, hid], [pm2], sig=(ch == 1))
                        s.op("dve", lambda h: h.tensor_tensor(out=vct[0:127, 0:64], in0=pm2[0:127, 0:64], in1=b2b[0:127, :], op=ALU.add), reads=[pm2, b2b], writes=[vct])
                        s.op("pool", lambda h: h.memset(vct[:, 64:65], 1.0), writes=[vct])
                        s.op("pool", lambda h: h.tensor_copy(out=vct[:, 65:97], in_=ovl[:]), reads=[ovl], writes=[vct])
                        s.dma("sp", Dr["VC_d"][hk], vct[:], reads=[vct], writes=[DR["VC_d"]])

    def load_heads(self, tile, slots, grouped):
        s = self.s
        for g, slot in enumerate(slots):
            dst0 = tile[0:64, g, :] if grouped else tile[0:64, :]
            dst1 = tile[64:68, g, :] if grouped else tile[64:68, :]
            s.dma("sp", dst0, self.Dr["QT_d"][slot], reads=[self.DR["QT_d"]], writes=[tile])
            s.dma("sp", dst1, self.I["aug"][slot], writes=[tile])

    def attn_stream(self, items, banks, pts):
        s = self.s
        n = len(items)

        def emit_qk(k):
            it = items[k]
            sc = banks[k % len(banks)]
            nq = len(it["qk"])
            for m, (outfn, lhsT, rhs, start, stop, reads) in enumerate(it["qk"]):
                self.mm(outfn(sc), lhsT, rhs, start, stop, reads, [sc], sig=(m == nq - 1))

        emit_qk(0)
        if n > 1:
            emit_qk(1)
        for k in range(n):
            if k + 2 < n:
                emit_qk(k + 2)
            it = items[k]
            sc = banks[k % len(banks)]
            pT = pts[k % len(pts)]
            kp, N = it["kp"], it["N"]
            s.op("act", lambda h: h.activation(out=pT[0:kp, 0:N], in_=sc[0:kp, 0:N], func=AF.Exp, scale=SCALE), reads=[sc], writes=[pT])
            it["pv"](pT)

    def load_const(self, ps, name, shape, dt):
        t = self.s.sb(shape, dt, name, ps)
        self.s.dma("sp", t[:], self.I[name], writes=[t])
        return t

    def phase_swa(self, l, q):
        s, I, Dr, DR = self.s, self.I, self.Dr, self.DR
        ident = self.ident
        with ExitStack() as ps:
            mdiag = self.load_const(ps, "mdiag", [128, 128], BF16)
            medge = self.load_const(ps, "medge", [128, 128], BF16)
            esink = s.sb([128, 8], F32, "esink", ps)
            s.dma("sp", esink[:], I["sinks"][l:l + 1, :].broadcast_to([128, 8]), writes=[esink])
            s.op("act", lambda h: h.activation(out=esink[:], in_=esink[:], func=AF.Exp), reads=[esink], writes=[esink])
            QG = s.sb([68, 4, S], BF16, "QG", ps)
            KT = s.sb([68, S], BF16, "KT", ps)
            V = s.sb([128, NT, 65], BF16, "V", ps)
            pts = [s.sb([128, 512], BF16, "pT", ps) for _ in range(3)]
            ot = [s.sb([128, 4, 64], BF16, "ot", ps) for _ in range(2)]
            den = [s.sb([128, 4], F32, "den", ps) for _ in range(2)]
            for hk in range(2):
                self.load_heads(QG, [SL_QA + hk * 4 + g for g in range(4)], True)
                self.load_heads(KT, [SL_KA + hk], False)
                s.dma("sp", V[:], Dr["VA_d"][:, hk, :].rearrange("(n p) e -> p n e", p=128), reads=[DR["VA_d"]], writes=[V])
                for j in range(NT):
                    acc = self.P[4 + j % 2]
                    tiles = [i for i in (j - 1, j) if i >= 0]
                    items = []
                    for idx, i in enumerate(tiles):
                        mask = mdiag if i == j else medge
                        o3 = lambda sc: sc[:, :].rearrange("p (g t) -> p g t", g=4)
                        qk = [(o3, KT[:, i * 128:(i + 1) * 128], QG[:, :, j * 128:(j + 1) * 128], True, False, [KT, QG]),
                              (o3, ident[:], mask[:].unsqueeze(1).broadcast_to([128, 4, 128]), False, True, [ident, mask])]

                        def pv(pT, i=i, idx=idx, acc=acc, last=len(tiles) - 1):
                            for g in range(4):
                                self.mm(acc[:, g * 65:(g + 1) * 65], pT[:, g * 128:(g + 1) * 128], V[:, i, :], idx == 0 and g == 0, idx == last, [pT, V], [acc], sig=(g == 3))
                        items.append(dict(kp=128, N=512, qk=qk, pv=pv))
                    self.attn_stream(items, self.P[0:3], pts)
                    a3 = acc[:, 0:260].rearrange("p (g e) -> p g e", e=65)
                    dn, o = den[j % 2], ot[j % 2]
                    s.op("dve", lambda h: h.tensor_tensor(out=dn[:].unsqueeze(2), in0=a3[:, :, 64:65], in1=esink[:, hk * 4:(hk + 1) * 4].unsqueeze(2), op=ALU.add), reads=[acc, esink], writes=[dn])
                    s.op("dve", lambda h: h.reciprocal(out=dn[:], in_=dn[:]), reads=[dn], writes=[dn])
                    s.op("dve", lambda h: h.tensor_tensor(out=o[:], in0=a3[:, :, 0:64], in1=dn[:].unsqueeze(2).broadcast_to([128, 4, 64]), op=ALU.mult), reads=[acc, dn], writes=[o])
                    s.dma("sp", Dr["O_d"][j * 128:(j + 1) * 128, hk * 256:(hk + 1) * 256], o[:].rearrange("p g d -> p (g d)"), reads=[o], writes=[DR["O_d"]])

    def phase_diff(self, l, q):
        s, I, Dr, DR = self.s, self.I, self.Dr, self.DR
        ident = self.ident
        lam_init = 0.8 - 0.6 * float(np.exp(-0.3 * l))
        with ExitStack() as ps:
            mdiag = self.load_const(ps, "mdiag", [128, 128], BF16)
            dl = s.sb([128, 256], F32, "dl", ps)
            s.dma("sp", dl[:], I["diff_lambda"][l:l + 1, :].broadcast_to([128, 256]), writes=[dl])
            pr = s.sb([128, 2, 64], F32, "pr", ps)
            d4 = dl[:].rearrange("p (a b d) -> p a b d", a=2, b=2)
            s.op("dve", lambda h: h.tensor_tensor(out=pr[:], in0=d4[:, :, 0, :], in1=d4[:, :, 1, :], op=ALU.mult), reads=[dl], writes=[pr])
            e2 = s.sb([128, 2], F32, "e2", ps)
            s.op("dve", lambda h: h.reduce_sum(out=e2[:], in_=pr[:], axis=AX.X), reads=[pr], writes=[e2])
            s.op("act", lambda h: h.activation(out=e2[:], in_=e2[:], func=AF.Exp), reads=[e2], writes=[e2])
            nlam = s.sb([128, 1], F32, "nlam", ps)
            s.op("dve", lambda h: h.scalar_tensor_tensor(out=nlam[:], in0=e2[:, 0:1], scalar=lam_init, in1=e2[:, 1:2], op0=ALU.add, op1=ALU.subtract), reads=[e2], writes=[nlam])
            s.op("dve", lambda h: h.tensor_scalar_mul(out=nlam[:], in0=nlam[:], scalar1=-1.0), reads=[nlam], writes=[nlam])
            gsub = s.sb([128, 128], F32, "gsub", ps)
            s.dma("sp", gsub[:], I["diff_subln_g"][l:l + 1, :].broadcast_to([128, 128]), writes=[gsub])
            s.op("dve", lambda h: h.tensor_scalar_mul(out=gsub[:], in0=gsub[:], scalar1=1.0 - lam_init), reads=[gsub], writes=[gsub])
            KT = [s.sb([68, S], BF16, "KTb", ps) for _ in range(2)]
            QT = [s.sb([68, S], BF16, "QTb", ps) for _ in range(2)]
            V = s.sb([128, NT, 129], BF16, "Vb", ps)
            pts = [s.sb([128, 512], BF16, "pT", ps) for _ in range(3)]
            t0 = [s.sb([128, 128], F32, "t0", ps) for _ in range(2)]
            junk = s.sb([128, 128], F32, "junkb", ps)
            rr = [s.sb([128, 4], F32, "rr", ps) for _ in range(2)]
            ob = [s.sb([128, 128], BF16, "ob", ps) for _ in range(2)]
            accc = [[s.sb([128, 258], F32, "accc", ps) for _ in range(4)] for _ in range(2)]
            nfin = 0
            for hd in range(4):
                for c in range(2):
                    self.load_heads(KT[c], [SL_KB + 2 * hd + c], False)
                    self.load_heads(QT[c], [SL_QB + 2 * hd + c], False)
                s.dma("sp", V[:], Dr["VB_d"][:, hd, :].rearrange("(n p) e -> p n e", p=128), reads=[DR["VB_d"]], writes=[V])
                for G in range(4):
                    def accap(c, jj):
                        return self.P[4 + c * 2 + jj // 2], (jj % 2) * 129
                    items = []
                    for c in range(2):
                        for i in range(0, 4 * G + 4):
                            j0 = max(i, 4 * G)
                            N = (4 * G + 4 - j0) * 128
                            kt = KT[c][:, i * 128:(i + 1) * 128]
                            qk = []
                            if i >= 4 * G:
                                qk.append((lambda sc: sc[:, 0:128], kt, QT[c][:, j0 * 128:(j0 + 1) * 128], True, False, [KT[c], QT[c]]))
                                qk.append((lambda sc: sc[:, 0:128], ident[:], mdiag[:], False, True, [ident, mdiag]))
                                if N > 128:
                                    qk.append((lambda sc, N=N: sc[:, 128:N], kt, QT[c][:, (j0 + 1) * 128:(4 * G + 4) * 128], True, True, [KT[c], QT[c]]))
                            else:
                                qk.append((lambda sc, N=N: sc[:, 0:N], kt, QT[c][:, j0 * 128:(4 * G + 4) * 128], True, True, [KT[c], QT[c]]))

                            def pv(pT, c=c, i=i, j0=j0, G=G):
                                for jj in range(j0, 4 * G + 4):
                                    bank, off = accap(c, jj - 4 * G)
                                    self.mm(bank[:, off:off + 129], pT[:, (jj - j0) * 128:(jj - j0 + 1) * 128], V[:, i, :], i == 0 and (jj - 4 * G) % 2 == 0, i == jj, [pT, V], [bank], sig=(jj == 4 * G + 3))
                            items.append(dict(kp=128, N=N, qk=qk, pv=pv))
                    self.attn_stream(items, self.P[0:3], pts)
                    cps = accc[G % 2]
                    for bi in range(4):
                        bank = self.P[4 + bi]
                        if bi % 2 == 0:
                            s.op("act", lambda h, bank=bank, bi=bi: h.copy(out=cps[bi][:], in_=bank[:, 0:258]), reads=[bank], writes=[cps[bi]])
                        else:
                            s.op("dve", lambda h, bank=bank, bi=bi: h.tensor_copy(out=cps[bi][:], in_=bank[:, 0:258]), reads=[bank], writes=[cps[bi]])
                    for jj in range(4):
                        b0, o0 = cps[0 * 2 + jj // 2], (jj % 2) * 129
                        b1, o1 = cps[1 * 2 + jj // 2], (jj % 2) * 129
                        r, t, o = rr[nfin % 2], t0[nfin % 2], ob[nfin % 2]
                        nfin += 1
                        s.op("dve", lambda h: h.reciprocal(out=r[:, 0:1], in_=b0[:, o0 + 128:o0 + 129]), reads=[b0], writes=[r])
                        s.op("dve", lambda h: h.reciprocal(out=r[:, 1:2], in_=b1[:, o1 + 128:o1 + 129]), reads=[b1], writes=[r])
                        s.op("dve", lambda h: h.tensor_tensor(out=r[:, 1:2], in0=r[:, 1:2], in1=nlam[:], op=ALU.mult), reads=[r, nlam], writes=[r])
                        s.op("dve", lambda h: h.tensor_scalar_mul(out=t[:], in0=b0[:, o0:o0 + 128], scalar1=r[:, 0:1]), reads=[b0, r], writes=[t])
                        s.op("dve", lambda h: h.scalar_tensor_tensor(out=t[:], in0=b1[:, o1:o1 + 128], scalar=r[:, 1:2], in1=t[:], op0=ALU.mult, op1=ALU.add), reads=[b1, r, t], writes=[t])
                        s.op("act", lambda h: h.activation(out=junk[:], in_=t[:], func=AF.Square, accum_out=r[:, 2:3]), reads=[t], writes=[junk, r])
                        s.op("dve", lambda h: h.tensor_scalar(out=r[:, 2:3], in0=r[:, 2:3], scalar1=1.0 / 128, scalar2=EPS, op0=ALU.mult, op1=ALU.add), reads=[r], writes=[r])
                        s.op("act", lambda h: h.sqrt(out=r[:, 2:3], in_=r[:, 2:3]), reads=[r], writes=[r])
                        s.op("dve", lambda h: h.reciprocal(out=r[:, 2:3], in_=r[:, 2:3]), reads=[r], writes=[r])
                        s.op("dve", lambda h: h.scalar_tensor_tensor(out=o[:], in0=t[:], scalar=r[:, 2:3], in1=gsub[:], op0=ALU.mult, op1=ALU.mult), reads=[t, r, gsub], writes=[o])
                        row0 = (4 * G + jj) * 128
                        s.dma("sp", Dr["O_d"][row0:row0 + 128, 512 + hd * 128:512 + (hd + 1) * 128], o[:], reads=[o], writes=[DR["O_d"]])

    def phase_nsa(self, l, q):
        s, I, Dr, DR = self.s, self.I, self.Dr, self.DR
        ident = self.ident
        with ExitStack() as ps:
            mdiag = self.load_const(ps, "mdiag", [128, 128], BF16)
            medge = self.load_const(ps, "medge", [128, 128], BF16)
            cmaskT = self.load_const(ps, "cmaskT", [128, S], BF16)
            eblk = self.load_const(ps, "eblk", [32, S], BF16)
            atab = s.sb([128, NT, 32], F32, "atab", ps)
            s.dma("sp", atab[:], I["atab"].rearrange("(n p) b -> p n b", p=128), writes=[atab])
            GN = s.sb([128, NT, 24], F32, "GN", ps)
            s.dma("sp", GN[:], Dr["GN_d"].rearrange("(n p) c -> p n c", p=128), reads=[DR["GN_d"]], writes=[GN])
            QG = s.sb([68, 4, S], BF16, "QGc", ps)
            KSL = s.sb([68, S], BF16, "KSL", ps)
            KWN = s.sb([68, S], BF16, "KWN", ps)
            KCT = s.sb([64, 128], BF16, "KCT", ps)
            VC = s.sb([128, 97], BF16, "VC", ps)
            VSL = s.sb([128, NT, 65], BF16, "VSL", ps)
            VWN = s.sb([128, NT, 65], BF16, "VWN", ps)
            OC = s.sb([128, NT, 4, 64], F32, "OC", ps)
            SELT = s.sb([32, S], BF16, "SELT", ps)
            pts = [s.sb([128, 512], BF16, "pT", ps) for _ in range(3)]
            dn = [s.sb([128, 12], F32, "dn", ps) for _ in range(2)]
            imp = [s.sb([128, 32], F32, "imp", ps) for _ in range(2)]
            tmp32 = [s.sb([128, 32], F32, "tmp32", ps) for _ in range(2)]
            m8 = [s.sb([128, 16], F32, "m8", ps) for _ in range(2)]
            oo = [s.sb([128, 4, 64], F32, "oo", ps) for _ in range(2)]
            ot = [s.sb([128, 4, 64], F32, "otmp", ps) for _ in range(2)]
            ob = [s.sb([128, 4, 64], BF16, "obf", ps) for _ in range(2)]
            o3 = lambda sc: sc[:, :].rearrange("p (g t) -> p g t", g=4)
            o3c = lambda sc: sc[0:127, :].rearrange("p (g t) -> p g t", g=4)
            b4 = lambda ap: ap.unsqueeze(1).broadcast_to([ap.shape[0], 4, 128])
            for hk in range(2):
                self.load_heads(QG, [SL_QC + hk * 4 + g for g in range(4)], True)
                self.load_heads(KSL, [SL_KSL + hk], False)
                self.load_heads(KWN, [SL_KWN + hk], False)
                s.dma("sp", KCT[:], Dr["KC_d"][hk], reads=[DR["KC_d"]], writes=[KCT])
                s.dma("sp", VC[:], Dr["VC_d"][hk], reads=[DR["VC_d"]], writes=[VC])
                s.dma("sp", VSL[:], Dr["VSL_d"][:, hk, :].rearrange("(n p) e -> p n e", p=128), reads=[DR["VSL_d"]], writes=[VSL])
                s.dma("sp", VWN[:], Dr["VWN_d"][:, hk, :].rearrange("(n p) e -> p n e", p=128), reads=[DR["VWN_d"]], writes=[VWN])
                for j in range(NT):
                    acc = self.P[4 + j % 2]
                    jc = slice(j * 128, (j + 1) * 128)
                    qk = [(o3c, KCT[:, 0:127], QG[0:64, :, jc], True, False, [KCT, QG]),
                          (o3c, ident[0:127, 0:127], b4(cmaskT[0:127, jc]), False, True, [ident, cmaskT])]

                    def pv(pT, acc=acc):
                        for g in range(4):
                            self.mm(acc[:, g * 97:(g + 1) * 97], pT[0:127, g * 128:(g + 1) * 128], VC[0:127, :], g == 0, True, [pT, VC], [acc], sig=(g == 3))
                    self.attn_stream([dict(kp=127, N=512, qk=qk, pv=pv)], self.P[0:3], pts)
                    a3 = acc[:, 0:388].rearrange("p (g e) -> p g e", e=97)
                    d, im, tm, mm8 = dn[j % 2], imp[j % 2], tmp32[j % 2], m8[j % 2]
                    s.op("dve", lambda h: h.tensor_scalar_max(out=d[:, 0:4].unsqueeze(2), in0=a3[:, :, 64:65], scalar1=1e-30), reads=[acc], writes=[d])
                    s.op("dve", lambda h: h.reciprocal(out=d[:, 0:4], in_=d[:, 0:4]), reads=[d], writes=[d])
                    s.op("dve", lambda h: h.tensor_tensor(out=OC[:, j], in0=a3[:, :, 0:64], in1=d[:, 0:4].unsqueeze(2).broadcast_to([128, 4, 64]), op=ALU.mult), reads=[acc, d], writes=[OC])
                    s.op("dve", lambda h: h.tensor_scalar_mul(out=im[:], in0=a3[:, 0, 65:97], scalar1=d[:, 0:1]), reads=[acc, d], writes=[im])
                    for g in range(1, 4):
                        s.op("dve", lambda h, g=g: h.scalar_tensor_tensor(out=im[:], in0=a3[:, g, 65:97], scalar=d[:, g:g + 1], in1=im[:], op0=ALU.mult, op1=ALU.add), reads=[acc, d, im], writes=[im])
                    s.op("dve", lambda h: h.tensor_tensor(out=im[:], in0=im[:], in1=atab[:, j, :], op=ALU.add), reads=[im, atab], writes=[im])
                    s.op("dve", lambda h: h.max(out=mm8[:, 0:8], in_=im[:]), reads=[im], writes=[mm8])
                    s.op("dve", lambda h: h.match_replace(out=tm[:], in_to_replace=mm8[:, 0:8], in_values=im[:], imm_value=-3e9), reads=[im, mm8], writes=[tm])
                    s.op("dve", lambda h: h.max(out=mm8[:, 8:16], in_=tm[:]), reads=[tm], writes=[mm8])
                    s.op("dve", lambda h: h.tensor_scalar(out=tm[:], in0=im[:], scalar1=mm8[:, 15:16], scalar2=NEG, op0=ALU.is_lt, op1=ALU.mult), reads=[im, mm8], writes=[tm])
                    pt = self.P[6 + j % 2]
                    self.tr(pt[0:32, 0:128], tm[:], self.ident32[:], [tm, self.ident32], [pt])
                    s.op("act", lambda h: h.copy(out=SELT[:, jc], in_=pt[0:32, 0:128]), reads=[pt], writes=[SELT])
                for j in range(NT):
                    jc = slice(j * 128, (j + 1) * 128)
                    accS, accW = self.P[4 + 2 * (j % 2)], self.P[5 + 2 * (j % 2)]
                    items = []
                    for i in range(0, j + 1):
                        ic = slice(i * 128, (i + 1) * 128)
                        qk = [(o3, KSL[:, ic], QG[:, :, jc], True, False, [KSL, QG]),
                              (o3, eblk[:, ic], b4(SELT[:, jc]), False, i != j, [eblk, SELT])]
                        if i == j:
                            qk.append((o3, ident[:], b4(mdiag[:]), False, True, [ident, mdiag]))

                        def pv(pT, i=i, j=j, acc=accS):
                            for g in range(4):
                                self.mm(acc[:, g * 65:(g + 1) * 65], pT[:, g * 128:(g + 1) * 128], VSL[:, i, :], i == 0 and g == 0, i == j, [pT, VSL], [acc], sig=(g == 3))
                        items.append(dict(kp=128, N=512, qk=qk, pv=pv))
                    i0 = max(0, j - 4)
                    for i in range(i0, j + 1):
                        ic = slice(i * 128, (i + 1) * 128)
                        masked = (i == j) or (i == j - 4)
                        qk = [(o3, KWN[:, ic], QG[:, :, jc], True, not masked, [KWN, QG])]
                        if masked:
                            mk = mdiag if i == j else medge
                            qk.append((o3, ident[:], b4(mk[:]), False, True, [ident, mk]))

                        def pv(pT, i=i, j=j, i0=i0, acc=accW):
                            for g in range(4):
                                self.mm(acc[:, g * 65:(g + 1) * 65], pT[:, g * 128:(g + 1) * 128], VWN[:, i, :], i == i0 and g == 0, i == j, [pT, VWN], [acc], sig=(g == 3))
                        items.append(dict(kp=128, N=512, qk=qk, pv=pv))
                    self.attn_stream(items, self.P[0:3], pts)
                    d, o, t, obf = dn[j % 2], oo[j % 2], ot[j % 2], ob[j % 2]
                    aS = accS[:, 0:260].rearrange("p (g e) -> p g e", e=65)
                    aW = accW[:, 0:260].rearrange("p (g e) -> p g e", e=65)
                    gv = GN[:, j, hk * 12:(hk + 1) * 12].rearrange("p (g r) -> p g r", r=3)
                    s.op("dve", lambda h: h.reciprocal(out=d[:, 4:8].unsqueeze(2), in_=aS[:, :, 64:65]), reads=[accS], writes=[d])
                    s.op("dve", lambda h: h.reciprocal(out=d[:, 8:12].unsqueeze(2), in_=aW[:, :, 64:65]), reads=[accW], writes=[d])
                    s.op("dve", lambda h: h.tensor_tensor(out=d[:, 4:8].unsqueeze(2), in0=d[:, 4:8].unsqueeze(2), in1=gv[:, :, 1:2], op=ALU.mult), reads=[d, GN], writes=[d])
                    s.op("dve", lambda h: h.tensor_tensor(out=d[:, 8:12].unsqueeze(2), in0=d[:, 8:12].unsqueeze(2), in1=gv[:, :, 2:3], op=ALU.mult), reads=[d, GN], writes=[d])
                    s.op("pool", lambda h: h.tensor_tensor(out=o[:], in0=OC[:, j], in1=gv[:, :, 0:1].broadcast_to([128, 4, 64]), op=ALU.mult), reads=[OC, GN], writes=[o])
                    s.op("dve", lambda h: h.tensor_tensor(out=t[:], in0=aS[:, :, 0:64], in1=d[:, 4:8].unsqueeze(2).broadcast_to([128, 4, 64]), op=ALU.mult), reads=[accS, d], writes=[t])
                    s.op("pool", lambda h: h.tensor_tensor(out=o[:], in0=o[:], in1=t[:], op=ALU.add), reads=[o, t], writes=[o])
                    s.op("dve", lambda h: h.tensor_tensor(out=t[:], in0=aW[:, :, 0:64], in1=d[:, 8:12].unsqueeze(2).broadcast_to([128, 4, 64]), op=ALU.mult), reads=[accW, d], writes=[t])
                    s.op("pool", lambda h: h.tensor_tensor(out=obf[:], in0=o[:], in1=t[:], op=ALU.add), reads=[o, t], writes=[obf])
                    s.dma("sp", Dr["O_d"][jc, 1024 + hk * 256:1024 + (hk + 1) * 256], obf[:].rearrange("p g d -> p (g d)"), reads=[obf], writes=[DR["O_d"]])

    def phase_merge(self, l, q):
        s, I, Dr, DR = self.s, self.I, self.Dr, self.DR
        xsrc, xres_r = self.xsrc(l, q)
        with ExitStack() as ps:
            wbr = s.sb([128, 12, D], BF16, "wbr", ps)
            wout = s.sb([128, 8, D], BF16, "wout", ps)
            for r in range(3):
                for hh in range(2):
                    s.dma("pool", wbr[:, r * 4:(r + 1) * 4, hh * 512:(hh + 1) * 512],
                          I["w_branch"][l, r].rearrange("(kc p) n -> p kc n", p=128)[:, :, hh * 512:(hh + 1) * 512], writes=[wbr])
            for k4 in range(2):
                for hh in range(2):
                    s.dma("pool", wout[:, k4 * 4:(k4 + 1) * 4, hh * 512:(hh + 1) * 512],
                          I["w_out"][l].rearrange("(kc p) n -> p kc n", p=128)[:, k4 * 4:(k4 + 1) * 4, hh * 512:(hh + 1) * 512], writes=[wout])
            g1 = s.sb([128, D], F32, "g1", ps)
            self.mod_bc(l, q, 2, g1)
            Ot = [s.sb([128, 1536], BF16, "Ot", ps) for _ in range(2)]
            GM = [s.sb([128, 3072], F32, "GMt", ps) for _ in range(2)]
            xt = [s.sb([128, D], F32, "xt", ps) for _ in range(2)]
            OT = [s.sb([128, 12, 128], BF16, "OT", ps) for _ in range(2)]
            MT = [s.sb([128, 8, 128], BF16, "MT", ps) for _ in range(2)]
            mg = s.sb([128, D], F32, "mg", ps)
            mgb = s.sb([128, D], BF16, "mgb", ps)
            tmp = [s.sb([128, 512], F32, "mtmp", ps) for _ in range(2)]
            nb = 0
            for tt in range(NT):
                rows = slice(tt * 128, (tt + 1) * 128)
                grow = slice(q * S + tt * 128, q * S + (tt + 1) * 128)
                O, G, x, ot, mt = Ot[tt % 2], GM[tt % 2], xt[tt % 2], OT[tt % 2], MT[tt % 2]
                s.dma("sp", O[:], Dr["O_d"][rows, :], reads=[DR["O_d"]], writes=[O])
                s.dma("sp", G[:], Dr["GM_d"][rows, :], reads=[DR["GM_d"]], writes=[G])
                s.dma("sp", x[:], xsrc[grow, :], reads=[], writes=[x])
                pa, pb = self.P[0], self.P[1]
                pab, pbb = pa[:].bitcast(BF16), pb[:].bitcast(BF16)
                for kc in range(8):
                    self.tr(pab[:, kc * 128:(kc + 1) * 128], O[:, kc * 128:(kc + 1) * 128], self.ident[:], [O, self.ident], [pa], sig=(kc == 7))
                for kc in range(4):
                    self.tr(pbb[:, kc * 128:(kc + 1) * 128], O[:, (8 + kc) * 128:(9 + kc) * 128], self.ident[:], [O, self.ident], [pb], sig=(kc == 3))
                s.op("act", lambda h: h.copy(out=ot[:, 0:8, :], in_=pab.rearrange("p (k t) -> p k t", k=8)), reads=[pa], writes=[ot])
                s.op("act", lambda h: h.copy(out=ot[:, 8:12, :], in_=pbb[:, 0:512].rearrange("p (k t) -> p k t", k=4)), reads=[pb], writes=[ot])
                for r in range(3):
                    for half in range(2):
                        pm = self.P[2 + nb % 4]
                        tp = tmp[nb % 2]
                        nb += 1
                        hc = slice(half * 512, (half + 1) * 512)
                        for kc in range(4):
                            self.mm(pm[:, :], ot[:, r * 4 + kc, :], wbr[:, r * 4 + kc, hc], kc == 0, kc == 3, [ot, wbr], [pm], sig=(kc == 3))
                        gsl = G[:, r * 1024 + half * 512:r * 1024 + (half + 1) * 512]
                        if r == 0:
                            s.op("dve", lambda h: h.tensor_tensor(out=mg[:, hc], in0=pm[:, :], in1=gsl, op=ALU.mult), reads=[pm, G], writes=[mg])
                        else:
                            s.op("dve", lambda h: h.tensor_tensor(out=tp[:], in0=pm[:, :], in1=gsl, op=ALU.mult), reads=[pm, G], writes=[tp])
                            s.op("pool", lambda h: h.tensor_tensor(out=mg[:, hc], in0=mg[:, hc], in1=tp[:], op=ALU.add), reads=[mg, tp], writes=[mg])
                s.op("act", lambda h: h.copy(out=mgb[:], in_=mg[:]), reads=[mg], writes=[mgb])
                for kc in range(8):
                    self.tr(pab[:, kc * 128:(kc + 1) * 128], mgb[:, kc * 128:(kc + 1) * 128], self.ident[:], [mgb, self.ident], [pa], sig=(kc == 7))
                s.op("act", lambda h: h.copy(out=mt[:], in_=pab.rearrange("p (k t) -> p k t", k=8)), reads=[pa], writes=[mt])
                for half in range(2):
                    pm = self.P[2 + nb % 4]
                    tp = tmp[nb % 2]
                    nb += 1
                    hc = slice(half * 512, (half + 1) * 512)
                    for kc in range(8):
                        self.mm(pm[:, :], mt[:, kc, :], wout[:, kc, hc], kc == 0, kc == 7, [mt, wout], [pm], sig=(kc == 7))
                    s.op("dve", lambda h: h.tensor_tensor(out=tp[:], in0=pm[:, :], in1=g1[:, hc], op=ALU.mult), reads=[pm, g1], writes=[tp])
                    s.op("pool", lambda h: h.tensor_tensor(out=x[:, hc], in0=x[:, hc], in1=tp[:], op=ALU.add), reads=[x, tp], writes=[x])
                s.dma("pool", Dr["xres"][grow, :], x[:], reads=[x], writes=[DR["xres"]])

    def phase_moe2(self, l):
        s, I, Dr, DR = self.s, self.I, self.Dr, self.DR
        last = (l == L_DEPTH - 1)
        NTT, NB = self.ntt, self.nblk
        with ExitStack() as ps:
            g2 = []
            for q in range(self.nseq):
                t = s.sb([128, D], F32, "g2", ps)
                self.mod_bc(l, q, 5, t)
                g2.append(t)
            GATE = s.sb([128, NTT, NE], F32, "GATE", ps)
            GT = s.sb([NE, NTT, 128], F32, "GT", ps)
            IDX4 = s.sb([128, NTT, 4], I32, "IDX4", ps)
            G4 = s.sb([128, NTT, 4], F32, "G4", ps)
            IDXW = s.sb([128, NB, 2], I32, "IDXW", ps)
            IDXB = s.sb([128, NB], I32, "IDXB", ps)
            b2 = s.sb([NE, D], F32, "b2", ps)
            s.dma("sp", b2[:], I["exp_b2"][l], writes=[b2])
            with ExitStack() as p1:
                HB = s.sb([128, NTT, D], BF16, "HB", p1)
                MASK = s.sb([128, NTT, NE], BF16, "MASK", p1)
                DEST = s.sb([128, NTT, NE], F32, "DEST", p1)
                rw = s.sb([128, 8, NE], F32, "rw", p1)
                rb = s.sb([128, NE], F32, "rb", p1)
                s.dma("sp", rw[:], I["router_w"][l].rearrange("(kc p) e -> p kc e", p=128), writes=[rw])
                s.dma("sp", rb[:], I["router_b"][l:l + 1, :].broadcast_to([128, NE]), writes=[rb])
                ltri = self.load_const(p1, "ltri", [128, 128], BF16)
                ones = self.load_const(p1, "ones128", [128, 128], BF16)
                b512 = self.load_const(p1, "b512", [128, 64], F32)
                rowiota = self.load_const(p1, "rowiota", [128, 8], F32)
                piota = self.load_const(p1, "piota", [128, 1], F32)
                xt = [s.sb([128, D], F32, "xt", p1) for _ in range(2)]
                hf = [s.sb([128, D], F32, "hf", p1) for _ in range(2)]
                hT32s = [s.sb([128, 8, 128], F32, "hT32", p1) for _ in range(2)]
                junk = s.sb([128, D], F32, "junk", p1)
                ss = [s.sb([128, 1], F32, "ss", p1) for _ in range(2)]
                lg = [s.sb([128, NE], F32, "lg", p1) for _ in range(2)]
                ex = [s.sb([128, NE], F32, "ex", p1) for _ in range(2)]
                m8 = [s.sb([128, 8], F32, "m8", p1) for _ in range(2)]
                sm = [s.sb([128, 2], F32, "sm", p1) for _ in range(2)]
                gsc = sh = None
                for tt in range(NTT):
                    q = tt // NT
                    if tt % NT == 0:
                        gsc, sh = self.norm_tiles(p1, l, q, "norm2_g", 4, 3)
                    grow = slice(tt * 128, (tt + 1) * 128)
                    x, h32, sq = xt[tt % 2], hf[tt % 2], ss[tt % 2]
                    s.dma("sp", x[:], Dr["xres"][grow, :], reads=[DR["xres"]], writes=[x])
                    self.rstd_of((x, x[:]), (junk, junk[:]), sq)
                    s.op("dve", lambda hh: hh.scalar_tensor_tensor(out=x[:], in0=x[:], scalar=sq[:, 0:1], in1=gsc[:], op0=ALU.mult, op1=ALU.mult), reads=[x, sq, gsc], writes=[x])
                    s.op("pool", lambda hh: hh.tensor_tensor(out=h32[:], in0=x[:], in1=sh[:], op=ALU.add), reads=[x, sh], writes=[h32])
                    s.op("act", lambda hh: hh.copy(out=HB[:, tt, :], in_=h32[:]), reads=[h32], writes=[HB])
                    hT32 = hT32s[tt % 2]
                    for hh2 in range(2):
                        pf = self.P[(tt % 2) * 2 + hh2]
                        for k4 in range(4):
                            kc = hh2 * 4 + k4
                            self.tr(pf[:, k4 * 128:(k4 + 1) * 128], h32[:, kc * 128:(kc + 1) * 128], self.ident32[:], [h32, self.ident32], [pf], sig=(k4 == 3))
                        s.op("dve", lambda hh: hh.tensor_copy(out=hT32[:, hh2 * 4:(hh2 + 1) * 4, :], in_=pf[:, :].rearrange("p (k t) -> p k t", k=4)), reads=[pf], writes=[hT32])
                    pl = self.P[4 + tt % 2]
                    for kc in range(8):
                        self.mm(pl[:, 0:NE], hT32[:, kc, :], rw[:, kc, :], kc == 0, kc == 7, [hT32, rw], [pl], sig=(kc == 7))
                    lgt, et, mt, st = lg[tt % 2], ex[tt % 2], m8[tt % 2], sm[tt % 2]
                    s.op("dve", lambda hh: hh.tensor_tensor(out=lgt[:], in0=pl[:, 0:NE], in1=rb[:], op=ALU.add), reads=[pl, rb], writes=[lgt])
                    s.op("dve", lambda hh: hh.max(out=mt[:], in_=lgt[:]), reads=[lgt], writes=[mt])
                    s.op("dve", lambda hh: hh.tensor_scalar_mul(out=st[:, 0:1], in0=mt[:, 0:1], scalar1=-1.0), reads=[mt], writes=[st])
                    s.op("act", lambda hh: hh.activation(out=et[:], in_=lgt[:], func=AF.Exp, bias=st[:, 0:1]), reads=[lgt, st], writes=[et])
                    s.op("dve", lambda hh: hh.tensor_scalar(out=lgt[:], in0=lgt[:], scalar1=mt[:, 3:4], scalar2=None, op0=ALU.is_ge), reads=[lgt, mt], writes=[lgt])
                    s.op("dve", lambda hh: hh.tensor_copy(out=MASK[:, tt, :], in_=lgt[:]), reads=[lgt], writes=[MASK])
                    s.op("dve", lambda hh: hh.tensor_tensor(out=et[:], in0=et[:], in1=lgt[:], op=ALU.mult), reads=[et, lgt], writes=[et])
                    s.op("dve", lambda hh: hh.reduce_sum(out=st[:, 1:2], in_=et[:], axis=AX.X), reads=[et], writes=[st])
                    s.op("dve", lambda hh: hh.reciprocal(out=st[:, 1:2], in_=st[:, 1:2]), reads=[st], writes=[st])
                    s.op("dve", lambda hh: hh.tensor_scalar_mul(out=GATE[:, tt, :], in0=et[:], scalar1=st[:, 1:2]), reads=[et, st], writes=[GATE])
                    pg = self.P[6 + tt % 2]
                    self.tr(pg[0:NE, 0:128], GATE[:, tt, :], self.ident32[:], [GATE, self.ident32], [pg])
                    s.op("act", lambda hh: hh.copy(out=GT[:, tt, :], in_=pg[0:NE, 0:128]), reads=[pg], writes=[GT])
                run = s.sb([128, NE], F32, "run", p1)
                s.op("dve", lambda hh: hh.memset(run[:], 0.0), writes=[run])
                for tt in range(NTT):
                    pr = self.P[6 + tt % 2]
                    self.mm(pr[:, 0:NE], ltri[:], MASK[:, tt, :], True, True, [ltri, MASK], [pr], sig=False)
                    self.mm(pr[:, NE:2 * NE], ones[:], MASK[:, tt, :], False, True, [ones, MASK], [pr])
                    s.op("dve", lambda hh: hh.tensor_tensor(out=DEST[:, tt, :], in0=pr[:, 0:NE], in1=run[:], op=ALU.add), reads=[pr, run], writes=[DEST])
                    s.op("dve", lambda hh: hh.tensor_tensor(out=run[:], in0=run[:], in1=pr[:, NE:2 * NE], op=ALU.add), reads=[pr, run], writes=[run])
                padded = s.sb([128, NE], F32, "padded", p1)
                tmpe = s.sb([128, NE], F32, "tmpe", p1)
                cum = [s.sb([128, NE], F32, "cum", p1) for _ in range(2)]
                s.op("dve", lambda hh: hh.memset(padded[:], 0.0), writes=[padded])
                for jb in range(NTT // 4):
                    s.op("dve", lambda hh, jb=jb: hh.scalar_tensor_tensor(out=padded[:], in0=run[:], scalar=512.0 * jb, in1=padded[:], op0=ALU.is_gt, op1=ALU.add), reads=[run, padded], writes=[padded])
                s.op("dve", lambda hh: hh.tensor_scalar_mul(out=padded[:], in0=padded[:], scalar1=512.0), reads=[padded], writes=[padded])
                s.op("dve", lambda hh: hh.tensor_copy(out=cum[0][:], in_=padded[:]), reads=[padded], writes=[cum[0]])
                ci = 0
                for shf in (1, 2, 4, 8, 16):
                    a, b = cum[ci], cum[1 - ci]
                    s.op("dve", lambda hh: hh.tensor_copy(out=b[:, 0:shf], in_=a[:, 0:shf]), reads=[a], writes=[b])
                    s.op("dve", lambda hh: hh.tensor_tensor(out=b[:, shf:NE], in0=a[:, shf:NE], in1=a[:, 0:NE - shf], op=ALU.add), reads=[a], writes=[b])
                    ci = 1 - ci
                pend = cum[ci]
                pstart = s.sb([128, NE], F32, "pstart", p1)
                s.op("dve", lambda hh: hh.tensor_tensor(out=pstart[:], in0=pend[:], in1=padded[:], op=ALU.subtract), reads=[pend, padded], writes=[pstart])
                s.op("dve", lambda hh: hh.tensor_tensor(out=DEST[:], in0=DEST[:], in1=pstart[:].unsqueeze(1).broadcast_to([128, NTT, NE]), op=ALU.add), reads=[DEST, pstart], writes=[DEST])
                s.op("dve", lambda hh: hh.scalar_tensor_tensor(out=DEST[:], in0=DEST[:], scalar=1.0, in1=MASK[:], op0=ALU.add, op1=ALU.mult), reads=[DEST, MASK], writes=[DEST])
                d4 = s.sb([128, NTT, 4], F32, "d4", p1)
                for tt in range(NTT):
                    mt, et = m8[tt % 2], ex[tt % 2]
                    s.op("dve", lambda hh: hh.max(out=mt[:], in_=DEST[:, tt, :]), reads=[DEST], writes=[mt])
                    s.op("dve", lambda hh: hh.tensor_scalar_add(out=d4[:, tt, :], in0=mt[:, 0:4], scalar1=-1.0), reads=[mt], writes=[d4])
                    for k in range(4):
                        s.op("dve", lambda hh: hh.scalar_tensor_tensor(out=et[:], in0=DEST[:, tt, :], scalar=mt[:, k:k + 1], in1=GATE[:, tt, :], op0=ALU.is_equal, op1=ALU.mult), reads=[DEST, mt, GATE], writes=[et])
                        s.op("dve", lambda hh: hh.reduce_sum(out=G4[:, tt, k:k + 1], in_=et[:], axis=AX.X), reads=[et], writes=[G4])
                s.op("dve", lambda hh: hh.tensor_copy(out=IDX4[:], in_=d4[:]), reads=[d4], writes=[IDX4])
                bexp = s.sb([128, 64], F32, "bexp", p1)
                s.op("dve", lambda hh: hh.memset(bexp[:], 0.0), writes=[bexp])
                for e in range(NE):
                    s.op("dve", lambda hh: hh.scalar_tensor_tensor(out=bexp[:], in0=b512[:], scalar=pend[:, e:e + 1], in1=bexp[:], op0=ALU.is_ge, op1=ALU.add), reads=[b512, pend, bexp], writes=[bexp])
                boob = s.sb([128, 64], F32, "boob", p1)
                s.op("dve", lambda hh: hh.tensor_scalar(out=boob[:], in0=bexp[:], scalar1=float(NE) - 0.5, scalar2=1.0e6, op0=ALU.is_ge, op1=ALU.mult), reads=[bexp], writes=[boob])
                s.op("dve", lambda hh: hh.tensor_scalar_min(out=bexp[:], in0=bexp[:], scalar1=float(NE - 1)), reads=[bexp], writes=[bexp])
                iwf = s.sb([128, NB, 2], F32, "iwf", p1)
                ibf = s.sb([128, NB], F32, "ibf", p1)
                e1k = s.sb([128, 64], F32, "e1k", p1)
                s.op("dve", lambda hh: hh.tensor_scalar(out=e1k[:], in0=bexp[:], scalar1=256.0, scalar2=float(l * NE * 256), op0=ALU.mult, op1=ALU.add), reads=[bexp], writes=[e1k])
                s.op("dve", lambda hh: hh.tensor_tensor(out=e1k[:], in0=e1k[:], in1=boob[:], op=ALU.add), reads=[e1k, boob], writes=[e1k])
                s.op("dve", lambda hh: hh.tensor_tensor(out=iwf[:], in0=rowiota[:, 0:2].unsqueeze(1).broadcast_to([128, NB, 2]), in1=e1k[:, 0:NB].unsqueeze(2).broadcast_to([128, NB, 2]), op=ALU.add), reads=[rowiota, e1k], writes=[iwf])
                s.op("dve", lambda hh: hh.tensor_copy(out=IDXW[:], in_=iwf[:]), reads=[iwf], writes=[IDXW])
                s.op("dve", lambda hh: hh.tensor_scalar(out=ibf[:], in0=bexp[:, 0:NB], scalar1=128.0, scalar2=piota[:, 0:1], op0=ALU.mult, op1=ALU.add), reads=[bexp, piota], writes=[ibf])
                s.op("dve", lambda hh: hh.tensor_scalar_add(out=ibf[:], in0=ibf[:], scalar1=float(l * NE * 128)), reads=[ibf], writes=[ibf])
                s.op("dve", lambda hh: hh.tensor_tensor(out=ibf[:], in0=ibf[:], in1=boob[:, 0:NB], op=ALU.add), reads=[ibf, boob], writes=[ibf])
                s.op("dve", lambda hh: hh.tensor_copy(out=IDXB[:], in_=ibf[:]), reads=[ibf], writes=[IDXB])
                if "dbg_d" in self.dbg:
                    dbt = s.sb([128, 4096], F32, "dbt", p1)
                    s.op("dve", lambda hh: hh.memset(dbt[:], 0.0), writes=[dbt])
                    s.op("dve", lambda hh: hh.tensor_copy(out=dbt[:, 0:32], in_=run[:]), reads=[run], writes=[dbt])
                    s.op("dve", lambda hh: hh.tensor_copy(out=dbt[:, 32:64], in_=pend[:]), reads=[pend], writes=[dbt])
                    s.op("dve", lambda hh: hh.tensor_copy(out=dbt[:, 64:128], in_=bexp[:]), reads=[bexp], writes=[dbt])
                    s.op("dve", lambda hh: hh.tensor_copy(out=dbt[:, 128:128 + NTT * 4], in_=d4[:].rearrange("p t k -> p (t k)")), reads=[d4], writes=[dbt])
                    s.op("dve", lambda hh: hh.tensor_copy(out=dbt[:, 512:512 + NTT * 4], in_=G4[:].rearrange("p t k -> p (t k)")), reads=[G4], writes=[dbt])
                    s.dma("sp", Dr["dbg_d"], dbt[:], reads=[dbt], writes=[DR["dbg_d"]])
                for tt in range(NTT):
                    for k in range(4):
                        s.idma(Dr["xs_d"][:, :], HB[:, tt, :], out_off=IDX4[:, tt, k:k + 1], reads=[HB, IDX4], writes=[])
                s.barrier()
            with ExitStack() as p2:
                W1 = [s.sb([128, 8, 2 * D], BF16, "W1", p2) for _ in range(2)]
                W2 = [s.sb([128, 8, D], BF16, "W2", p2) for _ in range(2)]
                B1 = [s.sb([128, 16], F32, "B1", p2) for _ in range(2)]
                XS = [s.sb([128, 4, D], BF16, "XS", p2) for _ in range(2)]
                XT = [s.sb([128, 8, 512], BF16, "XT", p2) for _ in range(2)]
                Ab = [s.sb([128, 8, 512], BF16, "Ab", p2) for _ in range(2)]
                Gt = [s.sb([128, 512], F32, "Gt", p2) for _ in range(2)]
                St = [s.sb([128, 512], F32, "St", p2) for _ in range(2)]
                Ut = [s.sb([128, 512], F32, "Ut", p2) for _ in range(2)]
                Yt = [s.sb([128, D], F32, "Yt", p2) for _ in range(2)]
                for wt_ in W1 + W2 + B1:
                    s.op("pool", lambda hh, wt_=wt_: hh.memset(wt_[:], 0.0), writes=[wt_])
                cnt = {"nf": 0, "ny": 0}

                def load_block(b):
                    w1, w2, b1, xs = W1[b % 2], W2[b % 2], B1[b % 2], XS[b % 2]
                    s.dma("sp", xs[:], Dr["xs_d"][b * 512:(b + 1) * 512, :].rearrange("(t p) d -> p t d", p=128), reads=[DR["xs_d"]], writes=[xs])
                    for j2 in range(2):
                        s.idma(w1[:, j2 * 4:(j2 + 1) * 4, :].rearrange("p a b -> p (a b)"), I["exp_w1"][:, :], in_off=IDXW[:, b, j2:j2 + 1], reads=[IDXW], writes=[w1], bounds=65535)
                    s.idma(b1[:, :], I["exp_b1E"][:, :], in_off=IDXB[:, b:b + 1], reads=[IDXB], writes=[b1], bounds=65535)

                def load_w2(b):
                    w2 = W2[b % 2]
                    s.idma(w2[:].rearrange("p a b -> p (a b)"), I["exp_w2"][:, :], in_off=IDXB[:, b:b + 1], reads=[IDXB], writes=[w2], bounds=65535)

                def transposes(b):
                    xs, xT = XS[b % 2], XT[b % 2]
                    for t4 in range(4):
                        pt = self.P[t4 % 2]
                        ptb = pt[:].bitcast(BF16)
                        for kc in range(8):
                            kb = (kc // 4) * 512 + (kc % 4)
                            self.tr(ptb[:, kc * 128:(kc + 1) * 128], xs[:, t4, kb:kb + 509:4], self.ident[:], [xs, self.ident], [pt], sig=(kc == 7))
                        if t4 % 2 == 0:
                            s.op("act", lambda hh: hh.copy(out=xT[:, :, t4 * 128:(t4 + 1) * 128], in_=ptb.rearrange("p (k t) -> p k t", k=8)), reads=[pt], writes=[xT])
                        else:
                            s.op("dve", lambda hh: hh.tensor_copy(out=xT[:, :, t4 * 128:(t4 + 1) * 128], in_=ptb.rearrange("p (k t) -> p k t", k=8)), reads=[pt], writes=[xT])

                def stage1_fc(b, fc):
                    w1, b1, xT, A = W1[b % 2], B1[b % 2], XT[b % 2], Ab[b % 2]
                    nf = cnt["nf"]
                    cnt["nf"] += 1
                    psG, psU = self.P[2 + (nf % 2) * 2], self.P[3 + (nf % 2) * 2]
                    G, Sg, U = Gt[nf % 2], St[nf % 2], Ut[nf % 2]
                    for kc in range(8):
                        self.mm(psG[:, :], w1[:, kc, fc:D:8], xT[:, kc, :], kc == 0, kc == 7, [w1, xT], [psG], sig=(kc == 7))
                    for kc in range(8):
                        self.mm(psU[:, :], w1[:, kc, D + fc:2 * D:8], xT[:, kc, :], kc == 0, kc == 7, [w1, xT], [psU], sig=(kc == 7))
                    s.op("dve", lambda hh: hh.tensor_scalar(out=G[:], in0=psG[:, :], scalar1=b1[:, fc:fc + 1], scalar2=7.0, op0=ALU.add, op1=ALU.min), reads=[psG, b1], writes=[G])
                    s.op("act", lambda hh: hh.activation(out=Sg[:], in_=G[:], func=AF.Sigmoid, scale=1.702), reads=[G], writes=[Sg])
                    s.op("act", lambda hh: hh.activation(out=U[:], in_=psU[:, :], func=AF.Identity, bias=b1[:, 8 + fc:8 + fc + 1]), reads=[psU, b1], writes=[U])
                    s.op("dve", lambda hh: hh.tensor_scalar(out=U[:], in0=U[:], scalar1=7.0, scalar2=-7.0, op0=ALU.min, op1=ALU.max), reads=[U], writes=[U])
                    s.op("dve", lambda hh: hh.tensor_tensor(out=G[:], in0=G[:], in1=Sg[:], op=ALU.mult), reads=[G, Sg], writes=[G])
                    s.op("dve", lambda hh: hh.scalar_tensor_tensor(out=A[:, fc, :], in0=U[:], scalar=1.0, in1=G[:], op0=ALU.add, op1=ALU.mult), reads=[U, G], writes=[A])

                def stage2_grp(b, g):
                    w2, A = W2[b % 2], Ab[b % 2]
                    t4, dh = g // 2, g % 2
                    ny = cnt["ny"]
                    y = Yt[(ny // 2) % 2]
                    psY = self.P[6 + ny % 2]
                    cnt["ny"] += 1
                    dc = slice(dh * 512, (dh + 1) * 512)
                    for fc in range(8):
                        self.mm(psY[:, :], A[:, fc, t4 * 128:(t4 + 1) * 128], w2[:, fc, dc], fc == 0, fc == 7, [A, w2], [psY], sig=(fc == 7))
                    s.op("act", lambda hh: hh.copy(out=y[:, dc], in_=psY[:, :]), reads=[psY], writes=[y])
                    if dh == 1:
                        r0 = b * 512 + t4 * 128
                        s.dma("sp", Dr["ys_d"][r0:r0 + 128, :], y[:], reads=[y], writes=[DR["ys_d"]])

                load_block(0)
                load_w2(0)
                load_block(1)
                load_w2(1)
                for b in range(NB):
                    if 1 <= b < NB - 1:
                        load_block(b + 1)
                    transposes(b)
                    for i in range(8):
                        stage1_fc(b, i)
                        if b >= 1:
                            stage2_grp(b - 1, i)
                    if 1 <= b < NB - 1:
                        load_w2(b + 1)
                for i in range(8):
                    stage2_grp(NB - 1, i)
                s.barrier()
            with ExitStack() as p3:
                xt = [s.sb([128, D], F32, "xt", p3) for _ in range(2)]
                acc = [s.sb([128, D], F32, "acc3", p3) for _ in range(2)]
                Yk = [s.sb([128, D], F32, "Yk", p3) for _ in range(8)]
                junk = s.sb([128, D], F32, "junk", p3)
                ss = [s.sb([128, 1], F32, "ss", p3) for _ in range(2)]
                if last:
                    fg = s.sb([128, D], F32, "fg", p3)
                    s.dma("sp", fg[:], I["final_g"].broadcast_to([128, D]), writes=[fg])
                nk = 0
                for tt in range(NTT):
                    q = tt // NT
                    grow = slice(tt * 128, (tt + 1) * 128)
                    x, ac, sq = xt[tt % 2], acc[tt % 2], ss[tt % 2]
                    if tt == 0:
                        s.dma("sp", x[:], Dr["xres"][grow, :], reads=[], writes=[x])
                    if tt + 1 < NTT:
                        s.dma("sp", xt[(tt + 1) % 2][:], Dr["xres"][(tt + 1) * 128:(tt + 2) * 128, :], reads=[], writes=[xt[(tt + 1) % 2]])
                    pa = [self.P[(tt % 2) * 2], self.P[(tt % 2) * 2 + 1]]
                    for half in range(2):
                        self.mm(pa[half][:, :], GT[:, tt, :], b2[:, half * 512:(half + 1) * 512], True, True, [GT, b2], [pa[half]])
                    for k in range(4):
                        yk = Yk[nk % 8]
                        nk += 1
                        s.idma(yk[:, :], Dr["ys_d"][:, :], in_off=IDX4[:, tt, k:k + 1], reads=[IDX4, DR["ys_d"]], writes=[yk])
                        for half in range(2):
                            hc = slice(half * 512, (half + 1) * 512)
                            in1 = pa[half][:, :] if k == 0 else ac[:, hc]
                            rd = [yk, G4] + ([pa[half]] if k == 0 else [ac])
                            s.op("dve", lambda hh, in1=in1, hc=hc, yk=yk, k=k: hh.scalar_tensor_tensor(out=ac[:, hc], in0=yk[:, hc], scalar=G4[:, tt, k:k + 1], in1=in1, op0=ALU.mult, op1=ALU.add), reads=rd, writes=[ac])
                    s.op("pool", lambda hh: hh.tensor_tensor(out=ac[:], in0=ac[:], in1=g2[q][:], op=ALU.mult), reads=[ac, g2[q]], writes=[ac])
                    s.op("dve", lambda hh: hh.tensor_tensor(out=x[:], in0=x[:], in1=ac[:], op=ALU.add), reads=[x, ac], writes=[x])
                    if last:
                        self.rstd_of((x, x[:]), (junk, junk[:]), sq)
                        s.op("dve", lambda hh: hh.scalar_tensor_tensor(out=x[:], in0=x[:], scalar=sq[:, 0:1], in1=fg[:], op0=ALU.mult, op1=ALU.mult), reads=[x, sq, fg], writes=[x])
                        s.dma("sp", self.out[grow, :], x[:], reads=[x], writes=[R("out")])
                    else:
                        s.dma("sp", Dr["xres"][grow, :], x[:], reads=[x], writes=[DR["xres"]])
                s.barrier()


_CONSTS = None


def run(inputs, dbg=(), layers=L_DEPTH, nseq=NSEQ, stop_after=None, cores=NCORES, trace=False, only=None):
    global _CONSTS
    if _CONSTS is None:
        _CONSTS = host_consts()
    inp = {k: np.asarray(v) for k, v in inputs.items()}
    k = K(dbg=dbg, layers=layers, nseq=nseq, stop_after=stop_after, only=only)
    nc = k.build()
    in_maps = []
    for c in range(cores):
        m = host_inputs(inp, c)
        m.update(_CONSTS)
        in_maps.append(m)
    res = run_bass_kernel_spmd(nc, in_maps, core_ids=list(range(cores)), trace=trace)
    return res


def kernel(**inputs):
    res = run(inputs)
    out = np.concatenate([np.asarray(r["out"]).reshape(NSEQ, S, D) for r in res.results], axis=0)
    return out.astype(np.float32)
```

```python
import numpy as np
import ml_dtypes
from contextlib import ExitStack
import concourse.bass as bass
import concourse.mybir as mybir
from concourse.bass_utils import run_bass_kernel_spmd

F32 = mybir.dt.float32
BF16 = mybir.dt.bfloat16
I32 = mybir.dt.int32
AF = mybir.ActivationFunctionType
ALU = mybir.AluOpType
AX = mybir.AxisListType
NDSEM = 8


class R:
    __slots__ = ("name", "lw", "rd")

    def __init__(self, name=""):
        self.name = name
        self.lw = {}
        self.rd = {}


class T(R):
    __slots__ = ("t",)

    def __init__(self, t, name=""):
        super().__init__(name)
        self.t = t

    def __getitem__(self, k):
        return self.t[k]


class Eng:
    def __init__(self, name, h, sem, dsems):
        self.name = name
        self.h = h
        self.sem = sem
        self.count = 0
        self.known = {}
        self.dsems = dsems
        self.ndma = 0


class Sched:
    def __init__(self, nc, stack):
        self.nc = nc
        self.stack = stack
        self.E = {}
        for name, h, nd in (("pe", nc.tensor, 0), ("act", nc.scalar, NDSEM), ("dve", nc.vector, 0),
                            ("pool", nc.gpsimd, NDSEM), ("sp", nc.sync, NDSEM)):
            sem = stack.enter_context(nc.semaphore("s_" + name))
            ds = [stack.enter_context(nc.semaphore("d_%s%d" % (name, i))) for i in range(nd)]
            self.E[name] = Eng(name, h, sem, ds)
        self.dma_tokens = []
        self.nuniq = 0

    def sb(self, shape, dt, name=None, stack=None):
        self.nuniq += 1
        name = (name or "t") + "_%d" % self.nuniq
        t = (stack or self.stack).enter_context(self.nc.sbuf_tensor(name, list(shape), dt))
        return T(t, name)

    def ps(self, shape, dt=F32, name=None, stack=None):
        self.nuniq += 1
        name = (name or "p") + "_%d" % self.nuniq
        t = (stack or self.stack).enter_context(self.nc.psum_tensor(name, list(shape), dt))
        return T(t, name)

    def _wait(self, eng, tok):
        sem, val = tok
        if eng.known.get(sem, 0) >= val:
            return
        eng.h.wait_ge(sem, val)
        eng.known[sem] = val

    def _deps(self, eng, reads, writes):
        deps = []
        for r in reads:
            deps.extend(r.lw.items())
        for w in writes:
            deps.extend(w.lw.items())
            deps.extend(w.rd.items())
        for tok in deps:
            if tok[0] is eng.sem:
                if eng.name == "pe":
                    continue
                if tok[1] > eng.count:
                    continue
            self._wait(eng, tok)

    def _commit(self, tok, reads, writes):
        sem, val = tok
        for r in reads:
            if r.rd.get(sem, 0) < val:
                r.rd[sem] = val
        for w in writes:
            if w.lw.get(sem, 0) < val:
                w.lw[sem] = val

    def op(self, engname, fn, reads=(), writes=(), sig=True):
        eng = self.E[engname]
        self._deps(eng, reads, writes)
        ins = fn(eng.h)
        if sig:
            eng.count += 1
            ins.then_inc(eng.sem, 1)
            tok = (eng.sem, eng.count)
        else:
            tok = (eng.sem, eng.count + 1)
        self._commit(tok, reads, writes)
        return tok

    def dma(self, qname, out, in_, reads=(), writes=(), **kw):
        q = self.E[qname]
        i = q.ndma
        q.ndma += 1
        sem = q.dsems[i % NDSEM]
        val = 16 * (i // NDSEM + 1)
        if i >= NDSEM:
            self._wait(q, (sem, val - 16))
        self._deps(q, reads, writes)
        q.h.dma_start(out=out, in_=in_, **kw).then_inc(sem, 16)
        tok = (sem, val)
        self._commit(tok, reads, writes)
        self.dma_tokens.append(tok)
        return tok

    def idma(self, out, in_, out_off=None, in_off=None, reads=(), writes=(), bounds=None):
        q = self.E["pool"]
        i = q.ndma
        q.ndma += 1
        sem = q.dsems[i % NDSEM]
        val = 16 * (i // NDSEM + 1)
        if i >= NDSEM:
            self._wait(q, (sem, val - 16))
        self._deps(q, reads, writes)
        oo = bass.IndirectOffsetOnAxis(ap=out_off, axis=0) if out_off is not None else None
        io = bass.IndirectOffsetOnAxis(ap=in_off, axis=0) if in_off is not None else None
        if bounds is None:
            q.h.indirect_dma_start(out=out, out_offset=oo, in_=in_, in_offset=io).then_inc(sem, 16)
        else:
            if getattr(self, "bound_reg", None) is None:
                self.bound_reg = q.h.alloc_register("bnd")
                q.h.reg_mov(self.bound_reg, 65535)
            q.h.indirect_dma_start(out=out, out_offset=oo, in_=in_, in_offset=io, bounds_check=self.bound_reg, oob_is_err=False).then_inc(sem, 16)
        tok = (sem, val)
        self._commit(tok, reads, writes)
        self.dma_tokens.append(tok)
        return tok

    def barrier(self):
        toks = []
        for e in self.E.values():
            if e.count > 0:
                toks.append((e.sem, e.count))
            for j, s in enumerate(e.dsems):
                n = (e.ndma - j + NDSEM - 1) // NDSEM
                if n > 0:
                    toks.append((s, 16 * n))
        for e in self.E.values():
            for tok in toks:
                if tok[0] is e.sem:
                    continue
                self._wait(e, tok)

    def finish(self):
        sp = self.E["sp"]
        for e in self.E.values():
            for j, s in enumerate(e.dsems):
                n = (e.ndma - j + NDSEM - 1) // NDSEM
                if n > 0:
                    self._wait(sp, (s, 16 * n))


NCORES = 8
L_DEPTH = 2
D = 1024
S = 2048
NSEQ = 2
NT = S // 128
EPS = 1e-5
INW = 6680
SCALE = 0.125
NEG = -30000.0
NE = 32
SLOT_COL = ([0 + 64 * i for i in range(8)] + [512 + 64 * i for i in range(2)] + [768 + 64 * i for i in range(8)]
            + [1280 + 64 * i for i in range(8)] + [2304 + 64 * i for i in range(8)] + [2816 + 64 * i for i in range(2)]
            + [2944 + 64 * i for i in range(2)] + [3072 + 64 * i for i in range(2)] + [3328 + 64 * i for i in range(2)])
NSLOT = len(SLOT_COL)
SL_QA, SL_KA, SL_QB, SL_KB, SL_QC, SL_KCM, SL_VCM, SL_KSL, SL_KWN = 0, 8, 10, 18, 26, 34, 36, 38, 40
C_VA, C_VB, C_VSL, C_VWN, C_GN, C_GM = 640, 1792, 3200, 3456, 3584, 3608


def host_consts():
    bf = ml_dtypes.bfloat16
    c = {}
    t = np.arange(S)
    a_t, b_t = (t // 128).astype(np.float32), (t % 128).astype(np.float32)
    aug = np.zeros((NSLOT, 4, S), np.float32)
    kaug = np.stack([a_t, b_t, np.ones(S, np.float32), np.ones(S, np.float32)])

    def qaug(slope):
        return np.stack([np.full(S, 1024.0 * slope, np.float32), np.full(S, 8.0 * slope, np.float32),
                         -1024.0 * slope * a_t, -8.0 * slope * b_t])
    for i in range(8):
        aug[SL_QA + i] = qaug(2.0 ** -(i + 1))
        aug[SL_QC + i] = qaug(2.0 ** -(i + 1))
        aug[SL_QB + i] = qaug(2.0 ** (-2.0 * (i // 2 + 1)))
        aug[SL_KB + i] = kaug
    for i in range(2):
        aug[SL_KA + i] = kaug
        aug[SL_KSL + i] = kaug
        aug[SL_KWN + i] = kaug
    c["aug"] = aug.astype(bf)
    c["ident"] = np.eye(128, dtype=np.float32).astype(bf)
    c["ident32"] = np.eye(128, dtype=np.float32)
    sk = np.arange(128)[:, None]
    tq = np.arange(128)[None, :]
    c["mdiag"] = np.where(tq >= sk, 0.0, NEG).astype(bf)
    c["medge"] = np.where(tq < sk, 0.0, NEG).astype(bf)
    cc = np.arange(128)[:, None]
    c["cmaskT"] = np.where((16 * cc + 31 <= t[None, :]) & (cc < 127), 0.0, NEG).astype(bf)
    nb = np.arange(32)
    c["eblk"] = (t[None, :] // 64 == nb[:, None]).astype(np.float32).astype(bf)
    cur = t // 64
    forced = (nb[None, :] == 0) | (nb[None, :] == cur[:, None]) | (nb[None, :] == cur[:, None] - 1)
    causal = nb[None, :] <= cur[:, None]
    A = np.where(forced, 1e9 + 1e6 * nb[None, :], np.where(causal, 0.0, -1e9 - 1e6 * nb[None, :]))
    c["atab"] = A.astype(np.float32)
    cstart = np.arange(127) * 16
    sstart = nb * 64
    ov = np.clip(np.minimum(cstart[:, None] + 32, sstart[None, :] + 64) - np.maximum(cstart[:, None], sstart[None, :]), 0, None) / 32.0
    ovp = np.zeros((128, 32), np.float32)
    ovp[:127] = ov
    c["overlap"] = ovp.astype(bf)
    c["ltri"] = (np.arange(128)[:, None] < np.arange(128)[None, :]).astype(np.float32).astype(bf)
    c["ones128"] = np.ones((128, 128), np.float32).astype(bf)
    c["b512"] = np.broadcast_to((512.0 * np.arange(64, dtype=np.float32))[None, :], (128, 64)).copy()
    c["rowiota"] = (np.arange(8, dtype=np.float32)[None, :] * 128 + np.arange(128, dtype=np.float32)[:, None]).copy()
    c["piota"] = np.arange(128, dtype=np.float32).reshape(128, 1).copy()
    return c


def host_inputs(inp, core):
    b0 = core * NSEQ
    m = {}
    m["x"] = np.ascontiguousarray(inp["x"][b0:b0 + NSEQ].reshape(NSEQ * S, D))
    m["cT"] = np.ascontiguousarray(inp["c"][b0:b0 + NSEQ].T)
    for k in ("mod_w", "mod_b", "norm1_g", "norm2_g", "w_in", "b_in", "sinks", "diff_subln_g", "cmp_w1", "cmp_w2",
              "cmp_b2", "w_branch", "w_out", "router_w", "router_b", "exp_b2"):
        m[k] = inp[k]
    m["final_g"] = inp["final_g"].reshape(1, D)
    m["b_inT"] = np.ascontiguousarray(np.stack([inp["b_in"][:, SLOT_COL[2 * i]:SLOT_COL[2 * i] + 128] for i in range(NSLOT // 2)], axis=2))
    m["diff_lambda"] = inp["diff_lambda"].reshape(L_DEPTH, 256)
    m["cmp_posT"] = np.ascontiguousarray(inp["cmp_pos"].transpose(0, 1, 3, 2))
    m["cmp_b1T"] = np.ascontiguousarray(inp["cmp_b1"].reshape(L_DEPTH, 2, 2, 128).transpose(0, 1, 3, 2))
    m["cmp_b2T"] = np.ascontiguousarray(inp["cmp_b2"].reshape(L_DEPTH, 2, 64, 1))
    m["exp_b1E"] = np.ascontiguousarray(inp["exp_b1"].reshape(L_DEPTH, NE, 2, 128, 8).transpose(0, 1, 3, 2, 4)).reshape(L_DEPTH * NE * 128, 16)
    m["exp_w1"] = inp["exp_w1"].reshape(L_DEPTH * NE * 256, 4 * 2 * D)
    m["exp_w2"] = inp["exp_w2"].reshape(L_DEPTH * NE * 128, 8 * D)
    return m


IN_SHAPES = {
    "x": ([NSEQ * S, D], F32), "cT": ([D, NSEQ], F32), "mod_w": ([L_DEPTH, D, 6 * D], F32), "mod_b": ([L_DEPTH, 6 * D], F32),
    "norm1_g": ([L_DEPTH, D], F32), "norm2_g": ([L_DEPTH, D], F32), "w_in": ([L_DEPTH, D, INW], F32), "b_in": ([L_DEPTH, INW], F32),
    "sinks": ([L_DEPTH, 8], F32), "diff_subln_g": ([L_DEPTH, 128], F32), "cmp_w1": ([L_DEPTH, 2, 2048, 256], F32),
    "cmp_w2": ([L_DEPTH, 2, 256, 64], F32), "cmp_b2": ([L_DEPTH, 2, 64], F32), "w_branch": ([L_DEPTH, 3, 512, D], F32),
    "w_out": ([L_DEPTH, D, D], F32), "router_w": ([L_DEPTH, D, NE], F32), "router_b": ([L_DEPTH, NE], F32),
    "exp_w1": ([L_DEPTH * NE * 256, 8 * D], F32), "exp_w2": ([L_DEPTH * NE * 128, 8 * D], F32), "exp_b2": ([L_DEPTH, NE, D], F32),
    "final_g": ([1, D], F32), "b_inT": ([L_DEPTH, 128, NSLOT // 2], F32), "diff_lambda": ([L_DEPTH, 256], F32),
    "cmp_posT": ([L_DEPTH, 2, 64, 32], F32), "cmp_b1T": ([L_DEPTH, 2, 128, 2], F32), "cmp_b2T": ([L_DEPTH, 2, 64, 1], F32),
    "exp_b1E": ([L_DEPTH * NE * 128, 16], F32),
    "ltri": ([128, 128], BF16), "ones128": ([128, 128], BF16), "b512": ([128, 64], F32), "rowiota": ([128, 8], F32), "piota": ([128, 1], F32),
    "aug": ([NSLOT, 4, S], BF16), "ident": ([128, 128], BF16), "ident32": ([128, 128], F32), "mdiag": ([128, 128], BF16),
    "medge": ([128, 128], BF16), "cmaskT": ([128, S], BF16), "eblk": ([32, S], BF16), "atab": ([S, 32], F32),
    "overlap": ([128, 32], BF16),
}


def bcast_rows(ap1d_row, nparts):
    return ap1d_row.broadcast_to([nparts, ap1d_row.shape[-1]])


class K:
    def __init__(self, dbg=(), layers=L_DEPTH, nseq=NSEQ, stop_after=None, only=None):
        self.dbg = set(dbg)
        self.only = only
        self.layers = layers
        self.nseq = nseq
        self.stop_after = stop_after
        nc = self.nc = bass.Bass("TRN2", target_bir_lowering=False)
        self.I = {k: nc.dram_tensor(k, list(sh), dt, kind="ExternalInput").ap() for k, (sh, dt) in IN_SHAPES.items()}
        self.out = nc.dram_tensor("out", [NSEQ * S, D], F32, kind="ExternalOutput").ap()
        self.Dr = {}
        self.DR = {}

    def dram(self, name, shape, dt):
        kind = "ExternalOutput" if name in self.dbg else "Internal"
        self.Dr[name] = self.nc.dram_tensor(name, list(shape), dt, kind=kind).ap()
        self.DR[name] = R(name)
        return self.Dr[name]

    def mm(self, out, lhsT, rhs, start, stop, reads, writes, sig=True):
        return self.s.op("pe", lambda h: h.matmul(out, lhsT=lhsT, rhs=rhs, start=start, stop=stop), reads=reads, writes=writes, sig=sig)

    def tr(self, out, in_, ident, reads, writes, sig=True):
        return self.s.op("pe", lambda h: h.transpose(out=out, in_=in_, identity=ident), reads=reads, writes=writes, sig=sig)

    def rstd_of(self, xt, junk, ss, n=D):
        s = self.s
        s.op("act", lambda h: h.activation(out=junk[1], in_=xt[1], func=AF.Square, accum_out=ss[:]), reads=[xt[0]], writes=[junk[0], ss])
        s.op("dve", lambda h: h.tensor_scalar(out=ss[:], in0=ss[:], scalar1=1.0 / n, scalar2=EPS, op0=ALU.mult, op1=ALU.add), reads=[ss], writes=[ss])
        s.op("act", lambda h: h.sqrt(out=ss[:], in_=ss[:]), reads=[ss], writes=[ss])
        s.op("dve", lambda h: h.reciprocal(out=ss[:], in_=ss[:]), reads=[ss], writes=[ss])

    def build(self):
        nc = self.nc
        with ExitStack() as st:
            s = self.s = Sched(nc, st)
            self.P = [s.ps([128, 512], F32, "bank%d" % i) for i in range(8)]
            self.ident = s.sb([128, 128], BF16, "ident")
            self.ident32 = s.sb([128, 128], F32, "ident32")
            s.dma("sp", self.ident[:], self.I["ident"], writes=[self.ident])
            s.dma("sp", self.ident32[:], self.I["ident32"], writes=[self.ident32])
            self.dram("mod_d", [L_DEPTH, NSEQ, 6 * D], F32)
            self.dram("xres", [NSEQ * S, D], F32)
            self.dram("QT_d", [NSLOT, 64, S], BF16)
            self.dram("VA_d", [S, 2, 65], BF16)
            self.dram("VB_d", [S, 4, 129], BF16)
            self.dram("VSL_d", [S, 2, 65], BF16)
            self.dram("VWN_d", [S, 2, 65], BF16)
            self.dram("GN_d", [S, 24], F32)
            self.dram("GM_d", [S, 3072], F32)
            self.dram("KC_d", [2, 64, 128], BF16)
            self.dram("VC_d", [2, 128, 97], BF16)
            self.dram("O_d", [S, 1536], BF16)
            self.ntt = self.nseq * NT
            self.nblk = self.ntt + NE - 1
            self.dram("xs_d", [self.nblk * 512, D], BF16)
            self.dram("ys_d", [self.nblk * 512, D], F32)
            self.dram("dbg_d", [128, 4096], F32)
            zt = s.sb([128, 2048], BF16, "zt")
            s.op("pool", lambda h: h.memset(zt[:], 0.0), writes=[zt])
            xv = self.Dr["xs_d"].rearrange("(a p r) d -> a p (r d)", p=128, r=2)
            for a in range(xv.shape[0]):
                s.dma("pool", xv[a], zt[:], reads=[zt], writes=[])
            try:
                self.body()
            except StopIteration:
                pass
            s.barrier()
            s.finish()
        return nc

    def phase_end(self, name):
        self.s.barrier()
        if self.stop_after == name:
            raise StopIteration

    def body(self):
        for l in range(self.layers):
            self.runp("mod%d" % l, self.phase_mod, l)
            for q in range(self.nseq):
                for nm, fn in (("inproj", self.phase_inproj), ("cmp", self.phase_cmp), ("swa", self.phase_swa), ("diff", self.phase_diff),
                               ("nsa", self.phase_nsa), ("merge", self.phase_merge)):
                    self.runp("%s%d_%d" % (nm, l, q), fn, l, q)
            self.runp("moe%d" % l, self.phase_moe2, l)

    def runp(self, name, fn, *args):
        if self.only is None or name in self.only:
            fn(*args)
        self.phase_end(name)

    def phase_mod(self, l):
        s, I = self.s, self.I
        with ExitStack() as ps:
            cT = s.sb([128, 8, NSEQ], F32, "cT", ps)
            cs = s.sb([128, 8, NSEQ], F32, "cs", ps)
            modb = s.sb([NSEQ, 6 * D], F32, "modb", ps)
            mods = s.sb([NSEQ, 6 * D], F32, "mods", ps)
            wb = [s.sb([128, 8, 512], F32, "modw", ps) for _ in range(2)]
            s.dma("sp", cT[:], I["cT"].rearrange("(kc p) b -> p kc b", p=128), writes=[cT])
            s.dma("sp", modb[:], I["mod_b"][l:l + 1, :].broadcast_to([NSEQ, 6 * D]), writes=[modb])
            s.op("act", lambda h: h.activation(out=cs[:], in_=cT[:], func=AF.Silu), reads=[cT], writes=[cs])
            wsrc = I["mod_w"][l].rearrange("(kc p) n -> p kc n", p=128)
            for cg in range(12):
                w = wb[cg % 2]
                s.dma("sp", w[:], wsrc[:, :, cg * 512:(cg + 1) * 512], writes=[w])
                pm = self.P[cg % 2]
                for kc in range(8):
                    self.mm(pm[0:NSEQ, :], cs[:, kc, :], w[:, kc, :], kc == 0, kc == 7, [cs, w], [pm], sig=(kc == 7))
                s.op("dve", lambda h: h.tensor_tensor(out=mods[:, cg * 512:(cg + 1) * 512], in0=pm[0:NSEQ, :], in1=modb[:, cg * 512:(cg + 1) * 512], op=ALU.add),
                     reads=[pm, modb], writes=[mods])
            for seg in (1, 4):
                s.op("dve", lambda h: h.tensor_scalar_add(out=mods[:, seg * D:(seg + 1) * D], in0=mods[:, seg * D:(seg + 1) * D], scalar1=1.0), reads=[mods], writes=[mods])
            s.dma("sp", self.Dr["mod_d"][l], mods[:], reads=[mods], writes=[self.DR["mod_d"]])

    def mod_bc(self, l, q, seg, tile):
        src = self.Dr["mod_d"][l, q:q + 1, seg * D:(seg + 1) * D].broadcast_to([128, D])
        self.s.dma("sp", tile[:], src, reads=[self.DR["mod_d"]], writes=[tile])

    def xsrc(self, l, q):
        return (self.I["x"] if l == 0 else self.Dr["xres"]), ([] if l == 0 else [self.DR["xres"]])

    def norm_tiles(self, ps, l, q, gname, seg_sc, seg_sh):
        s, I = self.s, self.I
        gsc = s.sb([128, D], F32, "gsc", ps)
        sh = s.sb([128, D], F32, "sh", ps)
        gt = s.sb([128, D], F32, "gt", ps)
        s.dma("sp", gt[:], I[gname][l:l + 1, :].broadcast_to([128, D]), writes=[gt])
        self.mod_bc(l, q, seg_sc, gsc)
        self.mod_bc(l, q, seg_sh, sh)
        s.op("dve", lambda h: h.tensor_tensor(out=gsc[:], in0=gsc[:], in1=gt[:], op=ALU.mult), reads=[gsc, gt], writes=[gsc])
        return gsc, sh

    def phase_inproj(self, l, q):
        s, I, Dr, DR = self.s, self.I, self.Dr, self.DR
        xsrc, xres_r = self.xsrc(l, q)
        with ExitStack() as ps:
            gsc, sh = self.norm_tiles(ps, l, q, "norm1_g", 1, 0)
            hT = s.sb([128, 8, S], BF16, "hT", ps)
            hTr = [R("hT%d" % i) for i in range(4)]
            xt = [s.sb([128, D], F32, "xt", ps) for _ in range(2)]
            junk = s.sb([128, D], F32, "junk", ps)
            hb = [s.sb([128, D], BF16, "hb", ps) for _ in range(2)]
            ss = [s.sb([128, 1], F32, "ss", ps) for _ in range(2)]
            for tt in range(NT):
                x, h, sq = xt[tt % 2], hb[tt % 2], ss[tt % 2]
                r0 = q * S + tt * 128
                s.dma("sp", x[:], xsrc[r0:r0 + 128, :], reads=xres_r, writes=[x])
                self.rstd_of((x, x[:]), (junk, junk[:]), sq)
                s.op("dve", lambda hh: hh.scalar_tensor_tensor(out=x[:], in0=x[:], scalar=sq[:, 0:1], in1=gsc[:], op0=ALU.mult, op1=ALU.mult), reads=[x, sq, gsc], writes=[x])
                s.op("pool", lambda hh: hh.tensor_tensor(out=h[:], in0=x[:], in1=sh[:], op=ALU.add), reads=[x, sh], writes=[h])
                pt = self.P[tt % 2]
                ptb = pt[:].bitcast(BF16)
                for kc in range(8):
                    self.tr(ptb[:, kc * 128:(kc + 1) * 128], h[:, kc * 128:(kc + 1) * 128], self.ident[:], [h, self.ident], [pt], sig=(kc == 7))
                s.op("act", lambda hh: hh.copy(out=hT[:, :, tt * 128:(tt + 1) * 128], in_=ptb.rearrange("p (k t) -> p k t", k=8)), reads=[pt], writes=[hTr[tt // 4]])
            binT = s.sb([128, NSLOT // 2], F32, "binT", ps)
            s.dma("sp", binT[:], I["b_inT"][l], writes=[binT])
            wsrc = I["w_in"][l].rearrange("(kc p) n -> p kc n", p=128)
            wbuf = [s.sb([128, 8, 512], BF16, "wblk", ps) for _ in range(2)]
            stg = [s.sb([128, S], BF16, "stg", ps) for _ in range(2)]
            groups = [(SL_QA, 8), (SL_KA, 2), (SL_QB, 8), (SL_KB, 8), (SL_QC, 8), (SL_KCM, 4), (SL_KSL, 2), (SL_KWN, 2)]
            nw = 0
            nmm = 0
            for (s0, ns) in groups:
                w = wbuf[nw % 2]
                nw += 1
                c0 = SLOT_COL[s0]
                s.dma("pool", w[:, :, 0:ns * 64], wsrc[:, :, c0:c0 + ns * 64], writes=[w])
                for si in range(0, ns, 2):
                    slot = s0 + si
                    pr_ = slot // 2
                    sg = stg[pr_ % 2]
                    for tg in range(4):
                        pm = self.P[2 + nmm % 4]
                        nmm += 1
                        for kc in range(8):
                            self.mm(pm[:, :], w[:, kc, si * 64:(si + 2) * 64], hT[:, kc, tg * 512:(tg + 1) * 512], kc == 0, kc == 7, [w, hTr[tg]], [pm], sig=(kc == 7))
                        s.op("act", lambda hh: hh.activation(out=sg[:, tg * 512:(tg + 1) * 512], in_=pm[:, :], func=AF.Identity, bias=binT[:, pr_:pr_ + 1]),
                             reads=[pm, binT], writes=[sg])
                    s.dma("sp", Dr["QT_d"][slot], sg[0:64, :], reads=[sg], writes=[DR["QT_d"]])
                    s.dma("sp", Dr["QT_d"][slot + 1], sg[64:128, :], reads=[sg], writes=[DR["QT_d"]])
            binb = s.sb([128, INW], F32, "binb", ps)
            s.dma("sp", binb[:], I["b_in"][l:l + 1, :].broadcast_to([128, INW]), writes=[binb])
            vt = {}
            for nm, nh, dv in (("VA_d", 2, 64), ("VB_d", 4, 128), ("VSL_d", 2, 64), ("VWN_d", 2, 64)):
                vt[nm] = [s.sb([128, nh, dv + 1], BF16, "vt", ps) for _ in range(2)]
                for v in vt[nm]:
                    s.op("pool", lambda hh: hh.memset(v[:, :, dv:dv + 1], 1.0), writes=[v])
            gnt = [s.sb([128, 24], F32, "gnt", ps) for _ in range(2)]
            gmt = [s.sb([128, 512], F32, "gmt", ps) for _ in range(2)]
            blocks = [("VA_d", C_VA, 128, 2, 64), ("VB_d", C_VB, 512, 4, 128), ("VSL_d", C_VSL, 128, 2, 64), ("VWN_d", C_VWN, 128, 2, 64),
                      ("GN_d", C_GN, 24, 0, 0)] + [("GM_d", C_GM + 512 * i, 512, i, 0) for i in range(6)]
            for (nm, c0, ncol, nh, dv) in blocks:
                w = wbuf[nw % 2]
                nw += 1
                s.dma("pool", w[:, :, 0:ncol], wsrc[:, :, c0:c0 + ncol], writes=[w])
                for tt in range(NT):
                    pm = self.P[2 + nmm % 4]
                    nmm += 1
                    for kc in range(8):
                        self.mm(pm[:, 0:ncol], hT[:, kc, tt * 128:(tt + 1) * 128], w[:, kc, 0:ncol], kc == 0, kc == 7, [w, hTr[tt // 4]], [pm], sig=(kc == 7))
                    rows = slice(tt * 128, (tt + 1) * 128)
                    if nm == "GN_d":
                        g = gnt[tt % 2]
                        s.op("dve", lambda hh: hh.tensor_tensor(out=g[:], in0=pm[:, 0:24], in1=binb[:, c0:c0 + 24], op=ALU.add), reads=[pm, binb], writes=[g])
                        s.op("act", lambda hh: hh.activation(out=g[:], in_=g[:], func=AF.Sigmoid), reads=[g], writes=[g])
                        s.dma("sp", Dr["GN_d"][rows, :], g[:], reads=[g], writes=[DR["GN_d"]])
                    elif nm == "GM_d":
                        g = gmt[tt % 2]
                        s.op("dve", lambda hh: hh.tensor_tensor(out=g[:], in0=pm[:, :], in1=binb[:, c0:c0 + 512], op=ALU.add), reads=[pm, binb], writes=[g])
                        s.op("act", lambda hh: hh.activation(out=g[:], in_=g[:], func=AF.Sigmoid), reads=[g], writes=[g])
                        s.dma("sp", Dr["GM_d"][rows, nh * 512:(nh + 1) * 512], g[:], reads=[g], writes=[DR["GM_d"]])
                    else:
                        v = vt[nm][tt % 2]
                        s.op("dve", lambda hh: hh.tensor_tensor(out=v[:, :, 0:dv], in0=pm[:, 0:ncol].rearrange("p (h d) -> p h d", d=dv),
                                                               in1=binb[:, c0:c0 + ncol].rearrange("p (h d) -> p h d", d=dv), op=ALU.add), reads=[pm, binb], writes=[v])
                        s.dma("sp", Dr[nm][rows], v[:], reads=[v], writes=[DR[nm]])

    def phase_cmp(self, l, q):
        s, I, Dr, DR = self.s, self.I, self.Dr, self.DR
        with ExitStack() as ps:
            ovl = self.load_const(ps, "overlap", [128, 32], BF16)
            w1 = s.sb([64, 32, 256], BF16, "cw1", ps)
            w2 = s.sb([128, 2, 64], BF16, "cw2", ps)
            posT = s.sb([64, 32], F32, "posT", ps)
            posb = s.sb([64, 32], BF16, "posb", ps)
            b1T = s.sb([128, 2], F32, "b1T", ps)
            bias = s.sb([128, 2], F32, "cbias", ps)
            b2T = s.sb([64, 1], F32, "b2T", ps)
            b2b = s.sb([128, 64], F32, "b2b", ps)
            xT = s.sb([64, S], BF16, "cxT", ps)
            hid = s.sb([128, 2, 128], BF16, "hid", ps)
            y = s.sb([128, 128], F32, "cy", ps)
            u = s.sb([128, 128], F32, "cu", ps)
            kct = s.sb([64, 128], BF16, "kct", ps)
            vct = s.sb([128, 97], BF16, "vct", ps)
            s.op("pool", lambda h: h.memset(kct[:], 0.0), writes=[kct])
            s.op("pool", lambda h: h.memset(vct[:], 0.0), writes=[vct])
            for which in range(2):
                s.dma("pool", w1[:], I["cmp_w1"][l, which].rearrange("(l d) f -> d l f", d=64), writes=[w1])
                s.dma("pool", w2[:], I["cmp_w2"][l, which].rearrange("(c p) d -> p c d", p=128), writes=[w2])
                s.dma("sp", posT[:], I["cmp_posT"][l, which], writes=[posT])
                s.dma("sp", b1T[:], I["cmp_b1T"][l, which], writes=[b1T])
                s.dma("sp", b2T[:], I["cmp_b2T"][l, which], writes=[b2T])
                s.dma("sp", b2b[:], I["cmp_b2"][l, which:which + 1, :].broadcast_to([128, 64]), writes=[b2b])
                s.op("act", lambda h: h.copy(out=posb[:], in_=posT[:]), reads=[posT], writes=[posb])
                for ch in range(2):
                    pm = self.P[ch]
                    for li in range(32):
                        self.mm(pm[:, 0:1], w1[:, li, ch * 128:(ch + 1) * 128], posb[:, li:li + 1], li == 0, li == 31, [w1, posb], [pm], sig=(li == 31))
                    s.op("dve", lambda h: h.tensor_tensor(out=bias[:, ch:ch + 1], in0=pm[:, 0:1], in1=b1T[:, ch:ch + 1], op=ALU.add), reads=[pm, b1T], writes=[bias])
                for hk in range(2):
                    s.dma("sp", xT[:], Dr["QT_d"][SL_KCM + which * 2 + hk], reads=[DR["QT_d"]], writes=[xT])
                    for ch in range(2):
                        pm = self.P[2 + ch]
                        for li in range(32):
                            self.mm(pm[:, 0:127], w1[:, li, ch * 128:(ch + 1) * 128], xT[:, li:li + 16 * 126 + 1:16], li == 0, li == 31, [w1, xT], [pm], sig=(li == 31))
                        s.op("act", lambda h: h.activation(out=y[:, 0:127], in_=pm[:, 0:127], func=AF.Identity, bias=bias[:, ch:ch + 1]), reads=[pm, bias], writes=[y])
                        s.op("dve", lambda h: h.tensor_tensor(out=u[:, 0:127], in0=y[:, 0:127], in1=y[:, 0:127], op=ALU.mult), reads=[y], writes=[u])
                        s.op("dve", lambda h: h.tensor_scalar(out=u[:, 0:127], in0=u[:, 0:127], scalar1=0.044715, scalar2=1.0, op0=ALU.mult, op1=ALU.add), reads=[u], writes=[u])
                        s.op("dve", lambda h: h.tensor_tensor(out=u[:, 0:127], in0=u[:, 0:127], in1=y[:, 0:127], op=ALU.mult), reads=[u, y], writes=[u])
                        s.op("act", lambda h: h.activation(out=u[:, 0:127], in_=u[:, 0:127], func=AF.Sigmoid, scale=1.5957691216057308), reads=[u], writes=[u])
                        s.op("dve", lambda h: h.tensor_tensor(out=hid[:, ch, 0:127], in0=u[:, 0:127], in1=y[:, 0:127], op=ALU.mult), reads=[u, y], writes=[hid])
                    pm2 = self.P[4 + hk]
                    if which == 0:
                        for ch in range(2):
                            self.mm(pm2[0:64, 0:127], w2[:, ch, :], hid[:, ch, 0:127], ch == 0, ch == 1, [w2, hid], [pm2], sig=(ch == 1))
                        s.op("act", lambda h: h.activation(out=kct[:, 0:127], in_=pm2[0:64, 0:127], func=AF.Identity, bias=b2T[:, 0:1]), reads=[pm2, b2T], writes=[kct])
                        s.dma("sp", Dr["KC_d"][hk], kct[:], reads=[kct], writes=[DR["KC_d"]])
                    else:
                        for ch in range(2):
                            self.mm(pm2[0:127, 0:64], hid[:, ch, 0:127], w2[:, ch, :], ch == 0, ch == 1, [w2, hid], [pm2], sig=(ch == 1))
                        s.op("dve", lambda h: h.tensor_tensor(out=vct[0:127, 0:64], in0=pm2[0:127, 0:64], in1=b2b[0:127, :], op=ALU.add), reads=[pm2, b2b], writes=[vct])
                        s.op("pool", lambda h: h.memset(vct[:, 64:65], 1.0), writes=[vct])
                        s.op("pool", lambda h: h.tensor_copy(out=vct[:, 65:97], in_=ovl[:]), reads=[ovl], writes=[vct])
                        s.dma("sp", Dr["VC_d"][hk], vct[:], reads=[vct], writes=[DR["VC_d"]])

    def load_heads(self, tile, slots, grouped):
        s = self.s
        for g, slot in enumerate(slots):
            dst0 = tile[0:64, g, :] if grouped else tile[0:64, :]
            dst1 = tile[64:68, g, :] if grouped else tile[64:68, :]
            s.dma("sp", dst0, self.Dr["QT_d"][slot], reads=[self.DR["QT_d"]], writes=[tile])
            s.dma("sp", dst1, self.I["aug"][slot], writes=[tile])

    def attn_stream(self, items, banks, pts):
        s = self.s
        n = len(items)

        def emit_qk(k):
            it = items[k]
            sc = banks[k % len(banks)]
            nq = len(it["qk"])
            for m, (outfn, lhsT, rhs, start, stop, reads) in enumerate(it["qk"]):
                self.mm(outfn(sc), lhsT, rhs, start, stop, reads, [sc], sig=(m == nq - 1))

        emit_qk(0)
        if n > 1:
            emit_qk(1)
        for k in range(n):
            if k + 2 < n:
                emit_qk(k + 2)
            it = items[k]
            sc = banks[k % len(banks)]
            pT = pts[k % len(pts)]
            kp, N = it["kp"], it["N"]
            s.op("act", lambda h: h.activation(out=pT[0:kp, 0:N], in_=sc[0:kp, 0:N], func=AF.Exp, scale=SCALE), reads=[sc], writes=[pT])
            it["pv"](pT)

    def load_const(self, ps, name, shape, dt):
        t = self.s.sb(shape, dt, name, ps)
        self.s.dma("sp", t[:], self.I[name], writes=[t])
        return t

    def phase_swa(self, l, q):
        s, I, Dr, DR = self.s, self.I, self.Dr, self.DR
        ident = self.ident
        with ExitStack() as ps:
            mdiag = self.load_const(ps, "mdiag", [128, 128], BF16)
            medge = self.load_const(ps, "medge", [128, 128], BF16)
            esink = s.sb([128, 8], F32, "esink", ps)
            s.dma("sp", esink[:], I["sinks"][l:l + 1, :].broadcast_to([128, 8]), writes=[esink])
            s.op("act", lambda h: h.activation(out=esink[:], in_=esink[:], func=AF.Exp), reads=[esink], writes=[esink])
            QG = s.sb([68, 4, S], BF16, "QG", ps)
            KT = s.sb([68, S], BF16, "KT", ps)
            V = s.sb([128, NT, 65], BF16, "V", ps)
            pts = [s.sb([128, 512], BF16, "pT", ps) for _ in range(3)]
            ot = [s.sb([128, 4, 64], BF16, "ot", ps) for _ in range(2)]
            den = [s.sb([128, 4], F32, "den", ps) for _ in range(2)]
            for hk in range(2):
                self.load_heads(QG, [SL_QA + hk * 4 + g for g in range(4)], True)
                self.load_heads(KT, [SL_KA + hk], False)
                s.dma("sp", V[:], Dr["VA_d"][:, hk, :].rearrange("(n p) e -> p n e", p=128), reads=[DR["VA_d"]], writes=[V])
                for j in range(NT):
                    acc = self.P[4 + j % 2]
                    tiles = [i for i in (j - 1, j) if i >= 0]
                    items = []
                    for idx, i in enumerate(tiles):
                        mask = mdiag if i == j else medge
                        o3 = lambda sc: sc[:, :].rearrange("p (g t) -> p g t", g=4)
                        qk = [(o3, KT[:, i * 128:(i + 1) * 128], QG[:, :, j * 128:(j + 1) * 128], True, False, [KT, QG]),
                              (o3, ident[:], mask[:].unsqueeze(1).broadcast_to([128, 4, 128]), False, True, [ident, mask])]

                        def pv(pT, i=i, idx=idx, acc=acc, last=len(tiles) - 1):
                            for g in range(4):
                                self.mm(acc[:, g * 65:(g + 1) * 65], pT[:, g * 128:(g + 1) * 128], V[:, i, :], idx == 0 and g == 0, idx == last, [pT, V], [acc], sig=(g == 3))
                        items.append(dict(kp=128, N=512, qk=qk, pv=pv))
                    self.attn_stream(items, self.P[0:3], pts)
                    a3 = acc[:, 0:260].rearrange("p (g e) -> p g e", e=65)
                    dn, o = den[j % 2], ot[j % 2]
                    s.op("dve", lambda h: h.tensor_tensor(out=dn[:].unsqueeze(2), in0=a3[:, :, 64:65], in1=esink[:, hk * 4:(hk + 1) * 4].unsqueeze(2), op=ALU.add), reads=[acc, esink], writes=[dn])
                    s.op("dve", lambda h: h.reciprocal(out=dn[:], in_=dn[:]), reads=[dn], writes=[dn])
                    s.op("dve", lambda h: h.tensor_tensor(out=o[:], in0=a3[:, :, 0:64], in1=dn[:].unsqueeze(2).broadcast_to([128, 4, 64]), op=ALU.mult), reads=[acc, dn], writes=[o])
                    s.dma("sp", Dr["O_d"][j * 128:(j + 1) * 128, hk * 256:(hk + 1) * 256], o[:].rearrange("p g d -> p (g d)"), reads=[o], writes=[DR["O_d"]])

    def phase_diff(self, l, q):
        s, I, Dr, DR = self.s, self.I, self.Dr, self.DR
        ident = self.ident
        lam_init = 0.8 - 0.6 * float(np.exp(-0.3 * l))
        with ExitStack() as ps:
            mdiag = self.load_const(ps, "mdiag", [128, 128], BF16)
            dl = s.sb([128, 256], F32, "dl", ps)
            s.dma("sp", dl[:], I["diff_lambda"][l:l + 1, :].broadcast_to([128, 256]), writes=[dl])
            pr = s.sb([128, 2, 64], F32, "pr", ps)
            d4 = dl[:].rearrange("p (a b d) -> p a b d", a=2, b=2)
            s.op("dve", lambda h: h.tensor_tensor(out=pr[:], in0=d4[:, :, 0, :], in1=d4[:, :, 1, :], op=ALU.mult), reads=[dl], writes=[pr])
            e2 = s.sb([128, 2], F32, "e2", ps)
            s.op("dve", lambda h: h.reduce_sum(out=e2[:], in_=pr[:], axis=AX.X), reads=[pr], writes=[e2])
            s.op("act", lambda h: h.activation(out=e2[:], in_=e2[:], func=AF.Exp), reads=[e2], writes=[e2])
            nlam = s.sb([128, 1], F32, "nlam", ps)
            s.op("dve", lambda h: h.scalar_tensor_tensor(out=nlam[:], in0=e2[:, 0:1], scalar=lam_init, in1=e2[:, 1:2], op0=ALU.add, op1=ALU.subtract), reads=[e2], writes=[nlam])
            s.op("dve", lambda h: h.tensor_scalar_mul(out=nlam[:], in0=nlam[:], scalar1=-1.0), reads=[nlam], writes=[nlam])
            gsub = s.sb([128, 128], F32, "gsub", ps)
            s.dma("sp", gsub[:], I["diff_subln_g"][l:l + 1, :].broadcast_to([128, 128]), writes=[gsub])
            s.op("dve", lambda h: h.tensor_scalar_mul(out=gsub[:], in0=gsub[:], scalar1=1.0 - lam_init), reads=[gsub], writes=[gsub])
            KT = [s.sb([68, S], BF16, "KTb", ps) for _ in range(2)]
            QT = [s.sb([68, S], BF16, "QTb", ps) for _ in range(2)]
            V = s.sb([128, NT, 129], BF16, "Vb", ps)
            pts = [s.sb([128, 512], BF16, "pT", ps) for _ in range(3)]
            t0 = [s.sb([128, 128], F32, "t0", ps) for _ in range(2)]
            junk = s.sb([128, 128], F32, "junkb", ps)
            rr = [s.sb([128, 4], F32, "rr", ps) for _ in range(2)]
            ob = [s.sb([128, 128], BF16, "ob", ps) for _ in range(2)]
            accc = [[s.sb([128, 258], F32, "accc", ps) for _ in range(4)] for _ in range(2)]
            nfin = 0
            for hd in range(4):
                for c in range(2):
                    self.load_heads(KT[c], [SL_KB + 2 * hd + c], False)
                    self.load_heads(QT[c], [SL_QB + 2 * hd + c], False)
                s.dma("sp", V[:], Dr["VB_d"][:, hd, :].rearrange("(n p) e -> p n e", p=128), reads=[DR["VB_d"]], writes=[V])
                for G in range(4):
                    def accap(c, jj):
                        return self.P[4 + c * 2 + jj // 2], (jj % 2) * 129
                    items = []
                    for c in range(2):
                        for i in range(0, 4 * G + 4):
                            j0 = max(i, 4 * G)
                            N = (4 * G + 4 - j0) * 128
                            kt = KT[c][:, i * 128:(i + 1) * 128]
                            qk = []
                            if i >= 4 * G:
                                qk.append((lambda sc: sc[:, 0:128], kt, QT[c][:, j0 * 128:(j0 + 1) * 128], True, False, [KT[c], QT[c]]))
                                qk.append((lambda sc: sc[:, 0:128], ident[:], mdiag[:], False, True, [ident, mdiag]))
                                if N > 128:
                                    qk.append((lambda sc, N=N: sc[:, 128:N], kt, QT[c][:, (j0 + 1) * 128:(4 * G + 4) * 128], True, True, [KT[c], QT[c]]))
                            else:
                                qk.append((lambda sc, N=N: sc[:, 0:N], kt, QT[c][:, j0 * 128:(4 * G + 4) * 128], True, True, [KT[c], QT[c]]))

                            def pv(pT, c=c, i=i, j0=j0, G=G):
                                for jj in range(j0, 4 * G + 4):
                                    bank, off = accap(c, jj - 4 * G)
                                    self.mm(bank[:, off:off + 129], pT[:, (jj - j0) * 128:(jj - j0 + 1) * 128], V[:, i, :], i == 0 and (jj - 4 * G) % 2 == 0, i == jj, [pT, V], [bank], sig=(jj == 4 * G + 3))
                            items.append(dict(kp=128, N=N, qk=qk, pv=pv))
                    self.attn_stream(items, self.P[0:3], pts)
                    cps = accc[G % 2]
                    for bi in range(4):
                        bank = self.P[4 + bi]
                        if bi % 2 == 0:
                            s.op("act", lambda h, bank=bank, bi=bi: h.copy(out=cps[bi][:], in_=bank[:, 0:258]), reads=[bank], writes=[cps[bi]])
                        else:
                            s.op("dve", lambda h, bank=bank, bi=bi: h.tensor_copy(out=cps[bi][:], in_=bank[:, 0:258]), reads=[bank], writes=[cps[bi]])
                    for jj in range(4):
                        b0, o0 = cps[0 * 2 + jj // 2], (jj % 2) * 129
                        b1, o1 = cps[1 * 2 + jj // 2], (jj % 2) * 129
                        r, t, o = rr[nfin % 2], t0[nfin % 2], ob[nfin % 2]
                        nfin += 1
                        s.op("dve", lambda h: h.reciprocal(out=r[:, 0:1], in_=b0[:, o0 + 128:o0 + 129]), reads=[b0], writes=[r])
                        s.op("dve", lambda h: h.reciprocal(out=r[:, 1:2], in_=b1[:, o1 + 128:o1 + 129]), reads=[b1], writes=[r])
                        s.op("dve", lambda h: h.tensor_tensor(out=r[:, 1:2], in0=r[:, 1:2], in1=nlam[:], op=ALU.mult), reads=[r, nlam], writes=[r])
                        s.op("dve", lambda h: h.tensor_scalar_mul(out=t[:], in0=b0[:, o0:o0 + 128], scalar1=r[:, 0:1]), reads=[b0, r], writes=[t])
                        s.op("dve", lambda h: h.scalar_tensor_tensor(out=t[:], in0=b1[:, o1:o1 + 128], scalar=r[:, 1:2], in1=t[:], op0=ALU.mult, op1=ALU.add), reads=[b1, r, t], writes=[t])
                        s.op("act", lambda h: h.activation(out=junk[:], in_=t[:], func=AF.Square, accum_out=r[:, 2:3]), reads=[t], writes=[junk, r])
                        s.op("dve", lambda h: h.tensor_scalar(out=r[:, 2:3], in0=r[:, 2:3], scalar1=1.0 / 128, scalar2=EPS, op0=ALU.mult, op1=ALU.add), reads=[r], writes=[r])
                        s.op("act", lambda h: h.sqrt(out=r[:, 2:3], in_=r[:, 2:3]), reads=[r], writes=[r])
                        s.op("dve", lambda h: h.reciprocal(out=r[:, 2:3], in_=r[:, 2:3]), reads=[r], writes=[r])
                        s.op("dve", lambda h: h.scalar_tensor_tensor(out=o[:], in0=t[:], scalar=r[:, 2:3], in1=gsub[:], op0=ALU.mult, op1=ALU.mult), reads=[t, r, gsub], writes=[o])
                        row0 = (4 * G + jj) * 128
                        s.dma("sp", Dr["O_d"][row0:row0 + 128, 512 + hd * 128:512 + (hd + 1) * 128], o[:], reads=[o], writes=[DR["O_d"]])

    def phase_nsa(self, l, q):
        s, I, Dr, DR = self.s, self.I, self.Dr, self.DR
        ident = self.ident
        with ExitStack() as ps:
            mdiag = self.load_const(ps, "mdiag", [128, 128], BF16)
            medge = self.load_const(ps, "medge", [128, 128], BF16)
            cmaskT = self.load_const(ps, "cmaskT", [128, S], BF16)
            eblk = self.load_const(ps, "eblk", [32, S], BF16)
            atab = s.sb([128, NT, 32], F32, "atab", ps)
            s.dma("sp", atab[:], I["atab"].rearrange("(n p) b -> p n b", p=128), writes=[atab])
            GN = s.sb([128, NT, 24], F32, "GN", ps)
            s.dma("sp", GN[:], Dr["GN_d"].rearrange("(n p) c -> p n c", p=128), reads=[DR["GN_d"]], writes=[GN])
            QG = s.sb([68, 4, S], BF16, "QGc", ps)
            KSL = s.sb([68, S], BF16, "KSL", ps)
            KWN = s.sb([68, S], BF16, "KWN", ps)
            KCT = s.sb([64, 128], BF16, "KCT", ps)
            VC = s.sb([128, 97], BF16, "VC", ps)
            VSL = s.sb([128, NT, 65], BF16, "VSL", ps)
            VWN = s.sb([128, NT, 65], BF16, "VWN", ps)
            OC = s.sb([128, NT, 4, 64], F32, "OC", ps)
            SELT = s.sb([32, S], BF16, "SELT", ps)
            pts = [s.sb([128, 512], BF16, "pT", ps) for _ in range(3)]
            dn = [s.sb([128, 12], F32, "dn", ps) for _ in range(2)]
            imp = [s.sb([128, 32], F32, "imp", ps) for _ in range(2)]
            tmp32 = [s.sb([128, 32], F32, "tmp32", ps) for _ in range(2)]
            m8 = [s.sb([128, 16], F32, "m8", ps) for _ in range(2)]
            oo = [s.sb([128, 4, 64], F32, "oo", ps) for _ in range(2)]
            ot = [s.sb([128, 4, 64], F32, "otmp", ps) for _ in range(2)]
            ob = [s.sb([128, 4, 64], BF16, "obf", ps) for _ in range(2)]
            o3 = lambda sc: sc[:, :].rearrange("p (g t) -> p g t", g=4)
            o3c = lambda sc: sc[0:127, :].rearrange("p (g t) -> p g t", g=4)
            b4 = lambda ap: ap.unsqueeze(1).broadcast_to([ap.shape[0], 4, 128])
            for hk in range(2):
                self.load_heads(QG, [SL_QC + hk * 4 + g for g in range(4)], True)
                self.load_heads(KSL, [SL_KSL + hk], False)
                self.load_heads(KWN, [SL_KWN + hk], False)
                s.dma("sp", KCT[:], Dr["KC_d"][hk], reads=[DR["KC_d"]], writes=[KCT])
                s.dma("sp", VC[:], Dr["VC_d"][hk], reads=[DR["VC_d"]], writes=[VC])
                s.dma("sp", VSL[:], Dr["VSL_d"][:, hk, :].rearrange("(n p) e -> p n e", p=128), reads=[DR["VSL_d"]], writes=[VSL])
                s.dma("sp", VWN[:], Dr["VWN_d"][:, hk, :].rearrange("(n p) e -> p n e", p=128), reads=[DR["VWN_d"]], writes=[VWN])
                for j in range(NT):
                    acc = self.P[4 + j % 2]
                    jc = slice(j * 128, (j + 1) * 128)
                    qk = [(o3c, KCT[:, 0:127], QG[0:64, :, jc], True, False, [KCT, QG]),
                          (o3c, ident[0:127, 0:127], b4(cmaskT[0:127, jc]), False, True, [ident, cmaskT])]

                    def pv(pT, acc=acc):
                        for g in range(4):
                            self.mm(acc[:, g * 97:(g + 1) * 97], pT[0:127, g * 128:(g + 1) * 128], VC[0:127, :], g == 0, True, [pT, VC], [acc], sig=(g == 3))
                    self.attn_stream([dict(kp=127, N=512, qk=qk, pv=pv)], self.P[0:3], pts)
                    a3 = acc[:, 0:388].rearrange("p (g e) -> p g e", e=97)
                    d, im, tm, mm8 = dn[j % 2], imp[j % 2], tmp32[j % 2], m8[j % 2]
                    s.op("dve", lambda h: h.tensor_scalar_max(out=d[:, 0:4].unsqueeze(2), in0=a3[:, :, 64:65], scalar1=1e-30), reads=[acc], writes=[d])
                    s.op("dve", lambda h: h.reciprocal(out=d[:, 0:4], in_=d[:, 0:4]), reads=[d], writes=[d])
                    s.op("dve", lambda h: h.tensor_tensor(out=OC[:, j], in0=a3[:, :, 0:64], in1=d[:, 0:4].unsqueeze(2).broadcast_to([128, 4, 64]), op=ALU.mult), reads=[acc, d], writes=[OC])
                    s.op("dve", lambda h: h.tensor_scalar_mul(out=im[:], in0=a3[:, 0, 65:97], scalar1=d[:, 0:1]), reads=[acc, d], writes=[im])
                    for g in range(1, 4):
                        s.op("dve", lambda h, g=g: h.scalar_tensor_tensor(out=im[:], in0=a3[:, g, 65:97], scalar=d[:, g:g + 1], in1=im[:], op0=ALU.mult, op1=ALU.add), reads=[acc, d, im], writes=[im])
                    s.op("dve", lambda h: h.tensor_tensor(out=im[:], in0=im[:], in1=atab[:, j, :], op=ALU.add), reads=[im, atab], writes=[im])
                    s.op("dve", lambda h: h.max(out=mm8[:, 0:8], in_=im[:]), reads=[im], writes=[mm8])
                    s.op("dve", lambda h: h.match_replace(out=tm[:], in_to_replace=mm8[:, 0:8], in_values=im[:], imm_value=-3e9), reads=[im, mm8], writes=[tm])
                    s.op("dve", lambda h: h.max(out=mm8[:, 8:16], in_=tm[:]), reads=[tm], writes=[mm8])
                    s.op("dve", lambda h: h.tensor_scalar(out=tm[:], in0=im[:], scalar1=mm8[:, 15:16], scalar2=NEG, op0=ALU.is_lt, op1=ALU.mult), reads=[im, mm8], writes=[tm])
                    pt = self.P[6 + j % 2]
                    self.tr(pt[0:32, 0:128], tm[:], self.ident32[:], [tm, self.ident32], [pt])
                    s.op("act", lambda h: h.copy(out=SELT[:, jc], in_=pt[0:32, 0:128]), reads=[pt], writes=[SELT])
                for j in range(NT):
                    jc = slice(j * 128, (j + 1) * 128)
                    accS, accW = self.P[4 + 2 * (j % 2)], self.P[5 + 2 * (j % 2)]
                    items = []
                    for i in range(0, j + 1):
                        ic = slice(i * 128, (i + 1) * 128)
                        use_sel = j >= 8
                        qk = [(o3, KSL[:, ic], QG[:, :, jc], True, (not use_sel) and i != j, [KSL, QG])]
                        if use_sel:
                            qk.append((o3, eblk[:, ic], b4(SELT[:, jc]), False, i != j, [eblk, SELT]))
                        if i == j:
                            qk.append((o3, ident[:], b4(mdiag[:]), False, True, [ident, mdiag]))

                        def pv(pT, i=i, j=j, acc=accS):
                            for g in range(4):
                                self.mm(acc[:, g * 65:(g + 1) * 65], pT[:, g * 128:(g + 1) * 128], VSL[:, i, :], i == 0 and g == 0, i == j, [pT, VSL], [acc], sig=(g == 3))
                        items.append(dict(kp=128, N=512, qk=qk, pv=pv))
                    i0 = max(0, j - 4)
                    for i in range(i0, j + 1):
                        ic = slice(i * 128, (i + 1) * 128)
                        masked = (i == j) or (i == j - 4)
                        qk = [(o3, KWN[:, ic], QG[:, :, jc], True, not masked, [KWN, QG])]
                        if masked:
                            mk = mdiag if i == j else medge
                            qk.append((o3, ident[:], b4(mk[:]), False, True, [ident, mk]))

                        def pv(pT, i=i, j=j, i0=i0, acc=accW):
                            for g in range(4):
                                self.mm(acc[:, g * 65:(g + 1) * 65], pT[:, g * 128:(g + 1) * 128], VWN[:, i, :], i == i0 and g == 0, i == j, [pT, VWN], [acc], sig=(g == 3))
                        items.append(dict(kp=128, N=512, qk=qk, pv=pv))
                    self.attn_stream(items, self.P[0:3], pts)
                    d, o, t, obf = dn[j % 2], oo[j % 2], ot[j % 2], ob[j % 2]
                    aS = accS[:, 0:260].rearrange("p (g e) -> p g e", e=65)
                    aW = accW[:, 0:260].rearrange("p (g e) -> p g e", e=65)
                    gv = GN[:, j, hk * 12:(hk + 1) * 12].rearrange("p (g r) -> p g r", r=3)
                    s.op("dve", lambda h: h.reciprocal(out=d[:, 4:8].unsqueeze(2), in_=aS[:, :, 64:65]), reads=[accS], writes=[d])
                    s.op("dve", lambda h: h.reciprocal(out=d[:, 8:12].unsqueeze(2), in_=aW[:, :, 64:65]), reads=[accW], writes=[d])
                    s.op("dve", lambda h: h.tensor_tensor(out=d[:, 4:8].unsqueeze(2), in0=d[:, 4:8].unsqueeze(2), in1=gv[:, :, 1:2], op=ALU.mult), reads=[d, GN], writes=[d])
                    s.op("dve", lambda h: h.tensor_tensor(out=d[:, 8:12].unsqueeze(2), in0=d[:, 8:12].unsqueeze(2), in1=gv[:, :, 2:3], op=ALU.mult), reads=[d, GN], writes=[d])
                    s.op("pool", lambda h: h.tensor_tensor(out=o[:], in0=OC[:, j], in1=gv[:, :, 0:1].broadcast_to([128, 4, 64]), op=ALU.mult), reads=[OC, GN], writes=[o])
                    s.op("dve", lambda h: h.tensor_tensor(out=t[:], in0=aS[:, :, 0:64], in1=d[:, 4:8].unsqueeze(2).broadcast_to([128, 4, 64]), op=ALU.mult), reads=[accS, d], writes=[t])
                    s.op("pool", lambda h: h.tensor_tensor(out=o[:], in0=o[:], in1=t[:], op=ALU.add), reads=[o, t], writes=[o])
                    s.op("dve", lambda h: h.tensor_tensor(out=t[:], in0=aW[:, :, 0:64], in1=d[:, 8:12].unsqueeze(2).broadcast_to([128, 4, 64]), op=ALU.mult), reads=[accW, d], writes=[t])
                    s.op("pool", lambda h: h.tensor_tensor(out=obf[:], in0=o[:], in1=t[:], op=ALU.add), reads=[o, t], writes=[obf])
                    s.dma("sp", Dr["O_d"][jc, 1024 + hk * 256:1024 + (hk + 1) * 256], obf[:].rearrange("p g d -> p (g d)"), reads=[obf], writes=[DR["O_d"]])

    def phase_merge(self, l, q):
        s, I, Dr, DR = self.s, self.I, self.Dr, self.DR
        xsrc, xres_r = self.xsrc(l, q)
        with ExitStack() as ps:
            wbr = s.sb([128, 12, D], BF16, "wbr", ps)
            wout = s.sb([128, 8, D], BF16, "wout", ps)
            for r in range(3):
                for hh in range(2):
                    s.dma("pool", wbr[:, r * 4:(r + 1) * 4, hh * 512:(hh + 1) * 512],
                          I["w_branch"][l, r].rearrange("(kc p) n -> p kc n", p=128)[:, :, hh * 512:(hh + 1) * 512], writes=[wbr])
            for k4 in range(2):
                for hh in range(2):
                    s.dma("pool", wout[:, k4 * 4:(k4 + 1) * 4, hh * 512:(hh + 1) * 512],
                          I["w_out"][l].rearrange("(kc p) n -> p kc n", p=128)[:, k4 * 4:(k4 + 1) * 4, hh * 512:(hh + 1) * 512], writes=[wout])
            g1 = s.sb([128, D], F32, "g1", ps)
            self.mod_bc(l, q, 2, g1)
            Ot = [s.sb([128, 1536], BF16, "Ot", ps) for _ in range(2)]
            GM = [s.sb([128, 3072], F32, "GMt", ps) for _ in range(2)]
            xt = [s.sb([128, D], F32, "xt", ps) for _ in range(2)]
            OT = [s.sb([128, 12, 128], BF16, "OT", ps) for _ in range(2)]
            MT = [s.sb([128, 8, 128], BF16, "MT", ps) for _ in range(2)]
            mg = s.sb([128, D], F32, "mg", ps)
            mgb = s.sb([128, D], BF16, "mgb", ps)
            tmp = [s.sb([128, 512], F32, "mtmp", ps) for _ in range(2)]
            nb = 0
            for tt in range(NT):
                rows = slice(tt * 128, (tt + 1) * 128)
                grow = slice(q * S + tt * 128, q * S + (tt + 1) * 128)
                O, G, x, ot, mt = Ot[tt % 2], GM[tt % 2], xt[tt % 2], OT[tt % 2], MT[tt % 2]
                s.dma("sp", O[:], Dr["O_d"][rows, :], reads=[DR["O_d"]], writes=[O])
                s.dma("sp", G[:], Dr["GM_d"][rows, :], reads=[DR["GM_d"]], writes=[G])
                s.dma("sp", x[:], xsrc[grow, :], reads=[], writes=[x])
                pa, pb = self.P[0], self.P[1]
                pab, pbb = pa[:].bitcast(BF16), pb[:].bitcast(BF16)
                for kc in range(8):
                    self.tr(pab[:, kc * 128:(kc + 1) * 128], O[:, kc * 128:(kc + 1) * 128], self.ident[:], [O, self.ident], [pa], sig=(kc == 7))
                for kc in range(4):
                    self.tr(pbb[:, kc * 128:(kc + 1) * 128], O[:, (8 + kc) * 128:(9 + kc) * 128], self.ident[:], [O, self.ident], [pb], sig=(kc == 3))
                s.op("act", lambda h: h.copy(out=ot[:, 0:8, :], in_=pab.rearrange("p (k t) -> p k t", k=8)), reads=[pa], writes=[ot])
                s.op("act", lambda h: h.copy(out=ot[:, 8:12, :], in_=pbb[:, 0:512].rearrange("p (k t) -> p k t", k=4)), reads=[pb], writes=[ot])
                for r in range(3):
                    for half in range(2):
                        pm = self.P[2 + nb % 4]
                        tp = tmp[nb % 2]
                        nb += 1
                        hc = slice(half * 512, (half + 1) * 512)
                        for kc in range(4):
                            self.mm(pm[:, :], ot[:, r * 4 + kc, :], wbr[:, r * 4 + kc, hc], kc == 0, kc == 3, [ot, wbr], [pm], sig=(kc == 3))
                        gsl = G[:, r * 1024 + half * 512:r * 1024 + (half + 1) * 512]
                        if r == 0:
                            s.op("dve", lambda h: h.tensor_tensor(out=mg[:, hc], in0=pm[:, :], in1=gsl, op=ALU.mult), reads=[pm, G], writes=[mg])
                        else:
                            s.op("dve", lambda h: h.tensor_tensor(out=tp[:], in0=pm[:, :], in1=gsl, op=ALU.mult), reads=[pm, G], writes=[tp])
                            s.op("pool", lambda h: h.tensor_tensor(out=mg[:, hc], in0=mg[:, hc], in1=tp[:], op=ALU.add), reads=[mg, tp], writes=[mg])
                s.op("act", lambda h: h.copy(out=mgb[:], in_=mg[:]), reads=[mg], writes=[mgb])
                for kc in range(8):
                    self.tr(pab[:, kc * 128:(kc + 1) * 128], mgb[:, kc * 128:(kc + 1) * 128], self.ident[:], [mgb, self.ident], [pa], sig=(kc == 7))
                s.op("act", lambda h: h.copy(out=mt[:], in_=pab.rearrange("p (k t) -> p k t", k=8)), reads=[pa], writes=[mt])
                for half in range(2):
                    pm = self.P[2 + nb % 4]
                    tp = tmp[nb % 2]
                    nb += 1
                    hc = slice(half * 512, (half + 1) * 512)
                    for kc in range(8):
                        self.mm(pm[:, :], mt[:, kc, :], wout[:, kc, hc], kc == 0, kc == 7, [mt, wout], [pm], sig=(kc == 7))
                    s.op("dve", lambda h: h.tensor_tensor(out=tp[:], in0=pm[:, :], in1=g1[:, hc], op=ALU.mult), reads=[pm, g1], writes=[tp])
                    s.op("pool", lambda h: h.tensor_tensor(out=x[:, hc], in0=x[:, hc], in1=tp[:], op=ALU.add), reads=[x, tp], writes=[x])
                s.dma("pool", Dr["xres"][grow, :], x[:], reads=[x], writes=[DR["xres"]])

    def phase_moe2(self, l):
        s, I, Dr, DR = self.s, self.I, self.Dr, self.DR
        last = (l == L_DEPTH - 1)
        NTT, NB = self.ntt, self.nblk
        with ExitStack() as ps:
            g2 = []
            for q in range(self.nseq):
                t = s.sb([128, D], F32, "g2", ps)
                self.mod_bc(l, q, 5, t)
                g2.append(t)
            GATE = s.sb([128, NTT, NE], F32, "GATE", ps)
            GT = s.sb([NE, NTT, 128], F32, "GT", ps)
            IDX4 = s.sb([128, NTT, 4], I32, "IDX4", ps)
            G4 = s.sb([128, NTT, 4], F32, "G4", ps)
            IDXW = s.sb([128, NB, 2], I32, "IDXW", ps)
            IDXB = s.sb([128, NB], I32, "IDXB", ps)
            b2 = s.sb([NE, D], F32, "b2", ps)
            s.dma("sp", b2[:], I["exp_b2"][l], writes=[b2])
            with ExitStack() as p1:
                HB = s.sb([128, NTT, D], BF16, "HB", p1)
                MASK = s.sb([128, NTT, NE], BF16, "MASK", p1)
                DEST = s.sb([128, NTT, NE], F32, "DEST", p1)
                rw = s.sb([128, 8, NE], F32, "rw", p1)
                rb = s.sb([128, NE], F32, "rb", p1)
                s.dma("sp", rw[:], I["router_w"][l].rearrange("(kc p) e -> p kc e", p=128), writes=[rw])
                s.dma("sp", rb[:], I["router_b"][l:l + 1, :].broadcast_to([128, NE]), writes=[rb])
                ltri = self.load_const(p1, "ltri", [128, 128], BF16)
                ones = self.load_const(p1, "ones128", [128, 128], BF16)
                b512 = self.load_const(p1, "b512", [128, 64], F32)
                rowiota = self.load_const(p1, "rowiota", [128, 8], F32)
                piota = self.load_const(p1, "piota", [128, 1], F32)
                xt = [s.sb([128, D], F32, "xt", p1) for _ in range(2)]
                hf = [s.sb([128, D], F32, "hf", p1) for _ in range(2)]
                hT32s = [s.sb([128, 8, 128], F32, "hT32", p1) for _ in range(2)]
                junk = s.sb([128, D], F32, "junk", p1)
                ss = [s.sb([128, 1], F32, "ss", p1) for _ in range(2)]
                lg = [s.sb([128, NE], F32, "lg", p1) for _ in range(2)]
                ex = [s.sb([128, NE], F32, "ex", p1) for _ in range(2)]
                m8 = [s.sb([128, 8], F32, "m8", p1) for _ in range(2)]
                sm = [s.sb([128, 2], F32, "sm", p1) for _ in range(2)]
                gsc = sh = None
                for tt in range(NTT):
                    q = tt // NT
                    if tt % NT == 0:
                        gsc, sh = self.norm_tiles(p1, l, q, "norm2_g", 4, 3)
                    grow = slice(tt * 128, (tt + 1) * 128)
                    x, h32, sq = xt[tt % 2], hf[tt % 2], ss[tt % 2]
                    s.dma("sp", x[:], Dr["xres"][grow, :], reads=[DR["xres"]], writes=[x])
                    self.rstd_of((x, x[:]), (junk, junk[:]), sq)
                    s.op("dve", lambda hh: hh.scalar_tensor_tensor(out=x[:], in0=x[:], scalar=sq[:, 0:1], in1=gsc[:], op0=ALU.mult, op1=ALU.mult), reads=[x, sq, gsc], writes=[x])
                    s.op("pool", lambda hh: hh.tensor_tensor(out=h32[:], in0=x[:], in1=sh[:], op=ALU.add), reads=[x, sh], writes=[h32])
                    s.op("act", lambda hh: hh.copy(out=HB[:, tt, :], in_=h32[:]), reads=[h32], writes=[HB])
                    hT32 = hT32s[tt % 2]
                    for hh2 in range(2):
                        pf = self.P[(tt % 2) * 2 + hh2]
                        for k4 in range(4):
                            kc = hh2 * 4 + k4
                            self.tr(pf[:, k4 * 128:(k4 + 1) * 128], h32[:, kc * 128:(kc + 1) * 128], self.ident32[:], [h32, self.ident32], [pf], sig=(k4 == 3))
                        s.op("dve", lambda hh: hh.tensor_copy(out=hT32[:, hh2 * 4:(hh2 + 1) * 4, :], in_=pf[:, :].rearrange("p (k t) -> p k t", k=4)), reads=[pf], writes=[hT32])
                    pl = self.P[4 + tt % 2]
                    for kc in range(8):
                        self.mm(pl[:, 0:NE], hT32[:, kc, :], rw[:, kc, :], kc == 0, kc == 7, [hT32, rw], [pl], sig=(kc == 7))
                    lgt, et, mt, st = lg[tt % 2], ex[tt % 2], m8[tt % 2], sm[tt % 2]
                    s.op("dve", lambda hh: hh.tensor_tensor(out=lgt[:], in0=pl[:, 0:NE], in1=rb[:], op=ALU.add), reads=[pl, rb], writes=[lgt])
                    s.op("dve", lambda hh: hh.max(out=mt[:], in_=lgt[:]), reads=[lgt], writes=[mt])
                    s.op("dve", lambda hh: hh.tensor_scalar_mul(out=st[:, 0:1], in0=mt[:, 0:1], scalar1=-1.0), reads=[mt], writes=[st])
                    s.op("act", lambda hh: hh.activation(out=et[:], in_=lgt[:], func=AF.Exp, bias=st[:, 0:1]), reads=[lgt, st], writes=[et])
                    s.op("dve", lambda hh: hh.tensor_scalar(out=lgt[:], in0=lgt[:], scalar1=mt[:, 3:4], scalar2=None, op0=ALU.is_ge), reads=[lgt, mt], writes=[lgt])
                    s.op("dve", lambda hh: hh.tensor_copy(out=MASK[:, tt, :], in_=lgt[:]), reads=[lgt], writes=[MASK])
                    s.op("dve", lambda hh: hh.tensor_tensor(out=et[:], in0=et[:], in1=lgt[:], op=ALU.mult), reads=[et, lgt], writes=[et])
                    s.op("dve", lambda hh: hh.reduce_sum(out=st[:, 1:2], in_=et[:], axis=AX.X), reads=[et], writes=[st])
                    s.op("dve", lambda hh: hh.reciprocal(out=st[:, 1:2], in_=st[:, 1:2]), reads=[st], writes=[st])
                    s.op("dve", lambda hh: hh.tensor_scalar_mul(out=GATE[:, tt, :], in0=et[:], scalar1=st[:, 1:2]), reads=[et, st], writes=[GATE])
                    pg = self.P[6 + tt % 2]
                    self.tr(pg[0:NE, 0:128], GATE[:, tt, :], self.ident32[:], [GATE, self.ident32], [pg])
                    s.op("act", lambda hh: hh.copy(out=GT[:, tt, :], in_=pg[0:NE, 0:128]), reads=[pg], writes=[GT])
                run = s.sb([128, NE], F32, "run", p1)
                s.op("dve", lambda hh: hh.memset(run[:], 0.0), writes=[run])
                for tt in range(NTT):
                    pr = self.P[6 + tt % 2]
                    self.mm(pr[:, 0:NE], ltri[:], MASK[:, tt, :], True, True, [ltri, MASK], [pr], sig=False)
                    self.mm(pr[:, NE:2 * NE], ones[:], MASK[:, tt, :], False, True, [ones, MASK], [pr])
                    s.op("dve", lambda hh: hh.tensor_tensor(out=DEST[:, tt, :], in0=pr[:, 0:NE], in1=run[:], op=ALU.add), reads=[pr, run], writes=[DEST])
                    s.op("dve", lambda hh: hh.tensor_tensor(out=run[:], in0=run[:], in1=pr[:, NE:2 * NE], op=ALU.add), reads=[pr, run], writes=[run])
                padded = s.sb([128, NE], F32, "padded", p1)
                tmpe = s.sb([128, NE], F32, "tmpe", p1)
                cum = [s.sb([128, NE], F32, "cum", p1) for _ in range(2)]
                s.op("dve", lambda hh: hh.memset(padded[:], 0.0), writes=[padded])
                for jb in range(NTT // 4):
                    s.op("dve", lambda hh, jb=jb: hh.scalar_tensor_tensor(out=padded[:], in0=run[:], scalar=512.0 * jb, in1=padded[:], op0=ALU.is_gt, op1=ALU.add), reads=[run, padded], writes=[padded])
                s.op("dve", lambda hh: hh.tensor_scalar_mul(out=padded[:], in0=padded[:], scalar1=512.0), reads=[padded], writes=[padded])
                s.op("dve", lambda hh: hh.tensor_copy(out=cum[0][:], in_=padded[:]), reads=[padded], writes=[cum[0]])
                ci = 0
                for shf in (1, 2, 4, 8, 16):
                    a, b = cum[ci], cum[1 - ci]
                    s.op("dve", lambda hh: hh.tensor_copy(out=b[:, 0:shf], in_=a[:, 0:shf]), reads=[a], writes=[b])
                    s.op("dve", lambda hh: hh.tensor_tensor(out=b[:, shf:NE], in0=a[:, shf:NE], in1=a[:, 0:NE - shf], op=ALU.add), reads=[a], writes=[b])
                    ci = 1 - ci
                pend = cum[ci]
                pstart = s.sb([128, NE], F32, "pstart", p1)
                s.op("dve", lambda hh: hh.tensor_tensor(out=pstart[:], in0=pend[:], in1=padded[:], op=ALU.subtract), reads=[pend, padded], writes=[pstart])
                s.op("dve", lambda hh: hh.tensor_tensor(out=DEST[:], in0=DEST[:], in1=pstart[:].unsqueeze(1).broadcast_to([128, NTT, NE]), op=ALU.add), reads=[DEST, pstart], writes=[DEST])
                s.op("dve", lambda hh: hh.scalar_tensor_tensor(out=DEST[:], in0=DEST[:], scalar=1.0, in1=MASK[:], op0=ALU.add, op1=ALU.mult), reads=[DEST, MASK], writes=[DEST])
                d4 = s.sb([128, NTT, 4], F32, "d4", p1)
                for tt in range(NTT):
                    mt, et = m8[tt % 2], ex[tt % 2]
                    s.op("dve", lambda hh: hh.max(out=mt[:], in_=DEST[:, tt, :]), reads=[DEST], writes=[mt])
                    s.op("dve", lambda hh: hh.tensor_scalar_add(out=d4[:, tt, :], in0=mt[:, 0:4], scalar1=-1.0), reads=[mt], writes=[d4])
                    for k in range(4):
                        s.op("dve", lambda hh: hh.scalar_tensor_tensor(out=et[:], in0=DEST[:, tt, :], scalar=mt[:, k:k + 1], in1=GATE[:, tt, :], op0=ALU.is_equal, op1=ALU.mult), reads=[DEST, mt, GATE], writes=[et])
                        s.op("dve", lambda hh: hh.reduce_sum(out=G4[:, tt, k:k + 1], in_=et[:], axis=AX.X), reads=[et], writes=[G4])
                s.op("dve", lambda hh: hh.tensor_copy(out=IDX4[:], in_=d4[:]), reads=[d4], writes=[IDX4])
                bexp = s.sb([128, 64], F32, "bexp", p1)
                s.op("dve", lambda hh: hh.memset(bexp[:], 0.0), writes=[bexp])
                for e in range(NE):
                    s.op("dve", lambda hh: hh.scalar_tensor_tensor(out=bexp[:], in0=b512[:], scalar=pend[:, e:e + 1], in1=bexp[:], op0=ALU.is_ge, op1=ALU.add), reads=[b512, pend, bexp], writes=[bexp])
                boob = s.sb([128, 64], F32, "boob", p1)
                s.op("dve", lambda hh: hh.tensor_scalar(out=boob[:], in0=bexp[:], scalar1=float(NE) - 0.5, scalar2=1.0e6, op0=ALU.is_ge, op1=ALU.mult), reads=[bexp], writes=[boob])
                s.op("dve", lambda hh: hh.tensor_scalar_min(out=bexp[:], in0=bexp[:], scalar1=float(NE - 1)), reads=[bexp], writes=[bexp])
                iwf = s.sb([128, NB, 2], F32, "iwf", p1)
                ibf = s.sb([128, NB], F32, "ibf", p1)
                e1k = s.sb([128, 64], F32, "e1k", p1)
                s.op("dve", lambda hh: hh.tensor_scalar(out=e1k[:], in0=bexp[:], scalar1=256.0, scalar2=float(l * NE * 256), op0=ALU.mult, op1=ALU.add), reads=[bexp], writes=[e1k])
                s.op("dve", lambda hh: hh.tensor_tensor(out=e1k[:], in0=e1k[:], in1=boob[:], op=ALU.add), reads=[e1k, boob], writes=[e1k])
                s.op("dve", lambda hh: hh.tensor_tensor(out=iwf[:], in0=rowiota[:, 0:2].unsqueeze(1).broadcast_to([128, NB, 2]), in1=e1k[:, 0:NB].unsqueeze(2).broadcast_to([128, NB, 2]), op=ALU.add), reads=[rowiota, e1k], writes=[iwf])
                s.op("dve", lambda hh: hh.tensor_copy(out=IDXW[:], in_=iwf[:]), reads=[iwf], writes=[IDXW])
                s.op("dve", lambda hh: hh.tensor_scalar(out=ibf[:], in0=bexp[:, 0:NB], scalar1=128.0, scalar2=piota[:, 0:1], op0=ALU.mult, op1=ALU.add), reads=[bexp, piota], writes=[ibf])
                s.op("dve", lambda hh: hh.tensor_scalar_add(out=ibf[:], in0=ibf[:], scalar1=float(l * NE * 128)), reads=[ibf], writes=[ibf])
                s.op("dve", lambda hh: hh.tensor_tensor(out=ibf[:], in0=ibf[:], in1=boob[:, 0:NB], op=ALU.add), reads=[ibf, boob], writes=[ibf])
                s.op("dve", lambda hh: hh.tensor_copy(out=IDXB[:], in_=ibf[:]), reads=[ibf], writes=[IDXB])
                if "dbg_d" in self.dbg:
                    dbt = s.sb([128, 4096], F32, "dbt", p1)
                    s.op("dve", lambda hh: hh.memset(dbt[:], 0.0), writes=[dbt])
                    s.op("dve", lambda hh: hh.tensor_copy(out=dbt[:, 0:32], in_=run[:]), reads=[run], writes=[dbt])
                    s.op("dve", lambda hh: hh.tensor_copy(out=dbt[:, 32:64], in_=pend[:]), reads=[pend], writes=[dbt])
                    s.op("dve", lambda hh: hh.tensor_copy(out=dbt[:, 64:128], in_=bexp[:]), reads=[bexp], writes=[dbt])
                    s.op("dve", lambda hh: hh.tensor_copy(out=dbt[:, 128:128 + NTT * 4], in_=d4[:].rearrange("p t k -> p (t k)")), reads=[d4], writes=[dbt])
                    s.op("dve", lambda hh: hh.tensor_copy(out=dbt[:, 512:512 + NTT * 4], in_=G4[:].rearrange("p t k -> p (t k)")), reads=[G4], writes=[dbt])
                    s.dma("sp", Dr["dbg_d"], dbt[:], reads=[dbt], writes=[DR["dbg_d"]])
                for tt in range(NTT):
                    for k in range(4):
                        s.idma(Dr["xs_d"][:, :], HB[:, tt, :], out_off=IDX4[:, tt, k:k + 1], reads=[HB, IDX4], writes=[])
                s.barrier()
            with ExitStack() as p2:
                W1 = [s.sb([128, 8, 2 * D], BF16, "W1", p2) for _ in range(2)]
                W2 = [s.sb([128, 8, D], BF16, "W2", p2) for _ in range(2)]
                B1 = [s.sb([128, 16], F32, "B1", p2) for _ in range(2)]
                XS = [s.sb([128, 4, D], BF16, "XS", p2) for _ in range(2)]
                XT = [s.sb([128, 8, 512], BF16, "XT", p2) for _ in range(2)]
                Ab = [s.sb([128, 8, 512], BF16, "Ab", p2) for _ in range(2)]
                Gt = [s.sb([128, 512], F32, "Gt", p2) for _ in range(2)]
                St = [s.sb([128, 512], F32, "St", p2) for _ in range(2)]
                Ut = [s.sb([128, 512], F32, "Ut", p2) for _ in range(2)]
                Yt = [s.sb([128, D], F32, "Yt", p2) for _ in range(2)]
                for wt_ in W1 + W2 + B1:
                    s.op("pool", lambda hh, wt_=wt_: hh.memset(wt_[:], 0.0), writes=[wt_])
                cnt = {"nf": 0, "ny": 0}

                def load_block(b):
                    w1, w2, b1, xs = W1[b % 2], W2[b % 2], B1[b % 2], XS[b % 2]
                    s.dma("sp", xs[:], Dr["xs_d"][b * 512:(b + 1) * 512, :].rearrange("(t p) d -> p t d", p=128), reads=[DR["xs_d"]], writes=[xs])
                    for j2 in range(2):
                        s.idma(w1[:, j2 * 4:(j2 + 1) * 4, :].rearrange("p a b -> p (a b)"), I["exp_w1"][:, :], in_off=IDXW[:, b, j2:j2 + 1], reads=[IDXW], writes=[w1], bounds=65535)
                    s.idma(b1[:, :], I["exp_b1E"][:, :], in_off=IDXB[:, b:b + 1], reads=[IDXB], writes=[b1], bounds=65535)

                def load_w2(b):
                    w2 = W2[b % 2]
                    s.idma(w2[:].rearrange("p a b -> p (a b)"), I["exp_w2"][:, :], in_off=IDXB[:, b:b + 1], reads=[IDXB], writes=[w2], bounds=65535)

                def transposes(b):
                    xs, xT = XS[b % 2], XT[b % 2]
                    for t4 in range(4):
                        pt = self.P[t4 % 2]
                        ptb = pt[:].bitcast(BF16)
                        for kc in range(8):
                            kb = (kc // 4) * 512 + (kc % 4)
                            self.tr(ptb[:, kc * 128:(kc + 1) * 128], xs[:, t4, kb:kb + 509:4], self.ident[:], [xs, self.ident], [pt], sig=(kc == 7))
                        if t4 % 2 == 0:
                            s.op("act", lambda hh: hh.copy(out=xT[:, :, t4 * 128:(t4 + 1) * 128], in_=ptb.rearrange("p (k t) -> p k t", k=8)), reads=[pt], writes=[xT])
                        else:
                            s.op("dve", lambda hh: hh.tensor_copy(out=xT[:, :, t4 * 128:(t4 + 1) * 128], in_=ptb.rearrange("p (k t) -> p k t", k=8)), reads=[pt], writes=[xT])

                def stage1_fc(b, fc):
                    w1, b1, xT, A = W1[b % 2], B1[b % 2], XT[b % 2], Ab[b % 2]
                    nf = cnt["nf"]
                    cnt["nf"] += 1
                    psG, psU = self.P[2 + (nf % 2) * 2], self.P[3 + (nf % 2) * 2]
                    G, Sg, U = Gt[nf % 2], St[nf % 2], Ut[nf % 2]
                    for kc in range(8):
                        self.mm(psG[:, :], w1[:, kc, fc:D:8], xT[:, kc, :], kc == 0, kc == 7, [w1, xT], [psG], sig=(kc == 7))
                    for kc in range(8):
                        self.mm(psU[:, :], w1[:, kc, D + fc:2 * D:8], xT[:, kc, :], kc == 0, kc == 7, [w1, xT], [psU], sig=(kc == 7))
                    s.op("dve", lambda hh: hh.tensor_scalar(out=G[:], in0=psG[:, :], scalar1=b1[:, fc:fc + 1], scalar2=7.0, op0=ALU.add, op1=ALU.min), reads=[psG, b1], writes=[G])
                    s.op("act", lambda hh: hh.activation(out=Sg[:], in_=G[:], func=AF.Sigmoid, scale=1.702), reads=[G], writes=[Sg])
                    s.op("act", lambda hh: hh.activation(out=U[:], in_=psU[:, :], func=AF.Identity, bias=b1[:, 8 + fc:8 + fc + 1]), reads=[psU, b1], writes=[U])
                    s.op("dve", lambda hh: hh.tensor_scalar(out=U[:], in0=U[:], scalar1=7.0, scalar2=-7.0, op0=ALU.min, op1=ALU.max), reads=[U], writes=[U])
                    s.op("dve", lambda hh: hh.tensor_tensor(out=G[:], in0=G[:], in1=Sg[:], op=ALU.mult), reads=[G, Sg], writes=[G])
                    s.op("dve", lambda hh: hh.scalar_tensor_tensor(out=A[:, fc, :], in0=U[:], scalar=1.0, in1=G[:], op0=ALU.add, op1=ALU.mult), reads=[U, G], writes=[A])

                def stage2_grp(b, g):
                    w2, A = W2[b % 2], Ab[b % 2]
                    t4, dh = g // 2, g % 2
                    ny = cnt["ny"]
                    y = Yt[(ny // 2) % 2]
                    psY = self.P[6 + ny % 2]
                    cnt["ny"] += 1
                    dc = slice(dh * 512, (dh + 1) * 512)
                    for fc in range(8):
                        self.mm(psY[:, :], A[:, fc, t4 * 128:(t4 + 1) * 128], w2[:, fc, dc], fc == 0, fc == 7, [A, w2], [psY], sig=(fc == 7))
                    s.op("act", lambda hh: hh.copy(out=y[:, dc], in_=psY[:, :]), reads=[psY], writes=[y])
                    if dh == 1:
                        r0 = b * 512 + t4 * 128
                        s.dma("sp", Dr["ys_d"][r0:r0 + 128, :], y[:], reads=[y], writes=[DR["ys_d"]])

                load_block(0)
                load_w2(0)
                load_block(1)
                load_w2(1)
                for b in range(NB):
                    if 1 <= b < NB - 1:
                        load_block(b + 1)
                    transposes(b)
                    for i in range(8):
                        stage1_fc(b, i)
                        if b >= 1:
                            stage2_grp(b - 1, i)
                    if 1 <= b < NB - 1:
                        load_w2(b + 1)
                for i in range(8):
                    stage2_grp(NB - 1, i)
                s.barrier()
            with ExitStack() as p3:
                xt = [s.sb([128, D], F32, "xt", p3) for _ in range(2)]
                acc = [s.sb([128, D], F32, "acc3", p3) for _ in range(2)]
                Yk = [s.sb([128, D], F32, "Yk", p3) for _ in range(8)]
                junk = s.sb([128, D], F32, "junk", p3)
                ss = [s.sb([128, 1], F32, "ss", p3) for _ in range(2)]
                if last:
                    fg = s.sb([128, D], F32, "fg", p3)
                    s.dma("sp", fg[:], I["final_g"].broadcast_to([128, D]), writes=[fg])
                nk = 0
                for tt in range(NTT):
                    q = tt // NT
                    grow = slice(tt * 128, (tt + 1) * 128)
                    x, ac, sq = xt[tt % 2], acc[tt % 2], ss[tt % 2]
                    if tt == 0:
                        s.dma("sp", x[:], Dr["xres"][grow, :], reads=[], writes=[x])
                    if tt + 1 < NTT:
                        s.dma("sp", xt[(tt + 1) % 2][:], Dr["xres"][(tt + 1) * 128:(tt + 2) * 128, :], reads=[], writes=[xt[(tt + 1) % 2]])
                    pa = [self.P[(tt % 2) * 2], self.P[(tt % 2) * 2 + 1]]
                    for half in range(2):
                        self.mm(pa[half][:, :], GT[:, tt, :], b2[:, half * 512:(half + 1) * 512], True, True, [GT, b2], [pa[half]])
                    for k in range(4):
                        yk = Yk[nk % 8]
                        nk += 1
                        s.idma(yk[:, :], Dr["ys_d"][:, :], in_off=IDX4[:, tt, k:k + 1], reads=[IDX4, DR["ys_d"]], writes=[yk])
                        for half in range(2):
                            hc = slice(half * 512, (half + 1) * 512)
                            in1 = pa[half][:, :] if k == 0 else ac[:, hc]
                            rd = [yk, G4] + ([pa[half]] if k == 0 else [ac])
                            s.op("dve", lambda hh, in1=in1, hc=hc, yk=yk, k=k: hh.scalar_tensor_tensor(out=ac[:, hc], in0=yk[:, hc], scalar=G4[:, tt, k:k + 1], in1=in1, op0=ALU.mult, op1=ALU.add), reads=rd, writes=[ac])
                    s.op("pool", lambda hh: hh.tensor_tensor(out=ac[:], in0=ac[:], in1=g2[q][:], op=ALU.mult), reads=[ac, g2[q]], writes=[ac])
                    s.op("dve", lambda hh: hh.tensor_tensor(out=x[:], in0=x[:], in1=ac[:], op=ALU.add), reads=[x, ac], writes=[x])
                    if last:
                        self.rstd_of((x, x[:]), (junk, junk[:]), sq)
                        s.op("dve", lambda hh: hh.scalar_tensor_tensor(out=x[:], in0=x[:], scalar=sq[:, 0:1], in1=fg[:], op0=ALU.mult, op1=ALU.mult), reads=[x, sq, fg], writes=[x])
                        s.dma("sp", self.out[grow, :], x[:], reads=[x], writes=[R("out")])
                    else:
                        s.dma("sp", Dr["xres"][grow, :], x[:], reads=[x], writes=[DR["xres"]])
                s.barrier()


_CONSTS = None


def run(inputs, dbg=(), layers=L_DEPTH, nseq=NSEQ, stop_after=None, cores=NCORES, trace=False, only=None):
    global _CONSTS
    if _CONSTS is None:
        _CONSTS = host_consts()
    inp = {k: np.asarray(v) for k, v in inputs.items()}
    k = K(dbg=dbg, layers=layers, nseq=nseq, stop_after=stop_after, only=only)
    nc = k.build()
    in_maps = []
    for c in range(cores):
        m = host_inputs(inp, c)
        m.update(_CONSTS)
        in_maps.append(m)
    res = run_bass_kernel_spmd(nc, in_maps, core_ids=list(range(cores)), trace=trace)
    return res


def kernel(**inputs):
    res = run(inputs)
    out = np.concatenate([np.asarray(r["out"]).reshape(NSEQ, S, D) for r in res.results], axis=0)
    return out.astype(np.float32)
```
